# Optimizing a Trainium2 kernel written in Bass

```python
import jax, jax.numpy as jnp
from jax import lax
import numpy as np

D_MODEL = 1024
BATCH = 8
SEQ = 4096
DEPTH = 4

GRID_W = 64
CTX_LEN = 256
N_MIXERS = 3
N_A = (DEPTH + 2) // 3
N_B = (DEPTH + 1) // 3
N_C = DEPTH // 3
ATTN_HEADS = 8
ATTN_HD = D_MODEL // (2 * ATTN_HEADS)
ROPE_FREQS = ATTN_HD // 4
ROPE_BASE = 10000.0
Q_BLOCK = 128
FNET_GROUPS = 8
FNET_GW = D_MODEL // FNET_GROUPS
CONV_WIDTH = 31
CONV_PAD = CONV_WIDTH // 2
N_EXPERTS = 32
TOP_K = 4
D_FF = D_MODEL
SWIGLU_ALPHA = 1.702
SWIGLU_LIMIT = 7.0
MOE_BLOCK = 128
DEEPNORM_ALPHA = (2 * DEPTH) ** 0.25
DEEPNORM_BETA = (8 * DEPTH) ** -0.25
LN_EPS = 1e-5

kernel_name = "hybrid_diffattn_fnet_conformer_moe_dit"


def layer_norm(x, g, b):
    xf = x.astype(jnp.float32)
    mu = jnp.mean(xf, axis=-1, keepdims=True)
    var = jnp.mean(jnp.square(xf - mu), axis=-1, keepdims=True)
    return ((xf - mu) * lax.rsqrt(var + LN_EPS) * g + b).astype(x.dtype)


def axial_rope_tables(n):
    rows_count = n // GRID_W
    rows = jnp.repeat(jnp.arange(rows_count), GRID_W).astype(jnp.float32)
    cols = jnp.tile(jnp.arange(GRID_W), rows_count).astype(jnp.float32)
    inv_freq = ROPE_BASE ** (-jnp.arange(ROPE_FREQS, dtype=jnp.float32) / ROPE_FREQS)
    ang_r = rows[:, None] * inv_freq
    ang_c = cols[:, None] * inv_freq
    shp = (1, n, 1, 1, ROPE_FREQS)
    return (jnp.cos(ang_r).reshape(shp), jnp.sin(ang_r).reshape(shp),
            jnp.cos(ang_c).reshape(shp), jnp.sin(ang_c).reshape(shp))


def rotate_half(xp, cos, sin):
    x1, x2 = jnp.split(xp, 2, axis=-1)
    return jnp.concatenate([x1 * cos - x2 * sin, x2 * cos + x1 * sin], axis=-1)


def axial_rope(x, tables):
    cr, sr, cc, sc = tables
    half = ATTN_HD // 2
    xr = rotate_half(x[..., :half], cr, sr)
    xc = rotate_half(x[..., half:], cc, sc)
    return jnp.concatenate([xr, xc], axis=-1).astype(x.dtype)


def diff_core(q, k, v, lam):
    s = jnp.einsum('bqhcd,bkhcd->cbhqk', q, k, preferred_element_type=jnp.float32) * (ATTN_HD ** -0.5)
    p = jax.nn.softmax(s, axis=-1)
    attn = p[0] - lam * p[1]
    return jnp.einsum('bhqk,bkhv->bqhv', attn.astype(v.dtype), v)


def diff_head_out(o, subln_g, lam_init, w_o):
    of = o.astype(jnp.float32)
    of = of * lax.rsqrt(jnp.mean(jnp.square(of), axis=-1, keepdims=True) + LN_EPS) * subln_g * (1.0 - lam_init)
    B, N = o.shape[0], o.shape[1]
    return of.astype(o.dtype).reshape(B, N, D_MODEL) @ w_o


def diff_attention(u_lat, u_ctx, w_qkv, w_o, lq1, lk1, lq2, lk2, subln_g, lam_init, tables, with_ctx_out):
    B, N, _ = u_lat.shape

    def project(u):
        q, k, v = jnp.split(u @ w_qkv, 3, axis=-1)
        n = u.shape[1]
        return (q.reshape(B, n, ATTN_HEADS, 2, ATTN_HD),
                k.reshape(B, n, ATTN_HEADS, 2, ATTN_HD),
                v.reshape(B, n, ATTN_HEADS, 2 * ATTN_HD))

    q_l, k_l, v_l = project(u_lat)
    q_c, k_c, v_c = project(u_ctx)
    q_l = axial_rope(q_l, tables)
    k_l = axial_rope(k_l, tables)
    lam = (jnp.exp(jnp.sum(lq1 * lk1).astype(jnp.float32))
           - jnp.exp(jnp.sum(lq2 * lk2).astype(jnp.float32)) + lam_init)

    k_all = jnp.concatenate([k_l, k_c], axis=1)
    v_all = jnp.concatenate([v_l, v_c], axis=1)
    nb = N // Q_BLOCK
    q_blocks = q_l.reshape(B, nb, Q_BLOCK, ATTN_HEADS, 2, ATTN_HD).transpose(1, 0, 2, 3, 4, 5)
    o_l = lax.map(lambda qb: diff_core(qb, k_all, v_all, lam), q_blocks)
    o_l = o_l.transpose(1, 0, 2, 3, 4).reshape(B, N, ATTN_HEADS, 2 * ATTN_HD)
    y_lat = diff_head_out(o_l, subln_g, lam_init, w_o)
    y_ctx = None
    if with_ctx_out:
        y_ctx = diff_head_out(diff_core(q_c, k_c, v_c, lam), subln_g, lam_init, w_o)
    return y_lat, y_ctx


def fourier_mix(u, w, b):
    B, N, _ = u.shape
    ug = u.astype(jnp.float32).reshape(B, N, FNET_GROUPS, FNET_GW)
    f = jnp.fft.fft2(ug, axes=(1, 3), norm='ortho').real
    return f.reshape(B, N, D_MODEL).astype(u.dtype) @ w + b


def conformer_conv(u, w_pw1, b_pw1, w_dw, b_dw, ln_g, ln_b, w_pw2, b_pw2):
    h = u @ w_pw1 + b_pw1
    a, g = jnp.split(h, 2, axis=-1)
    h = a * jax.nn.sigmoid(g)
    h = lax.conv_general_dilated(h, w_dw[:, None, :].astype(h.dtype), window_strides=(1,),
                                 padding=[(CONV_PAD, CONV_PAD)],
                                 dimension_numbers=('NWC', 'WIO', 'NWC'),
                                 feature_group_count=D_MODEL) + b_dw
    h = jax.nn.silu(layer_norm(h, ln_g, ln_b))
    return h @ w_pw2 + b_pw2


def clamped_swiglu(h):
    glu, lin = jnp.split(h, 2, axis=-1)
    glu = jnp.minimum(glu, SWIGLU_LIMIT)
    lin = jnp.clip(lin, -SWIGLU_LIMIT, SWIGLU_LIMIT)
    return glu * jax.nn.sigmoid(SWIGLU_ALPHA * glu) * (lin + 1.0)


def moe(tokens, w_router, b_router, w1, b1, w2, b2):
    T = tokens.shape[0]
    logits = (tokens @ w_router + b_router).astype(jnp.float32)
    top_val, top_idx = lax.top_k(logits, TOP_K)
    gates = jax.nn.softmax(top_val, axis=-1)
    A = T * TOP_K
    flat_e = top_idx.reshape(-1)
    order = jnp.argsort(flat_e)
    sorted_e = flat_e[order]
    counts = jnp.bincount(flat_e, length=N_EXPERTS)
    padded = (counts + MOE_BLOCK - 1) // MOE_BLOCK * MOE_BLOCK
    start = jnp.cumsum(counts) - counts
    pend = jnp.cumsum(padded)
    pstart = pend - padded
    dest = pstart[sorted_e] + (jnp.arange(A) - start[sorted_e])
    n_blocks = -(-A // MOE_BLOCK) + N_EXPERTS
    P = n_blocks * MOE_BLOCK
    row_tok = jnp.zeros((P,), jnp.int32).at[dest].set((order // TOP_K).astype(jnp.int32))
    row_gate = jnp.zeros((P,), jnp.float32).at[dest].set(gates.reshape(-1)[order])
    block_e = jnp.minimum(jnp.searchsorted(pend, jnp.arange(n_blocks) * MOE_BLOCK, side='right'),
                          N_EXPERTS - 1)
    xb = tokens[row_tok].reshape(n_blocks, MOE_BLOCK, D_MODEL)

    def expert_block(args):
        xe, e = args
        h = clamped_swiglu(xe @ w1[e] + b1[e])
        return h @ w2[e] + b2[e]

    yb = lax.map(expert_block, (xb, block_e)).reshape(P, D_MODEL)
    return jnp.zeros_like(tokens).at[row_tok].add(yb * row_gate[:, None].astype(tokens.dtype))


def setup_inputs(seed: int = 0) -> dict:
    key = jax.random.key(seed)
    keys = iter(jax.random.split(key, 48))

    def nrm(shape, scale):
        return jax.random.normal(next(keys), shape, jnp.float32) * scale

    D, E, F, HD = D_MODEL, N_EXPERTS, D_FF, ATTN_HD
    din = D ** -0.5
    return {
        "x": nrm((BATCH, SEQ, D), 1.0),
        "c": nrm((BATCH, D), 1.0),
        "ctx": nrm((BATCH, CTX_LEN, D), 1.0),
        "c_ctx": nrm((D,), 1.0),
        "w_mod": nrm((DEPTH, D, 6 * D), 0.5 * din),
        "b_mod": nrm((DEPTH, 6 * D), 0.02),
        "ln1_g": 1.0 + nrm((DEPTH, D), 0.02),
        "ln1_b": nrm((DEPTH, D), 0.02),
        "ln2_g": 1.0 + nrm((DEPTH, D), 0.02),
        "ln2_b": nrm((DEPTH, D), 0.02),
        "attn_w_qkv": nrm((N_A, D, 3 * D), din),
        "attn_w_o": nrm((N_A, D, D), din * DEEPNORM_BETA),
        "attn_lam_q1": nrm((N_A, HD), 0.1),
        "attn_lam_k1": nrm((N_A, HD), 0.1),
        "attn_lam_q2": nrm((N_A, HD), 0.1),
        "attn_lam_k2": nrm((N_A, HD), 0.1),
        "attn_subln_g": 1.0 + nrm((N_A, 2 * HD), 0.02),
        "fnet_w": nrm((N_B, D, D), din * DEEPNORM_BETA),
        "fnet_b": nrm((N_B, D), 0.02),
        "conv_w_pw1": nrm((N_C, D, 2 * D), din),
        "conv_b_pw1": nrm((N_C, 2 * D), 0.02),
        "conv_w_dw": nrm((N_C, CONV_WIDTH, D), CONV_WIDTH ** -0.5),
        "conv_b_dw": nrm((N_C, D), 0.02),
        "conv_ln_g": 1.0 + nrm((N_C, D), 0.02),
        "conv_ln_b": nrm((N_C, D), 0.02),
        "conv_w_pw2": nrm((N_C, D, D), din * DEEPNORM_BETA),
        "conv_b_pw2": nrm((N_C, D), 0.02),
        "moe_w_router": nrm((DEPTH, D, E), din),
        "moe_b_router": nrm((DEPTH, E), 0.01),
        "moe_w1": nrm((DEPTH, E, D, 2 * F), din),
        "moe_b1": nrm((DEPTH, E, 2 * F), 0.02),
        "moe_w2": nrm((DEPTH, E, F, D), F ** -0.5 * DEEPNORM_BETA),
        "moe_b2": nrm((DEPTH, E, D), 0.02),
    }


def reference(x, c, ctx, c_ctx, w_mod, b_mod, ln1_g, ln1_b, ln2_g, ln2_b,
              attn_w_qkv, attn_w_o, attn_lam_q1, attn_lam_k1, attn_lam_q2, attn_lam_k2, attn_subln_g,
              fnet_w, fnet_b,
              conv_w_pw1, conv_b_pw1, conv_w_dw, conv_b_dw, conv_ln_g, conv_ln_b, conv_w_pw2, conv_b_pw2,
              moe_w_router, moe_b_router, moe_w1, moe_b1, moe_w2, moe_b2):
    B, N, D = x.shape
    C = ctx.shape[1]
    tables = axial_rope_tables(N)
    cond_lat = jax.nn.silu(c)
    cond_ctx = jax.nn.silu(c_ctx)
    x_lat, x_ctx = x, ctx

    for i in range(DEPTH):
        last = i == DEPTH - 1
        kind = i % N_MIXERS
        j = i // N_MIXERS
        m_lat = cond_lat @ w_mod[i] + b_mod[i]
        m_ctx = cond_ctx @ w_mod[i] + b_mod[i]
        sh1, sc1, g1, sh2, sc2, g2 = jnp.split(m_lat[:, None, :], 6, axis=-1)
        ch1, cs1, cg1, ch2, cs2, cg2 = jnp.split(m_ctx, 6)

        u_lat = x_lat * (1.0 + sc1) + sh1
        need_ctx = (not last) or kind == 0
        u_ctx = x_ctx * (1.0 + cs1) + ch1 if need_ctx else None
        if kind == 0:
            lam_init = 0.8 - 0.6 * float(np.exp(-0.3 * i))
            y_lat, y_ctx = diff_attention(u_lat, u_ctx, attn_w_qkv[j], attn_w_o[j],
                                          attn_lam_q1[j], attn_lam_k1[j], attn_lam_q2[j], attn_lam_k2[j],
                                          attn_subln_g[j], lam_init, tables, with_ctx_out=not last)
        elif kind == 1:
            y_lat = fourier_mix(u_lat, fnet_w[j], fnet_b[j])
            y_ctx = None if last else fourier_mix(u_ctx, fnet_w[j], fnet_b[j])
        else:
            cw = (conv_w_pw1[j], conv_b_pw1[j], conv_w_dw[j], conv_b_dw[j],
                  conv_ln_g[j], conv_ln_b[j], conv_w_pw2[j], conv_b_pw2[j])
            y_lat = conformer_conv(u_lat, *cw)
            y_ctx = None if last else conformer_conv(u_ctx, *cw)
        x_lat = layer_norm(DEEPNORM_ALPHA * x_lat + g1 * y_lat, ln1_g[i], ln1_b[i])
        if not last:
            x_ctx = layer_norm(DEEPNORM_ALPHA * x_ctx + cg1 * y_ctx, ln1_g[i], ln1_b[i])

        mw = (moe_w_router[i], moe_b_router[i], moe_w1[i], moe_b1[i], moe_w2[i], moe_b2[i])
        v_lat = (x_lat * (1.0 + sc2) + sh2).reshape(B * N, D)
        if last:
            f_lat = moe(v_lat, *mw).reshape(B, N, D)
        else:
            v_ctx = (x_ctx * (1.0 + cs2) + ch2).reshape(B * C, D)
            f_all = moe(jnp.concatenate([v_lat, v_ctx], axis=0), *mw)
            f_lat = f_all[:B * N].reshape(B, N, D)
            f_ctx = f_all[B * N:].reshape(B, C, D)
            x_ctx = layer_norm(DEEPNORM_ALPHA * x_ctx + cg2 * f_ctx, ln2_g[i], ln2_b[i])
        x_lat = layer_norm(DEEPNORM_ALPHA * x_lat + g2 * f_lat, ln2_g[i], ln2_b[i])

    return x_lat
```

```python
import contextlib
import numpy as np
import ml_dtypes
import concourse.bass as bass
import concourse.mybir as mybir
from concourse.bass_utils import run_bass_kernel_spmd

F32 = mybir.dt.float32
BF16 = mybir.dt.bfloat16
AF = mybir.ActivationFunctionType
ALU = mybir.AluOpType
AX = mybir.AxisListType
P = 128
LN_EPS = 1e-5


class Cfg:
    def __init__(self, SEQ=4096, CTX=256, E=32, DEPTH=4):
        self.SEQ, self.CTX, self.E, self.DEPTH = SEQ, CTX, E, DEPTH
        self.D = 1024
        self.KC = 8
        self.H = 8
        self.T = SEQ + CTX
        self.N_A = (DEPTH + 2) // 3
        self.N_B = (DEPTH + 1) // 3
        self.N_C = DEPTH // 3
        self.alpha = float((2 * DEPTH) ** 0.25)
        self.lat_blocks = [(t, 512, False) for t in range(0, SEQ, 512)]
        self.ctx_blocks = [(SEQ + t, min(512, CTX - t), True) for t in range(0, CTX, 512)]
        self.blocks = self.lat_blocks + self.ctx_blocks
        self.SB_MAX = 1024


class TT:
    def __init__(self, h, name):
        self.h = h
        self.name = name
        self.w = None
        self.r = {}
        self.dsem = None
        self.dcnt = 0
        self.w_is_dma = False

    def __getitem__(self, k):
        return self.h[k]


class Sched:
    def __init__(self, nc, es):
        self.nc = nc
        self.eng = {"pe": nc.tensor, "act": nc.scalar, "dve": nc.vector, "pool": nc.gpsimd, "sp": nc.sync}
        self.sem = {k: es.enter_context(nc.semaphore("e_" + k)) for k in self.eng}
        self.cnt = {k: 0 for k in self.eng}
        self.seen = {k: {} for k in self.eng}
        self.semkey = {}
        for k in self.eng:
            self.semkey[id(self.sem[k])] = k
        self.free_dsems = []
        self.all_dsems = []
        self.es = es
        self.ndsem = 0
        self.live = []
        self.ninst = 0

    def _get_dsem(self):
        if self.free_dsems:
            return self.free_dsems.pop()
        s = self.es.enter_context(self.nc.semaphore("d%d" % self.ndsem))
        self.ndsem += 1
        rec = [s, 0]
        self.all_dsems.append(rec)
        return rec

    def track(self, h, name):
        t = TT(h, name)
        self.live.append(t)
        return t

    def _wait(self, e, evs):
        own = self.sem[e]
        for (sem, val) in evs:
            if sem is own and e == "pe":
                continue
            k = id(sem)
            if self.seen[e].get(k, 0) >= val:
                continue
            self.eng[e].wait_ge(sem, val)
            self.seen[e][k] = val

    def op(self, e, fn, reads=(), writes=(), sig=True):
        deps = []
        for t in reads:
            if t.w is not None:
                deps.append(t.w)
        for t in writes:
            if t.w is not None:
                deps.append(t.w)
            for k, v in t.r.items():
                deps.append((k, v))
        deps2 = []
        for d in deps:
            deps2.append(d)
        self._wait(e, deps2)
        ins = fn(self.eng[e])
        self.ninst += 1
        if sig:
            self.cnt[e] += 1
            ins.then_inc(self.sem[e], 1)
            ev = (self.sem[e], self.cnt[e])
        else:
            ev = (self.sem[e], self.cnt[e] + 1)
        for t in writes:
            t.w = ev
            t.w_is_dma = False
            t.r = {}
        for t in reads:
            if t.r.get(ev[0], 0) < ev[1]:
                t.r[ev[0]] = ev[1]
        return ins

    def dma(self, q, out_t, out_ap, in_t, in_ap):
        deps = []
        if in_t.w is not None:
            deps.append(in_t.w)
        if out_t.w is not None and not out_t.w_is_dma:
            deps.append(out_t.w)
        for k, v in out_t.r.items():
            deps.append((k, v))
        self._wait(q, deps)
        if out_t.dsem is None:
            out_t.dsem = self._get_dsem()
        rec = out_t.dsem
        rec[1] += 16
        self.eng[q].dma_start(out=out_ap, in_=in_ap).then_inc(rec[0], 16)
        self.ninst += 1
        ev = (rec[0], rec[1])
        out_t.w = ev
        out_t.w_is_dma = True
        out_t.r = {}
        if in_t.r.get(ev[0], 0) < ev[1]:
            in_t.r[ev[0]] = ev[1]

    def drain(self, release=()):
        evs = [(self.sem[k], self.cnt[k]) for k in self.eng if self.cnt[k] > 0]
        evs += [(r[0], r[1]) for r in self.all_dsems if r[1] > 0]
        for e in self.eng:
            own = self.sem[e]
            for (sem, val) in evs:
                if sem is own:
                    continue
                k = id(sem)
                if self.seen[e].get(k, 0) >= val:
                    continue
                self.eng[e].wait_ge(sem, val)
                self.seen[e][k] = val
        for t in self.live:
            t.w = None
            t.r = {}
        for t in release:
            if t.dsem is not None:
                self.free_dsems.append(t.dsem)
                t.dsem = None
            if t in self.live:
                self.live.remove(t)


_UID = [0]


def _uid():
    _UID[0] += 1
    return _UID[0]


class Scope:
    def __init__(self, S):
        self.S = S
        self.es = contextlib.ExitStack()
        self.tiles = []
        self.n = 0

    def sb(self, name, shape, dt):
        h = self.es.enter_context(self.S.nc.sbuf_tensor("%s_%d" % (name, _uid()), list(shape), dt))
        t = self.S.track(h, name)
        self.tiles.append(t)
        return t

    def ps(self, name, shape=(P, 512), dt=F32):
        h = self.es.enter_context(self.S.nc.psum_tensor("%s_%d" % (name, _uid()), list(shape), dt))
        t = self.S.track(h, name)
        self.tiles.append(t)
        return t

    def close(self):
        self.S.drain(release=self.tiles)
        self.es.close()


class Rot:
    def __init__(self, tiles):
        self.tiles = tiles
        self.i = 0

    def next(self):
        t = self.tiles[self.i % len(self.tiles)]
        self.i += 1
        return t


class Prog:
    def __init__(self, cfg):
        self.cfg = cfg
        self.nc = bass.Bass("TRN2", target_bir_lowering=False)
        self.io = {}

    def din(self, name, shape, dt=F32):
        ap = self.nc.dram_tensor(name, list(shape), dt, kind="ExternalInput").ap()
        self.io[name] = ap
        return ap

    def build(self):
        cfg = self.cfg
        nc = self.nc
        D, T, E, SEQ, CTX, DEPTH = cfg.D, cfg.T, cfg.E, cfg.SEQ, cfg.CTX, cfg.DEPTH
        d = self.din
        d("xT", [D, SEQ]); d("ctxT", [D, CTX]); d("cT", [P, 8]); d("cctxT", [P, 8])
        d("w_mod", [DEPTH, D, 6 * D]); d("b_modT", [P, DEPTH * 48])
        d("lnp", [P, DEPTH * 32])
        d("attn_w_qkv", [cfg.N_A, D, 3 * D]); d("attn_w_o", [cfg.N_A, D, D])
        d("lam", [cfg.N_A, 4, 64]); d("sublnT", [P, cfg.N_A])
        d("fnet_w", [max(cfg.N_B, 1), D, D]); d("fnet_bT", [P, max(cfg.N_B, 1) * 8])
        nC = max(cfg.N_C, 1)
        d("conv_w_pw1", [nC, D, 2 * D]); d("conv_b_pw1T", [P, nC * 16])
        d("conv_w_dwT", [P, nC * 8 * 31]); d("conv_pT", [P, nC * 4 * 8])
        d("conv_w_pw2", [nC, D, D])
        d("moe_w_router", [DEPTH, D, E]); d("moe_b_router", [DEPTH, E])
        d("moe_w1", [DEPTH, E, D, 2 * D]); d("moe_b1T", [P, DEPTH * E * 16])
        d("moe_w2", [DEPTH, E, D, D]); d("moe_b2", [DEPTH, E, D])
        d("ropeC", [P, SEQ]); d("ropeS", [P, SEQ])
        d("dftL", [2, SEQ, SEQ], BF16); d("dftC", [2, CTX, CTX], BF16); d("dft128", [P, 256], BF16)
        d("ident", [P, P])
        self.yT = nc.dram_tensor("yT", [D, SEQ], F32, kind="ExternalOutput").ap()
        self.XX = nc.dram_tensor("XX", [D, T], F32).ap()
        self.X1 = nc.dram_tensor("X1", [D, T], F32).ap()
        self.MIX = nc.dram_tensor("MIX", [D, T], BF16).ap()
        self.GD = nc.dram_tensor("GD", [E, T], F32).ap()

        with contextlib.ExitStack() as es:
            S = Sched(nc, es)
            self.S = S
            es.enter_context(nc.Block())
            self.t_in = S.track(None, "inputs")
            self.t_XX = S.track(None, "XX")
            self.t_X1 = S.track(None, "X1")
            self.t_MIX = S.track(None, "MIX")
            self.t_GD = S.track(None, "GD")
            self.t_out = S.track(None, "yT")
            G = Scope(S)
            self.G = G
            self.ones32 = G.sb("ones32", [P, P], F32)
            self.onesbf = G.sb("onesbf", [P, P], BF16)
            self.ident = G.sb("ident", [P, P], F32)
            self.MOD = G.sb("MOD", [P, DEPTH, 48, 2], F32)
            self.LNP = G.sb("LNP", [P, DEPTH, 4, 8], F32)
            self.GB1 = G.sb("GB1", [P, DEPTH, 8, 2], F32)
            self.cst = G.sb("cst", [P, 4], F32)
            S.op("dve", lambda e: e.memset(self.cst[:, 0:1], LN_EPS), writes=[self.cst])
            S.op("dve", lambda e: e.memset(self.cst[:, 1:2], 128.0 * LN_EPS), writes=[self.cst])
            S.op("dve", lambda e: e.memset(self.ones32[:], 1.0), writes=[self.ones32])
            S.op("dve", lambda e: e.memset(self.onesbf[:], 1.0), writes=[self.onesbf])
            S.dma("sp", self.ident, self.ident[:], self.t_in, self.io["ident"][:, :])
            S.dma("sp", self.LNP, self.LNP[:].rearrange("p a b c -> p (a b c)"), self.t_in, self.io["lnp"][:, :])
            self.stage_mod()
            for i in range(DEPTH):
                kind = i % 3
                j = i // 3
                last = i == DEPTH - 1
                if kind == 0:
                    self.mixer_attn(i, j, last)
                elif kind == 1:
                    self.mixer_fnet(i, j, last)
                else:
                    self.mixer_conv(i, j, last)
                self.post_moe(i, kind, j, last)
            S.drain()
            G.close()
        return nc

    def xsrc(self, i, blk):
        t0, n, is_ctx = blk
        cfg = self.cfg
        if i == 0:
            if is_ctx:
                ap = self.io["ctxT"].rearrange("(k p) t -> p k t", p=P)[:, :, t0 - cfg.SEQ:t0 - cfg.SEQ + n]
            else:
                ap = self.io["xT"].rearrange("(k p) t -> p k t", p=P)[:, :, t0:t0 + n]
            return self.t_in, ap
        return self.t_XX, self.XX.rearrange("(k p) t -> p k t", p=P)[:, :, t0:t0 + n]

    def wview(self, ap2d):
        return ap2d.rearrange("(k p) n -> p k n", p=P)

    def modulate(self, eng_rot, x, ut_ap_fn, i, n, col, s_sh, s_sc):
        S = self.S
        for c in range(8):
            S.op("act", lambda e, c=c: e.activation(
                out=ut_ap_fn(c), in_=x[:, c, 0:n], func=AF.Identity,
                bias=self.MOD[:, i, s_sh * 8 + c, col:col + 1], scale=self.MOD[:, i, s_sc * 8 + c, col:col + 1]),
                reads=[x, self.MOD], writes=eng_rot)

    def layer_norm(self, sc, z, n, gcol, bcol, outs, tmp, greads=()):
        S = self.S
        sq, pss, psq, mean, rstd, nb = tmp["sq"], tmp["pss"], tmp["psq"], tmp["mean"], tmp["rstd"], tmp["nb"]
        for c in range(8):
            S.op("act", lambda e, c=c: e.activation(out=sq[:, c, 0:n], in_=z[:, c, 0:n], func=AF.Square),
                 reads=[z], writes=[sq])
        for c in range(8):
            S.op("pe", lambda e, c=c: e.matmul(pss[:, 0:n], self.ones32[:], z[:, c, 0:n], start=(c == 0), stop=(c == 7)),
                 reads=[self.ones32, z], writes=[pss], sig=(c == 7))
        for c in range(8):
            S.op("pe", lambda e, c=c: e.matmul(psq[:, 0:n], self.ones32[:], sq[:, c, 0:n], start=(c == 0), stop=(c == 7)),
                 reads=[self.ones32, sq], writes=[psq], sig=(c == 7))
        invd = 1.0 / 1024.0
        S.op("dve", lambda e: e.tensor_scalar(out=mean[:, 0:n], in0=pss[:, 0:n], scalar1=invd, scalar2=None, op0=ALU.mult),
             reads=[pss], writes=[mean])
        S.op("dve", lambda e: e.tensor_tensor(out=nb[:, 0:n], in0=mean[:, 0:n], in1=mean[:, 0:n], op=ALU.mult),
             reads=[mean], writes=[nb])
        S.op("dve", lambda e: e.scalar_tensor_tensor(out=rstd[:, 0:n], in0=psq[:, 0:n], scalar=invd, in1=nb[:, 0:n],
                                                     op0=ALU.mult, op1=ALU.subtract),
             reads=[psq, nb], writes=[rstd])
        S.op("act", lambda e: e.activation(out=rstd[:, 0:n], in_=rstd[:, 0:n], func=AF.Sqrt, bias=self.cst[:, 0:1]),
             reads=[rstd, self.cst], writes=[rstd])
        S.op("dve", lambda e: e.reciprocal(out=rstd[:, 0:n], in_=rstd[:, 0:n]), reads=[rstd], writes=[rstd])
        S.op("dve", lambda e: e.scalar_tensor_tensor(out=nb[:, 0:n], in0=mean[:, 0:n], scalar=-1.0, in1=rstd[:, 0:n],
                                                     op0=ALU.mult, op1=ALU.mult),
             reads=[mean, rstd], writes=[nb])
        for c in range(8):
            t = tmp["t"].next()
            eng = "dve" if c % 2 == 0 else "pool"
            S.op(eng, lambda e, c=c, t=t: e.tensor_tensor(out=t[:, 0:n], in0=z[:, c, 0:n], in1=rstd[:, 0:n], op=ALU.mult),
                 reads=[z, rstd], writes=[t])
            S.op(eng, lambda e, t=t: e.tensor_tensor(out=t[:, 0:n], in0=t[:, 0:n], in1=nb[:, 0:n], op=ALU.add),
                 reads=[t, nb], writes=[t])
            for (ot, apf, func) in outs:
                S.op("act", lambda e, c=c, t=t, apf=apf, func=func: e.activation(
                    out=apf(c), in_=t[:, 0:n], func=func, bias=bcol(c), scale=gcol(c)),
                    reads=[t, self.LNP, self.MOD] + list(greads), writes=[ot])

    def stage_mod(self):
        S, cfg = self.S, self.cfg
        sc = Scope(S)
        craw = sc.sb("craw", [P, 16], F32)
        cond = sc.sb("cond", [P, 8, 2], F32)
        bmod = sc.sb("bmod", [P, cfg.DEPTH, 48], F32)
        wm = Rot([sc.sb("wm%d" % k, [P, 8, 768], F32) for k in range(2)])
        pst = Rot([sc.ps("pmod%d" % k) for k in range(2)])
        S.dma("sp", craw, craw[:, 0:8], self.t_in, self.io["cT"][:, :])
        S.dma("sp", craw, craw[:, 8:16], self.t_in, self.io["cctxT"][:, :])
        S.dma("sp", bmod, bmod[:].rearrange("p a b -> p (a b)"), self.t_in, self.io["b_modT"][:, :])
        S.op("act", lambda e: e.activation(out=cond[:, :, 0], in_=craw[:, 0:8], func=AF.Silu), reads=[craw], writes=[cond])
        S.op("act", lambda e: e.activation(out=cond[:, :, 1], in_=craw[:, 8:16], func=AF.Silu), reads=[craw], writes=[cond])
        for i in range(cfg.DEPTH):
            wv = self.wview(self.io["w_mod"][i])
            for nb in range(8):
                w = wm.next()
                S.dma("sp", w, w[:], self.t_in, wv[:, :, nb * 768:(nb + 1) * 768])
                for c6 in range(6):
                    ch = nb * 6 + c6
                    ps = pst.next()
                    for k in range(8):
                        S.op("pe", lambda e, k=k, c6=c6, w=w, ps=ps: e.matmul(
                            ps[:, 0:2], w[:, k, c6 * 128:(c6 + 1) * 128], cond[:, k, :], start=(k == 0), stop=(k == 7)),
                            reads=[w, cond], writes=[ps], sig=(k == 7))
                    S.op("dve", lambda e, ch=ch, ps=ps, i=i: e.tensor_scalar(
                        out=self.MOD[:, i, ch, :], in0=ps[:, 0:2], scalar1=bmod[:, i, ch:ch + 1], scalar2=None, op0=ALU.add),
                        reads=[ps, bmod], writes=[self.MOD])
            for s in (1, 4):
                S.op("dve", lambda e, s=s, i=i: e.tensor_scalar(
                    out=self.MOD[:, i, s * 8:(s + 1) * 8, :], in0=self.MOD[:, i, s * 8:(s + 1) * 8, :], scalar1=1.0,
                    scalar2=None, op0=ALU.add), reads=[self.MOD], writes=[self.MOD])
        sc.close()

    def mixer_attn(self, i, j, last):
        S, cfg = self.S, self.cfg
        SEQ, T = cfg.SEQ, cfg.T
        lam_init = 0.8 - 0.6 * float(np.exp(-0.3 * i))
        sc = Scope(S)
        UT = sc.sb("UT", [P, 8, T], BF16)
        sc0 = Scope(S)
        xb = Rot([sc0.sb("xb%d" % k, [P, 8, 512], F32) for k in range(2)])
        for blk in cfg.blocks:
            t0, n, is_ctx = blk
            x = xb.next()
            st, sap = self.xsrc(i, blk)
            S.dma("sp", x, x[:, :, 0:n], st, sap)
            self.modulate([UT], x, lambda c, t0=t0, n=n: UT[:, c, t0:t0 + n], i, n, 1 if is_ctx else 0, 0, 1)
        sc0.close()
        COS = sc.sb("COS", [P, SEQ], F32)
        SIN = sc.sb("SIN", [P, SEQ], F32)
        S.dma("sp", COS, COS[:], self.t_in, self.io["ropeC"][:, :])
        S.dma("sp", SIN, SIN[:], self.t_in, self.io["ropeS"][:, :])
        lamt = sc.sb("lamt", [P, 4, 64], F32)
        S.dma("sp", lamt, lamt[:].rearrange("p a b -> p (a b)"), self.t_in,
              self.io["lam"][j].rearrange("a b -> (a b)").partition_broadcast(P))
        lsc = sc.sb("lsc", [P, 8], F32)
        S.op("dve", lambda e: e.tensor_tensor(out=lamt[:, 0, :], in0=lamt[:, 0, :], in1=lamt[:, 1, :], op=ALU.mult),
             reads=[lamt], writes=[lamt])
        S.op("dve", lambda e: e.tensor_tensor(out=lamt[:, 2, :], in0=lamt[:, 2, :], in1=lamt[:, 3, :], op=ALU.mult),
             reads=[lamt], writes=[lamt])
        S.op("dve", lambda e: e.reduce_sum(out=lsc[:, 0:1], in_=lamt[:, 0, :], axis=AX.X), reads=[lamt], writes=[lsc])
        S.op("dve", lambda e: e.reduce_sum(out=lsc[:, 1:2], in_=lamt[:, 2, :], axis=AX.X), reads=[lamt], writes=[lsc])
        S.op("act", lambda e: e.activation(out=lsc[:, 2:4], in_=lsc[:, 0:2], func=AF.Exp), reads=[lsc], writes=[lsc])
        S.op("dve", lambda e: e.scalar_tensor_tensor(out=lsc[:, 4:5], in0=lsc[:, 3:4], scalar=-lam_init, in1=lsc[:, 2:3],
                                                     op0=ALU.add, op1=ALU.subtract), reads=[lsc], writes=[lsc])
        NA = cfg.N_A
        sgt0 = sc.sb("sgt0", [P, NA], F32)
        sgt = sc.sb("sgt", [P, 2], F32)
        S.dma("sp", sgt0, sgt0[:], self.t_in, self.io["sublnT"][:, :])
        S.op("dve", lambda e: e.tensor_scalar(out=sgt[:, 1:2], in0=sgt0[:, j:j + 1], scalar1=float((1.0 - lam_init) * np.sqrt(128.0)),
                                              scalar2=None, op0=ALU.mult), reads=[sgt0], writes=[sgt])
        WQ = Rot([sc.sb("WQ%d" % k, [P, 8, 3, 128], BF16) for k in range(2)])
        WR = Rot([sc.sb("WR%d" % k, [P, 8, 2, 128], BF16) for k in range(2)])
        QT = Rot([sc.sb("QT%d" % k, [P, T], BF16) for k in range(2)])
        KT = Rot([sc.sb("KT%d" % k, [P, T], BF16) for k in range(2)])
        VV = Rot([sc.sb("VV%d" % k, [P, T // 128, 128], BF16) for k in range(2)])
        pp = Rot([sc.ps("pp%d" % k) for k in range(3)])
        pO = [sc.ps("pO%d" % k) for k in range(2)]
        pL = [sc.ps("pL%d" % k) for k in range(2)]
        pR = sc.ps("pR")
        tmpa = Rot([sc.sb("tmpa%d" % k, [P, 512], F32) for k in range(4)])
        ET = Rot([sc.sb("ET%d" % k, [P, 512], BF16) for k in range(3)])
        ob = Rot([sc.sb("ob%d" % k, [P, 512], BF16) for k in range(2)])
        wq = self.wview(self.io["attn_w_qkv"][j])
        mixv = self.MIX
        qblocks = cfg.blocks if not last else cfg.lat_blocks
        for h in range(cfg.H):
            W = WQ.next()
            R = WR.next()
            for s3 in range(3):
                S.dma("pool", W, W[:, :, s3, :], self.t_in, wq[:, :, s3 * 1024 + h * 128: s3 * 1024 + (h + 1) * 128])
            for s2 in range(2):
                src = W[:, :, s2, :].rearrange("p k (b h j) -> p k b h j", b=4, h=2)
                dst = R[:, :, s2, :].rearrange("p k (b h j) -> p k b h j", b=4, h=2)
                for k in range(8):
                    S.op("dve", lambda e, src=src, dst=dst, k=k: e.tensor_scalar(
                        out=dst[:, k, :, 0, :], in0=src[:, k, :, 1, :], scalar1=-1.0, scalar2=None, op0=ALU.mult),
                        reads=[W], writes=[R])
                    S.op("dve", lambda e, src=src, dst=dst, k=k: e.tensor_copy(out=dst[:, k, :, 1, :], in_=src[:, k, :, 0, :]),
                         reads=[W], writes=[R])
            Q, K, V = QT.next(), KT.next(), VV.next()
            for blk in cfg.blocks:
                t0, n, is_ctx = blk
                for s2, dest in ((0, Q), (1, K)):
                    p1 = pp.next()
                    for k in range(8):
                        S.op("pe", lambda e, k=k, p1=p1, s2=s2: e.matmul(p1[:, 0:n], W[:, k, s2, :], UT[:, k, t0:t0 + n],
                                                                         start=(k == 0), stop=(k == 7)),
                             reads=[W, UT], writes=[p1], sig=(k == 7))
                    if is_ctx:
                        S.op("act", lambda e, p1=p1, dest=dest: e.copy(out=dest[:, t0:t0 + n], in_=p1[:, 0:n]),
                             reads=[p1], writes=[dest])
                    else:
                        p2 = pp.next()
                        for k in range(8):
                            S.op("pe", lambda e, k=k, p2=p2, s2=s2: e.matmul(p2[:, 0:n], R[:, k, s2, :], UT[:, k, t0:t0 + n],
                                                                             start=(k == 0), stop=(k == 7)),
                                 reads=[R, UT], writes=[p2], sig=(k == 7))
                        a1, a2 = tmpa.next(), tmpa.next()
                        S.op("dve", lambda e, p1=p1, a1=a1: e.tensor_tensor(out=a1[:, 0:n], in0=p1[:, 0:n], in1=COS[:, t0:t0 + n], op=ALU.mult),
                             reads=[p1, COS], writes=[a1])
                        S.op("dve", lambda e, p2=p2, a2=a2: e.tensor_tensor(out=a2[:, 0:n], in0=p2[:, 0:n], in1=SIN[:, t0:t0 + n], op=ALU.mult),
                             reads=[p2, SIN], writes=[a2])
                        S.op("pool", lambda e, a1=a1, a2=a2, dest=dest: e.tensor_tensor(out=dest[:, t0:t0 + n], in0=a1[:, 0:n], in1=a2[:, 0:n], op=ALU.add),
                             reads=[a1, a2], writes=[dest])
                for s in range(n // 128):
                    p1 = pp.next()
                    tt = t0 + s * 128
                    for k in range(8):
                        S.op("pe", lambda e, k=k, p1=p1, tt=tt: e.matmul(p1[:, 0:128], UT[:, k, tt:tt + 128], W[:, k, 2, :],
                                                                         start=(k == 0), stop=(k == 7)),
                             reads=[W, UT], writes=[p1], sig=(k == 7))
                    S.op("act", lambda e, p1=p1, tt=tt: e.copy(out=V[:, tt // 128, :], in_=p1[:, 0:128]), reads=[p1], writes=[V])
            for qb in qblocks:
                q0, nq, q_ctx = qb
                kchunks = list(range(SEQ // 128, T // 128)) if q_ctx else list(range(T // 128))
                for m in range(2):
                    lo, hi = m * 64, (m + 1) * 64
                    for ki, kc in enumerate(kchunks):
                        ps = pp.next()
                        S.op("pe", lambda e, ps=ps, kc=kc, lo=lo, hi=hi: e.matmul(
                            ps[:, 0:nq], K[lo:hi, kc * 128:(kc + 1) * 128], Q[lo:hi, q0:q0 + nq], start=True, stop=True),
                            reads=[K, Q], writes=[ps])
                        et = ET.next()
                        S.op("act", lambda e, ps=ps, et=et: e.activation(out=et[:, 0:nq], in_=ps[:, 0:nq], func=AF.Exp, scale=0.125),
                             reads=[ps], writes=[et])
                        fst, lst = ki == 0, ki == len(kchunks) - 1
                        S.op("pe", lambda e, et=et, kc=kc, m=m, fst=fst, lst=lst: e.matmul(
                            pO[m][:, 0:nq], V[:, kc, :], et[:, 0:nq], start=fst, stop=lst),
                            reads=[V, et], writes=[pO[m]], sig=False)
                        S.op("pe", lambda e, et=et, m=m, fst=fst, lst=lst: e.matmul(
                            pL[m][:, 0:nq], self.onesbf[:], et[:, 0:nq], start=fst, stop=lst),
                            reads=[self.onesbf, et], writes=[pL[m]], sig=True)
                r1, a1, r2, a2 = tmpa.next(), tmpa.next(), tmpa.next(), tmpa.next()
                S.op("dve", lambda e, r1=r1: e.reciprocal(out=r1[:, 0:nq], in_=pL[0][:, 0:nq]), reads=[pL[0]], writes=[r1])
                S.op("dve", lambda e, r1=r1, a1=a1: e.tensor_tensor(out=a1[:, 0:nq], in0=pO[0][:, 0:nq], in1=r1[:, 0:nq], op=ALU.mult),
                     reads=[pO[0], r1], writes=[a1])
                S.op("dve", lambda e, r2=r2: e.reciprocal(out=r2[:, 0:nq], in_=pL[1][:, 0:nq]), reads=[pL[1]], writes=[r2])
                S.op("dve", lambda e, r2=r2, a2=a2: e.tensor_tensor(out=a2[:, 0:nq], in0=pO[1][:, 0:nq], in1=r2[:, 0:nq], op=ALU.mult),
                     reads=[pO[1], r2], writes=[a2])
                S.op("dve", lambda e, a1=a1, a2=a2: e.scalar_tensor_tensor(out=a1[:, 0:nq], in0=a2[:, 0:nq], scalar=lsc[:, 4:5], in1=a1[:, 0:nq],
                                                                          op0=ALU.mult, op1=ALU.add), reads=[a1, a2, lsc], writes=[a1])
                S.op("act", lambda e, a1=a1, r1=r1: e.activation(out=r1[:, 0:nq], in_=a1[:, 0:nq], func=AF.Square), reads=[a1], writes=[r1])
                S.op("pe", lambda e, r1=r1: e.matmul(pR[:, 0:nq], self.ones32[:], r1[:, 0:nq], start=True, stop=True),
                     reads=[self.ones32, r1], writes=[pR])
                S.op("act", lambda e, r2=r2: e.activation(out=r2[:, 0:nq], in_=pR[:, 0:nq], func=AF.Sqrt, bias=self.cst[:, 1:2]),
                     reads=[pR, self.cst], writes=[r2])
                S.op("dve", lambda e, r2=r2: e.reciprocal(out=r2[:, 0:nq], in_=r2[:, 0:nq]), reads=[r2], writes=[r2])
                S.op("dve", lambda e, a1=a1, r2=r2: e.tensor_tensor(out=a1[:, 0:nq], in0=a1[:, 0:nq], in1=r2[:, 0:nq], op=ALU.mult),
                     reads=[a1, r2], writes=[a1])
                o = ob.next()
                S.op("act", lambda e, a1=a1, o=o: e.activation(out=o[:, 0:nq], in_=a1[:, 0:nq], func=AF.Identity, scale=sgt[:, 1:2]),
                     reads=[a1, sgt], writes=[o])
                S.dma("sp", self.t_MIX, mixv[h * 128:(h + 1) * 128, q0:q0 + nq], o, o[:, 0:nq])
        sc.close()

    def mixer_fnet(self, i, j, last):
        S, cfg = self.S, self.cfg
        parts = [(0, cfg.SEQ, cfg.lat_blocks, self.io["dftL"], 0)]
        if not last:
            parts.append((cfg.SEQ, cfg.CTX, cfg.ctx_blocks, self.io["dftC"], 1))
        for (off, N, blks, dft, col) in parts:
            sc = Scope(S)
            NCH = N // 128
            ACS = sc.sb("ACS", [P, NCH, 8, 256], BF16)
            cs128 = sc.sb("cs128", [P, 256], BF16)
            S.dma("sp", cs128, cs128[:], self.t_in, self.io["dft128"][:, :])
            sc1 = Scope(S)
            xb = Rot([sc1.sb("xb%d" % k, [P, 8, 512], F32) for k in range(2)])
            ub = Rot([sc1.sb("ub%d" % k, [P, 8, 512], BF16) for k in range(2)])
            pp = Rot([sc1.ps("pp%d" % k) for k in range(4)])
            for blk in blks:
                t0, n, is_ctx = blk
                x = xb.next()
                u = ub.next()
                st, sap = self.xsrc(i, blk)
                S.dma("sp", x, x[:, :, 0:n], st, sap)
                self.modulate([u], x, lambda c, u=u, n=n: u[:, c, 0:n], i, n, col, 0, 1)
                for g in range(8):
                    for s in range(n // 128):
                        p1 = pp.next()
                        nch = (t0 - off) // 128 + s
                        S.op("pe", lambda e, p1=p1, u=u, g=g, s=s: e.matmul(p1[:, 0:256], u[:, g, s * 128:(s + 1) * 128], cs128[:],
                                                                            start=True, stop=True), reads=[u, cs128], writes=[p1])
                        eng = "act" if (g + s) % 2 == 0 else "dve"
                        if eng == "act":
                            S.op("act", lambda e, p1=p1, nch=nch, g=g: e.copy(out=ACS[:, nch, g, :], in_=p1[:, 0:256]), reads=[p1], writes=[ACS])
                        else:
                            S.op("dve", lambda e, p1=p1, nch=nch, g=g: e.tensor_copy(out=ACS[:, nch, g, :], in_=p1[:, 0:256]), reads=[p1], writes=[ACS])
            sc1.close()
            sc2 = Scope(S)
            acc = [sc2.ps("acc%d" % g) for g in range(8)]
            CP = Rot([sc2.sb("CP%d" % k, [P, 2, 512], BF16) for k in range(4)])
            ob = Rot([sc2.sb("ob%d" % k, [P, 512], BF16) for k in range(4)])
            scale = float(1.0 / np.sqrt(N * 128.0))
            kbw = min(512, N)
            for kb in range(N // kbw):
                for nchk in range(NCH):
                    cp = CP.next()
                    for s2 in range(2):
                        S.dma("sp", cp, cp[:, s2, 0:kbw], self.t_in, dft[s2, nchk * 128:(nchk + 1) * 128, kb * kbw:(kb + 1) * kbw])
                    for g in range(8):
                        for s2 in range(2):
                            S.op("pe", lambda e, g=g, s2=s2, cp=cp, nchk=nchk: e.matmul(
                                acc[g][:, 0:kbw], ACS[:, nchk, g, s2 * 128:(s2 + 1) * 128], cp[:, s2, 0:kbw],
                                start=(nchk == 0 and s2 == 0), stop=(nchk == NCH - 1 and s2 == 1)),
                                reads=[ACS, cp], writes=[acc[g]], sig=(s2 == 1 and (g == 7 or nchk == NCH - 1)))
                for g in range(8):
                    o = ob.next()
                    if g % 2 == 0:
                        S.op("act", lambda e, o=o, g=g: e.activation(out=o[:, 0:kbw], in_=acc[g][:, 0:kbw], func=AF.Copy, scale=scale),
                             reads=[acc[g]], writes=[o])
                    else:
                        S.op("dve", lambda e, o=o, g=g: e.tensor_scalar(out=o[:, 0:kbw], in0=acc[g][:, 0:kbw], scalar1=scale, scalar2=None, op0=ALU.mult),
                             reads=[acc[g]], writes=[o])
                    S.dma("sp", self.t_MIX, self.MIX[g * 128:(g + 1) * 128, off + kb * kbw: off + (kb + 1) * kbw], o, o[:, 0:kbw])
            sc2.close()
            sc.close()

    def mixer_conv(self, i, j, last):
        S, cfg = self.S, self.cfg
        SEQ, CTX = cfg.SEQ, cfg.CTX
        sc = Scope(S)
        HW = 15 + SEQ + 30 + CTX + 15
        HG = sc.sb("HG", [P, 8, HW], BF16)
        lat_off, ctx_off = 15, 15 + SEQ + 30
        for (a, b) in ((0, 15), (15 + SEQ, 15 + SEQ + 30), (HW - 15, HW)):
            S.op("pool", lambda e, a=a, b=b: e.memset(HG[:, :, a:b], 0.0), writes=[HG])
        W1 = sc.sb("W1", [P, 8, 2048], BF16)
        w1v = self.wview(self.io["conv_w_pw1"][j])
        for q in range(4):
            S.dma("pool", W1, W1[:, :, q * 512:(q + 1) * 512], self.t_in, w1v[:, :, q * 512:(q + 1) * 512])
        b1 = sc.sb("b1", [P, 16], F32)
        S.dma("sp", b1, b1[:], self.t_in, self.io["conv_b_pw1T"][:, j * 16:(j + 1) * 16])
        wdw = sc.sb("wdw", [P, 8, 31], F32)
        S.dma("sp", wdw, wdw[:].rearrange("p a b -> p (a b)"), self.t_in, self.io["conv_w_dwT"][:, j * 248:(j + 1) * 248])
        cp = sc.sb("cp", [P, 4, 8], F32)
        S.dma("sp", cp, cp[:].rearrange("p a b -> p (a b)"), self.t_in, self.io["conv_pT"][:, j * 32:(j + 1) * 32])
        blks = cfg.blocks if not last else cfg.lat_blocks

        def hoff(blk):
            t0, n, is_ctx = blk
            return (ctx_off + t0 - SEQ) if is_ctx else (lat_off + t0)
        sc1 = Scope(S)
        xb = Rot([sc1.sb("xb%d" % k, [P, 8, 512], F32) for k in range(2)])
        ub = Rot([sc1.sb("ub%d" % k, [P, 8, 512], BF16) for k in range(2)])
        pp = Rot([sc1.ps("pp%d" % k) for k in range(6)])
        ta = Rot([sc1.sb("ta%d" % k, [P, 512], F32) for k in range(3)])
        tg = Rot([sc1.sb("tg%d" % k, [P, 512], F32) for k in range(3)])
        for blk in blks:
            t0, n, is_ctx = blk
            x, u = xb.next(), ub.next()
            st, sap = self.xsrc(i, blk)
            S.dma("sp", x, x[:, :, 0:n], st, sap)
            self.modulate([u], x, lambda c, u=u, n=n: u[:, c, 0:n], i, n, 1 if is_ctx else 0, 0, 1)
            ho = hoff(blk)
            for jj in range(8):
                pa, pg = pp.next(), pp.next()
                for (pt, cb) in ((pa, jj * 128), (pg, 1024 + jj * 128)):
                    for k in range(8):
                        S.op("pe", lambda e, pt=pt, cb=cb, k=k, u=u: e.matmul(pt[:, 0:n], W1[:, k, cb:cb + 128], u[:, k, 0:n],
                                                                             start=(k == 0), stop=(k == 7)),
                             reads=[W1, u], writes=[pt], sig=(k == 7))
                a, g = ta.next(), tg.next()
                S.op("dve", lambda e, pa=pa, a=a, jj=jj: e.tensor_scalar(out=a[:, 0:n], in0=pa[:, 0:n], scalar1=b1[:, jj:jj + 1], scalar2=None, op0=ALU.add),
                     reads=[pa, b1], writes=[a])
                S.op("act", lambda e, pg=pg, g=g, jj=jj: e.activation(out=g[:, 0:n], in_=pg[:, 0:n], func=AF.Sigmoid, bias=b1[:, 8 + jj:9 + jj]),
                     reads=[pg, b1], writes=[g])
                S.op("pool", lambda e, a=a, g=g, jj=jj, ho=ho: e.tensor_tensor(out=HG[:, jj, ho:ho + n], in0=a[:, 0:n], in1=g[:, 0:n], op=ALU.mult),
                     reads=[a, g], writes=[HG])
        sc1.close()
        sc2 = Scope(S)
        zb = Rot([sc2.sb("zb%d" % k, [P, 8, 512], F32) for k in range(2)])
        accB = Rot([sc2.sb("accB%d" % k, [P, 512], F32) for k in range(2)])
        tmp = dict(sq=sc2.sb("sq", [P, 8, 512], F32), pss=sc2.ps("pss"), psq=sc2.ps("psq"),
                   mean=sc2.sb("mean", [P, 512], F32), rstd=sc2.sb("rstd", [P, 512], F32), nb=sc2.sb("nb", [P, 512], F32),
                   t=Rot([sc2.sb("lt%d" % k, [P, 512], F32) for k in range(3)]))
        mb = Rot([sc2.sb("mb%d" % k, [P, 8, 512], BF16) for k in range(2)])
        for blk in blks:
            t0, n, is_ctx = blk
            ho = hoff(blk)
            z = zb.next()
            for c in range(8):
                ab = accB.next()
                for tap in range(31):
                    src_off = ho + tap - 15
                    if tap == 0:
                        S.op("dve", lambda e, c=c, src_off=src_off: e.tensor_scalar(
                            out=z[:, c, 0:n], in0=HG[:, c, src_off:src_off + n], scalar1=wdw[:, c, 0:1], scalar2=cp[:, 0, c:c + 1],
                            op0=ALU.mult, op1=ALU.add), reads=[HG, wdw, cp], writes=[z])
                    else:
                        S.op("dve", lambda e, c=c, src_off=src_off, tap=tap: e.scalar_tensor_tensor(
                            out=z[:, c, 0:n], in0=HG[:, c, src_off:src_off + n], scalar=wdw[:, c, tap:tap + 1], in1=z[:, c, 0:n],
                            op0=ALU.mult, op1=ALU.add), reads=[HG, wdw, z], writes=[z])
            m = mb.next()
            self.layer_norm(sc2, z, n, lambda c: cp[:, 1, c:c + 1], lambda c: cp[:, 2, c:c + 1],
                            [(m, lambda c, m=m, n=n: m[:, c, 0:n], AF.Silu)], tmp, greads=[cp])
            S.dma("sp", self.t_MIX, self.MIX.rearrange("(k p) t -> p k t", p=P)[:, :, t0:t0 + n], m, m[:, :, 0:n])
        sc2.close()
        sc.close()

    def post_moe(self, i, kind, j, last):
        S, cfg = self.S, self.cfg
        E = cfg.E
        blks = cfg.blocks if not last else cfg.lat_blocks
        sbs, cur, tot = [], [], 0
        for b in blks:
            if b[2] and cur and tot + b[1] <= cfg.SB_MAX + 256:
                cur.append(b)
                tot += b[1]
                continue
            if tot + b[1] > cfg.SB_MAX:
                sbs.append(cur)
                cur, tot = [], 0
            cur.append(b)
            tot += b[1]
        if cur:
            sbs.append(cur)
        if kind == 0:
            wo_ap, bo_ap = self.io["attn_w_o"][j], None
        elif kind == 1:
            wo_ap, bo_ap = self.io["fnet_w"][j], self.io["fnet_bT"][:, j * 8:(j + 1) * 8]
        else:
            wo_ap, bo_ap = self.io["conv_w_pw2"][j], self.io["conv_pT"][:, j * 32 + 24:j * 32 + 32]
        L = Scope(S)
        WO = L.sb("WO", [P, 8, 1024], BF16)
        wov = self.wview(wo_ap)
        for q in range(2):
            S.dma("pool", WO, WO[:, :, q * 512:(q + 1) * 512], self.t_in, wov[:, :, q * 512:(q + 1) * 512])
        bo = L.sb("bo", [P, 8], F32)
        if bo_ap is None:
            S.op("dve", lambda e: e.memset(bo[:], 0.0), writes=[bo])
        else:
            S.dma("sp", bo, bo[:], self.t_in, bo_ap)
        for col in range(2):
            S.op("dve", lambda e, col=col: e.tensor_tensor(out=self.GB1[:, i, :, col], in0=self.MOD[:, i, 16:24, col], in1=bo[:], op=ALU.mult),
                 reads=[self.MOD, bo], writes=[self.GB1])
        WR = L.sb("WR", [P, 8, E], F32)
        S.dma("sp", WR, WR[:], self.t_in, self.wview(self.io["moe_w_router"][i]))
        BR = L.sb("BR", [P, E], F32)
        S.dma("sp", BR, BR[:], self.t_in, self.io["moe_b_router"][i].partition_broadcast(P))
        B1 = L.sb("B1", [P, E, 16], F32)
        S.dma("sp", B1, B1[:].rearrange("p a b -> p (a b)"), self.t_in, self.io["moe_b1T"][:, i * E * 16:(i + 1) * E * 16])
        B2 = L.sb("B2", [E, 1024], F32)
        S.dma("sp", B2, B2[:], self.t_in, self.io["moe_b2"][i])
        xxv = self.XX.rearrange("(k p) t -> p k t", p=P)
        x1v = self.X1.rearrange("(k p) t -> p k t", p=P)
        mixv = self.MIX.rearrange("(k p) t -> p k t", p=P)
        outv = self.yT.rearrange("(k p) t -> p k t", p=P)
        for sb in sbs:
            Tb = sum(b[1] for b in sb)
            offs = []
            o = 0
            for b in sb:
                offs.append(o)
                o += b[1]
            sbt0 = sb[0][0]
            M = Scope(S)
            VT = M.sb("VT", [P, 8, Tb], BF16)
            A = Scope(S)
            xb = Rot([A.sb("xb%d" % k, [P, 8, 512], F32) for k in range(2)])
            mb = Rot([A.sb("mb%d" % k, [P, 8, 512], BF16) for k in range(2)])
            zb = A.sb("zb", [P, 8, 512], F32)
            x1b = Rot([A.sb("x1b%d" % k, [P, 8, 512], F32) for k in range(1)])
            v32 = A.sb("v32", [P, 8, 512], F32)
            tmp = dict(sq=A.sb("sq", [P, 8, 512], F32), pss=A.ps("pss"), psq=A.ps("psq"),
                       mean=A.sb("mean", [P, 512], F32), rstd=A.sb("rstd", [P, 512], F32), nb=A.sb("nb", [P, 512], F32),
                       t=Rot([A.sb("lt%d" % k, [P, 512], F32) for k in range(3)]))
            pp = Rot([A.ps("pp%d" % k) for k in range(3)])
            pl = Rot([A.ps("pl%d" % k) for k in range(2)])
            pt = A.ps("ptr")
            lg = Rot([A.sb("lg%d" % k, [P, E], F32) for k in range(2)])
            mx = Rot([A.sb("mx%d" % k, [P, 8], F32) for k in range(2)])
            gm = Rot([A.sb("gm%d" % k, [P, E], F32) for k in range(2)])
            ge = Rot([A.sb("ge%d" % k, [P, E], F32) for k in range(2)])
            gs = Rot([A.sb("gs%d" % k, [P, 2], F32) for k in range(2)])
            gt = Rot([A.sb("gt%d" % k, [E, 128], F32) for k in range(2)])
            for bi, blk in enumerate(sb):
                t0, n, is_ctx = blk
                col = 1 if is_ctx else 0
                x, m = xb.next(), mb.next()
                st, sap = self.xsrc(i, blk)
                S.dma("sp", x, x[:, :, 0:n], st, sap)
                S.dma("sp", m, m[:, :, 0:n], self.t_MIX, mixv[:, :, t0:t0 + n])
                for c in range(8):
                    S.op("act", lambda e, c=c, x=x: e.activation(out=x[:, c, 0:n], in_=x[:, c, 0:n], func=AF.Identity,
                                                                 scale=cfg.alpha, bias=self.GB1[:, i, c, col:col + 1]),
                         reads=[x, self.GB1], writes=[x])
                for dch in range(8):
                    py = pp.next()
                    for k in range(8):
                        S.op("pe", lambda e, py=py, k=k, dch=dch, m=m: e.matmul(py[:, 0:n], WO[:, k, dch * 128:(dch + 1) * 128], m[:, k, 0:n],
                                                                               start=(k == 0), stop=(k == 7)),
                             reads=[WO, m], writes=[py], sig=(k == 7))
                    S.op("dve", lambda e, py=py, dch=dch, x=x: e.scalar_tensor_tensor(
                        out=zb[:, dch, 0:n], in0=py[:, 0:n], scalar=self.MOD[:, i, 16 + dch, col:col + 1], in1=x[:, dch, 0:n],
                        op0=ALU.mult, op1=ALU.add), reads=[py, self.MOD, x], writes=[zb])
                x1 = x1b.next()
                self.layer_norm(A, zb, n, lambda c: self.LNP[:, i, 0, c:c + 1], lambda c: self.LNP[:, i, 1, c:c + 1],
                                [(x1, lambda c, x1=x1, n=n: x1[:, c, 0:n], AF.Identity)], tmp)
                S.dma("sp", self.t_X1, x1v[:, :, t0:t0 + n], x1, x1[:, :, 0:n])
                for c in range(8):
                    S.op("act", lambda e, c=c, x1=x1: e.activation(out=v32[:, c, 0:n], in_=x1[:, c, 0:n], func=AF.Identity,
                                                                   scale=self.MOD[:, i, 32 + c, col:col + 1], bias=self.MOD[:, i, 24 + c, col:col + 1]),
                         reads=[x1, self.MOD], writes=[v32])
                    eng = "dve" if c % 2 == 0 else "pool"
                    S.op(eng, lambda e, c=c, o=offs[bi]: e.tensor_copy(out=VT[:, c, o:o + n], in_=v32[:, c, 0:n]), reads=[v32], writes=[VT])
                for s in range(n // 128):
                    p1 = pl.next()
                    for k in range(8):
                        S.op("pe", lambda e, p1=p1, k=k, s=s: e.matmul(p1[:, 0:E], v32[:, k, s * 128:(s + 1) * 128], WR[:, k, :],
                                                                       start=(k == 0), stop=(k == 7)),
                             reads=[v32, WR], writes=[p1], sig=(k == 7))
                    l, mxx, gmm, gee, gss, gtt = lg.next(), mx.next(), gm.next(), ge.next(), gs.next(), gt.next()
                    S.op("dve", lambda e, l=l, p1=p1: e.tensor_tensor(out=l[:], in0=p1[:, 0:E], in1=BR[:], op=ALU.add), reads=[p1, BR], writes=[l])
                    S.op("dve", lambda e, l=l, mxx=mxx: e.max(out=mxx[:], in_=l[:]), reads=[l], writes=[mxx])
                    S.op("dve", lambda e, l=l, mxx=mxx, gmm=gmm: e.tensor_scalar(out=gmm[:], in0=l[:], scalar1=mxx[:, 3:4], scalar2=None, op0=ALU.is_ge),
                         reads=[l, mxx], writes=[gmm])
                    S.op("dve", lambda e, mxx=mxx, gss=gss: e.tensor_scalar(out=gss[:, 0:1], in0=mxx[:, 0:1], scalar1=-1.0, scalar2=None, op0=ALU.mult),
                         reads=[mxx], writes=[gss])
                    S.op("act", lambda e, l=l, gee=gee, gss=gss: e.activation(out=gee[:], in_=l[:], func=AF.Exp, bias=gss[:, 0:1]),
                         reads=[l, gss], writes=[gee])
                    S.op("dve", lambda e, gee=gee, gmm=gmm: e.tensor_tensor(out=gee[:], in0=gee[:], in1=gmm[:], op=ALU.mult), reads=[gee, gmm], writes=[gee])
                    S.op("dve", lambda e, gee=gee, gss=gss: e.reduce_sum(out=gss[:, 1:2], in_=gee[:], axis=AX.X), reads=[gee], writes=[gss])
                    S.op("dve", lambda e, gss=gss: e.reciprocal(out=gss[:, 1:2], in_=gss[:, 1:2]), reads=[gss], writes=[gss])
                    S.op("dve", lambda e, gee=gee, gss=gss: e.tensor_scalar(out=gee[:], in0=gee[:], scalar1=gss[:, 1:2], scalar2=None, op0=ALU.mult),
                         reads=[gee, gss], writes=[gee])
                    S.op("pe", lambda e, gee=gee: e.transpose(pt[0:E, 0:128], gee[:], self.ident[:]), reads=[gee, self.ident], writes=[pt])
                    S.op("act", lambda e, gtt=gtt: e.copy(out=gtt[:], in_=pt[0:E, 0:128]), reads=[pt], writes=[gtt])
                    tt = t0 + s * 128
                    S.dma("sp", self.t_GD, self.GD[:, tt:tt + 128], gtt, gtt[:])
            A.close()
            Mf = Scope(S)
            Fa = Mf.sb("Fa", [P, 8, Tb], F32)
            B = Scope(S)
            Hh = B.sb("Hh", [P, 8, Tb], BF16)
            GBt = Rot([B.sb("GBt%d" % k, [P, Tb], F32) for k in range(2)])
            GTs = B.sb("GTs", [E, Tb], F32)
            S.dma("sp", GTs, GTs[:], self.t_GD, self.GD[:, sbt0:sbt0 + Tb])
            w1r = Rot([B.sb("w1r%d" % k, [P, 8, 2, 256], BF16) for k in range(3)])
            w2r = Rot([B.sb("w2r%d" % k, [P, 8, 256], BF16) for k in range(3)])
            pg = Rot([B.ps("pg%d" % k) for k in range(3)])
            plin = Rot([B.ps("plin%d" % k) for k in range(3)])
            py2 = Rot([B.ps("py%d" % k) for k in range(2)])
            ta = Rot([B.sb("ta%d" % k, [P, 512], F32) for k in range(2)])
            ts_ = Rot([B.sb("ts%d" % k, [P, 512], F32) for k in range(2)])
            tl = Rot([B.sb("tl%d" % k, [P, 512], F32) for k in range(2)])
            tl2 = Rot([B.sb("tl2%d" % k, [P, 512], F32) for k in range(2)])
            tas = Rot([B.sb("tas%d" % k, [P, 512], F32) for k in range(2)])
            for ex in range(E):
                gb = GBt.next()
                S.dma("sp", gb, gb[:], self.t_GD, self.GD[ex, sbt0:sbt0 + Tb].partition_broadcast(P))
                w1v = self.wview(self.io["moe_w1"][i, ex])
                w2v = self.wview(self.io["moe_w2"][i, ex])
                for j4 in range(4):
                    w1 = w1r.next()
                    for s2 in range(2):
                        S.dma("pool", w1, w1[:, :, s2, :], self.t_in, w1v[:, :, s2 * 1024 + j4 * 256: s2 * 1024 + (j4 + 1) * 256])
                    for jj in range(2):
                        jc = j4 * 2 + jj
                        for bi, blk in enumerate(sb):
                            n, o = blk[1], offs[bi]
                            pG, pLn = pg.next(), plin.next()
                            for (ptile, s2) in ((pG, 0), (pLn, 1)):
                                for k in range(8):
                                    S.op("pe", lambda e, ptile=ptile, s2=s2, k=k, w1=w1, jj=jj, o=o, n=n: e.matmul(
                                        ptile[:, 0:n], w1[:, k, s2, jj * 128:(jj + 1) * 128], VT[:, k, o:o + n], start=(k == 0), stop=(k == 7)),
                                        reads=[w1, VT], writes=[ptile], sig=(k == 7))
                            a, sg, l, l2, as_ = ta.next(), ts_.next(), tl.next(), tl2.next(), tas.next()
                            S.op("dve", lambda e, a=a, pG=pG, n=n, jc=jc: e.tensor_scalar(
                                out=a[:, 0:n], in0=pG[:, 0:n], scalar1=B1[:, ex, jc:jc + 1], scalar2=7.0, op0=ALU.add, op1=ALU.min),
                                reads=[pG, B1], writes=[a])
                            S.op("act", lambda e, a=a, sg=sg, n=n: e.activation(out=sg[:, 0:n], in_=a[:, 0:n], func=AF.Sigmoid, scale=1.702),
                                 reads=[a], writes=[sg])
                            S.op("dve", lambda e, l=l, pLn=pLn, n=n, jc=jc: e.tensor_scalar(
                                out=l[:, 0:n], in0=pLn[:, 0:n], scalar1=B1[:, ex, 8 + jc:9 + jc], scalar2=7.0, op0=ALU.add, op1=ALU.min),
                                reads=[pLn, B1], writes=[l])
                            S.op("pool", lambda e, l=l, l2=l2, n=n: e.tensor_scalar(out=l2[:, 0:n], in0=l[:, 0:n], scalar1=-7.0, scalar2=1.0,
                                                                                   op0=ALU.max, op1=ALU.add), reads=[l], writes=[l2])
                            S.op("pool", lambda e, a=a, sg=sg, as_=as_, n=n: e.tensor_tensor(out=as_[:, 0:n], in0=a[:, 0:n], in1=sg[:, 0:n], op=ALU.mult),
                                 reads=[a, sg], writes=[as_])
                            S.op("pool", lambda e, as_=as_, l2=l2, n=n: e.tensor_tensor(out=as_[:, 0:n], in0=as_[:, 0:n], in1=l2[:, 0:n], op=ALU.mult),
                                 reads=[as_, l2], writes=[as_])
                            S.op("dve", lambda e, as_=as_, gb=gb, n=n, o=o, jc=jc: e.tensor_tensor(
                                out=Hh[:, jc, o:o + n], in0=as_[:, 0:n], in1=gb[:, o:o + n], op=ALU.mult), reads=[as_, gb], writes=[Hh])
                for d4 in range(4):
                    w2 = w2r.next()
                    S.dma("pool", w2, w2[:], self.t_in, w2v[:, :, d4 * 256:(d4 + 1) * 256])
                    for dd in range(2):
                        dch = d4 * 2 + dd
                        for bi, blk in enumerate(sb):
                            n, o = blk[1], offs[bi]
                            py = py2.next()
                            if ex == 0:
                                S.op("pe", lambda e, py=py, n=n, o=o, dch=dch: e.matmul(
                                    py[:, 0:n], B2[:, dch * 128:(dch + 1) * 128], GTs[:, o:o + n], start=True, stop=False),
                                    reads=[B2, GTs], writes=[py], sig=False)
                            for f in range(8):
                                S.op("pe", lambda e, py=py, n=n, o=o, f=f, w2=w2, dd=dd: e.matmul(
                                    py[:, 0:n], w2[:, f, dd * 128:(dd + 1) * 128], Hh[:, f, o:o + n], start=(f == 0 and ex != 0), stop=(f == 7)),
                                    reads=[w2, Hh], writes=[py], sig=(f == 7))
                            if ex == 0:
                                S.op("act", lambda e, py=py, n=n, o=o, dch=dch: e.copy(out=Fa[:, dch, o:o + n], in_=py[:, 0:n]), reads=[py], writes=[Fa])
                            else:
                                S.op("dve", lambda e, py=py, n=n, o=o, dch=dch: e.tensor_tensor(
                                    out=Fa[:, dch, o:o + n], in0=Fa[:, dch, o:o + n], in1=py[:, 0:n], op=ALU.add), reads=[py, Fa], writes=[Fa])
            B.close()
            C = Scope(S)
            xb = Rot([C.sb("xb%d" % k, [P, 8, 512], F32) for k in range(2)])
            zb = C.sb("zb", [P, 8, 512], F32)
            ob = Rot([C.sb("ob%d" % k, [P, 8, 512], F32) for k in range(2)])
            tmp = dict(sq=C.sb("sq", [P, 8, 512], F32), pss=C.ps("pss"), psq=C.ps("psq"),
                       mean=C.sb("mean", [P, 512], F32), rstd=C.sb("rstd", [P, 512], F32), nb=C.sb("nb", [P, 512], F32),
                       t=Rot([C.sb("lt%d" % k, [P, 512], F32) for k in range(3)]))
            for bi, blk in enumerate(sb):
                t0, n, is_ctx = blk
                col = 1 if is_ctx else 0
                o = offs[bi]
                x = xb.next()
                S.dma("sp", x, x[:, :, 0:n], self.t_X1, x1v[:, :, t0:t0 + n])
                for c in range(8):
                    S.op("act", lambda e, c=c, x=x: e.activation(out=x[:, c, 0:n], in_=x[:, c, 0:n], func=AF.Copy, scale=cfg.alpha),
                         reads=[x], writes=[x])
                    S.op("dve", lambda e, c=c, x=x, o=o: e.scalar_tensor_tensor(
                        out=zb[:, c, 0:n], in0=Fa[:, c, o:o + n], scalar=self.MOD[:, i, 40 + c, col:col + 1], in1=x[:, c, 0:n],
                        op0=ALU.mult, op1=ALU.add), reads=[Fa, self.MOD, x], writes=[zb])
                ot = ob.next()
                self.layer_norm(C, zb, n, lambda c: self.LNP[:, i, 2, c:c + 1], lambda c: self.LNP[:, i, 3, c:c + 1],
                                [(ot, lambda c, ot=ot, n=n: ot[:, c, 0:n], AF.Identity)], tmp)
                if last:
                    S.dma("sp", self.t_out, outv[:, :, t0:t0 + n], ot, ot[:, :, 0:n])
                else:
                    S.dma("sp", self.t_XX, xxv[:, :, t0:t0 + n], ot, ot[:, :, 0:n])
            C.close()
            Mf.close()
            M.close()
        L.close()


def pmajor(v, nch):
    v = np.asarray(v, np.float32)
    lead = v.shape[:-1]
    a = v.reshape(lead + (nch, P))
    a = np.moveaxis(a, -1, 0)
    return np.ascontiguousarray(a)


def host_constants(cfg):
    SEQ, CTX = cfg.SEQ, cfg.CTX
    GRID_W = 64
    t = np.arange(SEQ)
    rows = (t // GRID_W).astype(np.float64)
    cols = (t % GRID_W).astype(np.float64)
    inv_freq = (10000.0 ** (-np.arange(16, dtype=np.float64) / 16)).astype(np.float32).astype(np.float64)
    dd = np.arange(64)
    f = dd % 16
    pos = np.where(dd[:, None] < 32, rows[None, :], cols[None, :])
    ang = (pos.astype(np.float32) * inv_freq[f][:, None].astype(np.float32)).astype(np.float64)
    C = np.cos(ang).astype(np.float32)
    Sn = np.sin(ang).astype(np.float32)
    ropeC = np.concatenate([C, C], 0)
    ropeS = np.concatenate([Sn, Sn], 0)

    def dft(N):
        n = np.arange(N, dtype=np.int64)
        m = (n[:, None] * n[None, :]) % N
        a = 2.0 * np.pi * m.astype(np.float64) / N
        return np.stack([np.cos(a), -np.sin(a)]).astype(np.float32).astype(ml_dtypes.bfloat16)
    a128 = 2.0 * np.pi * ((np.arange(128)[:, None] * np.arange(128)[None, :]) % 128) / 128.0
    dft128 = np.concatenate([np.cos(a128), np.sin(a128)], 1).astype(np.float32).astype(ml_dtypes.bfloat16)
    return dict(ropeC=ropeC, ropeS=ropeS, dftL=dft(SEQ), dftC=dft(CTX), dft128=dft128, ident=np.eye(P, dtype=np.float32))


def host_inputs(cfg, inp, consts, b):
    f = lambda a: np.ascontiguousarray(np.asarray(a, np.float32))
    DEPTH, E = cfg.DEPTH, cfg.E
    m = {}
    m["xT"] = np.ascontiguousarray(np.asarray(inp["x"][b], np.float32).T)
    m["ctxT"] = np.ascontiguousarray(np.asarray(inp["ctx"][b], np.float32).T)
    m["cT"] = pmajor(inp["c"][b], 8)
    m["cctxT"] = pmajor(inp["c_ctx"], 8)
    m["w_mod"] = f(inp["w_mod"])
    m["b_modT"] = pmajor(inp["b_mod"], 48).reshape(P, DEPTH * 48)
    lnp = np.stack([np.asarray(inp[k], np.float32) for k in ("ln1_g", "ln1_b", "ln2_g", "ln2_b")], 1)
    m["lnp"] = pmajor(lnp, 8).reshape(P, DEPTH * 32)
    m["attn_w_qkv"] = f(inp["attn_w_qkv"])
    m["attn_w_o"] = f(inp["attn_w_o"])
    m["lam"] = np.ascontiguousarray(np.stack([np.asarray(inp[k], np.float32) for k in
                                             ("attn_lam_q1", "attn_lam_k1", "attn_lam_q2", "attn_lam_k2")], 1))
    m["sublnT"] = np.ascontiguousarray(np.asarray(inp["attn_subln_g"], np.float32).T)
    m["fnet_w"] = f(inp["fnet_w"])
    m["fnet_bT"] = pmajor(inp["fnet_b"], 8).reshape(P, -1)
    m["conv_w_pw1"] = f(inp["conv_w_pw1"])
    m["conv_b_pw1T"] = pmajor(inp["conv_b_pw1"], 16).reshape(P, -1)
    wdw = np.asarray(inp["conv_w_dw"], np.float32)
    wdw = np.moveaxis(wdw, 1, 2)
    nC = wdw.shape[0]
    wdw = wdw.reshape(nC, 8, P, 31)
    m["conv_w_dwT"] = np.ascontiguousarray(np.moveaxis(wdw, 2, 0)).reshape(P, nC * 8 * 31)
    cpar = np.stack([np.asarray(inp[k], np.float32) for k in ("conv_b_dw", "conv_ln_g", "conv_ln_b", "conv_b_pw2")], 1)
    m["conv_pT"] = pmajor(cpar, 8).reshape(P, nC * 32)
    m["conv_w_pw2"] = f(inp["conv_w_pw2"])
    m["moe_w_router"] = f(inp["moe_w_router"])
    m["moe_b_router"] = f(inp["moe_b_router"])
    m["moe_w1"] = f(inp["moe_w1"])
    m["moe_b1T"] = pmajor(inp["moe_b1"], 16).reshape(P, DEPTH * E * 16)
    m["moe_w2"] = f(inp["moe_w2"])
    m["moe_b2"] = f(inp["moe_b2"])
    m.update(consts)
    return m


_CACHE = {}


def run(cfg, inputs, n_cores):
    key = (cfg.SEQ, cfg.CTX, cfg.E, cfg.DEPTH)
    if key not in _CACHE:
        _CACHE[key] = Prog(cfg).build()
    nc = _CACHE[key]
    consts = host_constants(cfg)
    in_maps = [host_inputs(cfg, inputs, consts, b) for b in range(n_cores)]
    res = run_bass_kernel_spmd(nc, in_maps, core_ids=list(range(n_cores)))
    out = np.stack([np.ascontiguousarray(res.results[b]["yT"].T) for b in range(n_cores)], 0)
    return out.astype(np.float32)


def kernel(**inputs):
    cfg = Cfg()
    return run(cfg, inputs, 8)
```

```python
import contextlib
import numpy as np
import ml_dtypes
import concourse.bass as bass
import concourse.mybir as mybir
from concourse.bass_utils import run_bass_kernel_spmd

F32 = mybir.dt.float32
BF16 = mybir.dt.bfloat16
AF = mybir.ActivationFunctionType
ALU = mybir.AluOpType
AX = mybir.AxisListType
P = 128
LN_EPS = 1e-5


class Cfg:
    def __init__(self, SEQ=4096, CTX=256, E=32, DEPTH=4):
        self.SEQ, self.CTX, self.E, self.DEPTH = SEQ, CTX, E, DEPTH
        self.D = 1024
        self.KC = 8
        self.H = 8
        self.T = SEQ + CTX
        self.N_A = (DEPTH + 2) // 3
        self.N_B = (DEPTH + 1) // 3
        self.N_C = DEPTH // 3
        self.alpha = float((2 * DEPTH) ** 0.25)
        self.lat_blocks = [(t, 512, False) for t in range(0, SEQ, 512)]
        self.ctx_blocks = [(SEQ + t, min(512, CTX - t), True) for t in range(0, CTX, 512)]
        self.blocks = self.lat_blocks + self.ctx_blocks
        self.SB_MAX = 1024


class TT:
    def __init__(self, h, name):
        self.h = h
        self.name = name
        self.w = None
        self.r = {}
        self.dsem = None
        self.dcnt = 0
        self.w_is_dma = False

    def __getitem__(self, k):
        return self.h[k]


class Sched:
    def __init__(self, nc, es):
        self.nc = nc
        self.eng = {"pe": nc.tensor, "act": nc.scalar, "dve": nc.vector, "pool": nc.gpsimd, "sp": nc.sync}
        self.sem = {k: es.enter_context(nc.semaphore("e_" + k)) for k in self.eng}
        self.cnt = {k: 0 for k in self.eng}
        self.seen = {k: {} for k in self.eng}
        self.semkey = {}
        for k in self.eng:
            self.semkey[id(self.sem[k])] = k
        self.free_dsems = []
        self.all_dsems = []
        self.es = es
        self.ndsem = 0
        self.live = []
        self.ninst = 0

    def _get_dsem(self):
        if self.free_dsems:
            return self.free_dsems.pop()
        s = self.es.enter_context(self.nc.semaphore("d%d" % self.ndsem))
        self.ndsem += 1
        rec = [s, 0]
        self.all_dsems.append(rec)
        return rec

    def track(self, h, name):
        t = TT(h, name)
        self.live.append(t)
        return t

    def _wait(self, e, evs):
        own = self.sem[e]
        for (sem, val) in evs:
            if sem is own and e == "pe":
                continue
            k = id(sem)
            if self.seen[e].get(k, 0) >= val:
                continue
            self.eng[e].wait_ge(sem, val)
            self.seen[e][k] = val

    def op(self, e, fn, reads=(), writes=(), sig=True):
        deps = []
        for t in reads:
            if t.w is not None:
                deps.append(t.w)
        for t in writes:
            if t.w is not None:
                deps.append(t.w)
            for k, v in t.r.items():
                deps.append((k, v))
        deps2 = []
        for d in deps:
            deps2.append(d)
        self._wait(e, deps2)
        ins = fn(self.eng[e])
        self.ninst += 1
        if sig:
            self.cnt[e] += 1
            ins.then_inc(self.sem[e], 1)
            ev = (self.sem[e], self.cnt[e])
        else:
            ev = (self.sem[e], self.cnt[e] + 1)
        for t in writes:
            t.w = ev
            t.w_is_dma = False
            t.r = {}
        for t in reads:
            if t.r.get(ev[0], 0) < ev[1]:
                t.r[ev[0]] = ev[1]
        return ins

    def dma(self, q, out_t, out_ap, in_t, in_ap):
        deps = []
        if in_t.w is not None:
            deps.append(in_t.w)
        if out_t.w is not None and not out_t.w_is_dma:
            deps.append(out_t.w)
        for k, v in out_t.r.items():
            deps.append((k, v))
        self._wait(q, deps)
        if out_t.dsem is None:
            out_t.dsem = self._get_dsem()
        rec = out_t.dsem
        rec[1] += 16
        self.eng[q].dma_start(out=out_ap, in_=in_ap).then_inc(rec[0], 16)
        self.ninst += 1
        ev = (rec[0], rec[1])
        out_t.w = ev
        out_t.w_is_dma = True
        out_t.r = {}
        if in_t.r.get(ev[0], 0) < ev[1]:
            in_t.r[ev[0]] = ev[1]

    def drain(self, release=()):
        evs = [(self.sem[k], self.cnt[k]) for k in self.eng if self.cnt[k] > 0]
        evs += [(r[0], r[1]) for r in self.all_dsems if r[1] > 0]
        for e in self.eng:
            own = self.sem[e]
            for (sem, val) in evs:
                if sem is own:
                    continue
                k = id(sem)
                if self.seen[e].get(k, 0) >= val:
                    continue
                self.eng[e].wait_ge(sem, val)
                self.seen[e][k] = val
        for t in self.live:
            t.w = None
            t.r = {}
        for t in release:
            if t.dsem is not None:
                self.free_dsems.append(t.dsem)
                t.dsem = None
            if t in self.live:
                self.live.remove(t)


_UID = [0]


def _uid():
    _UID[0] += 1
    return _UID[0]


class Scope:
    def __init__(self, S):
        self.S = S
        self.es = contextlib.ExitStack()
        self.tiles = []
        self.n = 0

    def sb(self, name, shape, dt):
        h = self.es.enter_context(self.S.nc.sbuf_tensor("%s_%d" % (name, _uid()), list(shape), dt))
        t = self.S.track(h, name)
        self.tiles.append(t)
        return t

    def ps(self, name, shape=(P, 512), dt=F32):
        h = self.es.enter_context(self.S.nc.psum_tensor("%s_%d" % (name, _uid()), list(shape), dt))
        t = self.S.track(h, name)
        self.tiles.append(t)
        return t

    def close(self):
        self.S.drain(release=self.tiles)
        self.es.close()


class Rot:
    def __init__(self, tiles):
        self.tiles = tiles
        self.i = 0

    def next(self):
        t = self.tiles[self.i % len(self.tiles)]
        self.i += 1
        return t


class Prog:
    def __init__(self, cfg):
        self.cfg = cfg
        self.nc = bass.Bass("TRN2", target_bir_lowering=False)
        self.io = {}

    def din(self, name, shape, dt=F32):
        ap = self.nc.dram_tensor(name, list(shape), dt, kind="ExternalInput").ap()
        self.io[name] = ap
        return ap

    def build(self):
        cfg = self.cfg
        nc = self.nc
        D, T, E, SEQ, CTX, DEPTH = cfg.D, cfg.T, cfg.E, cfg.SEQ, cfg.CTX, cfg.DEPTH
        d = self.din
        d("xT", [D, SEQ]); d("ctxT", [D, CTX]); d("cT", [P, 8]); d("cctxT", [P, 8])
        d("w_mod", [DEPTH, D, 6 * D]); d("b_modT", [P, DEPTH * 48])
        d("lnp", [P, DEPTH * 32])
        d("attn_w_qkv", [cfg.N_A, D, 3 * D]); d("attn_w_o", [cfg.N_A, D, D])
        d("lam", [cfg.N_A, 4, 64]); d("sublnT", [P, cfg.N_A])
        d("fnet_w", [max(cfg.N_B, 1), D, D]); d("fnet_bT", [P, max(cfg.N_B, 1) * 8])
        nC = max(cfg.N_C, 1)
        d("conv_w_pw1", [nC, D, 2 * D]); d("conv_b_pw1T", [P, nC * 16])
        d("conv_w_dwT", [P, nC * 8 * 31]); d("conv_pT", [P, nC * 4 * 8])
        d("conv_w_pw2", [nC, D, D])
        d("moe_w_router", [DEPTH, D, E]); d("moe_b_router", [DEPTH, E])
        d("moe_w1", [DEPTH, E, D, 2 * D]); d("moe_b1T", [P, DEPTH * E * 16])
        d("moe_w2", [DEPTH, E, D, D]); d("moe_b2", [DEPTH, E, D])
        d("ropeC", [P, SEQ]); d("ropeS", [P, SEQ])
        d("dftL", [2, SEQ, SEQ], BF16); d("dftC", [2, CTX, CTX], BF16); d("dft128", [P, 256], BF16)
        d("ident", [P, P])
        self.yT = nc.dram_tensor("yT", [D, SEQ], F32, kind="ExternalOutput").ap()
        self.XX = nc.dram_tensor("XX", [D, T], F32).ap()
        self.X1 = nc.dram_tensor("X1", [D, T], F32).ap()
        self.MIX = nc.dram_tensor("MIX", [D, T], BF16).ap()
        self.GD = nc.dram_tensor("GD", [E, T], F32).ap()

        with contextlib.ExitStack() as es:
            S = Sched(nc, es)
            self.S = S
            es.enter_context(nc.Block())
            self.t_in = S.track(None, "inputs")
            self.t_XX = S.track(None, "XX")
            self.t_X1 = S.track(None, "X1")
            self.t_MIX = S.track(None, "MIX")
            self.t_GD = S.track(None, "GD")
            self.t_out = S.track(None, "yT")
            G = Scope(S)
            self.G = G
            self.ones32 = G.sb("ones32", [P, P], F32)
            self.onesbf = G.sb("onesbf", [P, P], BF16)
            self.ident = G.sb("ident", [P, P], F32)
            self.MOD = G.sb("MOD", [P, DEPTH, 48, 2], F32)
            self.LNP = G.sb("LNP", [P, DEPTH, 4, 8], F32)
            self.GB1 = G.sb("GB1", [P, DEPTH, 8, 2], F32)
            self.cst = G.sb("cst", [P, 4], F32)
            S.op("dve", lambda e: e.memset(self.cst[:, 0:1], LN_EPS), writes=[self.cst])
            S.op("dve", lambda e: e.memset(self.cst[:, 1:2], 128.0 * LN_EPS), writes=[self.cst])
            S.op("dve", lambda e: e.memset(self.ones32[:], 1.0), writes=[self.ones32])
            S.op("dve", lambda e: e.memset(self.onesbf[:], 1.0), writes=[self.onesbf])
            S.dma("sp", self.ident, self.ident[:], self.t_in, self.io["ident"][:, :])
            S.dma("sp", self.LNP, self.LNP[:].rearrange("p a b c -> p (a b c)"), self.t_in, self.io["lnp"][:, :])
            self.stage_mod()
            for i in range(DEPTH):
                kind = i % 3
                j = i // 3
                last = i == DEPTH - 1
                if kind == 0:
                    self.mixer_attn(i, j, last)
                elif kind == 1:
                    self.mixer_fnet(i, j, last)
                else:
                    self.mixer_conv(i, j, last)
                self.post_moe(i, kind, j, last)
            S.drain()
            G.close()
        return nc

    def xsrc(self, i, blk):
        t0, n, is_ctx = blk
        cfg = self.cfg
        if i == 0:
            if is_ctx:
                ap = self.io["ctxT"].rearrange("(k p) t -> p k t", p=P)[:, :, t0 - cfg.SEQ:t0 - cfg.SEQ + n]
            else:
                ap = self.io["xT"].rearrange("(k p) t -> p k t", p=P)[:, :, t0:t0 + n]
            return self.t_in, ap
        return self.t_XX, self.XX.rearrange("(k p) t -> p k t", p=P)[:, :, t0:t0 + n]

    def wview(self, ap2d):
        return ap2d.rearrange("(k p) n -> p k n", p=P)

    def modulate(self, eng_rot, x, ut_ap_fn, i, n, col, s_sh, s_sc):
        S = self.S
        for c in range(8):
            S.op("act", lambda e, c=c: e.activation(
                out=ut_ap_fn(c), in_=x[:, c, 0:n], func=AF.Identity,
                bias=self.MOD[:, i, s_sh * 8 + c, col:col + 1], scale=self.MOD[:, i, s_sc * 8 + c, col:col + 1]),
                reads=[x, self.MOD], writes=eng_rot)

    def layer_norm(self, sc, z, n, gcol, bcol, outs, tmp, greads=()):
        S = self.S
        sq, pss, psq, mean, rstd, nb = tmp["sq"], tmp["pss"], tmp["psq"], tmp["mean"], tmp["rstd"], tmp["nb"]
        for c in range(8):
            S.op("act", lambda e, c=c: e.activation(out=sq[:, c, 0:n], in_=z[:, c, 0:n], func=AF.Square),
                 reads=[z], writes=[sq])
        for c in range(8):
            S.op("pe", lambda e, c=c: e.matmul(pss[:, 0:n], self.ones32[:], z[:, c, 0:n], start=(c == 0), stop=(c == 7)),
                 reads=[self.ones32, z], writes=[pss], sig=(c == 7))
        for c in range(8):
            S.op("pe", lambda e, c=c: e.matmul(psq[:, 0:n], self.ones32[:], sq[:, c, 0:n], start=(c == 0), stop=(c == 7)),
                 reads=[self.ones32, sq], writes=[psq], sig=(c == 7))
        invd = 1.0 / 1024.0
        S.op("dve", lambda e: e.tensor_scalar(out=mean[:, 0:n], in0=pss[:, 0:n], scalar1=invd, scalar2=None, op0=ALU.mult),
             reads=[pss], writes=[mean])
        S.op("dve", lambda e: e.tensor_tensor(out=nb[:, 0:n], in0=mean[:, 0:n], in1=mean[:, 0:n], op=ALU.mult),
             reads=[mean], writes=[nb])
        S.op("dve", lambda e: e.scalar_tensor_tensor(out=rstd[:, 0:n], in0=psq[:, 0:n], scalar=invd, in1=nb[:, 0:n],
                                                     op0=ALU.mult, op1=ALU.subtract),
             reads=[psq, nb], writes=[rstd])
        S.op("act", lambda e: e.activation(out=rstd[:, 0:n], in_=rstd[:, 0:n], func=AF.Sqrt, bias=self.cst[:, 0:1]),
             reads=[rstd, self.cst], writes=[rstd])
        S.op("dve", lambda e: e.reciprocal(out=rstd[:, 0:n], in_=rstd[:, 0:n]), reads=[rstd], writes=[rstd])
        S.op("dve", lambda e: e.scalar_tensor_tensor(out=nb[:, 0:n], in0=mean[:, 0:n], scalar=-1.0, in1=rstd[:, 0:n],
                                                     op0=ALU.mult, op1=ALU.mult),
             reads=[mean, rstd], writes=[nb])
        for c in range(8):
            t = tmp["t"].next()
            eng = "dve" if c % 2 == 0 else "pool"
            S.op(eng, lambda e, c=c, t=t: e.tensor_tensor(out=t[:, 0:n], in0=z[:, c, 0:n], in1=rstd[:, 0:n], op=ALU.mult),
                 reads=[z, rstd], writes=[t])
            S.op(eng, lambda e, t=t: e.tensor_tensor(out=t[:, 0:n], in0=t[:, 0:n], in1=nb[:, 0:n], op=ALU.add),
                 reads=[t, nb], writes=[t])
            for (ot, apf, func) in outs:
                S.op("act", lambda e, c=c, t=t, apf=apf, func=func: e.activation(
                    out=apf(c), in_=t[:, 0:n], func=func, bias=bcol(c), scale=gcol(c)),
                    reads=[t, self.LNP, self.MOD] + list(greads), writes=[ot])

    def stage_mod(self):
        S, cfg = self.S, self.cfg
        sc = Scope(S)
        craw = sc.sb("craw", [P, 16], F32)
        cond = sc.sb("cond", [P, 8, 2], F32)
        bmod = sc.sb("bmod", [P, cfg.DEPTH, 48], F32)
        wm = Rot([sc.sb("wm%d" % k, [P, 8, 768], F32) for k in range(2)])
        pst = Rot([sc.ps("pmod%d" % k) for k in range(2)])
        S.dma("sp", craw, craw[:, 0:8], self.t_in, self.io["cT"][:, :])
        S.dma("sp", craw, craw[:, 8:16], self.t_in, self.io["cctxT"][:, :])
        S.dma("sp", bmod, bmod[:].rearrange("p a b -> p (a b)"), self.t_in, self.io["b_modT"][:, :])
        S.op("act", lambda e: e.activation(out=cond[:, :, 0], in_=craw[:, 0:8], func=AF.Silu), reads=[craw], writes=[cond])
        S.op("act", lambda e: e.activation(out=cond[:, :, 1], in_=craw[:, 8:16], func=AF.Silu), reads=[craw], writes=[cond])
        for i in range(cfg.DEPTH):
            wv = self.wview(self.io["w_mod"][i])
            for nb in range(8):
                w = wm.next()
                S.dma("sp", w, w[:], self.t_in, wv[:, :, nb * 768:(nb + 1) * 768])
                for c6 in range(6):
                    ch = nb * 6 + c6
                    ps = pst.next()
                    for k in range(8):
                        S.op("pe", lambda e, k=k, c6=c6, w=w, ps=ps: e.matmul(
                            ps[:, 0:2], w[:, k, c6 * 128:(c6 + 1) * 128], cond[:, k, :], start=(k == 0), stop=(k == 7)),
                            reads=[w, cond], writes=[ps], sig=(k == 7))
                    S.op("dve", lambda e, ch=ch, ps=ps, i=i: e.tensor_scalar(
                        out=self.MOD[:, i, ch, :], in0=ps[:, 0:2], scalar1=bmod[:, i, ch:ch + 1], scalar2=None, op0=ALU.add),
                        reads=[ps, bmod], writes=[self.MOD])
            for s in (1, 4):
                S.op("dve", lambda e, s=s, i=i: e.tensor_scalar(
                    out=self.MOD[:, i, s * 8:(s + 1) * 8, :], in0=self.MOD[:, i, s * 8:(s + 1) * 8, :], scalar1=1.0,
                    scalar2=None, op0=ALU.add), reads=[self.MOD], writes=[self.MOD])
        sc.close()

    def mixer_attn(self, i, j, last):
        S, cfg = self.S, self.cfg
        SEQ, T = cfg.SEQ, cfg.T
        lam_init = 0.8 - 0.6 * float(np.exp(-0.3 * i))
        sc = Scope(S)
        UT = sc.sb("UT", [P, 8, T], BF16)
        sc0 = Scope(S)
        xb = Rot([sc0.sb("xb%d" % k, [P, 8, 512], F32) for k in range(2)])
        for blk in cfg.blocks:
            t0, n, is_ctx = blk
            x = xb.next()
            st, sap = self.xsrc(i, blk)
            S.dma("sp", x, x[:, :, 0:n], st, sap)
            self.modulate([UT], x, lambda c, t0=t0, n=n: UT[:, c, t0:t0 + n], i, n, 1 if is_ctx else 0, 0, 1)
        sc0.close()
        COS = sc.sb("COS", [P, SEQ], F32)
        SIN = sc.sb("SIN", [P, SEQ], F32)
        S.dma("sp", COS, COS[:], self.t_in, self.io["ropeC"][:, :])
        S.dma("sp", SIN, SIN[:], self.t_in, self.io["ropeS"][:, :])
        lamt = sc.sb("lamt", [P, 4, 64], F32)
        S.dma("sp", lamt, lamt[:].rearrange("p a b -> p (a b)"), self.t_in,
              self.io["lam"][j].rearrange("a b -> (a b)").partition_broadcast(P))
        lsc = sc.sb("lsc", [P, 8], F32)
        S.op("dve", lambda e: e.tensor_tensor(out=lamt[:, 0, :], in0=lamt[:, 0, :], in1=lamt[:, 1, :], op=ALU.mult),
             reads=[lamt], writes=[lamt])
        S.op("dve", lambda e: e.tensor_tensor(out=lamt[:, 2, :], in0=lamt[:, 2, :], in1=lamt[:, 3, :], op=ALU.mult),
             reads=[lamt], writes=[lamt])
        S.op("dve", lambda e: e.reduce_sum(out=lsc[:, 0:1], in_=lamt[:, 0, :], axis=AX.X), reads=[lamt], writes=[lsc])
        S.op("dve", lambda e: e.reduce_sum(out=lsc[:, 1:2], in_=lamt[:, 2, :], axis=AX.X), reads=[lamt], writes=[lsc])
        S.op("act", lambda e: e.activation(out=lsc[:, 2:4], in_=lsc[:, 0:2], func=AF.Exp), reads=[lsc], writes=[lsc])
        S.op("dve", lambda e: e.scalar_tensor_tensor(out=lsc[:, 4:5], in0=lsc[:, 3:4], scalar=-lam_init, in1=lsc[:, 2:3],
                                                     op0=ALU.add, op1=ALU.subtract), reads=[lsc], writes=[lsc])
        NA = cfg.N_A
        sgt0 = sc.sb("sgt0", [P, NA], F32)
        sgt = sc.sb("sgt", [P, 2], F32)
        S.dma("sp", sgt0, sgt0[:], self.t_in, self.io["sublnT"][:, :])
        S.op("dve", lambda e: e.tensor_scalar(out=sgt[:, 1:2], in0=sgt0[:, j:j + 1], scalar1=float((1.0 - lam_init) * np.sqrt(128.0)),
                                              scalar2=None, op0=ALU.mult), reads=[sgt0], writes=[sgt])
        WQ = Rot([sc.sb("WQ%d" % k, [P, 8, 3, 128], BF16) for k in range(2)])
        WR = Rot([sc.sb("WR%d" % k, [P, 8, 2, 128], BF16) for k in range(2)])
        QT = Rot([sc.sb("QT%d" % k, [P, T], BF16) for k in range(2)])
        KT = Rot([sc.sb("KT%d" % k, [P, T], BF16) for k in range(2)])
        VV = Rot([sc.sb("VV%d" % k, [P, T // 128, 128], BF16) for k in range(2)])
        pp = Rot([sc.ps("pp%d" % k) for k in range(3)])
        pO = [sc.ps("pO%d" % k) for k in range(2)]
        pL = [sc.ps("pL%d" % k) for k in range(2)]
        pR = sc.ps("pR")
        tmpa = Rot([sc.sb("tmpa%d" % k, [P, 512], F32) for k in range(4)])
        ET = Rot([sc.sb("ET%d" % k, [P, 512], BF16) for k in range(3)])
        ob = Rot([sc.sb("ob%d" % k, [P, 512], BF16) for k in range(2)])
        wq = self.wview(self.io["attn_w_qkv"][j])
        mixv = self.MIX
        qblocks = cfg.blocks if not last else cfg.lat_blocks
        for h in range(cfg.H):
            W = WQ.next()
            R = WR.next()
            for s3 in range(3):
                S.dma("pool", W, W[:, :, s3, :], self.t_in, wq[:, :, s3 * 1024 + h * 128: s3 * 1024 + (h + 1) * 128])
            for s2 in range(2):
                src = W[:, :, s2, :].rearrange("p k (b h j) -> p k b h j", b=4, h=2)
                dst = R[:, :, s2, :].rearrange("p k (b h j) -> p k b h j", b=4, h=2)
                for k in range(8):
                    S.op("dve", lambda e, src=src, dst=dst, k=k: e.tensor_scalar(
                        out=dst[:, k, :, 0, :], in0=src[:, k, :, 1, :], scalar1=-1.0, scalar2=None, op0=ALU.mult),
                        reads=[W], writes=[R])
                    S.op("dve", lambda e, src=src, dst=dst, k=k: e.tensor_copy(out=dst[:, k, :, 1, :], in_=src[:, k, :, 0, :]),
                         reads=[W], writes=[R])
            Q, K, V = QT.next(), KT.next(), VV.next()
            for blk in cfg.blocks:
                t0, n, is_ctx = blk
                for s2, dest in ((0, Q), (1, K)):
                    p1 = pp.next()
                    for k in range(8):
                        S.op("pe", lambda e, k=k, p1=p1, s2=s2: e.matmul(p1[:, 0:n], W[:, k, s2, :], UT[:, k, t0:t0 + n],
                                                                         start=(k == 0), stop=(k == 7)),
                             reads=[W, UT], writes=[p1], sig=(k == 7))
                    if is_ctx:
                        S.op("act", lambda e, p1=p1, dest=dest: e.copy(out=dest[:, t0:t0 + n], in_=p1[:, 0:n]),
                             reads=[p1], writes=[dest])
                    else:
                        p2 = pp.next()
                        for k in range(8):
                            S.op("pe", lambda e, k=k, p2=p2, s2=s2: e.matmul(p2[:, 0:n], R[:, k, s2, :], UT[:, k, t0:t0 + n],
                                                                             start=(k == 0), stop=(k == 7)),
                                 reads=[R, UT], writes=[p2], sig=(k == 7))
                        a1, a2 = tmpa.next(), tmpa.next()
                        S.op("dve", lambda e, p1=p1, a1=a1: e.tensor_tensor(out=a1[:, 0:n], in0=p1[:, 0:n], in1=COS[:, t0:t0 + n], op=ALU.mult),
                             reads=[p1, COS], writes=[a1])
                        S.op("dve", lambda e, p2=p2, a2=a2: e.tensor_tensor(out=a2[:, 0:n], in0=p2[:, 0:n], in1=SIN[:, t0:t0 + n], op=ALU.mult),
                             reads=[p2, SIN], writes=[a2])
                        S.op("pool", lambda e, a1=a1, a2=a2, dest=dest: e.tensor_tensor(out=dest[:, t0:t0 + n], in0=a1[:, 0:n], in1=a2[:, 0:n], op=ALU.add),
                             reads=[a1, a2], writes=[dest])
                for s in range(n // 128):
                    p1 = pp.next()
                    tt = t0 + s * 128
                    for k in range(8):
                        S.op("pe", lambda e, k=k, p1=p1, tt=tt: e.matmul(p1[:, 0:128], UT[:, k, tt:tt + 128], W[:, k, 2, :],
                                                                         start=(k == 0), stop=(k == 7)),
                             reads=[W, UT], writes=[p1], sig=(k == 7))
                    S.op("act", lambda e, p1=p1, tt=tt: e.copy(out=V[:, tt // 128, :], in_=p1[:, 0:128]), reads=[p1], writes=[V])
            for qb in qblocks:
                q0, nq, q_ctx = qb
                kchunks = list(range(SEQ // 128, T // 128)) if q_ctx else list(range(T // 128))
                its = [(m, ki, kc) for m in range(2) for ki, kc in enumerate(kchunks)]
                LA = 2
                pss = {}

                def emit_scores(idx):
                    m, ki, kc = its[idx]
                    lo, hi = m * 64, (m + 1) * 64
                    ps = pp.next()
                    S.op("pe", lambda e, ps=ps, kc=kc, lo=lo, hi=hi: e.matmul(
                        ps[:, 0:nq], K[lo:hi, kc * 128:(kc + 1) * 128], Q[lo:hi, q0:q0 + nq], start=True, stop=True),
                        reads=[K, Q], writes=[ps])
                    pss[idx] = ps
                for idx in range(min(LA, len(its))):
                    emit_scores(idx)
                for idx in range(len(its)):
                    if idx + LA < len(its):
                        emit_scores(idx + LA)
                    m, ki, kc = its[idx]
                    ps = pss.pop(idx)
                    et = ET.next()
                    S.op("act", lambda e, ps=ps, et=et: e.activation(out=et[:, 0:nq], in_=ps[:, 0:nq], func=AF.Exp, scale=0.125),
                         reads=[ps], writes=[et])
                    fst, lst = ki == 0, ki == len(kchunks) - 1
                    S.op("pe", lambda e, et=et, kc=kc, m=m, fst=fst, lst=lst: e.matmul(
                        pO[m][:, 0:nq], V[:, kc, :], et[:, 0:nq], start=fst, stop=lst),
                        reads=[V, et], writes=[pO[m]], sig=False)
                    S.op("pe", lambda e, et=et, m=m, fst=fst, lst=lst: e.matmul(
                        pL[m][:, 0:nq], self.onesbf[:], et[:, 0:nq], start=fst, stop=lst),
                        reads=[self.onesbf, et], writes=[pL[m]], sig=True)
                r1, a1, r2, a2 = tmpa.next(), tmpa.next(), tmpa.next(), tmpa.next()
                S.op("dve", lambda e, r1=r1: e.reciprocal(out=r1[:, 0:nq], in_=pL[0][:, 0:nq]), reads=[pL[0]], writes=[r1])
                S.op("dve", lambda e, r1=r1, a1=a1: e.tensor_tensor(out=a1[:, 0:nq], in0=pO[0][:, 0:nq], in1=r1[:, 0:nq], op=ALU.mult),
                     reads=[pO[0], r1], writes=[a1])
                S.op("dve", lambda e, r2=r2: e.reciprocal(out=r2[:, 0:nq], in_=pL[1][:, 0:nq]), reads=[pL[1]], writes=[r2])
                S.op("dve", lambda e, r2=r2, a2=a2: e.tensor_tensor(out=a2[:, 0:nq], in0=pO[1][:, 0:nq], in1=r2[:, 0:nq], op=ALU.mult),
                     reads=[pO[1], r2], writes=[a2])
                S.op("dve", lambda e, a1=a1, a2=a2: e.scalar_tensor_tensor(out=a1[:, 0:nq], in0=a2[:, 0:nq], scalar=lsc[:, 4:5], in1=a1[:, 0:nq],
                                                                          op0=ALU.mult, op1=ALU.add), reads=[a1, a2, lsc], writes=[a1])
                S.op("act", lambda e, a1=a1, r1=r1: e.activation(out=r1[:, 0:nq], in_=a1[:, 0:nq], func=AF.Square), reads=[a1], writes=[r1])
                S.op("pe", lambda e, r1=r1: e.matmul(pR[:, 0:nq], self.ones32[:], r1[:, 0:nq], start=True, stop=True),
                     reads=[self.ones32, r1], writes=[pR])
                S.op("act", lambda e, r2=r2: e.activation(out=r2[:, 0:nq], in_=pR[:, 0:nq], func=AF.Sqrt, bias=self.cst[:, 1:2]),
                     reads=[pR, self.cst], writes=[r2])
                S.op("dve", lambda e, r2=r2: e.reciprocal(out=r2[:, 0:nq], in_=r2[:, 0:nq]), reads=[r2], writes=[r2])
                S.op("dve", lambda e, a1=a1, r2=r2: e.tensor_tensor(out=a1[:, 0:nq], in0=a1[:, 0:nq], in1=r2[:, 0:nq], op=ALU.mult),
                     reads=[a1, r2], writes=[a1])
                o = ob.next()
                S.op("act", lambda e, a1=a1, o=o: e.activation(out=o[:, 0:nq], in_=a1[:, 0:nq], func=AF.Identity, scale=sgt[:, 1:2]),
                     reads=[a1, sgt], writes=[o])
                S.dma("sp", self.t_MIX, mixv[h * 128:(h + 1) * 128, q0:q0 + nq], o, o[:, 0:nq])
        sc.close()

    def mixer_fnet(self, i, j, last):
        S, cfg = self.S, self.cfg
        parts = [(0, cfg.SEQ, cfg.lat_blocks, self.io["dftL"], 0)]
        if not last:
            parts.append((cfg.SEQ, cfg.CTX, cfg.ctx_blocks, self.io["dftC"], 1))
        for (off, N, blks, dft, col) in parts:
            sc = Scope(S)
            NCH = N // 128
            ACS = sc.sb("ACS", [P, NCH, 8, 256], BF16)
            cs128 = sc.sb("cs128", [P, 256], BF16)
            S.dma("sp", cs128, cs128[:], self.t_in, self.io["dft128"][:, :])
            sc1 = Scope(S)
            xb = Rot([sc1.sb("xb%d" % k, [P, 8, 512], F32) for k in range(2)])
            ub = Rot([sc1.sb("ub%d" % k, [P, 8, 512], BF16) for k in range(2)])
            pp = Rot([sc1.ps("pp%d" % k) for k in range(4)])
            for blk in blks:
                t0, n, is_ctx = blk
                x = xb.next()
                u = ub.next()
                st, sap = self.xsrc(i, blk)
                S.dma("sp", x, x[:, :, 0:n], st, sap)
                self.modulate([u], x, lambda c, u=u, n=n: u[:, c, 0:n], i, n, col, 0, 1)
                for g in range(8):
                    for s in range(n // 128):
                        p1 = pp.next()
                        nch = (t0 - off) // 128 + s
                        S.op("pe", lambda e, p1=p1, u=u, g=g, s=s: e.matmul(p1[:, 0:256], u[:, g, s * 128:(s + 1) * 128], cs128[:],
                                                                            start=True, stop=True), reads=[u, cs128], writes=[p1])
                        eng = "act" if (g + s) % 2 == 0 else "dve"
                        if eng == "act":
                            S.op("act", lambda e, p1=p1, nch=nch, g=g: e.copy(out=ACS[:, nch, g, :], in_=p1[:, 0:256]), reads=[p1], writes=[ACS])
                        else:
                            S.op("dve", lambda e, p1=p1, nch=nch, g=g: e.tensor_copy(out=ACS[:, nch, g, :], in_=p1[:, 0:256]), reads=[p1], writes=[ACS])
            sc1.close()
            sc2 = Scope(S)
            acc = [sc2.ps("acc%d" % g) for g in range(8)]
            CP = Rot([sc2.sb("CP%d" % k, [P, 2, 512], BF16) for k in range(4)])
            ob = Rot([sc2.sb("ob%d" % k, [P, 512], BF16) for k in range(4)])
            scale = float(1.0 / np.sqrt(N * 128.0))
            kbw = min(512, N)
            for kb in range(N // kbw):
                for nchk in range(NCH):
                    cp = CP.next()
                    for s2 in range(2):
                        S.dma("sp", cp, cp[:, s2, 0:kbw], self.t_in, dft[s2, nchk * 128:(nchk + 1) * 128, kb * kbw:(kb + 1) * kbw])
                    for g in range(8):
                        for s2 in range(2):
                            S.op("pe", lambda e, g=g, s2=s2, cp=cp, nchk=nchk: e.matmul(
                                acc[g][:, 0:kbw], ACS[:, nchk, g, s2 * 128:(s2 + 1) * 128], cp[:, s2, 0:kbw],
                                start=(nchk == 0 and s2 == 0), stop=(nchk == NCH - 1 and s2 == 1)),
                                reads=[ACS, cp], writes=[acc[g]], sig=(s2 == 1 and (g == 7 or nchk == NCH - 1)))
                for g in range(8):
                    o = ob.next()
                    if g % 2 == 0:
                        S.op("act", lambda e, o=o, g=g: e.activation(out=o[:, 0:kbw], in_=acc[g][:, 0:kbw], func=AF.Copy, scale=scale),
                             reads=[acc[g]], writes=[o])
                    else:
                        S.op("dve", lambda e, o=o, g=g: e.tensor_scalar(out=o[:, 0:kbw], in0=acc[g][:, 0:kbw], scalar1=scale, scalar2=None, op0=ALU.mult),
                             reads=[acc[g]], writes=[o])
                    S.dma("sp", self.t_MIX, self.MIX[g * 128:(g + 1) * 128, off + kb * kbw: off + (kb + 1) * kbw], o, o[:, 0:kbw])
            sc2.close()
            sc.close()

    def mixer_conv(self, i, j, last):
        S, cfg = self.S, self.cfg
        SEQ, CTX = cfg.SEQ, cfg.CTX
        sc = Scope(S)
        HW = 15 + SEQ + 30 + CTX + 15
        HG = sc.sb("HG", [P, 8, HW], BF16)
        lat_off, ctx_off = 15, 15 + SEQ + 30
        for (a, b) in ((0, 15), (15 + SEQ, 15 + SEQ + 30), (HW - 15, HW)):
            S.op("pool", lambda e, a=a, b=b: e.memset(HG[:, :, a:b], 0.0), writes=[HG])
        W1 = sc.sb("W1", [P, 8, 2048], BF16)
        w1v = self.wview(self.io["conv_w_pw1"][j])
        for q in range(4):
            S.dma("pool", W1, W1[:, :, q * 512:(q + 1) * 512], self.t_in, w1v[:, :, q * 512:(q + 1) * 512])
        b1 = sc.sb("b1", [P, 16], F32)
        S.dma("sp", b1, b1[:], self.t_in, self.io["conv_b_pw1T"][:, j * 16:(j + 1) * 16])
        wdw = sc.sb("wdw", [P, 8, 31], F32)
        S.dma("sp", wdw, wdw[:].rearrange("p a b -> p (a b)"), self.t_in, self.io["conv_w_dwT"][:, j * 248:(j + 1) * 248])
        cp = sc.sb("cp", [P, 4, 8], F32)
        S.dma("sp", cp, cp[:].rearrange("p a b -> p (a b)"), self.t_in, self.io["conv_pT"][:, j * 32:(j + 1) * 32])
        blks = cfg.blocks if not last else cfg.lat_blocks

        def hoff(blk):
            t0, n, is_ctx = blk
            return (ctx_off + t0 - SEQ) if is_ctx else (lat_off + t0)
        sc1 = Scope(S)
        xb = Rot([sc1.sb("xb%d" % k, [P, 8, 512], F32) for k in range(2)])
        ub = Rot([sc1.sb("ub%d" % k, [P, 8, 512], BF16) for k in range(2)])
        pp = Rot([sc1.ps("pp%d" % k) for k in range(6)])
        ta = Rot([sc1.sb("ta%d" % k, [P, 512], F32) for k in range(3)])
        tg = Rot([sc1.sb("tg%d" % k, [P, 512], F32) for k in range(3)])
        for blk in blks:
            t0, n, is_ctx = blk
            x, u = xb.next(), ub.next()
            st, sap = self.xsrc(i, blk)
            S.dma("sp", x, x[:, :, 0:n], st, sap)
            self.modulate([u], x, lambda c, u=u, n=n: u[:, c, 0:n], i, n, 1 if is_ctx else 0, 0, 1)
            ho = hoff(blk)
            for jj in range(8):
                pa, pg = pp.next(), pp.next()
                for (pt, cb) in ((pa, jj * 128), (pg, 1024 + jj * 128)):
                    for k in range(8):
                        S.op("pe", lambda e, pt=pt, cb=cb, k=k, u=u: e.matmul(pt[:, 0:n], W1[:, k, cb:cb + 128], u[:, k, 0:n],
                                                                             start=(k == 0), stop=(k == 7)),
                             reads=[W1, u], writes=[pt], sig=(k == 7))
                a, g = ta.next(), tg.next()
                S.op("dve", lambda e, pa=pa, a=a, jj=jj: e.tensor_scalar(out=a[:, 0:n], in0=pa[:, 0:n], scalar1=b1[:, jj:jj + 1], scalar2=None, op0=ALU.add),
                     reads=[pa, b1], writes=[a])
                S.op("act", lambda e, pg=pg, g=g, jj=jj: e.activation(out=g[:, 0:n], in_=pg[:, 0:n], func=AF.Sigmoid, bias=b1[:, 8 + jj:9 + jj]),
                     reads=[pg, b1], writes=[g])
                S.op("pool", lambda e, a=a, g=g, jj=jj, ho=ho: e.tensor_tensor(out=HG[:, jj, ho:ho + n], in0=a[:, 0:n], in1=g[:, 0:n], op=ALU.mult),
                     reads=[a, g], writes=[HG])
        sc1.close()
        sc2 = Scope(S)
        zb = Rot([sc2.sb("zb%d" % k, [P, 8, 512], F32) for k in range(2)])
        accB = Rot([sc2.sb("accB%d" % k, [P, 512], F32) for k in range(2)])
        tmp = dict(sq=sc2.sb("sq", [P, 8, 512], F32), pss=sc2.ps("pss"), psq=sc2.ps("psq"),
                   mean=sc2.sb("mean", [P, 512], F32), rstd=sc2.sb("rstd", [P, 512], F32), nb=sc2.sb("nb", [P, 512], F32),
                   t=Rot([sc2.sb("lt%d" % k, [P, 512], F32) for k in range(3)]))
        mb = Rot([sc2.sb("mb%d" % k, [P, 8, 512], BF16) for k in range(2)])
        for blk in blks:
            t0, n, is_ctx = blk
            ho = hoff(blk)
            z = zb.next()
            for c in range(8):
                ab = accB.next()
                for tap in range(31):
                    src_off = ho + tap - 15
                    if tap == 0:
                        S.op("dve", lambda e, c=c, src_off=src_off: e.tensor_scalar(
                            out=z[:, c, 0:n], in0=HG[:, c, src_off:src_off + n], scalar1=wdw[:, c, 0:1], scalar2=cp[:, 0, c:c + 1],
                            op0=ALU.mult, op1=ALU.add), reads=[HG, wdw, cp], writes=[z])
                    else:
                        S.op("dve", lambda e, c=c, src_off=src_off, tap=tap: e.scalar_tensor_tensor(
                            out=z[:, c, 0:n], in0=HG[:, c, src_off:src_off + n], scalar=wdw[:, c, tap:tap + 1], in1=z[:, c, 0:n],
                            op0=ALU.mult, op1=ALU.add), reads=[HG, wdw, z], writes=[z])
            m = mb.next()
            self.layer_norm(sc2, z, n, lambda c: cp[:, 1, c:c + 1], lambda c: cp[:, 2, c:c + 1],
                            [(m, lambda c, m=m, n=n: m[:, c, 0:n], AF.Silu)], tmp, greads=[cp])
            S.dma("sp", self.t_MIX, self.MIX.rearrange("(k p) t -> p k t", p=P)[:, :, t0:t0 + n], m, m[:, :, 0:n])
        sc2.close()
        sc.close()

    def post_moe(self, i, kind, j, last):
        S, cfg = self.S, self.cfg
        E = cfg.E
        blks = cfg.blocks if not last else cfg.lat_blocks
        sbs, cur, tot = [], [], 0
        for b in blks:
            if b[2] and cur and tot + b[1] <= cfg.SB_MAX + 256:
                cur.append(b)
                tot += b[1]
                continue
            if tot + b[1] > cfg.SB_MAX:
                sbs.append(cur)
                cur, tot = [], 0
            cur.append(b)
            tot += b[1]
        if cur:
            sbs.append(cur)
        if kind == 0:
            wo_ap, bo_ap = self.io["attn_w_o"][j], None
        elif kind == 1:
            wo_ap, bo_ap = self.io["fnet_w"][j], self.io["fnet_bT"][:, j * 8:(j + 1) * 8]
        else:
            wo_ap, bo_ap = self.io["conv_w_pw2"][j], self.io["conv_pT"][:, j * 32 + 24:j * 32 + 32]
        L = Scope(S)
        WO = L.sb("WO", [P, 8, 1024], BF16)
        wov = self.wview(wo_ap)
        for q in range(2):
            S.dma("pool", WO, WO[:, :, q * 512:(q + 1) * 512], self.t_in, wov[:, :, q * 512:(q + 1) * 512])
        bo = L.sb("bo", [P, 8], F32)
        if bo_ap is None:
            S.op("dve", lambda e: e.memset(bo[:], 0.0), writes=[bo])
        else:
            S.dma("sp", bo, bo[:], self.t_in, bo_ap)
        for col in range(2):
            S.op("dve", lambda e, col=col: e.tensor_tensor(out=self.GB1[:, i, :, col], in0=self.MOD[:, i, 16:24, col], in1=bo[:], op=ALU.mult),
                 reads=[self.MOD, bo], writes=[self.GB1])
        WR = L.sb("WR", [P, 8, E], F32)
        S.dma("sp", WR, WR[:], self.t_in, self.wview(self.io["moe_w_router"][i]))
        BR = L.sb("BR", [P, E], F32)
        S.dma("sp", BR, BR[:], self.t_in, self.io["moe_b_router"][i].partition_broadcast(P))
        B1 = L.sb("B1", [P, E, 16], F32)
        S.dma("sp", B1, B1[:].rearrange("p a b -> p (a b)"), self.t_in, self.io["moe_b1T"][:, i * E * 16:(i + 1) * E * 16])
        S.op("dve", lambda e: e.tensor_scalar(out=B1[:, :, 8:16], in0=B1[:, :, 8:16], scalar1=1.0, scalar2=None, op0=ALU.add),
             reads=[B1], writes=[B1])
        B2 = L.sb("B2", [E, 1024], F32)
        S.dma("sp", B2, B2[:], self.t_in, self.io["moe_b2"][i])
        xxv = self.XX.rearrange("(k p) t -> p k t", p=P)
        x1v = self.X1.rearrange("(k p) t -> p k t", p=P)
        mixv = self.MIX.rearrange("(k p) t -> p k t", p=P)
        outv = self.yT.rearrange("(k p) t -> p k t", p=P)
        for sb in sbs:
            Tb = sum(b[1] for b in sb)
            offs = []
            o = 0
            for b in sb:
                offs.append(o)
                o += b[1]
            sbt0 = sb[0][0]
            M = Scope(S)
            VT = M.sb("VT", [P, 8, Tb], BF16)
            A = Scope(S)
            xb = Rot([A.sb("xb%d" % k, [P, 8, 512], F32) for k in range(2)])
            mb = Rot([A.sb("mb%d" % k, [P, 8, 512], BF16) for k in range(2)])
            zb = A.sb("zb", [P, 8, 512], F32)
            x1b = Rot([A.sb("x1b%d" % k, [P, 8, 512], F32) for k in range(1)])
            v32 = A.sb("v32", [P, 8, 512], F32)
            tmp = dict(sq=A.sb("sq", [P, 8, 512], F32), pss=A.ps("pss"), psq=A.ps("psq"),
                       mean=A.sb("mean", [P, 512], F32), rstd=A.sb("rstd", [P, 512], F32), nb=A.sb("nb", [P, 512], F32),
                       t=Rot([A.sb("lt%d" % k, [P, 512], F32) for k in range(3)]))
            pp = Rot([A.ps("pp%d" % k) for k in range(3)])
            pl = Rot([A.ps("pl%d" % k) for k in range(2)])
            pt = A.ps("ptr")
            lg = Rot([A.sb("lg%d" % k, [P, E], F32) for k in range(2)])
            mx = Rot([A.sb("mx%d" % k, [P, 8], F32) for k in range(2)])
            gm = Rot([A.sb("gm%d" % k, [P, E], F32) for k in range(2)])
            ge = Rot([A.sb("ge%d" % k, [P, E], F32) for k in range(2)])
            gs = Rot([A.sb("gs%d" % k, [P, 2], F32) for k in range(2)])
            gt = Rot([A.sb("gt%d" % k, [E, 128], F32) for k in range(2)])
            for bi, blk in enumerate(sb):
                t0, n, is_ctx = blk
                col = 1 if is_ctx else 0
                x, m = xb.next(), mb.next()
                st, sap = self.xsrc(i, blk)
                S.dma("sp", x, x[:, :, 0:n], st, sap)
                S.dma("sp", m, m[:, :, 0:n], self.t_MIX, mixv[:, :, t0:t0 + n])
                for c in range(8):
                    S.op("act", lambda e, c=c, x=x: e.activation(out=x[:, c, 0:n], in_=x[:, c, 0:n], func=AF.Identity,
                                                                 scale=cfg.alpha, bias=self.GB1[:, i, c, col:col + 1]),
                         reads=[x, self.GB1], writes=[x])
                for dch in range(8):
                    py = pp.next()
                    for k in range(8):
                        S.op("pe", lambda e, py=py, k=k, dch=dch, m=m: e.matmul(py[:, 0:n], WO[:, k, dch * 128:(dch + 1) * 128], m[:, k, 0:n],
                                                                               start=(k == 0), stop=(k == 7)),
                             reads=[WO, m], writes=[py], sig=(k == 7))
                    S.op("dve", lambda e, py=py, dch=dch, x=x: e.scalar_tensor_tensor(
                        out=zb[:, dch, 0:n], in0=py[:, 0:n], scalar=self.MOD[:, i, 16 + dch, col:col + 1], in1=x[:, dch, 0:n],
                        op0=ALU.mult, op1=ALU.add), reads=[py, self.MOD, x], writes=[zb])
                x1 = x1b.next()
                self.layer_norm(A, zb, n, lambda c: self.LNP[:, i, 0, c:c + 1], lambda c: self.LNP[:, i, 1, c:c + 1],
                                [(x1, lambda c, x1=x1, n=n: x1[:, c, 0:n], AF.Identity)], tmp)
                S.dma("sp", self.t_X1, x1v[:, :, t0:t0 + n], x1, x1[:, :, 0:n])
                for c in range(8):
                    S.op("act", lambda e, c=c, x1=x1: e.activation(out=v32[:, c, 0:n], in_=x1[:, c, 0:n], func=AF.Identity,
                                                                   scale=self.MOD[:, i, 32 + c, col:col + 1], bias=self.MOD[:, i, 24 + c, col:col + 1]),
                         reads=[x1, self.MOD], writes=[v32])
                    eng = "dve" if c % 2 == 0 else "pool"
                    S.op(eng, lambda e, c=c, o=offs[bi]: e.tensor_copy(out=VT[:, c, o:o + n], in_=v32[:, c, 0:n]), reads=[v32], writes=[VT])
                for s in range(n // 128):
                    p1 = pl.next()
                    for k in range(8):
                        S.op("pe", lambda e, p1=p1, k=k, s=s: e.matmul(p1[:, 0:E], v32[:, k, s * 128:(s + 1) * 128], WR[:, k, :],
                                                                       start=(k == 0), stop=(k == 7)),
                             reads=[v32, WR], writes=[p1], sig=(k == 7))
                    l, mxx, gmm, gee, gss, gtt = lg.next(), mx.next(), gm.next(), ge.next(), gs.next(), gt.next()
                    S.op("dve", lambda e, l=l, p1=p1: e.tensor_tensor(out=l[:], in0=p1[:, 0:E], in1=BR[:], op=ALU.add), reads=[p1, BR], writes=[l])
                    S.op("dve", lambda e, l=l, mxx=mxx: e.max(out=mxx[:], in_=l[:]), reads=[l], writes=[mxx])
                    S.op("dve", lambda e, l=l, mxx=mxx, gmm=gmm: e.tensor_scalar(out=gmm[:], in0=l[:], scalar1=mxx[:, 3:4], scalar2=None, op0=ALU.is_ge),
                         reads=[l, mxx], writes=[gmm])
                    S.op("dve", lambda e, mxx=mxx, gss=gss: e.tensor_scalar(out=gss[:, 0:1], in0=mxx[:, 0:1], scalar1=-1.0, scalar2=None, op0=ALU.mult),
                         reads=[mxx], writes=[gss])
                    S.op("act", lambda e, l=l, gee=gee, gss=gss: e.activation(out=gee[:], in_=l[:], func=AF.Exp, bias=gss[:, 0:1]),
                         reads=[l, gss], writes=[gee])
                    S.op("dve", lambda e, gee=gee, gmm=gmm: e.tensor_tensor(out=gee[:], in0=gee[:], in1=gmm[:], op=ALU.mult), reads=[gee, gmm], writes=[gee])
                    S.op("dve", lambda e, gee=gee, gss=gss: e.reduce_sum(out=gss[:, 1:2], in_=gee[:], axis=AX.X), reads=[gee], writes=[gss])
                    S.op("dve", lambda e, gss=gss: e.reciprocal(out=gss[:, 1:2], in_=gss[:, 1:2]), reads=[gss], writes=[gss])
                    S.op("dve", lambda e, gee=gee, gss=gss: e.tensor_scalar(out=gee[:], in0=gee[:], scalar1=gss[:, 1:2], scalar2=None, op0=ALU.mult),
                         reads=[gee, gss], writes=[gee])
                    S.op("pe", lambda e, gee=gee: e.transpose(pt[0:E, 0:128], gee[:], self.ident[:]), reads=[gee, self.ident], writes=[pt])
                    S.op("act", lambda e, gtt=gtt: e.copy(out=gtt[:], in_=pt[0:E, 0:128]), reads=[pt], writes=[gtt])
                    tt = t0 + s * 128
                    S.dma("sp", self.t_GD, self.GD[:, tt:tt + 128], gtt, gtt[:])
            A.close()
            Mf = Scope(S)
            Fa = Mf.sb("Fa", [P, 8, Tb], F32)
            B = Scope(S)
            HH = [B.sb("Hh%d" % k, [P, 8, Tb], BF16) for k in range(2)]
            GBt = Rot([B.sb("GBt%d" % k, [P, Tb], F32) for k in range(2)])
            GTs = B.sb("GTs", [E, Tb], F32)
            S.dma("sp", GTs, GTs[:], self.t_GD, self.GD[:, sbt0:sbt0 + Tb])
            NW1 = 4
            w1bufs = [B.sb("w1r%d" % k, [P, 8, 2, 256], BF16) for k in range(NW1)]
            w2bufs = [B.sb("w2r%d" % k, [P, 8, 256], BF16) for k in range(4)]
            pg = Rot([B.ps("pg%d" % k) for k in range(3)])
            plin = Rot([B.ps("plin%d" % k) for k in range(3)])
            py2 = Rot([B.ps("py%d" % k) for k in range(2)])
            ta = Rot([B.sb("ta%d" % k, [P, 512], F32) for k in range(2)])
            ts_ = Rot([B.sb("ts%d" % k, [P, 512], F32) for k in range(2)])
            tl = Rot([B.sb("tl%d" % k, [P, 512], F32) for k in range(2)])
            nld = [0]

            def load_w1(q):
                while nld[0] <= q and nld[0] < E * 4:
                    qq = nld[0]
                    ex_, j4_ = qq // 4, qq % 4
                    w1 = w1bufs[qq % NW1]
                    w1v_ = self.wview(self.io["moe_w1"][i, ex_])
                    for s2 in range(2):
                        S.dma("pool", w1, w1[:, :, s2, :], self.t_in, w1v_[:, :, s2 * 1024 + j4_ * 256: s2 * 1024 + (j4_ + 1) * 256])
                    nld[0] += 1

            def load_w2(ex, d4):
                w2 = w2bufs[d4]
                w2v_ = self.wview(self.io["moe_w2"][i, ex])
                S.dma("pool", w2, w2[:], self.t_in, w2v_[:, :, d4 * 256:(d4 + 1) * 256])

            def emit_w1(ex):
                Hh = HH[ex % 2]
                gb = GBt.next()
                S.dma("sp", gb, gb[:], self.t_GD, self.GD[ex, sbt0:sbt0 + Tb].partition_broadcast(P))
                for j4 in range(4):
                    q = ex * 4 + j4
                    load_w1(q + 2)
                    if ex >= 1:
                        load_w2(ex - 1, j4)
                    w1 = w1bufs[q % NW1]
                    for jj in range(2):
                        jc = j4 * 2 + jj
                        for bi, blk in enumerate(sb):
                            n, o = blk[1], offs[bi]
                            pG, pLn = pg.next(), plin.next()
                            for (ptile, s2) in ((pG, 0), (pLn, 1)):
                                for k in range(8):
                                    S.op("pe", lambda e, ptile=ptile, s2=s2, k=k, w1=w1, jj=jj, o=o, n=n: e.matmul(
                                        ptile[:, 0:n], w1[:, k, s2, jj * 128:(jj + 1) * 128], VT[:, k, o:o + n], start=(k == 0), stop=(k == 7)),
                                        reads=[w1, VT], writes=[ptile], sig=(k == 7))
                            a, sg, l = ta.next(), ts_.next(), tl.next()
                            S.op("dve", lambda e, a=a, pG=pG, n=n, jc=jc, ex=ex: e.tensor_scalar(
                                out=a[:, 0:n], in0=pG[:, 0:n], scalar1=B1[:, ex, jc:jc + 1], scalar2=7.0, op0=ALU.add, op1=ALU.min),
                                reads=[pG, B1], writes=[a])
                            S.op("act", lambda e, a=a, sg=sg, n=n: e.activation(out=sg[:, 0:n], in_=a[:, 0:n], func=AF.Sigmoid, scale=1.702),
                                 reads=[a], writes=[sg])
                            S.op("act", lambda e, l=l, pLn=pLn, n=n, jc=jc, ex=ex: e.activation(
                                out=l[:, 0:n], in_=pLn[:, 0:n], func=AF.Identity, bias=B1[:, ex, 8 + jc:9 + jc]),
                                reads=[pLn, B1], writes=[l])
                            S.op("dve", lambda e, l=l, n=n: e.tensor_scalar(out=l[:, 0:n], in0=l[:, 0:n], scalar1=-6.0, scalar2=8.0,
                                                                          op0=ALU.max, op1=ALU.min), reads=[l], writes=[l])
                            S.op("pool", lambda e, a=a, sg=sg, n=n: e.tensor_tensor(out=sg[:, 0:n], in0=a[:, 0:n], in1=sg[:, 0:n], op=ALU.mult),
                                 reads=[a, sg], writes=[sg])
                            S.op("pool", lambda e, sg=sg, l=l, n=n: e.tensor_tensor(out=sg[:, 0:n], in0=sg[:, 0:n], in1=l[:, 0:n], op=ALU.mult),
                                 reads=[sg, l], writes=[sg])
                            S.op("pool", lambda e, sg=sg, gb=gb, n=n, o=o, jc=jc, Hh=Hh: e.tensor_tensor(
                                out=Hh[:, jc, o:o + n], in0=sg[:, 0:n], in1=gb[:, o:o + n], op=ALU.mult), reads=[sg, gb], writes=[Hh])

            def emit_w2(ex):
                Hh = HH[ex % 2]
                for d4 in range(4):
                    if ex == E - 1:
                        load_w2(ex, d4)
                    w2 = w2bufs[d4]
                    for dd in range(2):
                        dch = d4 * 2 + dd
                        for bi, blk in enumerate(sb):
                            n, o = blk[1], offs[bi]
                            py = py2.next()
                            if ex == 0:
                                S.op("pe", lambda e, py=py, n=n, o=o, dch=dch: e.matmul(
                                    py[:, 0:n], B2[:, dch * 128:(dch + 1) * 128], GTs[:, o:o + n], start=True, stop=False),
                                    reads=[B2, GTs], writes=[py], sig=False)
                            for f in range(8):
                                S.op("pe", lambda e, py=py, n=n, o=o, f=f, w2=w2, dd=dd, Hh=Hh: e.matmul(
                                    py[:, 0:n], w2[:, f, dd * 128:(dd + 1) * 128], Hh[:, f, o:o + n], start=(f == 0 and ex != 0), stop=(f == 7)),
                                    reads=[w2, Hh], writes=[py], sig=(f == 7))
                            if ex == 0:
                                S.op("act", lambda e, py=py, n=n, o=o, dch=dch: e.copy(out=Fa[:, dch, o:o + n], in_=py[:, 0:n]), reads=[py], writes=[Fa])
                            else:
                                S.op("dve", lambda e, py=py, n=n, o=o, dch=dch: e.tensor_tensor(
                                    out=Fa[:, dch, o:o + n], in0=Fa[:, dch, o:o + n], in1=py[:, 0:n], op=ALU.add), reads=[py, Fa], writes=[Fa])

            load_w1(1)
            emit_w1(0)
            for ex in range(E):
                if ex + 1 < E:
                    emit_w1(ex + 1)
                emit_w2(ex)
            B.close()
            C = Scope(S)
            xb = Rot([C.sb("xb%d" % k, [P, 8, 512], F32) for k in range(2)])
            zb = C.sb("zb", [P, 8, 512], F32)
            ob = Rot([C.sb("ob%d" % k, [P, 8, 512], F32) for k in range(2)])
            tmp = dict(sq=C.sb("sq", [P, 8, 512], F32), pss=C.ps("pss"), psq=C.ps("psq"),
                       mean=C.sb("mean", [P, 512], F32), rstd=C.sb("rstd", [P, 512], F32), nb=C.sb("nb", [P, 512], F32),
                       t=Rot([C.sb("lt%d" % k, [P, 512], F32) for k in range(3)]))
            for bi, blk in enumerate(sb):
                t0, n, is_ctx = blk
                col = 1 if is_ctx else 0
                o = offs[bi]
                x = xb.next()
                S.dma("sp", x, x[:, :, 0:n], self.t_X1, x1v[:, :, t0:t0 + n])
                for c in range(8):
                    S.op("act", lambda e, c=c, x=x: e.activation(out=x[:, c, 0:n], in_=x[:, c, 0:n], func=AF.Copy, scale=cfg.alpha),
                         reads=[x], writes=[x])
                    S.op("dve", lambda e, c=c, x=x, o=o: e.scalar_tensor_tensor(
                        out=zb[:, c, 0:n], in0=Fa[:, c, o:o + n], scalar=self.MOD[:, i, 40 + c, col:col + 1], in1=x[:, c, 0:n],
                        op0=ALU.mult, op1=ALU.add), reads=[Fa, self.MOD, x], writes=[zb])
                ot = ob.next()
                self.layer_norm(C, zb, n, lambda c: self.LNP[:, i, 2, c:c + 1], lambda c: self.LNP[:, i, 3, c:c + 1],
                                [(ot, lambda c, ot=ot, n=n: ot[:, c, 0:n], AF.Identity)], tmp)
                if last:
                    S.dma("sp", self.t_out, outv[:, :, t0:t0 + n], ot, ot[:, :, 0:n])
                else:
                    S.dma("sp", self.t_XX, xxv[:, :, t0:t0 + n], ot, ot[:, :, 0:n])
            C.close()
            Mf.close()
            M.close()
        L.close()


def pmajor(v, nch):
    v = np.asarray(v, np.float32)
    lead = v.shape[:-1]
    a = v.reshape(lead + (nch, P))
    a = np.moveaxis(a, -1, 0)
    return np.ascontiguousarray(a)


def host_constants(cfg):
    SEQ, CTX = cfg.SEQ, cfg.CTX
    GRID_W = 64
    t = np.arange(SEQ)
    rows = (t // GRID_W).astype(np.float64)
    cols = (t % GRID_W).astype(np.float64)
    inv_freq = (10000.0 ** (-np.arange(16, dtype=np.float64) / 16)).astype(np.float32).astype(np.float64)
    dd = np.arange(64)
    f = dd % 16
    pos = np.where(dd[:, None] < 32, rows[None, :], cols[None, :])
    ang = (pos.astype(np.float32) * inv_freq[f][:, None].astype(np.float32)).astype(np.float64)
    C = np.cos(ang).astype(np.float32)
    Sn = np.sin(ang).astype(np.float32)
    ropeC = np.concatenate([C, C], 0)
    ropeS = np.concatenate([Sn, Sn], 0)

    def dft(N):
        n = np.arange(N, dtype=np.int64)
        m = (n[:, None] * n[None, :]) % N
        a = 2.0 * np.pi * m.astype(np.float64) / N
        return np.stack([np.cos(a), -np.sin(a)]).astype(np.float32).astype(ml_dtypes.bfloat16)
    a128 = 2.0 * np.pi * ((np.arange(128)[:, None] * np.arange(128)[None, :]) % 128) / 128.0
    dft128 = np.concatenate([np.cos(a128), np.sin(a128)], 1).astype(np.float32).astype(ml_dtypes.bfloat16)
    return dict(ropeC=ropeC, ropeS=ropeS, dftL=dft(SEQ), dftC=dft(CTX), dft128=dft128, ident=np.eye(P, dtype=np.float32))


def host_inputs(cfg, inp, consts, b):
    f = lambda a: np.ascontiguousarray(np.asarray(a, np.float32))
    DEPTH, E = cfg.DEPTH, cfg.E
    m = {}
    m["xT"] = np.ascontiguousarray(np.asarray(inp["x"][b], np.float32).T)
    m["ctxT"] = np.ascontiguousarray(np.asarray(inp["ctx"][b], np.float32).T)
    m["cT"] = pmajor(inp["c"][b], 8)
    m["cctxT"] = pmajor(inp["c_ctx"], 8)
    m["w_mod"] = f(inp["w_mod"])
    m["b_modT"] = pmajor(inp["b_mod"], 48).reshape(P, DEPTH * 48)
    lnp = np.stack([np.asarray(inp[k], np.float32) for k in ("ln1_g", "ln1_b", "ln2_g", "ln2_b")], 1)
    m["lnp"] = pmajor(lnp, 8).reshape(P, DEPTH * 32)
    m["attn_w_qkv"] = f(inp["attn_w_qkv"])
    m["attn_w_o"] = f(inp["attn_w_o"])
    m["lam"] = np.ascontiguousarray(np.stack([np.asarray(inp[k], np.float32) for k in
                                             ("attn_lam_q1", "attn_lam_k1", "attn_lam_q2", "attn_lam_k2")], 1))
    m["sublnT"] = np.ascontiguousarray(np.asarray(inp["attn_subln_g"], np.float32).T)
    m["fnet_w"] = f(inp["fnet_w"])
    m["fnet_bT"] = pmajor(inp["fnet_b"], 8).reshape(P, -1)
    m["conv_w_pw1"] = f(inp["conv_w_pw1"])
    m["conv_b_pw1T"] = pmajor(inp["conv_b_pw1"], 16).reshape(P, -1)
    wdw = np.asarray(inp["conv_w_dw"], np.float32)
    wdw = np.moveaxis(wdw, 1, 2)
    nC = wdw.shape[0]
    wdw = wdw.reshape(nC, 8, P, 31)
    m["conv_w_dwT"] = np.ascontiguousarray(np.moveaxis(wdw, 2, 0)).reshape(P, nC * 8 * 31)
    cpar = np.stack([np.asarray(inp[k], np.float32) for k in ("conv_b_dw", "conv_ln_g", "conv_ln_b", "conv_b_pw2")], 1)
    m["conv_pT"] = pmajor(cpar, 8).reshape(P, nC * 32)
    m["conv_w_pw2"] = f(inp["conv_w_pw2"])
    m["moe_w_router"] = f(inp["moe_w_router"])
    m["moe_b_router"] = f(inp["moe_b_router"])
    m["moe_w1"] = f(inp["moe_w1"])
    m["moe_b1T"] = pmajor(inp["moe_b1"], 16).reshape(P, DEPTH * E * 16)
    m["moe_w2"] = f(inp["moe_w2"])
    m["moe_b2"] = f(inp["moe_b2"])
    m.update(consts)
    return m


_CACHE = {}


def run(cfg, inputs, n_cores):
    key = (cfg.SEQ, cfg.CTX, cfg.E, cfg.DEPTH)
    if key not in _CACHE:
        _CACHE[key] = Prog(cfg).build()
    nc = _CACHE[key]
    consts = host_constants(cfg)
    in_maps = [host_inputs(cfg, inputs, consts, b) for b in range(n_cores)]
    res = run_bass_kernel_spmd(nc, in_maps, core_ids=list(range(n_cores)))
    out = np.stack([np.ascontiguousarray(res.results[b]["yT"].T) for b in range(n_cores)], 0)
    return out.astype(np.float32)


def kernel(**inputs):
    cfg = Cfg()
    return run(cfg, inputs, 8)
```

```python
import contextlib
import numpy as np
import ml_dtypes
import concourse.bass as bass
import concourse.mybir as mybir
from concourse.bass_utils import run_bass_kernel_spmd

F32 = mybir.dt.float32
BF16 = mybir.dt.bfloat16
AF = mybir.ActivationFunctionType
ALU = mybir.AluOpType
AX = mybir.AxisListType
P = 128
LN_EPS = 1e-5


class Cfg:
    def __init__(self, SEQ=4096, CTX=256, E=32, DEPTH=4):
        self.SEQ, self.CTX, self.E, self.DEPTH = SEQ, CTX, E, DEPTH
        self.D = 1024
        self.KC = 8
        self.H = 8
        self.T = SEQ + CTX
        self.N_A = (DEPTH + 2) // 3
        self.N_B = (DEPTH + 1) // 3
        self.N_C = DEPTH // 3
        self.alpha = float((2 * DEPTH) ** 0.25)
        self.lat_blocks = [(t, 512, False) for t in range(0, SEQ, 512)]
        self.ctx_blocks = [(SEQ + t, min(512, CTX - t), True) for t in range(0, CTX, 512)]
        self.blocks = self.lat_blocks + self.ctx_blocks
        self.SB_MAX = 1024


class TT:
    def __init__(self, h, name):
        self.h = h
        self.name = name
        self.w = None
        self.r = {}
        self.dsem = None
        self.dcnt = 0
        self.w_is_dma = False

    def __getitem__(self, k):
        return self.h[k]


class Sched:
    def __init__(self, nc, es):
        self.nc = nc
        self.eng = {"pe": nc.tensor, "act": nc.scalar, "dve": nc.vector, "pool": nc.gpsimd, "sp": nc.sync}
        self.sem = {k: es.enter_context(nc.semaphore("e_" + k)) for k in self.eng}
        self.cnt = {k: 0 for k in self.eng}
        self.seen = {k: {} for k in self.eng}
        self.semkey = {}
        for k in self.eng:
            self.semkey[id(self.sem[k])] = k
        self.free_dsems = []
        self.all_dsems = []
        self.es = es
        self.ndsem = 0
        self.live = []
        self.ninst = 0

    def _get_dsem(self):
        if self.free_dsems:
            return self.free_dsems.pop()
        s = self.es.enter_context(self.nc.semaphore("d%d" % self.ndsem))
        self.ndsem += 1
        rec = [s, 0]
        self.all_dsems.append(rec)
        return rec

    def track(self, h, name):
        t = TT(h, name)
        self.live.append(t)
        return t

    def _wait(self, e, evs):
        own = self.sem[e]
        for (sem, val) in evs:
            if sem is own and e == "pe":
                continue
            k = id(sem)
            if self.seen[e].get(k, 0) >= val:
                continue
            self.eng[e].wait_ge(sem, val)
            self.seen[e][k] = val

    def op(self, e, fn, reads=(), writes=(), sig=True):
        deps = []
        for t in reads:
            if t.w is not None:
                deps.append(t.w)
        for t in writes:
            if t.w is not None:
                deps.append(t.w)
            for k, v in t.r.items():
                deps.append((k, v))
        deps2 = []
        for d in deps:
            deps2.append(d)
        self._wait(e, deps2)
        ins = fn(self.eng[e])
        self.ninst += 1
        if sig:
            self.cnt[e] += 1
            ins.then_inc(self.sem[e], 1)
            ev = (self.sem[e], self.cnt[e])
        else:
            ev = (self.sem[e], self.cnt[e] + 1)
        for t in writes:
            t.w = ev
            t.w_is_dma = False
            t.r = {}
        for t in reads:
            if t.r.get(ev[0], 0) < ev[1]:
                t.r[ev[0]] = ev[1]
        return ins

    def dma(self, q, out_t, out_ap, in_t, in_ap):
        deps = []
        if in_t.w is not None:
            deps.append(in_t.w)
        if out_t.w is not None and not out_t.w_is_dma:
            deps.append(out_t.w)
        for k, v in out_t.r.items():
            deps.append((k, v))
        self._wait(q, deps)
        if out_t.dsem is None:
            out_t.dsem = self._get_dsem()
        rec = out_t.dsem
        rec[1] += 16
        self.eng[q].dma_start(out=out_ap, in_=in_ap).then_inc(rec[0], 16)
        self.ninst += 1
        ev = (rec[0], rec[1])
        out_t.w = ev
        out_t.w_is_dma = True
        out_t.r = {}
        if in_t.r.get(ev[0], 0) < ev[1]:
            in_t.r[ev[0]] = ev[1]

    def drain(self, release=()):
        evs = [(self.sem[k], self.cnt[k]) for k in self.eng if self.cnt[k] > 0]
        evs += [(r[0], r[1]) for r in self.all_dsems if r[1] > 0]
        for e in self.eng:
            own = self.sem[e]
            for (sem, val) in evs:
                if sem is own:
                    continue
                k = id(sem)
                if self.seen[e].get(k, 0) >= val:
                    continue
                self.eng[e].wait_ge(sem, val)
                self.seen[e][k] = val
        for t in self.live:
            t.w = None
            t.r = {}
        for t in release:
            if t.dsem is not None:
                self.free_dsems.append(t.dsem)
                t.dsem = None
            if t in self.live:
                self.live.remove(t)


_UID = [0]


def _uid():
    _UID[0] += 1
    return _UID[0]


class Scope:
    def __init__(self, S):
        self.S = S
        self.es = contextlib.ExitStack()
        self.tiles = []
        self.n = 0

    def sb(self, name, shape, dt):
        h = self.es.enter_context(self.S.nc.sbuf_tensor("%s_%d" % (name, _uid()), list(shape), dt))
        t = self.S.track(h, name)
        self.tiles.append(t)
        return t

    def ps(self, name, shape=(P, 512), dt=F32):
        h = self.es.enter_context(self.S.nc.psum_tensor("%s_%d" % (name, _uid()), list(shape), dt))
        t = self.S.track(h, name)
        self.tiles.append(t)
        return t

    def close(self):
        self.S.drain(release=self.tiles)
        self.es.close()


class Rot:
    def __init__(self, tiles):
        self.tiles = tiles
        self.i = 0

    def next(self):
        t = self.tiles[self.i % len(self.tiles)]
        self.i += 1
        return t


class Prog:
    def __init__(self, cfg):
        self.cfg = cfg
        self.nc = bass.Bass("TRN2", target_bir_lowering=False)
        self.io = {}

    def din(self, name, shape, dt=F32):
        ap = self.nc.dram_tensor(name, list(shape), dt, kind="ExternalInput").ap()
        self.io[name] = ap
        return ap

    def build(self):
        cfg = self.cfg
        nc = self.nc
        D, T, E, SEQ, CTX, DEPTH = cfg.D, cfg.T, cfg.E, cfg.SEQ, cfg.CTX, cfg.DEPTH
        d = self.din
        d("xT", [D, SEQ]); d("ctxT", [D, CTX]); d("cT", [P, 8]); d("cctxT", [P, 8])
        d("w_mod", [DEPTH, D, 6 * D]); d("b_modT", [P, DEPTH * 48])
        d("lnp", [P, DEPTH * 32])
        d("attn_w_qkv", [cfg.N_A, D, 3 * D]); d("attn_w_o", [cfg.N_A, D, D])
        d("lam", [cfg.N_A, 4, 64]); d("sublnT", [P, cfg.N_A])
        d("fnet_w", [max(cfg.N_B, 1), D, D]); d("fnet_bT", [P, max(cfg.N_B, 1) * 8])
        nC = max(cfg.N_C, 1)
        d("conv_w_pw1", [nC, D, 2 * D]); d("conv_b_pw1T", [P, nC * 16])
        d("conv_w_dwT", [P, nC * 8 * 31]); d("conv_pT", [P, nC * 4 * 8])
        d("conv_w_pw2", [nC, D, D])
        d("moe_w_router", [DEPTH, D, E]); d("moe_b_router", [DEPTH, E])
        d("moe_w1", [DEPTH, E, D, 2 * D]); d("moe_b1T", [P, DEPTH * E * 16])
        d("moe_w2", [DEPTH, E, D, D]); d("moe_b2", [DEPTH, E, D])
        d("ropeC", [P, SEQ]); d("ropeS", [P, SEQ])
        d("dftL", [2, SEQ, SEQ], BF16); d("dftC", [2, CTX, CTX], BF16); d("dft128", [P, 256], BF16)
        d("ident", [P, P])
        self.yT = nc.dram_tensor("yT", [D, SEQ], F32, kind="ExternalOutput").ap()
        self.XX = nc.dram_tensor("XX", [D, T], F32).ap()
        self.X1 = nc.dram_tensor("X1", [D, T], F32).ap()
        self.MIX = nc.dram_tensor("MIX", [D, T], BF16).ap()
        self.GD = nc.dram_tensor("GD", [E, T], F32).ap()

        with contextlib.ExitStack() as es:
            S = Sched(nc, es)
            self.S = S
            es.enter_context(nc.Block())
            self.t_in = S.track(None, "inputs")
            self.t_XX = S.track(None, "XX")
            self.t_X1 = S.track(None, "X1")
            self.t_MIX = S.track(None, "MIX")
            self.t_GD = S.track(None, "GD")
            self.t_out = S.track(None, "yT")
            G = Scope(S)
            self.G = G
            self.ones32 = G.sb("ones32", [P, P], F32)
            self.onesbf = G.sb("onesbf", [P, P], BF16)
            self.ident = G.sb("ident", [P, P], F32)
            self.MOD = G.sb("MOD", [P, DEPTH, 48, 2], F32)
            self.LNP = G.sb("LNP", [P, DEPTH, 4, 8], F32)
            self.GB1 = G.sb("GB1", [P, DEPTH, 8, 2], F32)
            self.cst = G.sb("cst", [P, 4], F32)
            S.op("dve", lambda e: e.memset(self.cst[:, 0:1], LN_EPS), writes=[self.cst])
            S.op("dve", lambda e: e.memset(self.cst[:, 1:2], 128.0 * LN_EPS), writes=[self.cst])
            S.op("dve", lambda e: e.memset(self.ones32[:], 1.0), writes=[self.ones32])
            S.op("dve", lambda e: e.memset(self.onesbf[:], 1.0), writes=[self.onesbf])
            S.dma("sp", self.ident, self.ident[:], self.t_in, self.io["ident"][:, :])
            S.dma("sp", self.LNP, self.LNP[:].rearrange("p a b c -> p (a b c)"), self.t_in, self.io["lnp"][:, :])
            self.stage_mod()
            for i in range(DEPTH):
                kind = i % 3
                j = i // 3
                last = i == DEPTH - 1
                if kind == 0:
                    self.mixer_attn(i, j, last)
                elif kind == 1:
                    self.mixer_fnet(i, j, last)
                else:
                    self.mixer_conv(i, j, last)
                self.post_moe(i, kind, j, last)
            S.drain()
            G.close()
        return nc

    def xsrc(self, i, blk):
        t0, n, is_ctx = blk
        cfg = self.cfg
        if i == 0:
            if is_ctx:
                ap = self.io["ctxT"].rearrange("(k p) t -> p k t", p=P)[:, :, t0 - cfg.SEQ:t0 - cfg.SEQ + n]
            else:
                ap = self.io["xT"].rearrange("(k p) t -> p k t", p=P)[:, :, t0:t0 + n]
            return self.t_in, ap
        return self.t_XX, self.XX.rearrange("(k p) t -> p k t", p=P)[:, :, t0:t0 + n]

    def wview(self, ap2d):
        return ap2d.rearrange("(k p) n -> p k n", p=P)

    def modulate(self, eng_rot, x, ut_ap_fn, i, n, col, s_sh, s_sc):
        S = self.S
        for c in range(8):
            S.op("act", lambda e, c=c: e.activation(
                out=ut_ap_fn(c), in_=x[:, c, 0:n], func=AF.Identity,
                bias=self.MOD[:, i, s_sh * 8 + c, col:col + 1], scale=self.MOD[:, i, s_sc * 8 + c, col:col + 1]),
                reads=[x, self.MOD], writes=eng_rot)

    def layer_norm(self, sc, z, n, gcol, bcol, outs, tmp, greads=()):
        S = self.S
        sq, pss, psq, mean, rstd, nb = tmp["sq"], tmp["pss"], tmp["psq"], tmp["mean"], tmp["rstd"], tmp["nb"]
        for c in range(8):
            S.op("act", lambda e, c=c: e.activation(out=sq[:, c, 0:n], in_=z[:, c, 0:n], func=AF.Square),
                 reads=[z], writes=[sq])
        for c in range(8):
            S.op("pe", lambda e, c=c: e.matmul(pss[:, 0:n], self.ones32[:], z[:, c, 0:n], start=(c == 0), stop=(c == 7)),
                 reads=[self.ones32, z], writes=[pss], sig=(c == 7))
        for c in range(8):
            S.op("pe", lambda e, c=c: e.matmul(psq[:, 0:n], self.ones32[:], sq[:, c, 0:n], start=(c == 0), stop=(c == 7)),
                 reads=[self.ones32, sq], writes=[psq], sig=(c == 7))
        invd = 1.0 / 1024.0
        S.op("dve", lambda e: e.tensor_scalar(out=mean[:, 0:n], in0=pss[:, 0:n], scalar1=invd, scalar2=None, op0=ALU.mult),
             reads=[pss], writes=[mean])
        S.op("dve", lambda e: e.tensor_tensor(out=nb[:, 0:n], in0=mean[:, 0:n], in1=mean[:, 0:n], op=ALU.mult),
             reads=[mean], writes=[nb])
        S.op("dve", lambda e: e.scalar_tensor_tensor(out=rstd[:, 0:n], in0=psq[:, 0:n], scalar=invd, in1=nb[:, 0:n],
                                                     op0=ALU.mult, op1=ALU.subtract),
             reads=[psq, nb], writes=[rstd])
        S.op("act", lambda e: e.activation(out=rstd[:, 0:n], in_=rstd[:, 0:n], func=AF.Sqrt, bias=self.cst[:, 0:1]),
             reads=[rstd, self.cst], writes=[rstd])
        S.op("dve", lambda e: e.reciprocal(out=rstd[:, 0:n], in_=rstd[:, 0:n]), reads=[rstd], writes=[rstd])
        S.op("dve", lambda e: e.scalar_tensor_tensor(out=nb[:, 0:n], in0=mean[:, 0:n], scalar=-1.0, in1=rstd[:, 0:n],
                                                     op0=ALU.mult, op1=ALU.mult),
             reads=[mean, rstd], writes=[nb])
        for c in range(8):
            t = tmp["t"].next()
            eng = "dve" if c % 2 == 0 else "pool"
            S.op(eng, lambda e, c=c, t=t: e.tensor_tensor(out=t[:, 0:n], in0=z[:, c, 0:n], in1=rstd[:, 0:n], op=ALU.mult),
                 reads=[z, rstd], writes=[t])
            S.op(eng, lambda e, t=t: e.tensor_tensor(out=t[:, 0:n], in0=t[:, 0:n], in1=nb[:, 0:n], op=ALU.add),
                 reads=[t, nb], writes=[t])
            for (ot, apf, func) in outs:
                S.op("act", lambda e, c=c, t=t, apf=apf, func=func: e.activation(
                    out=apf(c), in_=t[:, 0:n], func=func, bias=bcol(c), scale=gcol(c)),
                    reads=[t, self.LNP, self.MOD] + list(greads), writes=[ot])

    def stage_mod(self):
        S, cfg = self.S, self.cfg
        sc = Scope(S)
        craw = sc.sb("craw", [P, 16], F32)
        cond = sc.sb("cond", [P, 8, 2], F32)
        bmod = sc.sb("bmod", [P, cfg.DEPTH, 48], F32)
        wm = Rot([sc.sb("wm%d" % k, [P, 8, 768], F32) for k in range(2)])
        pst = Rot([sc.ps("pmod%d" % k) for k in range(2)])
        S.dma("sp", craw, craw[:, 0:8], self.t_in, self.io["cT"][:, :])
        S.dma("sp", craw, craw[:, 8:16], self.t_in, self.io["cctxT"][:, :])
        S.dma("sp", bmod, bmod[:].rearrange("p a b -> p (a b)"), self.t_in, self.io["b_modT"][:, :])
        S.op("act", lambda e: e.activation(out=cond[:, :, 0], in_=craw[:, 0:8], func=AF.Silu), reads=[craw], writes=[cond])
        S.op("act", lambda e: e.activation(out=cond[:, :, 1], in_=craw[:, 8:16], func=AF.Silu), reads=[craw], writes=[cond])
        for i in range(cfg.DEPTH):
            wv = self.wview(self.io["w_mod"][i])
            for nb in range(8):
                w = wm.next()
                S.dma("sp", w, w[:], self.t_in, wv[:, :, nb * 768:(nb + 1) * 768])
                for c6 in range(6):
                    ch = nb * 6 + c6
                    ps = pst.next()
                    for k in range(8):
                        S.op("pe", lambda e, k=k, c6=c6, w=w, ps=ps: e.matmul(
                            ps[:, 0:2], w[:, k, c6 * 128:(c6 + 1) * 128], cond[:, k, :], start=(k == 0), stop=(k == 7)),
                            reads=[w, cond], writes=[ps], sig=(k == 7))
                    S.op("dve", lambda e, ch=ch, ps=ps, i=i: e.tensor_scalar(
                        out=self.MOD[:, i, ch, :], in0=ps[:, 0:2], scalar1=bmod[:, i, ch:ch + 1], scalar2=None, op0=ALU.add),
                        reads=[ps, bmod], writes=[self.MOD])
            for s in (1, 4):
                S.op("dve", lambda e, s=s, i=i: e.tensor_scalar(
                    out=self.MOD[:, i, s * 8:(s + 1) * 8, :], in0=self.MOD[:, i, s * 8:(s + 1) * 8, :], scalar1=1.0,
                    scalar2=None, op0=ALU.add), reads=[self.MOD], writes=[self.MOD])
        sc.close()

    def mixer_attn(self, i, j, last):
        S, cfg = self.S, self.cfg
        SEQ, T = cfg.SEQ, cfg.T
        lam_init = 0.8 - 0.6 * float(np.exp(-0.3 * i))
        sc = Scope(S)
        UT = sc.sb("UT", [P, 8, T], BF16)
        sc0 = Scope(S)
        xb = Rot([sc0.sb("xb%d" % k, [P, 8, 512], F32) for k in range(2)])
        for blk in cfg.blocks:
            t0, n, is_ctx = blk
            x = xb.next()
            st, sap = self.xsrc(i, blk)
            S.dma("sp", x, x[:, :, 0:n], st, sap)
            self.modulate([UT], x, lambda c, t0=t0, n=n: UT[:, c, t0:t0 + n], i, n, 1 if is_ctx else 0, 0, 1)
        sc0.close()
        COS = sc.sb("COS", [P, SEQ], F32)
        SIN = sc.sb("SIN", [P, SEQ], F32)
        S.dma("sp", COS, COS[:], self.t_in, self.io["ropeC"][:, :])
        S.dma("sp", SIN, SIN[:], self.t_in, self.io["ropeS"][:, :])
        lamt = sc.sb("lamt", [P, 4, 64], F32)
        S.dma("sp", lamt, lamt[:].rearrange("p a b -> p (a b)"), self.t_in,
              self.io["lam"][j].rearrange("a b -> (a b)").partition_broadcast(P))
        lsc = sc.sb("lsc", [P, 8], F32)
        S.op("dve", lambda e: e.tensor_tensor(out=lamt[:, 0, :], in0=lamt[:, 0, :], in1=lamt[:, 1, :], op=ALU.mult),
             reads=[lamt], writes=[lamt])
        S.op("dve", lambda e: e.tensor_tensor(out=lamt[:, 2, :], in0=lamt[:, 2, :], in1=lamt[:, 3, :], op=ALU.mult),
             reads=[lamt], writes=[lamt])
        S.op("dve", lambda e: e.reduce_sum(out=lsc[:, 0:1], in_=lamt[:, 0, :], axis=AX.X), reads=[lamt], writes=[lsc])
        S.op("dve", lambda e: e.reduce_sum(out=lsc[:, 1:2], in_=lamt[:, 2, :], axis=AX.X), reads=[lamt], writes=[lsc])
        S.op("act", lambda e: e.activation(out=lsc[:, 2:4], in_=lsc[:, 0:2], func=AF.Exp), reads=[lsc], writes=[lsc])
        S.op("dve", lambda e: e.scalar_tensor_tensor(out=lsc[:, 4:5], in0=lsc[:, 3:4], scalar=-lam_init, in1=lsc[:, 2:3],
                                                     op0=ALU.add, op1=ALU.subtract), reads=[lsc], writes=[lsc])
        NA = cfg.N_A
        sgt0 = sc.sb("sgt0", [P, NA], F32)
        sgt = sc.sb("sgt", [P, 2], F32)
        S.dma("sp", sgt0, sgt0[:], self.t_in, self.io["sublnT"][:, :])
        S.op("dve", lambda e: e.tensor_scalar(out=sgt[:, 1:2], in0=sgt0[:, j:j + 1], scalar1=float((1.0 - lam_init) * np.sqrt(128.0)),
                                              scalar2=None, op0=ALU.mult), reads=[sgt0], writes=[sgt])
        WQ = Rot([sc.sb("WQ%d" % k, [P, 8, 3, 128], BF16) for k in range(2)])
        WR = Rot([sc.sb("WR%d" % k, [P, 8, 2, 128], BF16) for k in range(2)])
        QT = Rot([[sc.sb("QA%d" % k, [P, T], BF16), sc.sb("QB%d" % k, [P, T], BF16)] for k in range(2)])
        for qpair in QT.tiles:
            for qt_ in qpair:
                S.op("pool", lambda e, qt_=qt_: e.memset(qt_[:], 0.0), writes=[qt_])
        KT = Rot([sc.sb("KT%d" % k, [P, T], BF16) for k in range(2)])
        VV = Rot([sc.sb("VV%d" % k, [P, T // 128, 128], BF16) for k in range(2)])
        pp = Rot([sc.ps("pp%d" % k) for k in range(3)])
        pO = [sc.ps("pO%d" % k) for k in range(2)]
        pL = [sc.ps("pL%d" % k) for k in range(2)]
        pR = sc.ps("pR")
        tmpa = Rot([sc.sb("tmpa%d" % k, [P, 512], F32) for k in range(4)])
        ET = Rot([sc.sb("ET%d" % k, [P, 512], BF16) for k in range(3)])
        ob = Rot([sc.sb("ob%d" % k, [P, 512], BF16) for k in range(2)])
        wq = self.wview(self.io["attn_w_qkv"][j])
        mixv = self.MIX
        qblocks = cfg.blocks if not last else cfg.lat_blocks
        Wb, Rb = WQ.tiles, WR.tiles

        def prep(h):
            W, R = Wb[h % 2], Rb[h % 2]
            for s3 in range(3):
                S.dma("pool", W, W[:, :, s3, :], self.t_in, wq[:, :, s3 * 1024 + h * 128: s3 * 1024 + (h + 1) * 128])
            for s2 in range(2):
                src = W[:, :, s2, :].rearrange("p k (b h j) -> p k b h j", b=4, h=2)
                dst = R[:, :, s2, :].rearrange("p k (b h j) -> p k b h j", b=4, h=2)
                for k in range(8):
                    S.op("dve", lambda e, src=src, dst=dst, k=k: e.tensor_scalar(
                        out=dst[:, k, :, 0, :], in0=src[:, k, :, 1, :], scalar1=-1.0, scalar2=None, op0=ALU.mult),
                        reads=[W], writes=[R])
                    S.op("dve", lambda e, src=src, dst=dst, k=k: e.tensor_copy(out=dst[:, k, :, 1, :], in_=src[:, k, :, 0, :]),
                         reads=[W], writes=[R])
        pending = [None, None]

        def flush_pending(stage):
            for st in range(stage + 1):
                if pending[st] is not None:
                    f = pending[st]
                    pending[st] = None
                    f()
        prep(0)
        for h in range(cfg.H):
            W, R = Wb[h % 2], Rb[h % 2]
            Q, K, V = QT.next(), KT.next(), VV.next()
            for blk in cfg.blocks:
                t0, n, is_ctx = blk
                for s2, dest in ((0, None), (1, K)):
                    p1 = pp.next()
                    for k in range(8):
                        S.op("pe", lambda e, k=k, p1=p1, s2=s2: e.matmul(p1[:, 0:n], W[:, k, s2, :], UT[:, k, t0:t0 + n],
                                                                         start=(k == 0), stop=(k == 7)),
                             reads=[W, UT], writes=[p1], sig=(k == 7))
                    if is_ctx:
                        if dest is None:
                            S.op("act", lambda e, p1=p1: e.copy(out=Q[0][0:64, t0:t0 + n], in_=p1[0:64, 0:n]),
                                 reads=[p1], writes=[Q[0]])
                            S.op("dve", lambda e, p1=p1: e.tensor_copy(out=Q[1][64:128, t0:t0 + n], in_=p1[64:128, 0:n]),
                                 reads=[p1], writes=[Q[1]])
                        else:
                            S.op("act", lambda e, p1=p1, dest=dest: e.copy(out=dest[:, t0:t0 + n], in_=p1[:, 0:n]),
                                 reads=[p1], writes=[dest])
                    else:
                        p2 = pp.next()
                        for k in range(8):
                            S.op("pe", lambda e, k=k, p2=p2, s2=s2: e.matmul(p2[:, 0:n], R[:, k, s2, :], UT[:, k, t0:t0 + n],
                                                                             start=(k == 0), stop=(k == 7)),
                                 reads=[R, UT], writes=[p2], sig=(k == 7))
                        a1, a2 = tmpa.next(), tmpa.next()
                        S.op("dve", lambda e, p1=p1, a1=a1: e.tensor_tensor(out=a1[:, 0:n], in0=p1[:, 0:n], in1=COS[:, t0:t0 + n], op=ALU.mult),
                             reads=[p1, COS], writes=[a1])
                        S.op("dve", lambda e, p2=p2, a2=a2: e.tensor_tensor(out=a2[:, 0:n], in0=p2[:, 0:n], in1=SIN[:, t0:t0 + n], op=ALU.mult),
                             reads=[p2, SIN], writes=[a2])
                        if dest is None:
                            for qi, (lo_, hi_) in enumerate(((0, 64), (64, 128))):
                                S.op("pool", lambda e, a1=a1, a2=a2, qi=qi, lo_=lo_, hi_=hi_: e.tensor_tensor(
                                    out=Q[qi][lo_:hi_, t0:t0 + n], in0=a1[lo_:hi_, 0:n], in1=a2[lo_:hi_, 0:n], op=ALU.add),
                                    reads=[a1, a2], writes=[Q[qi]])
                        else:
                            S.op("pool", lambda e, a1=a1, a2=a2, dest=dest: e.tensor_tensor(out=dest[:, t0:t0 + n], in0=a1[:, 0:n], in1=a2[:, 0:n], op=ALU.add),
                                 reads=[a1, a2], writes=[dest])
                for s in range(n // 128):
                    p1 = pp.next()
                    tt = t0 + s * 128
                    for k in range(8):
                        S.op("pe", lambda e, k=k, p1=p1, tt=tt: e.matmul(p1[:, 0:128], UT[:, k, tt:tt + 128], W[:, k, 2, :],
                                                                         start=(k == 0), stop=(k == 7)),
                             reads=[W, UT], writes=[p1], sig=(k == 7))
                    S.op("act", lambda e, p1=p1, tt=tt: e.copy(out=V[:, tt // 128, :], in_=p1[:, 0:128]), reads=[p1], writes=[V])
            if h + 1 < cfg.H:
                prep(h + 1)
            for qb in qblocks:
                q0, nq, q_ctx = qb
                kchunks = list(range(SEQ // 128, T // 128)) if q_ctx else list(range(T // 128))
                nk = len(kchunks)
                its = [(m, ki, kc) for m in range(2) for ki, kc in enumerate(kchunks)]
                LA = 2
                pss = {}
                r1, a1, r2, a2 = tmpa.next(), tmpa.next(), tmpa.next(), tmpa.next()

                def emit_scores(idx):
                    m, ki, kc = its[idx]
                    ps = pp.next()
                    S.op("pe", lambda e, ps=ps, kc=kc, m=m: e.matmul(
                        ps[:, 0:nq], K[:, kc * 128:(kc + 1) * 128], Q[m][:, q0:q0 + nq], start=True, stop=True),
                        reads=[K, Q[m]], writes=[ps])
                    pss[idx] = ps
                for idx in range(min(LA, len(its))):
                    emit_scores(idx)
                for idx in range(len(its)):
                    if idx + LA < len(its):
                        emit_scores(idx + LA)
                    m, ki, kc = its[idx]
                    ps = pss.pop(idx)
                    et = ET.next()
                    S.op("act", lambda e, ps=ps, et=et: e.activation(out=et[:, 0:nq], in_=ps[:, 0:nq], func=AF.Exp, scale=0.125),
                         reads=[ps], writes=[et])
                    fst, lst = ki == 0, ki == nk - 1
                    S.op("pe", lambda e, et=et, kc=kc, m=m, fst=fst, lst=lst: e.matmul(
                        pO[m][:, 0:nq], V[:, kc, :], et[:, 0:nq], start=fst, stop=lst),
                        reads=[V, et], writes=[pO[m]], sig=False)
                    S.op("pe", lambda e, et=et, m=m, fst=fst, lst=lst: e.matmul(
                        pL[m][:, 0:nq], self.onesbf[:], et[:, 0:nq], start=fst, stop=lst),
                        reads=[self.onesbf, et], writes=[pL[m]], sig=True)
                    if idx == min(20, nk - 1):
                        flush_pending(0)
                    if idx == min(23, nk - 1):
                        flush_pending(1)
                    if idx == nk - 1:
                        S.op("dve", lambda e, r1=r1: e.reciprocal(out=r1[:, 0:nq], in_=pL[0][:, 0:nq]), reads=[pL[0]], writes=[r1])
                        S.op("dve", lambda e, r1=r1, a1=a1: e.tensor_tensor(out=a1[:, 0:nq], in0=pO[0][:, 0:nq], in1=r1[:, 0:nq], op=ALU.mult),
                             reads=[pO[0], r1], writes=[a1])
                S.op("dve", lambda e, r2=r2: e.reciprocal(out=r2[:, 0:nq], in_=pL[1][:, 0:nq]), reads=[pL[1]], writes=[r2])
                S.op("dve", lambda e, r2=r2, a2=a2: e.tensor_tensor(out=a2[:, 0:nq], in0=pO[1][:, 0:nq], in1=r2[:, 0:nq], op=ALU.mult),
                     reads=[pO[1], r2], writes=[a2])
                S.op("dve", lambda e, a1=a1, a2=a2: e.scalar_tensor_tensor(out=a1[:, 0:nq], in0=a2[:, 0:nq], scalar=lsc[:, 4:5], in1=a1[:, 0:nq],
                                                                          op0=ALU.mult, op1=ALU.add), reads=[a1, a2, lsc], writes=[a1])
                S.op("pool", lambda e, a1=a1, r1=r1: e.tensor_tensor(out=r1[:, 0:nq], in0=a1[:, 0:nq], in1=a1[:, 0:nq], op=ALU.mult),
                     reads=[a1], writes=[r1])

                def tail1(r1=r1, nq=nq):
                    S.op("pe", lambda e: e.matmul(pR[:, 0:nq], self.ones32[:], r1[:, 0:nq], start=True, stop=True),
                         reads=[self.ones32, r1], writes=[pR])

                def tail2(a1=a1, r2=r2, q0=q0, nq=nq, h=h):
                    S.op("act", lambda e: e.activation(out=r2[:, 0:nq], in_=pR[:, 0:nq], func=AF.Ln, bias=self.cst[:, 1:2]),
                         reads=[pR, self.cst], writes=[r2])
                    S.op("act", lambda e: e.activation(out=r2[:, 0:nq], in_=r2[:, 0:nq], func=AF.Exp, scale=-0.5),
                         reads=[r2], writes=[r2])
                    o = ob.next()
                    S.op("dve", lambda e: e.scalar_tensor_tensor(out=o[:, 0:nq], in0=a1[:, 0:nq], scalar=sgt[:, 1:2], in1=r2[:, 0:nq],
                                                                 op0=ALU.mult, op1=ALU.mult), reads=[a1, sgt, r2], writes=[o])
                    S.dma("sp", self.t_MIX, mixv[h * 128:(h + 1) * 128, q0:q0 + nq], o, o[:, 0:nq])
                pending[0], pending[1] = tail1, tail2
            flush_pending(1)
        sc.close()

    def mixer_fnet(self, i, j, last):
        S, cfg = self.S, self.cfg
        parts = [(0, cfg.SEQ, cfg.lat_blocks, self.io["dftL"], 0)]
        if not last:
            parts.append((cfg.SEQ, cfg.CTX, cfg.ctx_blocks, self.io["dftC"], 1))
        for (off, N, blks, dft, col) in parts:
            sc = Scope(S)
            NCH = N // 128
            ACS = sc.sb("ACS", [P, NCH, 8, 256], BF16)
            cs128 = sc.sb("cs128", [P, 256], BF16)
            S.dma("sp", cs128, cs128[:], self.t_in, self.io["dft128"][:, :])
            sc1 = Scope(S)
            xb = Rot([sc1.sb("xb%d" % k, [P, 8, 512], F32) for k in range(2)])
            ub = Rot([sc1.sb("ub%d" % k, [P, 8, 512], BF16) for k in range(2)])
            pp = Rot([sc1.ps("pp%d" % k) for k in range(4)])
            for blk in blks:
                t0, n, is_ctx = blk
                x = xb.next()
                u = ub.next()
                st, sap = self.xsrc(i, blk)
                S.dma("sp", x, x[:, :, 0:n], st, sap)
                self.modulate([u], x, lambda c, u=u, n=n: u[:, c, 0:n], i, n, col, 0, 1)
                for g in range(8):
                    for s in range(n // 128):
                        p1 = pp.next()
                        nch = (t0 - off) // 128 + s
                        S.op("pe", lambda e, p1=p1, u=u, g=g, s=s: e.matmul(p1[:, 0:256], u[:, g, s * 128:(s + 1) * 128], cs128[:],
                                                                            start=True, stop=True), reads=[u, cs128], writes=[p1])
                        eng = "act" if (g + s) % 2 == 0 else "dve"
                        if eng == "act":
                            S.op("act", lambda e, p1=p1, nch=nch, g=g: e.copy(out=ACS[:, nch, g, :], in_=p1[:, 0:256]), reads=[p1], writes=[ACS])
                        else:
                            S.op("dve", lambda e, p1=p1, nch=nch, g=g: e.tensor_copy(out=ACS[:, nch, g, :], in_=p1[:, 0:256]), reads=[p1], writes=[ACS])
            sc1.close()
            sc2 = Scope(S)
            acc = [sc2.ps("acc%d" % g) for g in range(8)]
            CP = Rot([sc2.sb("CP%d" % k, [P, 2, 512], BF16) for k in range(4)])
            ob = Rot([sc2.sb("ob%d" % k, [P, 512], BF16) for k in range(4)])
            scale = float(1.0 / np.sqrt(N * 128.0))
            kbw = min(512, N)
            for kb in range(N // kbw):
                for nchk in range(NCH):
                    cp = CP.next()
                    for s2 in range(2):
                        S.dma("sp", cp, cp[:, s2, 0:kbw], self.t_in, dft[s2, nchk * 128:(nchk + 1) * 128, kb * kbw:(kb + 1) * kbw])
                    for g in range(8):
                        for s2 in range(2):
                            S.op("pe", lambda e, g=g, s2=s2, cp=cp, nchk=nchk: e.matmul(
                                acc[g][:, 0:kbw], ACS[:, nchk, g, s2 * 128:(s2 + 1) * 128], cp[:, s2, 0:kbw],
                                start=(nchk == 0 and s2 == 0), stop=(nchk == NCH - 1 and s2 == 1)),
                                reads=[ACS, cp], writes=[acc[g]], sig=(s2 == 1 and (g == 7 or nchk == NCH - 1)))
                for g in range(8):
                    o = ob.next()
                    if g % 2 == 0:
                        S.op("act", lambda e, o=o, g=g: e.activation(out=o[:, 0:kbw], in_=acc[g][:, 0:kbw], func=AF.Copy, scale=scale),
                             reads=[acc[g]], writes=[o])
                    else:
                        S.op("dve", lambda e, o=o, g=g: e.tensor_scalar(out=o[:, 0:kbw], in0=acc[g][:, 0:kbw], scalar1=scale, scalar2=None, op0=ALU.mult),
                             reads=[acc[g]], writes=[o])
                    S.dma("sp", self.t_MIX, self.MIX[g * 128:(g + 1) * 128, off + kb * kbw: off + (kb + 1) * kbw], o, o[:, 0:kbw])
            sc2.close()
            sc.close()

    def mixer_conv(self, i, j, last):
        S, cfg = self.S, self.cfg
        SEQ, CTX = cfg.SEQ, cfg.CTX
        sc = Scope(S)
        HW = 15 + SEQ + 30 + CTX + 15
        HG = sc.sb("HG", [P, 8, HW], BF16)
        lat_off, ctx_off = 15, 15 + SEQ + 30
        for (a, b) in ((0, 15), (15 + SEQ, 15 + SEQ + 30), (HW - 15, HW)):
            S.op("pool", lambda e, a=a, b=b: e.memset(HG[:, :, a:b], 0.0), writes=[HG])
        W1 = sc.sb("W1", [P, 8, 2048], BF16)
        w1v = self.wview(self.io["conv_w_pw1"][j])
        for q in range(4):
            S.dma("pool", W1, W1[:, :, q * 512:(q + 1) * 512], self.t_in, w1v[:, :, q * 512:(q + 1) * 512])
        b1 = sc.sb("b1", [P, 16], F32)
        S.dma("sp", b1, b1[:], self.t_in, self.io["conv_b_pw1T"][:, j * 16:(j + 1) * 16])
        wdw = sc.sb("wdw", [P, 8, 31], F32)
        S.dma("sp", wdw, wdw[:].rearrange("p a b -> p (a b)"), self.t_in, self.io["conv_w_dwT"][:, j * 248:(j + 1) * 248])
        cp = sc.sb("cp", [P, 4, 8], F32)
        S.dma("sp", cp, cp[:].rearrange("p a b -> p (a b)"), self.t_in, self.io["conv_pT"][:, j * 32:(j + 1) * 32])
        blks = cfg.blocks if not last else cfg.lat_blocks

        def hoff(blk):
            t0, n, is_ctx = blk
            return (ctx_off + t0 - SEQ) if is_ctx else (lat_off + t0)
        sc1 = Scope(S)
        xb = Rot([sc1.sb("xb%d" % k, [P, 8, 512], F32) for k in range(2)])
        ub = Rot([sc1.sb("ub%d" % k, [P, 8, 512], BF16) for k in range(2)])
        pp = Rot([sc1.ps("pp%d" % k) for k in range(6)])
        ta = Rot([sc1.sb("ta%d" % k, [P, 512], F32) for k in range(3)])
        tg = Rot([sc1.sb("tg%d" % k, [P, 512], F32) for k in range(3)])
        for blk in blks:
            t0, n, is_ctx = blk
            x, u = xb.next(), ub.next()
            st, sap = self.xsrc(i, blk)
            S.dma("sp", x, x[:, :, 0:n], st, sap)
            self.modulate([u], x, lambda c, u=u, n=n: u[:, c, 0:n], i, n, 1 if is_ctx else 0, 0, 1)
            ho = hoff(blk)
            for jj in range(8):
                pa, pg = pp.next(), pp.next()
                for (pt, cb) in ((pa, jj * 128), (pg, 1024 + jj * 128)):
                    for k in range(8):
                        S.op("pe", lambda e, pt=pt, cb=cb, k=k, u=u: e.matmul(pt[:, 0:n], W1[:, k, cb:cb + 128], u[:, k, 0:n],
                                                                             start=(k == 0), stop=(k == 7)),
                             reads=[W1, u], writes=[pt], sig=(k == 7))
                a, g = ta.next(), tg.next()
                S.op("dve", lambda e, pa=pa, a=a, jj=jj: e.tensor_scalar(out=a[:, 0:n], in0=pa[:, 0:n], scalar1=b1[:, jj:jj + 1], scalar2=None, op0=ALU.add),
                     reads=[pa, b1], writes=[a])
                S.op("act", lambda e, pg=pg, g=g, jj=jj: e.activation(out=g[:, 0:n], in_=pg[:, 0:n], func=AF.Sigmoid, bias=b1[:, 8 + jj:9 + jj]),
                     reads=[pg, b1], writes=[g])
                S.op("pool", lambda e, a=a, g=g, jj=jj, ho=ho: e.tensor_tensor(out=HG[:, jj, ho:ho + n], in0=a[:, 0:n], in1=g[:, 0:n], op=ALU.mult),
                     reads=[a, g], writes=[HG])
        sc1.close()
        sc2 = Scope(S)
        zb = Rot([sc2.sb("zb%d" % k, [P, 8, 512], F32) for k in range(2)])
        accB = Rot([sc2.sb("accB%d" % k, [P, 512], F32) for k in range(2)])
        tmp = dict(sq=sc2.sb("sq", [P, 8, 512], F32), pss=sc2.ps("pss"), psq=sc2.ps("psq"),
                   mean=sc2.sb("mean", [P, 512], F32), rstd=sc2.sb("rstd", [P, 512], F32), nb=sc2.sb("nb", [P, 512], F32),
                   t=Rot([sc2.sb("lt%d" % k, [P, 512], F32) for k in range(3)]))
        mb = Rot([sc2.sb("mb%d" % k, [P, 8, 512], BF16) for k in range(2)])
        for blk in blks:
            t0, n, is_ctx = blk
            ho = hoff(blk)
            z = zb.next()
            for c in range(8):
                ab = accB.next()
                for tap in range(31):
                    src_off = ho + tap - 15
                    if tap == 0:
                        S.op("dve", lambda e, c=c, src_off=src_off: e.tensor_scalar(
                            out=z[:, c, 0:n], in0=HG[:, c, src_off:src_off + n], scalar1=wdw[:, c, 0:1], scalar2=cp[:, 0, c:c + 1],
                            op0=ALU.mult, op1=ALU.add), reads=[HG, wdw, cp], writes=[z])
                    else:
                        S.op("dve", lambda e, c=c, src_off=src_off, tap=tap: e.scalar_tensor_tensor(
                            out=z[:, c, 0:n], in0=HG[:, c, src_off:src_off + n], scalar=wdw[:, c, tap:tap + 1], in1=z[:, c, 0:n],
                            op0=ALU.mult, op1=ALU.add), reads=[HG, wdw, z], writes=[z])
            m = mb.next()
            self.layer_norm(sc2, z, n, lambda c: cp[:, 1, c:c + 1], lambda c: cp[:, 2, c:c + 1],
                            [(m, lambda c, m=m, n=n: m[:, c, 0:n], AF.Silu)], tmp, greads=[cp])
            S.dma("sp", self.t_MIX, self.MIX.rearrange("(k p) t -> p k t", p=P)[:, :, t0:t0 + n], m, m[:, :, 0:n])
        sc2.close()
        sc.close()

    def post_moe(self, i, kind, j, last):
        S, cfg = self.S, self.cfg
        E = cfg.E
        blks = cfg.blocks if not last else cfg.lat_blocks
        sbs, cur, tot = [], [], 0
        for b in blks:
            if b[2] and cur and tot + b[1] <= cfg.SB_MAX + 256:
                cur.append(b)
                tot += b[1]
                continue
            if tot + b[1] > cfg.SB_MAX:
                sbs.append(cur)
                cur, tot = [], 0
            cur.append(b)
            tot += b[1]
        if cur:
            sbs.append(cur)
        if kind == 0:
            wo_ap, bo_ap = self.io["attn_w_o"][j], None
        elif kind == 1:
            wo_ap, bo_ap = self.io["fnet_w"][j], self.io["fnet_bT"][:, j * 8:(j + 1) * 8]
        else:
            wo_ap, bo_ap = self.io["conv_w_pw2"][j], self.io["conv_pT"][:, j * 32 + 24:j * 32 + 32]
        L = Scope(S)
        WO = L.sb("WO", [P, 8, 1024], BF16)
        wov = self.wview(wo_ap)
        for q in range(2):
            S.dma("pool", WO, WO[:, :, q * 512:(q + 1) * 512], self.t_in, wov[:, :, q * 512:(q + 1) * 512])
        bo = L.sb("bo", [P, 8], F32)
        if bo_ap is None:
            S.op("dve", lambda e: e.memset(bo[:], 0.0), writes=[bo])
        else:
            S.dma("sp", bo, bo[:], self.t_in, bo_ap)
        for col in range(2):
            S.op("dve", lambda e, col=col: e.tensor_tensor(out=self.GB1[:, i, :, col], in0=self.MOD[:, i, 16:24, col], in1=bo[:], op=ALU.mult),
                 reads=[self.MOD, bo], writes=[self.GB1])
        WR = L.sb("WR", [P, 8, E], F32)
        S.dma("sp", WR, WR[:], self.t_in, self.wview(self.io["moe_w_router"][i]))
        BR = L.sb("BR", [P, E], F32)
        S.dma("sp", BR, BR[:], self.t_in, self.io["moe_b_router"][i].partition_broadcast(P))
        B1 = L.sb("B1", [P, E, 16], F32)
        S.dma("sp", B1, B1[:].rearrange("p a b -> p (a b)"), self.t_in, self.io["moe_b1T"][:, i * E * 16:(i + 1) * E * 16])
        S.op("dve", lambda e: e.tensor_scalar(out=B1[:, :, 8:16], in0=B1[:, :, 8:16], scalar1=1.0, scalar2=None, op0=ALU.add),
             reads=[B1], writes=[B1])
        B2 = L.sb("B2", [E, 1024], F32)
        S.dma("sp", B2, B2[:], self.t_in, self.io["moe_b2"][i])
        xxv = self.XX.rearrange("(k p) t -> p k t", p=P)
        x1v = self.X1.rearrange("(k p) t -> p k t", p=P)
        mixv = self.MIX.rearrange("(k p) t -> p k t", p=P)
        outv = self.yT.rearrange("(k p) t -> p k t", p=P)
        for sb in sbs:
            Tb = sum(b[1] for b in sb)
            offs = []
            o = 0
            for b in sb:
                offs.append(o)
                o += b[1]
            sbt0 = sb[0][0]
            M = Scope(S)
            VT = M.sb("VT", [P, 8, Tb], BF16)
            A = Scope(S)
            xb = Rot([A.sb("xb%d" % k, [P, 8, 512], F32) for k in range(2)])
            mb = Rot([A.sb("mb%d" % k, [P, 8, 512], BF16) for k in range(2)])
            zb = A.sb("zb", [P, 8, 512], F32)
            x1b = Rot([A.sb("x1b%d" % k, [P, 8, 512], F32) for k in range(1)])
            v32 = A.sb("v32", [P, 8, 512], F32)
            tmp = dict(sq=A.sb("sq", [P, 8, 512], F32), pss=A.ps("pss"), psq=A.ps("psq"),
                       mean=A.sb("mean", [P, 512], F32), rstd=A.sb("rstd", [P, 512], F32), nb=A.sb("nb", [P, 512], F32),
                       t=Rot([A.sb("lt%d" % k, [P, 512], F32) for k in range(3)]))
            pp = Rot([A.ps("pp%d" % k) for k in range(3)])
            pl = Rot([A.ps("pl%d" % k) for k in range(2)])
            pt = A.ps("ptr")
            lg = Rot([A.sb("lg%d" % k, [P, E], F32) for k in range(2)])
            mx = Rot([A.sb("mx%d" % k, [P, 8], F32) for k in range(2)])
            gm = Rot([A.sb("gm%d" % k, [P, E], F32) for k in range(2)])
            ge = Rot([A.sb("ge%d" % k, [P, E], F32) for k in range(2)])
            gs = Rot([A.sb("gs%d" % k, [P, 2], F32) for k in range(2)])
            gt = Rot([A.sb("gt%d" % k, [E, 128], F32) for k in range(2)])
            for bi, blk in enumerate(sb):
                t0, n, is_ctx = blk
                col = 1 if is_ctx else 0
                x, m = xb.next(), mb.next()
                st, sap = self.xsrc(i, blk)
                S.dma("sp", x, x[:, :, 0:n], st, sap)
                S.dma("sp", m, m[:, :, 0:n], self.t_MIX, mixv[:, :, t0:t0 + n])
                for c in range(8):
                    S.op("act", lambda e, c=c, x=x: e.activation(out=x[:, c, 0:n], in_=x[:, c, 0:n], func=AF.Identity,
                                                                 scale=cfg.alpha, bias=self.GB1[:, i, c, col:col + 1]),
                         reads=[x, self.GB1], writes=[x])
                for dch in range(8):
                    py = pp.next()
                    for k in range(8):
                        S.op("pe", lambda e, py=py, k=k, dch=dch, m=m: e.matmul(py[:, 0:n], WO[:, k, dch * 128:(dch + 1) * 128], m[:, k, 0:n],
                                                                               start=(k == 0), stop=(k == 7)),
                             reads=[WO, m], writes=[py], sig=(k == 7))
                    S.op("dve", lambda e, py=py, dch=dch, x=x: e.scalar_tensor_tensor(
                        out=zb[:, dch, 0:n], in0=py[:, 0:n], scalar=self.MOD[:, i, 16 + dch, col:col + 1], in1=x[:, dch, 0:n],
                        op0=ALU.mult, op1=ALU.add), reads=[py, self.MOD, x], writes=[zb])
                x1 = x1b.next()
                self.layer_norm(A, zb, n, lambda c: self.LNP[:, i, 0, c:c + 1], lambda c: self.LNP[:, i, 1, c:c + 1],
                                [(x1, lambda c, x1=x1, n=n: x1[:, c, 0:n], AF.Identity)], tmp)
                S.dma("sp", self.t_X1, x1v[:, :, t0:t0 + n], x1, x1[:, :, 0:n])
                for c in range(8):
                    S.op("act", lambda e, c=c, x1=x1: e.activation(out=v32[:, c, 0:n], in_=x1[:, c, 0:n], func=AF.Identity,
                                                                   scale=self.MOD[:, i, 32 + c, col:col + 1], bias=self.MOD[:, i, 24 + c, col:col + 1]),
                         reads=[x1, self.MOD], writes=[v32])
                    eng = "dve" if c % 2 == 0 else "pool"
                    S.op(eng, lambda e, c=c, o=offs[bi]: e.tensor_copy(out=VT[:, c, o:o + n], in_=v32[:, c, 0:n]), reads=[v32], writes=[VT])
                for s in range(n // 128):
                    p1 = pl.next()
                    for k in range(8):
                        S.op("pe", lambda e, p1=p1, k=k, s=s: e.matmul(p1[:, 0:E], v32[:, k, s * 128:(s + 1) * 128], WR[:, k, :],
                                                                       start=(k == 0), stop=(k == 7)),
                             reads=[v32, WR], writes=[p1], sig=(k == 7))
                    l, mxx, gmm, gee, gss, gtt = lg.next(), mx.next(), gm.next(), ge.next(), gs.next(), gt.next()
                    S.op("dve", lambda e, l=l, p1=p1: e.tensor_tensor(out=l[:], in0=p1[:, 0:E], in1=BR[:], op=ALU.add), reads=[p1, BR], writes=[l])
                    S.op("dve", lambda e, l=l, mxx=mxx: e.max(out=mxx[:], in_=l[:]), reads=[l], writes=[mxx])
                    S.op("dve", lambda e, l=l, mxx=mxx, gmm=gmm: e.tensor_scalar(out=gmm[:], in0=l[:], scalar1=mxx[:, 3:4], scalar2=None, op0=ALU.is_ge),
                         reads=[l, mxx], writes=[gmm])
                    S.op("dve", lambda e, mxx=mxx, gss=gss: e.tensor_scalar(out=gss[:, 0:1], in0=mxx[:, 0:1], scalar1=-1.0, scalar2=None, op0=ALU.mult),
                         reads=[mxx], writes=[gss])
                    S.op("act", lambda e, l=l, gee=gee, gss=gss: e.activation(out=gee[:], in_=l[:], func=AF.Exp, bias=gss[:, 0:1]),
                         reads=[l, gss], writes=[gee])
                    S.op("dve", lambda e, gee=gee, gmm=gmm: e.tensor_tensor(out=gee[:], in0=gee[:], in1=gmm[:], op=ALU.mult), reads=[gee, gmm], writes=[gee])
                    S.op("dve", lambda e, gee=gee, gss=gss: e.reduce_sum(out=gss[:, 1:2], in_=gee[:], axis=AX.X), reads=[gee], writes=[gss])
                    S.op("dve", lambda e, gss=gss: e.reciprocal(out=gss[:, 1:2], in_=gss[:, 1:2]), reads=[gss], writes=[gss])
                    S.op("dve", lambda e, gee=gee, gss=gss: e.tensor_scalar(out=gee[:], in0=gee[:], scalar1=gss[:, 1:2], scalar2=None, op0=ALU.mult),
                         reads=[gee, gss], writes=[gee])
                    S.op("pe", lambda e, gee=gee: e.transpose(pt[0:E, 0:128], gee[:], self.ident[:]), reads=[gee, self.ident], writes=[pt])
                    S.op("act", lambda e, gtt=gtt: e.copy(out=gtt[:], in_=pt[0:E, 0:128]), reads=[pt], writes=[gtt])
                    tt = t0 + s * 128
                    S.dma("sp", self.t_GD, self.GD[:, tt:tt + 128], gtt, gtt[:])
            A.close()
            Mf = Scope(S)
            Fa = Mf.sb("Fa", [P, 8, Tb], F32)
            B = Scope(S)
            HH = [B.sb("Hh%d" % k, [P, 8, Tb], BF16) for k in range(2)]
            GBt = Rot([B.sb("GBt%d" % k, [P, Tb], F32) for k in range(2)])
            GTs = B.sb("GTs", [E, Tb], F32)
            S.dma("sp", GTs, GTs[:], self.t_GD, self.GD[:, sbt0:sbt0 + Tb])
            NW1 = 4
            w1bufs = [B.sb("w1r%d" % k, [P, 8, 2, 256], BF16) for k in range(NW1)]
            w2bufs = [B.sb("w2r%d" % k, [P, 8, 256], BF16) for k in range(4)]
            pg = Rot([B.ps("pg%d" % k) for k in range(3)])
            plin = Rot([B.ps("plin%d" % k) for k in range(3)])
            py2 = Rot([B.ps("py%d" % k) for k in range(2)])
            ta = Rot([B.sb("ta%d" % k, [P, 512], F32) for k in range(2)])
            ts_ = Rot([B.sb("ts%d" % k, [P, 512], F32) for k in range(2)])
            tl = Rot([B.sb("tl%d" % k, [P, 512], F32) for k in range(2)])
            nld = [0]

            def load_w1(q):
                while nld[0] <= q and nld[0] < E * 4:
                    qq = nld[0]
                    ex_, j4_ = qq // 4, qq % 4
                    w1 = w1bufs[qq % NW1]
                    w1v_ = self.wview(self.io["moe_w1"][i, ex_])
                    for s2 in range(2):
                        S.dma("pool", w1, w1[:, :, s2, :], self.t_in, w1v_[:, :, s2 * 1024 + j4_ * 256: s2 * 1024 + (j4_ + 1) * 256])
                    nld[0] += 1

            def load_w2(ex, d4):
                w2 = w2bufs[d4]
                w2v_ = self.wview(self.io["moe_w2"][i, ex])
                S.dma("pool", w2, w2[:], self.t_in, w2v_[:, :, d4 * 256:(d4 + 1) * 256])

            def emit_w1(ex):
                Hh = HH[ex % 2]
                gb = GBt.next()
                S.dma("sp", gb, gb[:], self.t_GD, self.GD[ex, sbt0:sbt0 + Tb].partition_broadcast(P))
                for j4 in range(4):
                    q = ex * 4 + j4
                    load_w1(q + 2)
                    if ex >= 1:
                        load_w2(ex - 1, j4)
                    w1 = w1bufs[q % NW1]
                    for jj in range(2):
                        jc = j4 * 2 + jj
                        for bi, blk in enumerate(sb):
                            n, o = blk[1], offs[bi]
                            pG, pLn = pg.next(), plin.next()
                            for (ptile, s2) in ((pG, 0), (pLn, 1)):
                                for k in range(8):
                                    S.op("pe", lambda e, ptile=ptile, s2=s2, k=k, w1=w1, jj=jj, o=o, n=n: e.matmul(
                                        ptile[:, 0:n], w1[:, k, s2, jj * 128:(jj + 1) * 128], VT[:, k, o:o + n], start=(k == 0), stop=(k == 7)),
                                        reads=[w1, VT], writes=[ptile], sig=(k == 7))
                            a, sg, l = ta.next(), ts_.next(), tl.next()
                            S.op("dve", lambda e, a=a, pG=pG, n=n, jc=jc, ex=ex: e.tensor_scalar(
                                out=a[:, 0:n], in0=pG[:, 0:n], scalar1=B1[:, ex, jc:jc + 1], scalar2=7.0, op0=ALU.add, op1=ALU.min),
                                reads=[pG, B1], writes=[a])
                            S.op("act", lambda e, a=a, sg=sg, n=n: e.activation(out=sg[:, 0:n], in_=a[:, 0:n], func=AF.Sigmoid, scale=1.702),
                                 reads=[a], writes=[sg])
                            S.op("act", lambda e, l=l, pLn=pLn, n=n, jc=jc, ex=ex: e.activation(
                                out=l[:, 0:n], in_=pLn[:, 0:n], func=AF.Identity, bias=B1[:, ex, 8 + jc:9 + jc]),
                                reads=[pLn, B1], writes=[l])
                            S.op("dve", lambda e, l=l, n=n: e.tensor_scalar(out=l[:, 0:n], in0=l[:, 0:n], scalar1=-6.0, scalar2=8.0,
                                                                          op0=ALU.max, op1=ALU.min), reads=[l], writes=[l])
                            S.op("pool", lambda e, a=a, sg=sg, n=n: e.tensor_tensor(out=sg[:, 0:n], in0=a[:, 0:n], in1=sg[:, 0:n], op=ALU.mult),
                                 reads=[a, sg], writes=[sg])
                            S.op("pool", lambda e, sg=sg, l=l, n=n: e.tensor_tensor(out=sg[:, 0:n], in0=sg[:, 0:n], in1=l[:, 0:n], op=ALU.mult),
                                 reads=[sg, l], writes=[sg])
                            S.op("pool", lambda e, sg=sg, gb=gb, n=n, o=o, jc=jc, Hh=Hh: e.tensor_tensor(
                                out=Hh[:, jc, o:o + n], in0=sg[:, 0:n], in1=gb[:, o:o + n], op=ALU.mult), reads=[sg, gb], writes=[Hh])

            def emit_w2(ex):
                Hh = HH[ex % 2]
                for d4 in range(4):
                    if ex == E - 1:
                        load_w2(ex, d4)
                    w2 = w2bufs[d4]
                    for dd in range(2):
                        dch = d4 * 2 + dd
                        for bi, blk in enumerate(sb):
                            n, o = blk[1], offs[bi]
                            py = py2.next()
                            if ex == 0:
                                S.op("pe", lambda e, py=py, n=n, o=o, dch=dch: e.matmul(
                                    py[:, 0:n], B2[:, dch * 128:(dch + 1) * 128], GTs[:, o:o + n], start=True, stop=False),
                                    reads=[B2, GTs], writes=[py], sig=False)
                            for f in range(8):
                                S.op("pe", lambda e, py=py, n=n, o=o, f=f, w2=w2, dd=dd, Hh=Hh: e.matmul(
                                    py[:, 0:n], w2[:, f, dd * 128:(dd + 1) * 128], Hh[:, f, o:o + n], start=(f == 0 and ex != 0), stop=(f == 7)),
                                    reads=[w2, Hh], writes=[py], sig=(f == 7))
                            if ex == 0:
                                S.op("act", lambda e, py=py, n=n, o=o, dch=dch: e.copy(out=Fa[:, dch, o:o + n], in_=py[:, 0:n]), reads=[py], writes=[Fa])
                            else:
                                S.op("dve", lambda e, py=py, n=n, o=o, dch=dch: e.tensor_tensor(
                                    out=Fa[:, dch, o:o + n], in0=Fa[:, dch, o:o + n], in1=py[:, 0:n], op=ALU.add), reads=[py, Fa], writes=[Fa])

            load_w1(1)
            emit_w1(0)
            for ex in range(E):
                if ex + 1 < E:
                    emit_w1(ex + 1)
                emit_w2(ex)
            B.close()
            C = Scope(S)
            xb = Rot([C.sb("xb%d" % k, [P, 8, 512], F32) for k in range(2)])
            zb = C.sb("zb", [P, 8, 512], F32)
            ob = Rot([C.sb("ob%d" % k, [P, 8, 512], F32) for k in range(2)])
            tmp = dict(sq=C.sb("sq", [P, 8, 512], F32), pss=C.ps("pss"), psq=C.ps("psq"),
                       mean=C.sb("mean", [P, 512], F32), rstd=C.sb("rstd", [P, 512], F32), nb=C.sb("nb", [P, 512], F32),
                       t=Rot([C.sb("lt%d" % k, [P, 512], F32) for k in range(3)]))
            for bi, blk in enumerate(sb):
                t0, n, is_ctx = blk
                col = 1 if is_ctx else 0
                o = offs[bi]
                x = xb.next()
                S.dma("sp", x, x[:, :, 0:n], self.t_X1, x1v[:, :, t0:t0 + n])
                for c in range(8):
                    S.op("act", lambda e, c=c, x=x: e.activation(out=x[:, c, 0:n], in_=x[:, c, 0:n], func=AF.Copy, scale=cfg.alpha),
                         reads=[x], writes=[x])
                    S.op("dve", lambda e, c=c, x=x, o=o: e.scalar_tensor_tensor(
                        out=zb[:, c, 0:n], in0=Fa[:, c, o:o + n], scalar=self.MOD[:, i, 40 + c, col:col + 1], in1=x[:, c, 0:n],
                        op0=ALU.mult, op1=ALU.add), reads=[Fa, self.MOD, x], writes=[zb])
                ot = ob.next()
                self.layer_norm(C, zb, n, lambda c: self.LNP[:, i, 2, c:c + 1], lambda c: self.LNP[:, i, 3, c:c + 1],
                                [(ot, lambda c, ot=ot, n=n: ot[:, c, 0:n], AF.Identity)], tmp)
                if last:
                    S.dma("sp", self.t_out, outv[:, :, t0:t0 + n], ot, ot[:, :, 0:n])
                else:
                    S.dma("sp", self.t_XX, xxv[:, :, t0:t0 + n], ot, ot[:, :, 0:n])
            C.close()
            Mf.close()
            M.close()
        L.close()


def pmajor(v, nch):
    v = np.asarray(v, np.float32)
    lead = v.shape[:-1]
    a = v.reshape(lead + (nch, P))
    a = np.moveaxis(a, -1, 0)
    return np.ascontiguousarray(a)


def host_constants(cfg):
    SEQ, CTX = cfg.SEQ, cfg.CTX
    GRID_W = 64
    t = np.arange(SEQ)
    rows = (t // GRID_W).astype(np.float64)
    cols = (t % GRID_W).astype(np.float64)
    inv_freq = (10000.0 ** (-np.arange(16, dtype=np.float64) / 16)).astype(np.float32).astype(np.float64)
    dd = np.arange(64)
    f = dd % 16
    pos = np.where(dd[:, None] < 32, rows[None, :], cols[None, :])
    ang = (pos.astype(np.float32) * inv_freq[f][:, None].astype(np.float32)).astype(np.float64)
    C = np.cos(ang).astype(np.float32)
    Sn = np.sin(ang).astype(np.float32)
    ropeC = np.concatenate([C, C], 0)
    ropeS = np.concatenate([Sn, Sn], 0)

    def dft(N):
        n = np.arange(N, dtype=np.int64)
        m = (n[:, None] * n[None, :]) % N
        a = 2.0 * np.pi * m.astype(np.float64) / N
        return np.stack([np.cos(a), -np.sin(a)]).astype(np.float32).astype(ml_dtypes.bfloat16)
    a128 = 2.0 * np.pi * ((np.arange(128)[:, None] * np.arange(128)[None, :]) % 128) / 128.0
    dft128 = np.concatenate([np.cos(a128), np.sin(a128)], 1).astype(np.float32).astype(ml_dtypes.bfloat16)
    return dict(ropeC=ropeC, ropeS=ropeS, dftL=dft(SEQ), dftC=dft(CTX), dft128=dft128, ident=np.eye(P, dtype=np.float32))


def host_inputs(cfg, inp, consts, b):
    f = lambda a: np.ascontiguousarray(np.asarray(a, np.float32))
    DEPTH, E = cfg.DEPTH, cfg.E
    m = {}
    m["xT"] = np.ascontiguousarray(np.asarray(inp["x"][b], np.float32).T)
    m["ctxT"] = np.ascontiguousarray(np.asarray(inp["ctx"][b], np.float32).T)
    m["cT"] = pmajor(inp["c"][b], 8)
    m["cctxT"] = pmajor(inp["c_ctx"], 8)
    m["w_mod"] = f(inp["w_mod"])
    m["b_modT"] = pmajor(inp["b_mod"], 48).reshape(P, DEPTH * 48)
    lnp = np.stack([np.asarray(inp[k], np.float32) for k in ("ln1_g", "ln1_b", "ln2_g", "ln2_b")], 1)
    m["lnp"] = pmajor(lnp, 8).reshape(P, DEPTH * 32)
    m["attn_w_qkv"] = f(inp["attn_w_qkv"])
    m["attn_w_o"] = f(inp["attn_w_o"])
    m["lam"] = np.ascontiguousarray(np.stack([np.asarray(inp[k], np.float32) for k in
                                             ("attn_lam_q1", "attn_lam_k1", "attn_lam_q2", "attn_lam_k2")], 1))
    m["sublnT"] = np.ascontiguousarray(np.asarray(inp["attn_subln_g"], np.float32).T)
    m["fnet_w"] = f(inp["fnet_w"])
    m["fnet_bT"] = pmajor(inp["fnet_b"], 8).reshape(P, -1)
    m["conv_w_pw1"] = f(inp["conv_w_pw1"])
    m["conv_b_pw1T"] = pmajor(inp["conv_b_pw1"], 16).reshape(P, -1)
    wdw = np.asarray(inp["conv_w_dw"], np.float32)
    wdw = np.moveaxis(wdw, 1, 2)
    nC = wdw.shape[0]
    wdw = wdw.reshape(nC, 8, P, 31)
    m["conv_w_dwT"] = np.ascontiguousarray(np.moveaxis(wdw, 2, 0)).reshape(P, nC * 8 * 31)
    cpar = np.stack([np.asarray(inp[k], np.float32) for k in ("conv_b_dw", "conv_ln_g", "conv_ln_b", "conv_b_pw2")], 1)
    m["conv_pT"] = pmajor(cpar, 8).reshape(P, nC * 32)
    m["conv_w_pw2"] = f(inp["conv_w_pw2"])
    m["moe_w_router"] = f(inp["moe_w_router"])
    m["moe_b_router"] = f(inp["moe_b_router"])
    m["moe_w1"] = f(inp["moe_w1"])
    m["moe_b1T"] = pmajor(inp["moe_b1"], 16).reshape(P, DEPTH * E * 16)
    m["moe_w2"] = f(inp["moe_w2"])
    m["moe_b2"] = f(inp["moe_b2"])
    m.update(consts)
    return m


_CACHE = {}


def run(cfg, inputs, n_cores):
    key = (cfg.SEQ, cfg.CTX, cfg.E, cfg.DEPTH)
    if key not in _CACHE:
        _CACHE[key] = Prog(cfg).build()
    nc = _CACHE[key]
    consts = host_constants(cfg)
    in_maps = [host_inputs(cfg, inputs, consts, b) for b in range(n_cores)]
    res = run_bass_kernel_spmd(nc, in_maps, core_ids=list(range(n_cores)))
    out = np.stack([np.ascontiguousarray(res.results[b]["yT"].T) for b in range(n_cores)], 0)
    return out.astype(np.float32)


def kernel(**inputs):
    cfg = Cfg()
    return run(cfg, inputs, 8)
```

```python
import contextlib
import numpy as np
import ml_dtypes
import concourse.bass as bass
import concourse.mybir as mybir
from concourse.bass_utils import run_bass_kernel_spmd

F32 = mybir.dt.float32
BF16 = mybir.dt.bfloat16
AF = mybir.ActivationFunctionType
ALU = mybir.AluOpType
AX = mybir.AxisListType
P = 128
LN_EPS = 1e-5


class Cfg:
    def __init__(self, SEQ=4096, CTX=256, E=32, DEPTH=4):
        self.SEQ, self.CTX, self.E, self.DEPTH = SEQ, CTX, E, DEPTH
        self.D = 1024
        self.KC = 8
        self.H = 8
        self.T = SEQ + CTX
        self.N_A = (DEPTH + 2) // 3
        self.N_B = (DEPTH + 1) // 3
        self.N_C = DEPTH // 3
        self.alpha = float((2 * DEPTH) ** 0.25)
        self.lat_blocks = [(t, 512, False) for t in range(0, SEQ, 512)]
        self.ctx_blocks = [(SEQ + t, min(512, CTX - t), True) for t in range(0, CTX, 512)]
        self.blocks = self.lat_blocks + self.ctx_blocks
        self.SB_MAX = 1024


class TT:
    def __init__(self, h, name):
        self.h = h
        self.name = name
        self.w = None
        self.r = {}
        self.dsem = None
        self.dcnt = 0
        self.w_is_dma = False

    def __getitem__(self, k):
        return self.h[k]


class Sched:
    def __init__(self, nc, es):
        self.nc = nc
        self.eng = {"pe": nc.tensor, "act": nc.scalar, "dve": nc.vector, "pool": nc.gpsimd, "sp": nc.sync}
        self.sem = {k: es.enter_context(nc.semaphore("e_" + k)) for k in self.eng}
        self.cnt = {k: 0 for k in self.eng}
        self.seen = {k: {} for k in self.eng}
        self.semkey = {}
        for k in self.eng:
            self.semkey[id(self.sem[k])] = k
        self.free_dsems = []
        self.all_dsems = []
        self.es = es
        self.ndsem = 0
        self.live = []
        self.ninst = 0

    def _get_dsem(self):
        if self.free_dsems:
            return self.free_dsems.pop()
        s = self.es.enter_context(self.nc.semaphore("d%d" % self.ndsem))
        self.ndsem += 1
        rec = [s, 0]
        self.all_dsems.append(rec)
        return rec

    def track(self, h, name):
        t = TT(h, name)
        self.live.append(t)
        return t

    def _wait(self, e, evs):
        own = self.sem[e]
        for (sem, val) in evs:
            if sem is own and e == "pe":
                continue
            k = id(sem)
            if self.seen[e].get(k, 0) >= val:
                continue
            self.eng[e].wait_ge(sem, val)
            self.seen[e][k] = val

    def op(self, e, fn, reads=(), writes=(), sig=True):
        deps = []
        for t in reads:
            if t.w is not None:
                deps.append(t.w)
        for t in writes:
            if t.w is not None:
                deps.append(t.w)
            for k, v in t.r.items():
                deps.append((k, v))
        deps2 = []
        for d in deps:
            deps2.append(d)
        self._wait(e, deps2)
        ins = fn(self.eng[e])
        self.ninst += 1
        if sig:
            self.cnt[e] += 1
            ins.then_inc(self.sem[e], 1)
            ev = (self.sem[e], self.cnt[e])
        else:
            ev = (self.sem[e], self.cnt[e] + 1)
        for t in writes:
            t.w = ev
            t.w_is_dma = False
            t.r = {}
        for t in reads:
            if t.r.get(ev[0], 0) < ev[1]:
                t.r[ev[0]] = ev[1]
        return ins

    def dma(self, q, out_t, out_ap, in_t, in_ap):
        deps = []
        if in_t.w is not None:
            deps.append(in_t.w)
        if out_t.w is not None and not out_t.w_is_dma:
            deps.append(out_t.w)
        for k, v in out_t.r.items():
            deps.append((k, v))
        self._wait(q, deps)
        if out_t.dsem is None:
            out_t.dsem = self._get_dsem()
        rec = out_t.dsem
        rec[1] += 16
        self.eng[q].dma_start(out=out_ap, in_=in_ap).then_inc(rec[0], 16)
        self.ninst += 1
        ev = (rec[0], rec[1])
        out_t.w = ev
        out_t.w_is_dma = True
        out_t.r = {}
        if in_t.r.get(ev[0], 0) < ev[1]:
            in_t.r[ev[0]] = ev[1]

    def drain(self, release=()):
        evs = [(self.sem[k], self.cnt[k]) for k in self.eng if self.cnt[k] > 0]
        evs += [(r[0], r[1]) for r in self.all_dsems if r[1] > 0]
        for e in self.eng:
            own = self.sem[e]
            for (sem, val) in evs:
                if sem is own:
                    continue
                k = id(sem)
                if self.seen[e].get(k, 0) >= val:
                    continue
                self.eng[e].wait_ge(sem, val)
                self.seen[e][k] = val
        for t in self.live:
            t.w = None
            t.r = {}
        for t in release:
            if t.dsem is not None:
                self.free_dsems.append(t.dsem)
                t.dsem = None
            if t in self.live:
                self.live.remove(t)


_UID = [0]


def _uid():
    _UID[0] += 1
    return _UID[0]


class Scope:
    def __init__(self, S):
        self.S = S
        self.es = contextlib.ExitStack()
        self.tiles = []
        self.n = 0

    def sb(self, name, shape, dt):
        h = self.es.enter_context(self.S.nc.sbuf_tensor("%s_%d" % (name, _uid()), list(shape), dt))
        t = self.S.track(h, name)
        self.tiles.append(t)
        return t

    def ps(self, name, shape=(P, 512), dt=F32):
        h = self.es.enter_context(self.S.nc.psum_tensor("%s_%d" % (name, _uid()), list(shape), dt))
        t = self.S.track(h, name)
        self.tiles.append(t)
        return t

    def close(self):
        self.S.drain(release=self.tiles)
        self.es.close()


class Rot:
    def __init__(self, tiles):
        self.tiles = tiles
        self.i = 0

    def next(self):
        t = self.tiles[self.i % len(self.tiles)]
        self.i += 1
        return t


class Prog:
    def __init__(self, cfg):
        self.cfg = cfg
        self.nc = bass.Bass("TRN2", target_bir_lowering=False)
        self.io = {}

    def din(self, name, shape, dt=F32):
        ap = self.nc.dram_tensor(name, list(shape), dt, kind="ExternalInput").ap()
        self.io[name] = ap
        return ap

    def build(self):
        cfg = self.cfg
        nc = self.nc
        D, T, E, SEQ, CTX, DEPTH = cfg.D, cfg.T, cfg.E, cfg.SEQ, cfg.CTX, cfg.DEPTH
        d = self.din
        d("xT", [D, SEQ]); d("ctxT", [D, CTX]); d("cT", [P, 8]); d("cctxT", [P, 8])
        d("w_mod", [DEPTH, D, 6 * D]); d("b_modT", [P, DEPTH * 48])
        d("lnp", [P, DEPTH * 32])
        d("attn_w_qkv", [cfg.N_A, D, 3 * D]); d("attn_w_o", [cfg.N_A, D, D])
        d("lam", [cfg.N_A, 4, 64]); d("sublnT", [P, cfg.N_A])
        d("fnet_w", [max(cfg.N_B, 1), D, D]); d("fnet_bT", [P, max(cfg.N_B, 1) * 8])
        nC = max(cfg.N_C, 1)
        d("conv_w_pw1", [nC, D, 2 * D]); d("conv_b_pw1T", [P, nC * 16])
        d("conv_w_dwT", [P, nC * 8 * 31]); d("conv_pT", [P, nC * 4 * 8])
        d("conv_w_pw2", [nC, D, D])
        d("moe_w_router", [DEPTH, D, E]); d("moe_b_router", [DEPTH, E])
        d("moe_w1", [DEPTH, E, D, 2 * D]); d("moe_b1T", [P, DEPTH * E * 16])
        d("moe_w2", [DEPTH, E, D, D]); d("moe_b2", [DEPTH, E, D])
        d("ropeC", [P, SEQ]); d("ropeS", [P, SEQ])
        d("dftL", [2, SEQ, SEQ], BF16); d("dftC", [2, CTX, CTX], BF16); d("dft128", [P, 256], BF16)
        d("ident", [P, P])
        self.yT = nc.dram_tensor("yT", [D, SEQ], F32, kind="ExternalOutput").ap()
        self.XX = nc.dram_tensor("XX", [D, T], F32).ap()
        self.X1 = nc.dram_tensor("X1", [D, T], F32).ap()
        self.MIX = nc.dram_tensor("MIX", [D, T], BF16).ap()
        self.GD = nc.dram_tensor("GD", [E, T], F32).ap()

        with contextlib.ExitStack() as es:
            S = Sched(nc, es)
            self.S = S
            es.enter_context(nc.Block())
            self.t_in = S.track(None, "inputs")
            self.t_XX = S.track(None, "XX")
            self.t_X1 = S.track(None, "X1")
            self.t_MIX = S.track(None, "MIX")
            self.t_GD = S.track(None, "GD")
            self.t_out = S.track(None, "yT")
            G = Scope(S)
            self.G = G
            self.ones32 = G.sb("ones32", [P, P], F32)
            self.onesbf = G.sb("onesbf", [P, P], BF16)
            self.ident = G.sb("ident", [P, P], F32)
            self.MOD = G.sb("MOD", [P, DEPTH, 48, 2], F32)
            self.LNP = G.sb("LNP", [P, DEPTH, 4, 8], F32)
            self.GB1 = G.sb("GB1", [P, DEPTH, 8, 2], F32)
            self.cst = G.sb("cst", [P, 4], F32)
            S.op("dve", lambda e: e.memset(self.cst[:, 0:1], LN_EPS), writes=[self.cst])
            S.op("dve", lambda e: e.memset(self.cst[:, 1:2], 128.0 * LN_EPS), writes=[self.cst])
            S.op("dve", lambda e: e.memset(self.ones32[:], 1.0), writes=[self.ones32])
            S.op("dve", lambda e: e.memset(self.onesbf[:], 1.0), writes=[self.onesbf])
            S.dma("sp", self.ident, self.ident[:], self.t_in, self.io["ident"][:, :])
            S.dma("sp", self.LNP, self.LNP[:].rearrange("p a b c -> p (a b c)"), self.t_in, self.io["lnp"][:, :])
            self.stage_mod()
            for i in range(DEPTH):
                kind = i % 3
                j = i // 3
                last = i == DEPTH - 1
                if kind == 0:
                    self.mixer_attn(i, j, last)
                elif kind == 1:
                    self.mixer_fnet(i, j, last)
                else:
                    self.mixer_conv(i, j, last)
                self.post_moe(i, kind, j, last)
            S.drain()
            G.close()
        return nc

    def xsrc(self, i, blk):
        t0, n, is_ctx = blk
        cfg = self.cfg
        if i == 0:
            if is_ctx:
                ap = self.io["ctxT"].rearrange("(k p) t -> p k t", p=P)[:, :, t0 - cfg.SEQ:t0 - cfg.SEQ + n]
            else:
                ap = self.io["xT"].rearrange("(k p) t -> p k t", p=P)[:, :, t0:t0 + n]
            return self.t_in, ap
        return self.t_XX, self.XX.rearrange("(k p) t -> p k t", p=P)[:, :, t0:t0 + n]

    def wview(self, ap2d):
        return ap2d.rearrange("(k p) n -> p k n", p=P)

    def modulate(self, eng_rot, x, ut_ap_fn, i, n, col, s_sh, s_sc):
        S = self.S
        for c in range(8):
            S.op("act", lambda e, c=c: e.activation(
                out=ut_ap_fn(c), in_=x[:, c, 0:n], func=AF.Identity,
                bias=self.MOD[:, i, s_sh * 8 + c, col:col + 1], scale=self.MOD[:, i, s_sc * 8 + c, col:col + 1]),
                reads=[x, self.MOD], writes=eng_rot)

    def layer_norm(self, sc, z, n, gcol, bcol, outs, tmp, greads=()):
        S = self.S
        sq, pss, psq, mean, rstd, nb = tmp["sq"], tmp["pss"], tmp["psq"], tmp["mean"], tmp["rstd"], tmp["nb"]
        for c in range(8):
            S.op("act", lambda e, c=c: e.activation(out=sq[:, c, 0:n], in_=z[:, c, 0:n], func=AF.Square),
                 reads=[z], writes=[sq])
        for c in range(8):
            S.op("pe", lambda e, c=c: e.matmul(pss[:, 0:n], self.ones32[:], z[:, c, 0:n], start=(c == 0), stop=(c == 7)),
                 reads=[self.ones32, z], writes=[pss], sig=(c == 7))
        for c in range(8):
            S.op("pe", lambda e, c=c: e.matmul(psq[:, 0:n], self.ones32[:], sq[:, c, 0:n], start=(c == 0), stop=(c == 7)),
                 reads=[self.ones32, sq], writes=[psq], sig=(c == 7))
        invd = 1.0 / 1024.0
        S.op("dve", lambda e: e.tensor_scalar(out=mean[:, 0:n], in0=pss[:, 0:n], scalar1=invd, scalar2=None, op0=ALU.mult),
             reads=[pss], writes=[mean])
        S.op("dve", lambda e: e.tensor_tensor(out=nb[:, 0:n], in0=mean[:, 0:n], in1=mean[:, 0:n], op=ALU.mult),
             reads=[mean], writes=[nb])
        S.op("dve", lambda e: e.scalar_tensor_tensor(out=rstd[:, 0:n], in0=psq[:, 0:n], scalar=invd, in1=nb[:, 0:n],
                                                     op0=ALU.mult, op1=ALU.subtract),
             reads=[psq, nb], writes=[rstd])
        S.op("act", lambda e: e.activation(out=rstd[:, 0:n], in_=rstd[:, 0:n], func=AF.Sqrt, bias=self.cst[:, 0:1]),
             reads=[rstd, self.cst], writes=[rstd])
        S.op("dve", lambda e: e.reciprocal(out=rstd[:, 0:n], in_=rstd[:, 0:n]), reads=[rstd], writes=[rstd])
        S.op("dve", lambda e: e.scalar_tensor_tensor(out=nb[:, 0:n], in0=mean[:, 0:n], scalar=-1.0, in1=rstd[:, 0:n],
                                                     op0=ALU.mult, op1=ALU.mult),
             reads=[mean, rstd], writes=[nb])
        for c in range(8):
            t = tmp["t"].next()
            eng = "dve" if c % 2 == 0 else "pool"
            S.op(eng, lambda e, c=c, t=t: e.tensor_tensor(out=t[:, 0:n], in0=z[:, c, 0:n], in1=rstd[:, 0:n], op=ALU.mult),
                 reads=[z, rstd], writes=[t])
            S.op(eng, lambda e, t=t: e.tensor_tensor(out=t[:, 0:n], in0=t[:, 0:n], in1=nb[:, 0:n], op=ALU.add),
                 reads=[t, nb], writes=[t])
            for (ot, apf, func) in outs:
                S.op("act", lambda e, c=c, t=t, apf=apf, func=func: e.activation(
                    out=apf(c), in_=t[:, 0:n], func=func, bias=bcol(c), scale=gcol(c)),
                    reads=[t, self.LNP, self.MOD] + list(greads), writes=[ot])

    def stage_mod(self):
        S, cfg = self.S, self.cfg
        sc = Scope(S)
        craw = sc.sb("craw", [P, 16], F32)
        cond = sc.sb("cond", [P, 8, 2], F32)
        bmod = sc.sb("bmod", [P, cfg.DEPTH, 48], F32)
        wm = Rot([sc.sb("wm%d" % k, [P, 8, 768], F32) for k in range(2)])
        pst = Rot([sc.ps("pmod%d" % k) for k in range(2)])
        S.dma("sp", craw, craw[:, 0:8], self.t_in, self.io["cT"][:, :])
        S.dma("sp", craw, craw[:, 8:16], self.t_in, self.io["cctxT"][:, :])
        S.dma("sp", bmod, bmod[:].rearrange("p a b -> p (a b)"), self.t_in, self.io["b_modT"][:, :])
        S.op("act", lambda e: e.activation(out=cond[:, :, 0], in_=craw[:, 0:8], func=AF.Silu), reads=[craw], writes=[cond])
        S.op("act", lambda e: e.activation(out=cond[:, :, 1], in_=craw[:, 8:16], func=AF.Silu), reads=[craw], writes=[cond])
        for i in range(cfg.DEPTH):
            wv = self.wview(self.io["w_mod"][i])
            for nb in range(8):
                w = wm.next()
                S.dma("sp", w, w[:], self.t_in, wv[:, :, nb * 768:(nb + 1) * 768])
                for c6 in range(6):
                    ch = nb * 6 + c6
                    ps = pst.next()
                    for k in range(8):
                        S.op("pe", lambda e, k=k, c6=c6, w=w, ps=ps: e.matmul(
                            ps[:, 0:2], w[:, k, c6 * 128:(c6 + 1) * 128], cond[:, k, :], start=(k == 0), stop=(k == 7)),
                            reads=[w, cond], writes=[ps], sig=(k == 7))
                    S.op("dve", lambda e, ch=ch, ps=ps, i=i: e.tensor_scalar(
                        out=self.MOD[:, i, ch, :], in0=ps[:, 0:2], scalar1=bmod[:, i, ch:ch + 1], scalar2=None, op0=ALU.add),
                        reads=[ps, bmod], writes=[self.MOD])
            for s in (1, 4):
                S.op("dve", lambda e, s=s, i=i: e.tensor_scalar(
                    out=self.MOD[:, i, s * 8:(s + 1) * 8, :], in0=self.MOD[:, i, s * 8:(s + 1) * 8, :], scalar1=1.0,
                    scalar2=None, op0=ALU.add), reads=[self.MOD], writes=[self.MOD])
        sc.close()

    def mixer_attn(self, i, j, last):
        S, cfg = self.S, self.cfg
        SEQ, T = cfg.SEQ, cfg.T
        lam_init = 0.8 - 0.6 * float(np.exp(-0.3 * i))
        sc = Scope(S)
        UT = sc.sb("UT", [P, 8, T], BF16)
        sc0 = Scope(S)
        xb = Rot([sc0.sb("xb%d" % k, [P, 8, 512], F32) for k in range(2)])
        for blk in cfg.blocks:
            t0, n, is_ctx = blk
            x = xb.next()
            st, sap = self.xsrc(i, blk)
            S.dma("sp", x, x[:, :, 0:n], st, sap)
            self.modulate([UT], x, lambda c, t0=t0, n=n: UT[:, c, t0:t0 + n], i, n, 1 if is_ctx else 0, 0, 1)
        sc0.close()
        COS = sc.sb("COS", [P, SEQ], F32)
        SIN = sc.sb("SIN", [P, SEQ], F32)
        S.dma("sp", COS, COS[:], self.t_in, self.io["ropeC"][:, :])
        S.dma("sp", SIN, SIN[:], self.t_in, self.io["ropeS"][:, :])
        lamt = sc.sb("lamt", [P, 4, 64], F32)
        S.dma("sp", lamt, lamt[:].rearrange("p a b -> p (a b)"), self.t_in,
              self.io["lam"][j].rearrange("a b -> (a b)").partition_broadcast(P))
        lsc = sc.sb("lsc", [P, 8], F32)
        S.op("dve", lambda e: e.tensor_tensor(out=lamt[:, 0, :], in0=lamt[:, 0, :], in1=lamt[:, 1, :], op=ALU.mult),
             reads=[lamt], writes=[lamt])
        S.op("dve", lambda e: e.tensor_tensor(out=lamt[:, 2, :], in0=lamt[:, 2, :], in1=lamt[:, 3, :], op=ALU.mult),
             reads=[lamt], writes=[lamt])
        S.op("dve", lambda e: e.reduce_sum(out=lsc[:, 0:1], in_=lamt[:, 0, :], axis=AX.X), reads=[lamt], writes=[lsc])
        S.op("dve", lambda e: e.reduce_sum(out=lsc[:, 1:2], in_=lamt[:, 2, :], axis=AX.X), reads=[lamt], writes=[lsc])
        S.op("act", lambda e: e.activation(out=lsc[:, 2:4], in_=lsc[:, 0:2], func=AF.Exp), reads=[lsc], writes=[lsc])
        S.op("dve", lambda e: e.scalar_tensor_tensor(out=lsc[:, 4:5], in0=lsc[:, 3:4], scalar=-lam_init, in1=lsc[:, 2:3],
                                                     op0=ALU.add, op1=ALU.subtract), reads=[lsc], writes=[lsc])
        NA = cfg.N_A
        sgt0 = sc.sb("sgt0", [P, NA], F32)
        sgt = sc.sb("sgt", [P, 2], F32)
        S.dma("sp", sgt0, sgt0[:], self.t_in, self.io["sublnT"][:, :])
        S.op("dve", lambda e: e.tensor_scalar(out=sgt[:, 1:2], in0=sgt0[:, j:j + 1], scalar1=float((1.0 - lam_init) * np.sqrt(128.0)),
                                              scalar2=None, op0=ALU.mult), reads=[sgt0], writes=[sgt])
        WQ = Rot([sc.sb("WQ%d" % k, [P, 8, 3, 128], BF16) for k in range(2)])
        WR = Rot([sc.sb("WR%d" % k, [P, 8, 2, 128], BF16) for k in range(2)])
        QT = Rot([[sc.sb("QA%d" % k, [P, T], BF16), sc.sb("QB%d" % k, [P, T], BF16)] for k in range(2)])
        for qpair in QT.tiles:
            for qt_ in qpair:
                S.op("pool", lambda e, qt_=qt_: e.memset(qt_[:], 0.0), writes=[qt_])
        KT = Rot([sc.sb("KT%d" % k, [P, T], BF16) for k in range(2)])
        VV = Rot([sc.sb("VV%d" % k, [P, T // 128, 128], BF16) for k in range(2)])
        pp = Rot([sc.ps("pp%d" % k) for k in range(3)])
        pO = [sc.ps("pO%d" % k) for k in range(2)]
        pL = [sc.ps("pL%d" % k) for k in range(2)]
        pR = sc.ps("pR")
        tmpa = Rot([sc.sb("tmpa%d" % k, [P, 512], F32) for k in range(4)])
        ET = Rot([sc.sb("ET%d" % k, [P, 512], BF16) for k in range(3)])
        ob = Rot([sc.sb("ob%d" % k, [P, 512], BF16) for k in range(2)])
        wq = self.wview(self.io["attn_w_qkv"][j])
        mixv = self.MIX
        qblocks = cfg.blocks if not last else cfg.lat_blocks
        Wb, Rb = WQ.tiles, WR.tiles

        def prep(h):
            W, R = Wb[h % 2], Rb[h % 2]
            for s3 in range(3):
                S.dma("pool", W, W[:, :, s3, :], self.t_in, wq[:, :, s3 * 1024 + h * 128: s3 * 1024 + (h + 1) * 128])
            for s2 in range(2):
                src = W[:, :, s2, :].rearrange("p k (b h j) -> p k b h j", b=4, h=2)
                dst = R[:, :, s2, :].rearrange("p k (b h j) -> p k b h j", b=4, h=2)
                for k in range(8):
                    S.op("dve", lambda e, src=src, dst=dst, k=k: e.tensor_scalar(
                        out=dst[:, k, :, 0, :], in0=src[:, k, :, 1, :], scalar1=-1.0, scalar2=None, op0=ALU.mult),
                        reads=[W], writes=[R])
                    S.op("dve", lambda e, src=src, dst=dst, k=k: e.tensor_copy(out=dst[:, k, :, 1, :], in_=src[:, k, :, 0, :]),
                         reads=[W], writes=[R])
        pending = [None, None]

        def flush_pending(stage):
            for st in range(stage + 1):
                if pending[st] is not None:
                    f = pending[st]
                    pending[st] = None
                    f()
        prep(0)
        for h in range(cfg.H):
            W, R = Wb[h % 2], Rb[h % 2]
            Q, K, V = QT.next(), KT.next(), VV.next()
            for blk in cfg.blocks:
                t0, n, is_ctx = blk
                for s2, dest in ((0, None), (1, K)):
                    p1 = pp.next()
                    for k in range(8):
                        S.op("pe", lambda e, k=k, p1=p1, s2=s2: e.matmul(p1[:, 0:n], W[:, k, s2, :], UT[:, k, t0:t0 + n],
                                                                         start=(k == 0), stop=(k == 7)),
                             reads=[W, UT], writes=[p1], sig=(k == 7))
                    if is_ctx:
                        if dest is None:
                            S.op("act", lambda e, p1=p1: e.copy(out=Q[0][0:64, t0:t0 + n], in_=p1[0:64, 0:n]),
                                 reads=[p1], writes=[Q[0]])
                            S.op("dve", lambda e, p1=p1: e.tensor_copy(out=Q[1][64:128, t0:t0 + n], in_=p1[64:128, 0:n]),
                                 reads=[p1], writes=[Q[1]])
                        else:
                            S.op("act", lambda e, p1=p1, dest=dest: e.copy(out=dest[:, t0:t0 + n], in_=p1[:, 0:n]),
                                 reads=[p1], writes=[dest])
                    else:
                        p2 = pp.next()
                        for k in range(8):
                            S.op("pe", lambda e, k=k, p2=p2, s2=s2: e.matmul(p2[:, 0:n], R[:, k, s2, :], UT[:, k, t0:t0 + n],
                                                                             start=(k == 0), stop=(k == 7)),
                                 reads=[R, UT], writes=[p2], sig=(k == 7))
                        a1, a2 = tmpa.next(), tmpa.next()
                        S.op("dve", lambda e, p1=p1, a1=a1: e.tensor_tensor(out=a1[:, 0:n], in0=p1[:, 0:n], in1=COS[:, t0:t0 + n], op=ALU.mult),
                             reads=[p1, COS], writes=[a1])
                        S.op("dve", lambda e, p2=p2, a2=a2: e.tensor_tensor(out=a2[:, 0:n], in0=p2[:, 0:n], in1=SIN[:, t0:t0 + n], op=ALU.mult),
                             reads=[p2, SIN], writes=[a2])
                        if dest is None:
                            for qi, (lo_, hi_) in enumerate(((0, 64), (64, 128))):
                                S.op("pool", lambda e, a1=a1, a2=a2, qi=qi, lo_=lo_, hi_=hi_: e.tensor_tensor(
                                    out=Q[qi][lo_:hi_, t0:t0 + n], in0=a1[lo_:hi_, 0:n], in1=a2[lo_:hi_, 0:n], op=ALU.add),
                                    reads=[a1, a2], writes=[Q[qi]])
                        else:
                            S.op("pool", lambda e, a1=a1, a2=a2, dest=dest: e.tensor_tensor(out=dest[:, t0:t0 + n], in0=a1[:, 0:n], in1=a2[:, 0:n], op=ALU.add),
                                 reads=[a1, a2], writes=[dest])
                for s in range(n // 128):
                    p1 = pp.next()
                    tt = t0 + s * 128
                    for k in range(8):
                        S.op("pe", lambda e, k=k, p1=p1, tt=tt: e.matmul(p1[:, 0:128], UT[:, k, tt:tt + 128], W[:, k, 2, :],
                                                                         start=(k == 0), stop=(k == 7)),
                             reads=[W, UT], writes=[p1], sig=(k == 7))
                    S.op("act", lambda e, p1=p1, tt=tt: e.copy(out=V[:, tt // 128, :], in_=p1[:, 0:128]), reads=[p1], writes=[V])
            if h + 1 < cfg.H:
                prep(h + 1)
            for qb in qblocks:
                q0, nq, q_ctx = qb
                kchunks = list(range(SEQ // 128, T // 128)) if q_ctx else list(range(T // 128))
                nk = len(kchunks)
                its = [(m, ki, kc) for m in range(2) for ki, kc in enumerate(kchunks)]
                LA = 2
                pss = {}
                r1, a1, r2, a2 = tmpa.next(), tmpa.next(), tmpa.next(), tmpa.next()

                def emit_scores(idx):
                    m, ki, kc = its[idx]
                    ps = pp.next()
                    S.op("pe", lambda e, ps=ps, kc=kc, m=m: e.matmul(
                        ps[:, 0:nq], K[:, kc * 128:(kc + 1) * 128], Q[m][:, q0:q0 + nq], start=True, stop=True),
                        reads=[K, Q[m]], writes=[ps])
                    pss[idx] = ps
                for idx in range(min(LA, len(its))):
                    emit_scores(idx)
                for idx in range(len(its)):
                    if idx + LA < len(its):
                        emit_scores(idx + LA)
                    m, ki, kc = its[idx]
                    ps = pss.pop(idx)
                    et = ET.next()
                    S.op("act", lambda e, ps=ps, et=et: e.activation(out=et[:, 0:nq], in_=ps[:, 0:nq], func=AF.Exp, scale=0.125),
                         reads=[ps], writes=[et])
                    fst, lst = ki == 0, ki == nk - 1
                    S.op("pe", lambda e, et=et, kc=kc, m=m, fst=fst, lst=lst: e.matmul(
                        pO[m][:, 0:nq], V[:, kc, :], et[:, 0:nq], start=fst, stop=lst),
                        reads=[V, et], writes=[pO[m]], sig=False)
                    S.op("pe", lambda e, et=et, m=m, fst=fst, lst=lst: e.matmul(
                        pL[m][:, 0:nq], self.onesbf[:], et[:, 0:nq], start=fst, stop=lst),
                        reads=[self.onesbf, et], writes=[pL[m]], sig=True)
                    if idx == min(20, nk - 1):
                        flush_pending(0)
                    if idx == min(23, nk - 1):
                        flush_pending(1)
                    if idx == nk - 1:
                        S.op("dve", lambda e, r1=r1: e.reciprocal(out=r1[:, 0:nq], in_=pL[0][:, 0:nq]), reads=[pL[0]], writes=[r1])
                        S.op("dve", lambda e, r1=r1, a1=a1: e.tensor_tensor(out=a1[:, 0:nq], in0=pO[0][:, 0:nq], in1=r1[:, 0:nq], op=ALU.mult),
                             reads=[pO[0], r1], writes=[a1])
                S.op("dve", lambda e, r2=r2: e.reciprocal(out=r2[:, 0:nq], in_=pL[1][:, 0:nq]), reads=[pL[1]], writes=[r2])
                S.op("dve", lambda e, r2=r2, a2=a2: e.tensor_tensor(out=a2[:, 0:nq], in0=pO[1][:, 0:nq], in1=r2[:, 0:nq], op=ALU.mult),
                     reads=[pO[1], r2], writes=[a2])
                S.op("dve", lambda e, a1=a1, a2=a2: e.scalar_tensor_tensor(out=a1[:, 0:nq], in0=a2[:, 0:nq], scalar=lsc[:, 4:5], in1=a1[:, 0:nq],
                                                                          op0=ALU.mult, op1=ALU.add), reads=[a1, a2, lsc], writes=[a1])
                S.op("pool", lambda e, a1=a1, r1=r1: e.tensor_tensor(out=r1[:, 0:nq], in0=a1[:, 0:nq], in1=a1[:, 0:nq], op=ALU.mult),
                     reads=[a1], writes=[r1])

                def tail1(r1=r1, nq=nq):
                    S.op("pe", lambda e: e.matmul(pR[:, 0:nq], self.ones32[:], r1[:, 0:nq], start=True, stop=True),
                         reads=[self.ones32, r1], writes=[pR])

                def tail2(a1=a1, r2=r2, q0=q0, nq=nq, h=h):
                    S.op("act", lambda e: e.activation(out=r2[:, 0:nq], in_=pR[:, 0:nq], func=AF.Ln, bias=self.cst[:, 1:2]),
                         reads=[pR, self.cst], writes=[r2])
                    S.op("act", lambda e: e.activation(out=r2[:, 0:nq], in_=r2[:, 0:nq], func=AF.Exp, scale=-0.5),
                         reads=[r2], writes=[r2])
                    o = ob.next()
                    S.op("dve", lambda e: e.scalar_tensor_tensor(out=o[:, 0:nq], in0=a1[:, 0:nq], scalar=sgt[:, 1:2], in1=r2[:, 0:nq],
                                                                 op0=ALU.mult, op1=ALU.mult), reads=[a1, sgt, r2], writes=[o])
                    S.dma("sp", self.t_MIX, mixv[h * 128:(h + 1) * 128, q0:q0 + nq], o, o[:, 0:nq])
                pending[0], pending[1] = tail1, tail2
            flush_pending(1)
        sc.close()

    def mixer_fnet(self, i, j, last):
        S, cfg = self.S, self.cfg
        parts = [(0, cfg.SEQ, cfg.lat_blocks, self.io["dftL"], 0)]
        if not last:
            parts.append((cfg.SEQ, cfg.CTX, cfg.ctx_blocks, self.io["dftC"], 1))
        for (off, N, blks, dft, col) in parts:
            sc = Scope(S)
            NCH = N // 128
            ACS = sc.sb("ACS", [P, NCH, 8, 256], BF16)
            cs128 = sc.sb("cs128", [P, 256], BF16)
            S.dma("sp", cs128, cs128[:], self.t_in, self.io["dft128"][:, :])
            sc1 = Scope(S)
            xb = Rot([sc1.sb("xb%d" % k, [P, 8, 512], F32) for k in range(2)])
            ub = Rot([sc1.sb("ub%d" % k, [P, 8, 512], BF16) for k in range(2)])
            pp = Rot([sc1.ps("pp%d" % k) for k in range(4)])
            for blk in blks:
                t0, n, is_ctx = blk
                x = xb.next()
                u = ub.next()
                st, sap = self.xsrc(i, blk)
                S.dma("sp", x, x[:, :, 0:n], st, sap)
                self.modulate([u], x, lambda c, u=u, n=n: u[:, c, 0:n], i, n, col, 0, 1)
                for g in range(8):
                    for s in range(n // 128):
                        p1 = pp.next()
                        nch = (t0 - off) // 128 + s
                        S.op("pe", lambda e, p1=p1, u=u, g=g, s=s: e.matmul(p1[:, 0:256], u[:, g, s * 128:(s + 1) * 128], cs128[:],
                                                                            start=True, stop=True), reads=[u, cs128], writes=[p1])
                        eng = "act" if (g + s) % 2 == 0 else "dve"
                        if eng == "act":
                            S.op("act", lambda e, p1=p1, nch=nch, g=g: e.copy(out=ACS[:, nch, g, :], in_=p1[:, 0:256]), reads=[p1], writes=[ACS])
                        else:
                            S.op("dve", lambda e, p1=p1, nch=nch, g=g: e.tensor_copy(out=ACS[:, nch, g, :], in_=p1[:, 0:256]), reads=[p1], writes=[ACS])
            sc1.close()
            sc2 = Scope(S)
            acc = [sc2.ps("acc%d" % g) for g in range(8)]
            CP = Rot([sc2.sb("CP%d" % k, [P, 2, 512], BF16) for k in range(4)])
            ob = Rot([sc2.sb("ob%d" % k, [P, 512], BF16) for k in range(4)])
            scale = float(1.0 / np.sqrt(N * 128.0))
            kbw = min(512, N)
            for kb in range(N // kbw):
                for nchk in range(NCH):
                    cp = CP.next()
                    for s2 in range(2):
                        S.dma("sp", cp, cp[:, s2, 0:kbw], self.t_in, dft[s2, nchk * 128:(nchk + 1) * 128, kb * kbw:(kb + 1) * kbw])
                    for g in range(8):
                        for s2 in range(2):
                            S.op("pe", lambda e, g=g, s2=s2, cp=cp, nchk=nchk: e.matmul(
                                acc[g][:, 0:kbw], ACS[:, nchk, g, s2 * 128:(s2 + 1) * 128], cp[:, s2, 0:kbw],
                                start=(nchk == 0 and s2 == 0), stop=(nchk == NCH - 1 and s2 == 1)),
                                reads=[ACS, cp], writes=[acc[g]], sig=(s2 == 1 and (g == 7 or nchk == NCH - 1)))
                for g in range(8):
                    o = ob.next()
                    if g % 2 == 0:
                        S.op("act", lambda e, o=o, g=g: e.activation(out=o[:, 0:kbw], in_=acc[g][:, 0:kbw], func=AF.Copy, scale=scale),
                             reads=[acc[g]], writes=[o])
                    else:
                        S.op("dve", lambda e, o=o, g=g: e.tensor_scalar(out=o[:, 0:kbw], in0=acc[g][:, 0:kbw], scalar1=scale, scalar2=None, op0=ALU.mult),
                             reads=[acc[g]], writes=[o])
                    S.dma("sp", self.t_MIX, self.MIX[g * 128:(g + 1) * 128, off + kb * kbw: off + (kb + 1) * kbw], o, o[:, 0:kbw])
            sc2.close()
            sc.close()

    def mixer_conv(self, i, j, last):
        S, cfg = self.S, self.cfg
        SEQ, CTX = cfg.SEQ, cfg.CTX
        sc = Scope(S)
        HW = 15 + SEQ + 30 + CTX + 15
        HG = sc.sb("HG", [P, 8, HW], BF16)
        lat_off, ctx_off = 15, 15 + SEQ + 30
        for (a, b) in ((0, 15), (15 + SEQ, 15 + SEQ + 30), (HW - 15, HW)):
            S.op("pool", lambda e, a=a, b=b: e.memset(HG[:, :, a:b], 0.0), writes=[HG])
        W1 = sc.sb("W1", [P, 8, 2048], BF16)
        w1v = self.wview(self.io["conv_w_pw1"][j])
        for q in range(4):
            S.dma("pool", W1, W1[:, :, q * 512:(q + 1) * 512], self.t_in, w1v[:, :, q * 512:(q + 1) * 512])
        b1 = sc.sb("b1", [P, 16], F32)
        S.dma("sp", b1, b1[:], self.t_in, self.io["conv_b_pw1T"][:, j * 16:(j + 1) * 16])
        wdw = sc.sb("wdw", [P, 8, 31], F32)
        S.dma("sp", wdw, wdw[:].rearrange("p a b -> p (a b)"), self.t_in, self.io["conv_w_dwT"][:, j * 248:(j + 1) * 248])
        cp = sc.sb("cp", [P, 4, 8], F32)
        S.dma("sp", cp, cp[:].rearrange("p a b -> p (a b)"), self.t_in, self.io["conv_pT"][:, j * 32:(j + 1) * 32])
        blks = cfg.blocks if not last else cfg.lat_blocks

        def hoff(blk):
            t0, n, is_ctx = blk
            return (ctx_off + t0 - SEQ) if is_ctx else (lat_off + t0)
        sc1 = Scope(S)
        xb = Rot([sc1.sb("xb%d" % k, [P, 8, 512], F32) for k in range(2)])
        ub = Rot([sc1.sb("ub%d" % k, [P, 8, 512], BF16) for k in range(2)])
        pp = Rot([sc1.ps("pp%d" % k) for k in range(6)])
        ta = Rot([sc1.sb("ta%d" % k, [P, 512], F32) for k in range(3)])
        tg = Rot([sc1.sb("tg%d" % k, [P, 512], F32) for k in range(3)])
        for blk in blks:
            t0, n, is_ctx = blk
            x, u = xb.next(), ub.next()
            st, sap = self.xsrc(i, blk)
            S.dma("sp", x, x[:, :, 0:n], st, sap)
            self.modulate([u], x, lambda c, u=u, n=n: u[:, c, 0:n], i, n, 1 if is_ctx else 0, 0, 1)
            ho = hoff(blk)
            for jj in range(8):
                pa, pg = pp.next(), pp.next()
                for (pt, cb) in ((pa, jj * 128), (pg, 1024 + jj * 128)):
                    for k in range(8):
                        S.op("pe", lambda e, pt=pt, cb=cb, k=k, u=u: e.matmul(pt[:, 0:n], W1[:, k, cb:cb + 128], u[:, k, 0:n],
                                                                             start=(k == 0), stop=(k == 7)),
                             reads=[W1, u], writes=[pt], sig=(k == 7))
                a, g = ta.next(), tg.next()
                S.op("dve", lambda e, pa=pa, a=a, jj=jj: e.tensor_scalar(out=a[:, 0:n], in0=pa[:, 0:n], scalar1=b1[:, jj:jj + 1], scalar2=None, op0=ALU.add),
                     reads=[pa, b1], writes=[a])
                S.op("act", lambda e, pg=pg, g=g, jj=jj: e.activation(out=g[:, 0:n], in_=pg[:, 0:n], func=AF.Sigmoid, bias=b1[:, 8 + jj:9 + jj]),
                     reads=[pg, b1], writes=[g])
                S.op("pool", lambda e, a=a, g=g, jj=jj, ho=ho: e.tensor_tensor(out=HG[:, jj, ho:ho + n], in0=a[:, 0:n], in1=g[:, 0:n], op=ALU.mult),
                     reads=[a, g], writes=[HG])
        sc1.close()
        sc2 = Scope(S)
        zb = Rot([sc2.sb("zb%d" % k, [P, 8, 512], F32) for k in range(2)])
        accB = Rot([sc2.sb("accB%d" % k, [P, 512], F32) for k in range(2)])
        tmp = dict(sq=sc2.sb("sq", [P, 8, 512], F32), pss=sc2.ps("pss"), psq=sc2.ps("psq"),
                   mean=sc2.sb("mean", [P, 512], F32), rstd=sc2.sb("rstd", [P, 512], F32), nb=sc2.sb("nb", [P, 512], F32),
                   t=Rot([sc2.sb("lt%d" % k, [P, 512], F32) for k in range(3)]))
        mb = Rot([sc2.sb("mb%d" % k, [P, 8, 512], BF16) for k in range(2)])
        for blk in blks:
            t0, n, is_ctx = blk
            ho = hoff(blk)
            z = zb.next()
            for c in range(8):
                ab = accB.next()
                for tap in range(31):
                    src_off = ho + tap - 15
                    if tap == 0:
                        S.op("dve", lambda e, c=c, src_off=src_off: e.tensor_scalar(
                            out=z[:, c, 0:n], in0=HG[:, c, src_off:src_off + n], scalar1=wdw[:, c, 0:1], scalar2=cp[:, 0, c:c + 1],
                            op0=ALU.mult, op1=ALU.add), reads=[HG, wdw, cp], writes=[z])
                    else:
                        S.op("dve", lambda e, c=c, src_off=src_off, tap=tap: e.scalar_tensor_tensor(
                            out=z[:, c, 0:n], in0=HG[:, c, src_off:src_off + n], scalar=wdw[:, c, tap:tap + 1], in1=z[:, c, 0:n],
                            op0=ALU.mult, op1=ALU.add), reads=[HG, wdw, z], writes=[z])
            m = mb.next()
            self.layer_norm(sc2, z, n, lambda c: cp[:, 1, c:c + 1], lambda c: cp[:, 2, c:c + 1],
                            [(m, lambda c, m=m, n=n: m[:, c, 0:n], AF.Silu)], tmp, greads=[cp])
            S.dma("sp", self.t_MIX, self.MIX.rearrange("(k p) t -> p k t", p=P)[:, :, t0:t0 + n], m, m[:, :, 0:n])
        sc2.close()
        sc.close()

    def post_moe(self, i, kind, j, last):
        S, cfg = self.S, self.cfg
        E = cfg.E
        blks = cfg.blocks if not last else cfg.lat_blocks
        sbs, cur, tot = [], [], 0
        for b in blks:
            if b[2] and cur and tot + b[1] <= cfg.SB_MAX + 256:
                cur.append(b)
                tot += b[1]
                continue
            if tot + b[1] > cfg.SB_MAX:
                sbs.append(cur)
                cur, tot = [], 0
            cur.append(b)
            tot += b[1]
        if cur:
            sbs.append(cur)
        if kind == 0:
            wo_ap, bo_ap = self.io["attn_w_o"][j], None
        elif kind == 1:
            wo_ap, bo_ap = self.io["fnet_w"][j], self.io["fnet_bT"][:, j * 8:(j + 1) * 8]
        else:
            wo_ap, bo_ap = self.io["conv_w_pw2"][j], self.io["conv_pT"][:, j * 32 + 24:j * 32 + 32]
        L = Scope(S)
        WO = L.sb("WO", [P, 8, 1024], BF16)
        wov = self.wview(wo_ap)
        for q in range(2):
            S.dma("pool", WO, WO[:, :, q * 512:(q + 1) * 512], self.t_in, wov[:, :, q * 512:(q + 1) * 512])
        bo = L.sb("bo", [P, 8], F32)
        if bo_ap is None:
            S.op("dve", lambda e: e.memset(bo[:], 0.0), writes=[bo])
        else:
            S.dma("sp", bo, bo[:], self.t_in, bo_ap)
        for col in range(2):
            S.op("dve", lambda e, col=col: e.tensor_tensor(out=self.GB1[:, i, :, col], in0=self.MOD[:, i, 16:24, col], in1=bo[:], op=ALU.mult),
                 reads=[self.MOD, bo], writes=[self.GB1])
        WR = L.sb("WR", [P, 8, E], F32)
        S.dma("sp", WR, WR[:], self.t_in, self.wview(self.io["moe_w_router"][i]))
        BR = L.sb("BR", [P, E], F32)
        S.dma("sp", BR, BR[:], self.t_in, self.io["moe_b_router"][i].partition_broadcast(P))
        B1 = L.sb("B1", [P, E, 16], F32)
        S.dma("sp", B1, B1[:].rearrange("p a b -> p (a b)"), self.t_in, self.io["moe_b1T"][:, i * E * 16:(i + 1) * E * 16])
        S.op("dve", lambda e: e.tensor_scalar(out=B1[:, :, 8:16], in0=B1[:, :, 8:16], scalar1=1.0, scalar2=None, op0=ALU.add),
             reads=[B1], writes=[B1])
        B2 = L.sb("B2", [E, 1024], F32)
        S.dma("sp", B2, B2[:], self.t_in, self.io["moe_b2"][i])
        xxv = self.XX.rearrange("(k p) t -> p k t", p=P)
        x1v = self.X1.rearrange("(k p) t -> p k t", p=P)
        mixv = self.MIX.rearrange("(k p) t -> p k t", p=P)
        outv = self.yT.rearrange("(k p) t -> p k t", p=P)
        for sb in sbs:
            Tb = sum(b[1] for b in sb)
            offs = []
            o = 0
            for b in sb:
                offs.append(o)
                o += b[1]
            sbt0 = sb[0][0]
            M = Scope(S)
            VT = M.sb("VT", [P, 8, Tb], BF16)
            A = Scope(S)
            xb = Rot([A.sb("xb%d" % k, [P, 8, 512], F32) for k in range(2)])
            mb = Rot([A.sb("mb%d" % k, [P, 8, 512], BF16) for k in range(2)])
            zb = A.sb("zb", [P, 8, 512], F32)
            x1b = Rot([A.sb("x1b%d" % k, [P, 8, 512], F32) for k in range(1)])
            v32 = A.sb("v32", [P, 8, 512], F32)
            tmp = dict(sq=A.sb("sq", [P, 8, 512], F32), pss=A.ps("pss"), psq=A.ps("psq"),
                       mean=A.sb("mean", [P, 512], F32), rstd=A.sb("rstd", [P, 512], F32), nb=A.sb("nb", [P, 512], F32),
                       t=Rot([A.sb("lt%d" % k, [P, 512], F32) for k in range(3)]))
            pp = Rot([A.ps("pp%d" % k) for k in range(3)])
            pl = Rot([A.ps("pl%d" % k) for k in range(2)])
            pt = A.ps("ptr")
            lg = Rot([A.sb("lg%d" % k, [P, E], F32) for k in range(2)])
            mx = Rot([A.sb("mx%d" % k, [P, 8], F32) for k in range(2)])
            gm = Rot([A.sb("gm%d" % k, [P, E], F32) for k in range(2)])
            ge = Rot([A.sb("ge%d" % k, [P, E], F32) for k in range(2)])
            gs = Rot([A.sb("gs%d" % k, [P, 2], F32) for k in range(2)])
            gt = Rot([A.sb("gt%d" % k, [E, 128], F32) for k in range(2)])
            for bi, blk in enumerate(sb):
                t0, n, is_ctx = blk
                col = 1 if is_ctx else 0
                x, m = xb.next(), mb.next()
                st, sap = self.xsrc(i, blk)
                S.dma("sp", x, x[:, :, 0:n], st, sap)
                S.dma("sp", m, m[:, :, 0:n], self.t_MIX, mixv[:, :, t0:t0 + n])
                for c in range(8):
                    S.op("act", lambda e, c=c, x=x: e.activation(out=x[:, c, 0:n], in_=x[:, c, 0:n], func=AF.Identity,
                                                                 scale=cfg.alpha, bias=self.GB1[:, i, c, col:col + 1]),
                         reads=[x, self.GB1], writes=[x])
                for dch in range(8):
                    py = pp.next()
                    for k in range(8):
                        S.op("pe", lambda e, py=py, k=k, dch=dch, m=m: e.matmul(py[:, 0:n], WO[:, k, dch * 128:(dch + 1) * 128], m[:, k, 0:n],
                                                                               start=(k == 0), stop=(k == 7)),
                             reads=[WO, m], writes=[py], sig=(k == 7))
                    S.op("dve", lambda e, py=py, dch=dch, x=x: e.scalar_tensor_tensor(
                        out=zb[:, dch, 0:n], in0=py[:, 0:n], scalar=self.MOD[:, i, 16 + dch, col:col + 1], in1=x[:, dch, 0:n],
                        op0=ALU.mult, op1=ALU.add), reads=[py, self.MOD, x], writes=[zb])
                x1 = x1b.next()
                self.layer_norm(A, zb, n, lambda c: self.LNP[:, i, 0, c:c + 1], lambda c: self.LNP[:, i, 1, c:c + 1],
                                [(x1, lambda c, x1=x1, n=n: x1[:, c, 0:n], AF.Identity)], tmp)
                S.dma("sp", self.t_X1, x1v[:, :, t0:t0 + n], x1, x1[:, :, 0:n])
                for c in range(8):
                    S.op("act", lambda e, c=c, x1=x1: e.activation(out=v32[:, c, 0:n], in_=x1[:, c, 0:n], func=AF.Identity,
                                                                   scale=self.MOD[:, i, 32 + c, col:col + 1], bias=self.MOD[:, i, 24 + c, col:col + 1]),
                         reads=[x1, self.MOD], writes=[v32])
                    eng = "dve" if c % 2 == 0 else "pool"
                    S.op(eng, lambda e, c=c, o=offs[bi]: e.tensor_copy(out=VT[:, c, o:o + n], in_=v32[:, c, 0:n]), reads=[v32], writes=[VT])
                for s in range(n // 128):
                    p1 = pl.next()
                    for k in range(8):
                        S.op("pe", lambda e, p1=p1, k=k, s=s: e.matmul(p1[:, 0:E], v32[:, k, s * 128:(s + 1) * 128], WR[:, k, :],
                                                                       start=(k == 0), stop=(k == 7)),
                             reads=[v32, WR], writes=[p1], sig=(k == 7))
                    l, mxx, gmm, gee, gss, gtt = lg.next(), mx.next(), gm.next(), ge.next(), gs.next(), gt.next()
                    S.op("dve", lambda e, l=l, p1=p1: e.tensor_tensor(out=l[:], in0=p1[:, 0:E], in1=BR[:], op=ALU.add), reads=[p1, BR], writes=[l])
                    S.op("dve", lambda e, l=l, mxx=mxx: e.max(out=mxx[:], in_=l[:]), reads=[l], writes=[mxx])
                    S.op("dve", lambda e, l=l, mxx=mxx, gmm=gmm: e.tensor_scalar(out=gmm[:], in0=l[:], scalar1=mxx[:, 3:4], scalar2=None, op0=ALU.is_ge),
                         reads=[l, mxx], writes=[gmm])
                    S.op("dve", lambda e, mxx=mxx, gss=gss: e.tensor_scalar(out=gss[:, 0:1], in0=mxx[:, 0:1], scalar1=-1.0, scalar2=None, op0=ALU.mult),
                         reads=[mxx], writes=[gss])
                    S.op("act", lambda e, l=l, gee=gee, gss=gss: e.activation(out=gee[:], in_=l[:], func=AF.Exp, bias=gss[:, 0:1]),
                         reads=[l, gss], writes=[gee])
                    S.op("dve", lambda e, gee=gee, gmm=gmm: e.tensor_tensor(out=gee[:], in0=gee[:], in1=gmm[:], op=ALU.mult), reads=[gee, gmm], writes=[gee])
                    S.op("dve", lambda e, gee=gee, gss=gss: e.reduce_sum(out=gss[:, 1:2], in_=gee[:], axis=AX.X), reads=[gee], writes=[gss])
                    S.op("dve", lambda e, gss=gss: e.reciprocal(out=gss[:, 1:2], in_=gss[:, 1:2]), reads=[gss], writes=[gss])
                    S.op("dve", lambda e, gee=gee, gss=gss: e.tensor_scalar(out=gee[:], in0=gee[:], scalar1=gss[:, 1:2], scalar2=None, op0=ALU.mult),
                         reads=[gee, gss], writes=[gee])
                    S.op("pe", lambda e, gee=gee: e.transpose(pt[0:E, 0:128], gee[:], self.ident[:]), reads=[gee, self.ident], writes=[pt])
                    S.op("act", lambda e, gtt=gtt: e.copy(out=gtt[:], in_=pt[0:E, 0:128]), reads=[pt], writes=[gtt])
                    tt = t0 + s * 128
                    S.dma("sp", self.t_GD, self.GD[:, tt:tt + 128], gtt, gtt[:])
            A.close()
            Mf = Scope(S)
            Fa = Mf.sb("Fa", [P, 8, Tb], F32)
            B = Scope(S)
            HH = [[B.sb("Hh%d_%d" % (k, bi), [P, 8, blk[1]], BF16) for bi, blk in enumerate(sb)] for k in range(2)]
            GBt = Rot([B.sb("GBt%d" % k, [P, Tb], F32) for k in range(2)])
            GTs = B.sb("GTs", [E, Tb], F32)
            S.dma("sp", GTs, GTs[:], self.t_GD, self.GD[:, sbt0:sbt0 + Tb])
            NW1 = 4
            w1bufs = [B.sb("w1r%d" % k, [P, 8, 2, 256], BF16) for k in range(NW1)]
            w2bufs = [B.sb("w2r%d" % k, [P, 8, 256], BF16) for k in range(4)]
            pg = Rot([B.ps("pg%d" % k) for k in range(3)])
            plin = Rot([B.ps("plin%d" % k) for k in range(3)])
            py2 = Rot([B.ps("py%d" % k) for k in range(2)])
            ta = Rot([B.sb("ta%d" % k, [P, 512], F32) for k in range(3)])
            ts_ = Rot([B.sb("ts%d" % k, [P, 512], F32) for k in range(3)])
            tl = Rot([B.sb("tl%d" % k, [P, 512], F32) for k in range(3)])
            nld = [0]

            def load_w1(q):
                while nld[0] <= q and nld[0] < E * 4:
                    qq = nld[0]
                    ex_, j4_ = qq // 4, qq % 4
                    w1 = w1bufs[qq % NW1]
                    w1v_ = self.wview(self.io["moe_w1"][i, ex_])
                    for s2 in range(2):
                        S.dma("pool", w1, w1[:, :, s2, :], self.t_in, w1v_[:, :, s2 * 1024 + j4_ * 256: s2 * 1024 + (j4_ + 1) * 256])
                    nld[0] += 1

            def load_w2(ex, d4):
                w2 = w2bufs[d4]
                w2v_ = self.wview(self.io["moe_w2"][i, ex])
                S.dma("pool", w2, w2[:], self.t_in, w2v_[:, :, d4 * 256:(d4 + 1) * 256])
            late = [None]

            def flush_late():
                if late[0] is not None:
                    f = late[0]
                    late[0] = None
                    f()

            def w1_unit(ex, w1, gb, jj, jc, n, o, bi):
                Hh = HH[ex % 2][bi]
                pG, pLn = pg.next(), plin.next()
                for (ptile, s2) in ((pG, 0), (pLn, 1)):
                    for k in range(8):
                        S.op("pe", lambda e, ptile=ptile, s2=s2, k=k: e.matmul(
                            ptile[:, 0:n], w1[:, k, s2, jj * 128:(jj + 1) * 128], VT[:, k, o:o + n], start=(k == 0), stop=(k == 7)),
                            reads=[w1, VT], writes=[ptile], sig=(k == 7))
                a, sg, l = ta.next(), ts_.next(), tl.next()
                S.op("dve", lambda e: e.tensor_scalar(
                    out=a[:, 0:n], in0=pG[:, 0:n], scalar1=B1[:, ex, jc:jc + 1], scalar2=7.0, op0=ALU.add, op1=ALU.min),
                    reads=[pG, B1], writes=[a])
                S.op("act", lambda e: e.activation(out=sg[:, 0:n], in_=a[:, 0:n], func=AF.Sigmoid, scale=1.702),
                     reads=[a], writes=[sg])
                S.op("act", lambda e: e.activation(out=l[:, 0:n], in_=pLn[:, 0:n], func=AF.Identity, bias=B1[:, ex, 8 + jc:9 + jc]),
                     reads=[pLn, B1], writes=[l])
                flush_late()

                def second():
                    S.op("dve", lambda e: e.tensor_scalar(out=l[:, 0:n], in0=l[:, 0:n], scalar1=-6.0, scalar2=8.0,
                                                          op0=ALU.max, op1=ALU.min), reads=[l], writes=[l])
                    S.op("dve", lambda e: e.tensor_tensor(out=sg[:, 0:n], in0=a[:, 0:n], in1=sg[:, 0:n], op=ALU.mult),
                         reads=[a, sg], writes=[sg])
                    S.op("pool", lambda e: e.tensor_tensor(out=sg[:, 0:n], in0=sg[:, 0:n], in1=l[:, 0:n], op=ALU.mult),
                         reads=[sg, l], writes=[sg])
                    S.op("pool", lambda e: e.tensor_tensor(out=Hh[:, jc, 0:n], in0=sg[:, 0:n], in1=gb[:, o:o + n], op=ALU.mult),
                         reads=[sg, gb], writes=[Hh])
                late[0] = second

            def w2_group(ex, w2, dd, dch, n, o, bi):
                Hh = HH[ex % 2][bi]
                py = py2.next()
                if ex == 0:
                    S.op("pe", lambda e: e.matmul(py[:, 0:n], B2[:, dch * 128:(dch + 1) * 128], GTs[:, o:o + n], start=True, stop=False),
                         reads=[B2, GTs], writes=[py], sig=False)
                for f in range(8):
                    S.op("pe", lambda e, f=f: e.matmul(
                        py[:, 0:n], w2[:, f, dd * 128:(dd + 1) * 128], Hh[:, f, 0:n], start=(f == 0 and ex != 0), stop=(f == 7)),
                        reads=[w2, Hh], writes=[py], sig=(f == 7))
                if ex == 0:
                    S.op("act", lambda e: e.copy(out=Fa[:, dch, o:o + n], in_=py[:, 0:n]), reads=[py], writes=[Fa])
                else:
                    S.op("dve", lambda e: e.tensor_tensor(out=Fa[:, dch, o:o + n], in0=Fa[:, dch, o:o + n], in1=py[:, 0:n], op=ALU.add),
                         reads=[py, Fa], writes=[Fa])

            load_w1(1)
            w2q = []
            LAG = min(2, len(sb))
            for st in range(E + 1):
                gb = None
                if st < E:
                    gb = GBt.next()
                    S.dma("sp", gb, gb[:], self.t_GD, self.GD[st, sbt0:sbt0 + Tb].partition_broadcast(P))
                for j4 in range(4):
                    if st < E:
                        load_w1(st * 4 + j4 + 2)
                    if j4 < 3:
                        if st >= 1:
                            load_w2(st - 1, j4 + 1)
                    elif st < E:
                        load_w2(st, 0)
                    w1 = w1bufs[(st * 4 + j4) % NW1]
                    w2 = w2bufs[j4]
                    for jj in range(2):
                        jc = j4 * 2 + jj
                        for bi, blk in enumerate(sb):
                            n, o = blk[1], offs[bi]
                            if st < E:
                                w1_unit(st, w1, gb, jj, jc, n, o, bi)
                            if st >= 1:
                                if st == E:
                                    flush_late()
                                w2q.append(lambda st=st, w2=w2, jj=jj, jc=jc, n=n, o=o, bi=bi: w2_group(st - 1, w2, jj, jc, n, o, bi))
                                while len(w2q) > LAG:
                                    w2q.pop(0)()
            flush_late()
            while w2q:
                w2q.pop(0)()
            B.close()
            C = Scope(S)
            xb = Rot([C.sb("xb%d" % k, [P, 8, 512], F32) for k in range(2)])
            zb = C.sb("zb", [P, 8, 512], F32)
            ob = Rot([C.sb("ob%d" % k, [P, 8, 512], F32) for k in range(2)])
            tmp = dict(sq=C.sb("sq", [P, 8, 512], F32), pss=C.ps("pss"), psq=C.ps("psq"),
                       mean=C.sb("mean", [P, 512], F32), rstd=C.sb("rstd", [P, 512], F32), nb=C.sb("nb", [P, 512], F32),
                       t=Rot([C.sb("lt%d" % k, [P, 512], F32) for k in range(3)]))
            for bi, blk in enumerate(sb):
                t0, n, is_ctx = blk
                col = 1 if is_ctx else 0
                o = offs[bi]
                x = xb.next()
                S.dma("sp", x, x[:, :, 0:n], self.t_X1, x1v[:, :, t0:t0 + n])
                for c in range(8):
                    S.op("act", lambda e, c=c, x=x: e.activation(out=x[:, c, 0:n], in_=x[:, c, 0:n], func=AF.Copy, scale=cfg.alpha),
                         reads=[x], writes=[x])
                    S.op("dve", lambda e, c=c, x=x, o=o: e.scalar_tensor_tensor(
                        out=zb[:, c, 0:n], in0=Fa[:, c, o:o + n], scalar=self.MOD[:, i, 40 + c, col:col + 1], in1=x[:, c, 0:n],
                        op0=ALU.mult, op1=ALU.add), reads=[Fa, self.MOD, x], writes=[zb])
                ot = ob.next()
                self.layer_norm(C, zb, n, lambda c: self.LNP[:, i, 2, c:c + 1], lambda c: self.LNP[:, i, 3, c:c + 1],
                                [(ot, lambda c, ot=ot, n=n: ot[:, c, 0:n], AF.Identity)], tmp)
                if last:
                    S.dma("sp", self.t_out, outv[:, :, t0:t0 + n], ot, ot[:, :, 0:n])
                else:
                    S.dma("sp", self.t_XX, xxv[:, :, t0:t0 + n], ot, ot[:, :, 0:n])
            C.close()
            Mf.close()
            M.close()
        L.close()


def pmajor(v, nch):
    v = np.asarray(v, np.float32)
    lead = v.shape[:-1]
    a = v.reshape(lead + (nch, P))
    a = np.moveaxis(a, -1, 0)
    return np.ascontiguousarray(a)


def host_constants(cfg):
    SEQ, CTX = cfg.SEQ, cfg.CTX
    GRID_W = 64
    t = np.arange(SEQ)
    rows = (t // GRID_W).astype(np.float64)
    cols = (t % GRID_W).astype(np.float64)
    inv_freq = (10000.0 ** (-np.arange(16, dtype=np.float64) / 16)).astype(np.float32).astype(np.float64)
    dd = np.arange(64)
    f = dd % 16
    pos = np.where(dd[:, None] < 32, rows[None, :], cols[None, :])
    ang = (pos.astype(np.float32) * inv_freq[f][:, None].astype(np.float32)).astype(np.float64)
    C = np.cos(ang).astype(np.float32)
    Sn = np.sin(ang).astype(np.float32)
    ropeC = np.concatenate([C, C], 0)
    ropeS = np.concatenate([Sn, Sn], 0)

    def dft(N):
        n = np.arange(N, dtype=np.int64)
        m = (n[:, None] * n[None, :]) % N
        a = 2.0 * np.pi * m.astype(np.float64) / N
        return np.stack([np.cos(a), -np.sin(a)]).astype(np.float32).astype(ml_dtypes.bfloat16)
    a128 = 2.0 * np.pi * ((np.arange(128)[:, None] * np.arange(128)[None, :]) % 128) / 128.0
    dft128 = np.concatenate([np.cos(a128), np.sin(a128)], 1).astype(np.float32).astype(ml_dtypes.bfloat16)
    return dict(ropeC=ropeC, ropeS=ropeS, dftL=dft(SEQ), dftC=dft(CTX), dft128=dft128, ident=np.eye(P, dtype=np.float32))


def host_inputs(cfg, inp, consts, b):
    f = lambda a: np.ascontiguousarray(np.asarray(a, np.float32))
    DEPTH, E = cfg.DEPTH, cfg.E
    m = {}
    m["xT"] = np.ascontiguousarray(np.asarray(inp["x"][b], np.float32).T)
    m["ctxT"] = np.ascontiguousarray(np.asarray(inp["ctx"][b], np.float32).T)
    m["cT"] = pmajor(inp["c"][b], 8)
    m["cctxT"] = pmajor(inp["c_ctx"], 8)
    m["w_mod"] = f(inp["w_mod"])
    m["b_modT"] = pmajor(inp["b_mod"], 48).reshape(P, DEPTH * 48)
    lnp = np.stack([np.asarray(inp[k], np.float32) for k in ("ln1_g", "ln1_b", "ln2_g", "ln2_b")], 1)
    m["lnp"] = pmajor(lnp, 8).reshape(P, DEPTH * 32)
    m["attn_w_qkv"] = f(inp["attn_w_qkv"])
    m["attn_w_o"] = f(inp["attn_w_o"])
    m["lam"] = np.ascontiguousarray(np.stack([np.asarray(inp[k], np.float32) for k in
                                             ("attn_lam_q1", "attn_lam_k1", "attn_lam_q2", "attn_lam_k2")], 1))
    m["sublnT"] = np.ascontiguousarray(np.asarray(inp["attn_subln_g"], np.float32).T)
    m["fnet_w"] = f(inp["fnet_w"])
    m["fnet_bT"] = pmajor(inp["fnet_b"], 8).reshape(P, -1)
    m["conv_w_pw1"] = f(inp["conv_w_pw1"])
    m["conv_b_pw1T"] = pmajor(inp["conv_b_pw1"], 16).reshape(P, -1)
    wdw = np.asarray(inp["conv_w_dw"], np.float32)
    wdw = np.moveaxis(wdw, 1, 2)
    nC = wdw.shape[0]
    wdw = wdw.reshape(nC, 8, P, 31)
    m["conv_w_dwT"] = np.ascontiguousarray(np.moveaxis(wdw, 2, 0)).reshape(P, nC * 8 * 31)
    cpar = np.stack([np.asarray(inp[k], np.float32) for k in ("conv_b_dw", "conv_ln_g", "conv_ln_b", "conv_b_pw2")], 1)
    m["conv_pT"] = pmajor(cpar, 8).reshape(P, nC * 32)
    m["conv_w_pw2"] = f(inp["conv_w_pw2"])
    m["moe_w_router"] = f(inp["moe_w_router"])
    m["moe_b_router"] = f(inp["moe_b_router"])
    m["moe_w1"] = f(inp["moe_w1"])
    m["moe_b1T"] = pmajor(inp["moe_b1"], 16).reshape(P, DEPTH * E * 16)
    m["moe_w2"] = f(inp["moe_w2"])
    m["moe_b2"] = f(inp["moe_b2"])
    m.update(consts)
    return m


_CACHE = {}


def run(cfg, inputs, n_cores):
    key = (cfg.SEQ, cfg.CTX, cfg.E, cfg.DEPTH)
    if key not in _CACHE:
        _CACHE[key] = Prog(cfg).build()
    nc = _CACHE[key]
    consts = host_constants(cfg)
    in_maps = [host_inputs(cfg, inputs, consts, b) for b in range(n_cores)]
    res = run_bass_kernel_spmd(nc, in_maps, core_ids=list(range(n_cores)))
    out = np.stack([np.ascontiguousarray(res.results[b]["yT"].T) for b in range(n_cores)], 0)
    return out.astype(np.float32)


def kernel(**inputs):
    cfg = Cfg()
    return run(cfg, inputs, 8)
```

```python
import contextlib
import numpy as np
import ml_dtypes
import concourse.bass as bass
import concourse.mybir as mybir
from concourse.bass_utils import run_bass_kernel_spmd

F32 = mybir.dt.float32
BF16 = mybir.dt.bfloat16
AF = mybir.ActivationFunctionType
ALU = mybir.AluOpType
AX = mybir.AxisListType
P = 128
LN_EPS = 1e-5


class Cfg:
    def __init__(self, SEQ=4096, CTX=256, E=32, DEPTH=4):
        self.SEQ, self.CTX, self.E, self.DEPTH = SEQ, CTX, E, DEPTH
        self.D = 1024
        self.KC = 8
        self.H = 8
        self.T = SEQ + CTX
        self.N_A = (DEPTH + 2) // 3
        self.N_B = (DEPTH + 1) // 3
        self.N_C = DEPTH // 3
        self.alpha = float((2 * DEPTH) ** 0.25)
        self.lat_blocks = [(t, 512, False) for t in range(0, SEQ, 512)]
        self.ctx_blocks = [(SEQ + t, min(512, CTX - t), True) for t in range(0, CTX, 512)]
        self.blocks = self.lat_blocks + self.ctx_blocks
        self.SB_MAX = 1024


class TT:
    def __init__(self, h, name):
        self.h = h
        self.name = name
        self.w = None
        self.r = {}
        self.dsem = None
        self.dcnt = 0
        self.w_is_dma = False

    def __getitem__(self, k):
        return self.h[k]


class Sched:
    def __init__(self, nc, es):
        self.nc = nc
        self.eng = {"pe": nc.tensor, "act": nc.scalar, "dve": nc.vector, "pool": nc.gpsimd, "sp": nc.sync}
        self.sem = {k: es.enter_context(nc.semaphore("e_" + k)) for k in self.eng}
        self.cnt = {k: 0 for k in self.eng}
        self.seen = {k: {} for k in self.eng}
        self.semkey = {}
        for k in self.eng:
            self.semkey[id(self.sem[k])] = k
        self.free_dsems = []
        self.all_dsems = []
        self.es = es
        self.ndsem = 0
        self.live = []
        self.ninst = 0

    def _get_dsem(self):
        if self.free_dsems:
            return self.free_dsems.pop()
        s = self.es.enter_context(self.nc.semaphore("d%d" % self.ndsem))
        self.ndsem += 1
        rec = [s, 0]
        self.all_dsems.append(rec)
        return rec

    def track(self, h, name):
        t = TT(h, name)
        self.live.append(t)
        return t

    def _wait(self, e, evs):
        own = self.sem[e]
        for (sem, val) in evs:
            if sem is own and e == "pe":
                continue
            k = id(sem)
            if self.seen[e].get(k, 0) >= val:
                continue
            self.eng[e].wait_ge(sem, val)
            self.seen[e][k] = val

    def op(self, e, fn, reads=(), writes=(), sig=True):
        deps = []
        for t in reads:
            if t.w is not None:
                deps.append(t.w)
        for t in writes:
            if t.w is not None:
                deps.append(t.w)
            for k, v in t.r.items():
                deps.append((k, v))
        deps2 = []
        for d in deps:
            deps2.append(d)
        self._wait(e, deps2)
        ins = fn(self.eng[e])
        self.ninst += 1
        if sig:
            self.cnt[e] += 1
            ins.then_inc(self.sem[e], 1)
            ev = (self.sem[e], self.cnt[e])
        else:
            ev = (self.sem[e], self.cnt[e] + 1)
        for t in writes:
            t.w = ev
            t.w_is_dma = False
            t.r = {}
        for t in reads:
            if t.r.get(ev[0], 0) < ev[1]:
                t.r[ev[0]] = ev[1]
        return ins

    def dma(self, q, out_t, out_ap, in_t, in_ap):
        deps = []
        if in_t.w is not None:
            deps.append(in_t.w)
        if out_t.w is not None and not out_t.w_is_dma:
            deps.append(out_t.w)
        for k, v in out_t.r.items():
            deps.append((k, v))
        self._wait(q, deps)
        if out_t.dsem is None:
            out_t.dsem = self._get_dsem()
        rec = out_t.dsem
        rec[1] += 16
        self.eng[q].dma_start(out=out_ap, in_=in_ap).then_inc(rec[0], 16)
        self.ninst += 1
        ev = (rec[0], rec[1])
        out_t.w = ev
        out_t.w_is_dma = True
        out_t.r = {}
        if in_t.r.get(ev[0], 0) < ev[1]:
            in_t.r[ev[0]] = ev[1]

    def drain(self, release=()):
        evs = [(self.sem[k], self.cnt[k]) for k in self.eng if self.cnt[k] > 0]
        evs += [(r[0], r[1]) for r in self.all_dsems if r[1] > 0]
        for e in self.eng:
            own = self.sem[e]
            for (sem, val) in evs:
                if sem is own:
                    continue
                k = id(sem)
                if self.seen[e].get(k, 0) >= val:
                    continue
                self.eng[e].wait_ge(sem, val)
                self.seen[e][k] = val
        for t in self.live:
            t.w = None
            t.r = {}
        for t in release:
            if t.dsem is not None:
                self.free_dsems.append(t.dsem)
                t.dsem = None
            if t in self.live:
                self.live.remove(t)


_UID = [0]


def _uid():
    _UID[0] += 1
    return _UID[0]


class Scope:
    def __init__(self, S):
        self.S = S
        self.es = contextlib.ExitStack()
        self.tiles = []
        self.n = 0

    def sb(self, name, shape, dt):
        h = self.es.enter_context(self.S.nc.sbuf_tensor("%s_%d" % (name, _uid()), list(shape), dt))
        t = self.S.track(h, name)
        self.tiles.append(t)
        return t

    def ps(self, name, shape=(P, 512), dt=F32):
        h = self.es.enter_context(self.S.nc.psum_tensor("%s_%d" % (name, _uid()), list(shape), dt))
        t = self.S.track(h, name)
        self.tiles.append(t)
        return t

    def close(self):
        self.S.drain(release=self.tiles)
        self.es.close()


class Rot:
    def __init__(self, tiles):
        self.tiles = tiles
        self.i = 0

    def next(self):
        t = self.tiles[self.i % len(self.tiles)]
        self.i += 1
        return t


class Prog:
    def __init__(self, cfg):
        self.cfg = cfg
        self.nc = bass.Bass("TRN2", target_bir_lowering=False)
        self.io = {}

    def din(self, name, shape, dt=F32):
        ap = self.nc.dram_tensor(name, list(shape), dt, kind="ExternalInput").ap()
        self.io[name] = ap
        return ap

    def build(self):
        cfg = self.cfg
        nc = self.nc
        D, T, E, SEQ, CTX, DEPTH = cfg.D, cfg.T, cfg.E, cfg.SEQ, cfg.CTX, cfg.DEPTH
        d = self.din
        d("xT", [D, SEQ]); d("ctxT", [D, CTX]); d("cT", [P, 8]); d("cctxT", [P, 8])
        d("w_mod", [DEPTH, D, 6 * D]); d("b_modT", [P, DEPTH * 48])
        d("lnp", [P, DEPTH * 32])
        d("attn_w_qkv", [cfg.N_A, D, 3 * D]); d("attn_w_o", [cfg.N_A, D, D])
        d("lam", [cfg.N_A, 4, 64]); d("sublnT", [P, cfg.N_A])
        d("fnet_w", [max(cfg.N_B, 1), D, D]); d("fnet_bT", [P, max(cfg.N_B, 1) * 8])
        nC = max(cfg.N_C, 1)
        d("conv_w_pw1", [nC, D, 2 * D]); d("conv_b_pw1T", [P, nC * 16])
        d("conv_w_dwT", [P, nC * 8 * 31]); d("conv_pT", [P, nC * 4 * 8])
        d("conv_w_pw2", [nC, D, D])
        d("moe_w_router", [DEPTH, D, E]); d("moe_b_router", [DEPTH, E])
        d("moe_w1", [DEPTH, E, D, 2 * D]); d("moe_b1T", [P, DEPTH * E * 16])
        d("moe_w2", [DEPTH, E, D, D]); d("moe_b2", [DEPTH, E, D])
        d("ropeC", [P, SEQ]); d("ropeS", [P, SEQ])
        d("dftL", [2, SEQ, SEQ], BF16); d("dftC", [2, CTX, CTX], BF16); d("dft128", [P, 256], BF16)
        d("ident", [P, P])
        self.yT = nc.dram_tensor("yT", [D, SEQ], F32, kind="ExternalOutput").ap()
        self.XX = nc.dram_tensor("XX", [D, T], F32).ap()
        self.X1 = nc.dram_tensor("X1", [D, T], F32).ap()
        self.MIX = nc.dram_tensor("MIX", [D, T], BF16).ap()
        self.GD = nc.dram_tensor("GD", [E, T], F32).ap()

        with contextlib.ExitStack() as es:
            S = Sched(nc, es)
            self.S = S
            es.enter_context(nc.Block())
            self.t_in = S.track(None, "inputs")
            self.t_XX = S.track(None, "XX")
            self.t_X1 = S.track(None, "X1")
            self.t_MIX = S.track(None, "MIX")
            self.t_GD = S.track(None, "GD")
            self.t_out = S.track(None, "yT")
            G = Scope(S)
            self.G = G
            self.ones32 = G.sb("ones32", [P, P], F32)
            self.onesbf = G.sb("onesbf", [P, P], BF16)
            self.ident = G.sb("ident", [P, P], F32)
            self.MOD = G.sb("MOD", [P, DEPTH, 48, 2], F32)
            self.LNP = G.sb("LNP", [P, DEPTH, 4, 8], F32)
            self.GB1 = G.sb("GB1", [P, DEPTH, 8, 2], F32)
            self.cst = G.sb("cst", [P, 4], F32)
            S.op("dve", lambda e: e.memset(self.cst[:, 0:1], LN_EPS), writes=[self.cst])
            S.op("dve", lambda e: e.memset(self.cst[:, 1:2], 128.0 * LN_EPS), writes=[self.cst])
            S.op("dve", lambda e: e.memset(self.ones32[:], 1.0), writes=[self.ones32])
            S.op("dve", lambda e: e.memset(self.onesbf[:], 1.0), writes=[self.onesbf])
            S.dma("sp", self.ident, self.ident[:], self.t_in, self.io["ident"][:, :])
            S.dma("sp", self.LNP, self.LNP[:].rearrange("p a b c -> p (a b c)"), self.t_in, self.io["lnp"][:, :])
            self.stage_mod()
            for i in range(DEPTH):
                kind = i % 3
                j = i // 3
                last = i == DEPTH - 1
                if kind == 0:
                    self.mixer_attn(i, j, last)
                elif kind == 1:
                    self.mixer_fnet(i, j, last)
                else:
                    self.mixer_conv(i, j, last)
                self.post_moe(i, kind, j, last)
            S.drain()
            G.close()
        return nc

    def xsrc(self, i, blk):
        t0, n, is_ctx = blk
        cfg = self.cfg
        if i == 0:
            if is_ctx:
                ap = self.io["ctxT"].rearrange("(k p) t -> p k t", p=P)[:, :, t0 - cfg.SEQ:t0 - cfg.SEQ + n]
            else:
                ap = self.io["xT"].rearrange("(k p) t -> p k t", p=P)[:, :, t0:t0 + n]
            return self.t_in, ap
        return self.t_XX, self.XX.rearrange("(k p) t -> p k t", p=P)[:, :, t0:t0 + n]

    def wview(self, ap2d):
        return ap2d.rearrange("(k p) n -> p k n", p=P)

    def modulate(self, eng_rot, x, ut_ap_fn, i, n, col, s_sh, s_sc):
        S = self.S
        for c in range(8):
            S.op("act", lambda e, c=c: e.activation(
                out=ut_ap_fn(c), in_=x[:, c, 0:n], func=AF.Identity,
                bias=self.MOD[:, i, s_sh * 8 + c, col:col + 1], scale=self.MOD[:, i, s_sc * 8 + c, col:col + 1]),
                reads=[x, self.MOD], writes=eng_rot)

    def layer_norm(self, sc, z, n, gcol, bcol, outs, tmp, greads=()):
        S = self.S
        sq, pss, psq, mean, rstd, nb = tmp["sq"], tmp["pss"], tmp["psq"], tmp["mean"], tmp["rstd"], tmp["nb"]
        for c in range(8):
            S.op("act", lambda e, c=c: e.activation(out=sq[:, c, 0:n], in_=z[:, c, 0:n], func=AF.Square),
                 reads=[z], writes=[sq])
        for c in range(8):
            S.op("pe", lambda e, c=c: e.matmul(pss[:, 0:n], self.ones32[:], z[:, c, 0:n], start=(c == 0), stop=(c == 7)),
                 reads=[self.ones32, z], writes=[pss], sig=(c == 7))
        for c in range(8):
            S.op("pe", lambda e, c=c: e.matmul(psq[:, 0:n], self.ones32[:], sq[:, c, 0:n], start=(c == 0), stop=(c == 7)),
                 reads=[self.ones32, sq], writes=[psq], sig=(c == 7))
        invd = 1.0 / 1024.0
        S.op("dve", lambda e: e.tensor_scalar(out=mean[:, 0:n], in0=pss[:, 0:n], scalar1=invd, scalar2=None, op0=ALU.mult),
             reads=[pss], writes=[mean])
        S.op("dve", lambda e: e.tensor_tensor(out=nb[:, 0:n], in0=mean[:, 0:n], in1=mean[:, 0:n], op=ALU.mult),
             reads=[mean], writes=[nb])
        S.op("dve", lambda e: e.scalar_tensor_tensor(out=rstd[:, 0:n], in0=psq[:, 0:n], scalar=invd, in1=nb[:, 0:n],
                                                     op0=ALU.mult, op1=ALU.subtract),
             reads=[psq, nb], writes=[rstd])
        S.op("act", lambda e: e.activation(out=rstd[:, 0:n], in_=rstd[:, 0:n], func=AF.Sqrt, bias=self.cst[:, 0:1]),
             reads=[rstd, self.cst], writes=[rstd])
        S.op("dve", lambda e: e.reciprocal(out=rstd[:, 0:n], in_=rstd[:, 0:n]), reads=[rstd], writes=[rstd])
        S.op("dve", lambda e: e.scalar_tensor_tensor(out=nb[:, 0:n], in0=mean[:, 0:n], scalar=-1.0, in1=rstd[:, 0:n],
                                                     op0=ALU.mult, op1=ALU.mult),
             reads=[mean, rstd], writes=[nb])
        for c in range(8):
            t = tmp["t"].next()
            eng = "dve" if c % 2 == 0 else "pool"
            S.op(eng, lambda e, c=c, t=t: e.tensor_tensor(out=t[:, 0:n], in0=z[:, c, 0:n], in1=rstd[:, 0:n], op=ALU.mult),
                 reads=[z, rstd], writes=[t])
            S.op(eng, lambda e, t=t: e.tensor_tensor(out=t[:, 0:n], in0=t[:, 0:n], in1=nb[:, 0:n], op=ALU.add),
                 reads=[t, nb], writes=[t])
            for (ot, apf, func) in outs:
                S.op("act", lambda e, c=c, t=t, apf=apf, func=func: e.activation(
                    out=apf(c), in_=t[:, 0:n], func=func, bias=bcol(c), scale=gcol(c)),
                    reads=[t, self.LNP, self.MOD] + list(greads), writes=[ot])

    def stage_mod(self):
        S, cfg = self.S, self.cfg
        sc = Scope(S)
        craw = sc.sb("craw", [P, 16], F32)
        cond = sc.sb("cond", [P, 8, 2], F32)
        bmod = sc.sb("bmod", [P, cfg.DEPTH, 48], F32)
        wm = Rot([sc.sb("wm%d" % k, [P, 8, 768], F32) for k in range(2)])
        pst = Rot([sc.ps("pmod%d" % k) for k in range(2)])
        S.dma("sp", craw, craw[:, 0:8], self.t_in, self.io["cT"][:, :])
        S.dma("sp", craw, craw[:, 8:16], self.t_in, self.io["cctxT"][:, :])
        S.dma("sp", bmod, bmod[:].rearrange("p a b -> p (a b)"), self.t_in, self.io["b_modT"][:, :])
        S.op("act", lambda e: e.activation(out=cond[:, :, 0], in_=craw[:, 0:8], func=AF.Silu), reads=[craw], writes=[cond])
        S.op("act", lambda e: e.activation(out=cond[:, :, 1], in_=craw[:, 8:16], func=AF.Silu), reads=[craw], writes=[cond])
        for i in range(cfg.DEPTH):
            wv = self.wview(self.io["w_mod"][i])
            for nb in range(8):
                w = wm.next()
                S.dma("sp", w, w[:], self.t_in, wv[:, :, nb * 768:(nb + 1) * 768])
                for c6 in range(6):
                    ch = nb * 6 + c6
                    ps = pst.next()
                    for k in range(8):
                        S.op("pe", lambda e, k=k, c6=c6, w=w, ps=ps: e.matmul(
                            ps[:, 0:2], w[:, k, c6 * 128:(c6 + 1) * 128], cond[:, k, :], start=(k == 0), stop=(k == 7)),
                            reads=[w, cond], writes=[ps], sig=(k == 7))
                    S.op("dve", lambda e, ch=ch, ps=ps, i=i: e.tensor_scalar(
                        out=self.MOD[:, i, ch, :], in0=ps[:, 0:2], scalar1=bmod[:, i, ch:ch + 1], scalar2=None, op0=ALU.add),
                        reads=[ps, bmod], writes=[self.MOD])
            for s in (1, 4):
                S.op("dve", lambda e, s=s, i=i: e.tensor_scalar(
                    out=self.MOD[:, i, s * 8:(s + 1) * 8, :], in0=self.MOD[:, i, s * 8:(s + 1) * 8, :], scalar1=1.0,
                    scalar2=None, op0=ALU.add), reads=[self.MOD], writes=[self.MOD])
        sc.close()

    def mixer_attn(self, i, j, last):
        S, cfg = self.S, self.cfg
        SEQ, T = cfg.SEQ, cfg.T
        lam_init = 0.8 - 0.6 * float(np.exp(-0.3 * i))
        sc = Scope(S)
        UT = sc.sb("UT", [P, 8, T], BF16)
        sc0 = Scope(S)
        xb = Rot([sc0.sb("xb%d" % k, [P, 8, 512], F32) for k in range(2)])
        for blk in cfg.blocks:
            t0, n, is_ctx = blk
            x = xb.next()
            st, sap = self.xsrc(i, blk)
            S.dma("sp", x, x[:, :, 0:n], st, sap)
            self.modulate([UT], x, lambda c, t0=t0, n=n: UT[:, c, t0:t0 + n], i, n, 1 if is_ctx else 0, 0, 1)
        sc0.close()
        COS = sc.sb("COS", [P, SEQ], F32)
        SIN = sc.sb("SIN", [P, SEQ], F32)
        S.dma("sp", COS, COS[:], self.t_in, self.io["ropeC"][:, :])
        S.dma("sp", SIN, SIN[:], self.t_in, self.io["ropeS"][:, :])
        lamt = sc.sb("lamt", [P, 4, 64], F32)
        S.dma("sp", lamt, lamt[:].rearrange("p a b -> p (a b)"), self.t_in,
              self.io["lam"][j].rearrange("a b -> (a b)").partition_broadcast(P))
        lsc = sc.sb("lsc", [P, 8], F32)
        S.op("dve", lambda e: e.tensor_tensor(out=lamt[:, 0, :], in0=lamt[:, 0, :], in1=lamt[:, 1, :], op=ALU.mult),
             reads=[lamt], writes=[lamt])
        S.op("dve", lambda e: e.tensor_tensor(out=lamt[:, 2, :], in0=lamt[:, 2, :], in1=lamt[:, 3, :], op=ALU.mult),
             reads=[lamt], writes=[lamt])
        S.op("dve", lambda e: e.reduce_sum(out=lsc[:, 0:1], in_=lamt[:, 0, :], axis=AX.X), reads=[lamt], writes=[lsc])
        S.op("dve", lambda e: e.reduce_sum(out=lsc[:, 1:2], in_=lamt[:, 2, :], axis=AX.X), reads=[lamt], writes=[lsc])
        S.op("act", lambda e: e.activation(out=lsc[:, 2:4], in_=lsc[:, 0:2], func=AF.Exp), reads=[lsc], writes=[lsc])
        S.op("dve", lambda e: e.scalar_tensor_tensor(out=lsc[:, 4:5], in0=lsc[:, 3:4], scalar=-lam_init, in1=lsc[:, 2:3],
                                                     op0=ALU.add, op1=ALU.subtract), reads=[lsc], writes=[lsc])
        NA = cfg.N_A
        sgt0 = sc.sb("sgt0", [P, NA], F32)
        sgt = sc.sb("sgt", [P, 2], F32)
        S.dma("sp", sgt0, sgt0[:], self.t_in, self.io["sublnT"][:, :])
        S.op("dve", lambda e: e.tensor_scalar(out=sgt[:, 1:2], in0=sgt0[:, j:j + 1], scalar1=float((1.0 - lam_init) * np.sqrt(128.0)),
                                              scalar2=None, op0=ALU.mult), reads=[sgt0], writes=[sgt])
        WQ = Rot([sc.sb("WQ%d" % k, [P, 8, 3, 128], BF16) for k in range(2)])
        WR = Rot([sc.sb("WR%d" % k, [P, 8, 2, 128], BF16) for k in range(2)])
        QT = Rot([[sc.sb("QA%d" % k, [P, T], BF16), sc.sb("QB%d" % k, [P, T], BF16)] for k in range(2)])
        for qpair in QT.tiles:
            for qt_ in qpair:
                S.op("pool", lambda e, qt_=qt_: e.memset(qt_[:], 0.0), writes=[qt_])
        KT = Rot([sc.sb("KT%d" % k, [P, T], BF16) for k in range(2)])
        VV = Rot([sc.sb("VV%d" % k, [P, T // 128, 128], BF16) for k in range(2)])
        pp = Rot([sc.ps("pp%d" % k) for k in range(3)])
        pO = [sc.ps("pO%d" % k) for k in range(2)]
        pL = [sc.ps("pL%d" % k) for k in range(2)]
        pR = sc.ps("pR")
        tmpa = Rot([sc.sb("tmpa%d" % k, [P, 512], F32) for k in range(4)])
        ET = Rot([sc.sb("ET%d" % k, [P, 512], BF16) for k in range(3)])
        ob = Rot([sc.sb("ob%d" % k, [P, 512], BF16) for k in range(2)])
        wq = self.wview(self.io["attn_w_qkv"][j])
        mixv = self.MIX
        qblocks = cfg.blocks if not last else cfg.lat_blocks
        Wb, Rb = WQ.tiles, WR.tiles

        def prep(h):
            W, R = Wb[h % 2], Rb[h % 2]
            for s3 in range(3):
                S.dma("pool", W, W[:, :, s3, :], self.t_in, wq[:, :, s3 * 1024 + h * 128: s3 * 1024 + (h + 1) * 128])
            for s2 in range(2):
                src = W[:, :, s2, :].rearrange("p k (b h j) -> p k b h j", b=4, h=2)
                dst = R[:, :, s2, :].rearrange("p k (b h j) -> p k b h j", b=4, h=2)
                for k in range(8):
                    S.op("dve", lambda e, src=src, dst=dst, k=k: e.tensor_scalar(
                        out=dst[:, k, :, 0, :], in0=src[:, k, :, 1, :], scalar1=-1.0, scalar2=None, op0=ALU.mult),
                        reads=[W], writes=[R])
                    S.op("dve", lambda e, src=src, dst=dst, k=k: e.tensor_copy(out=dst[:, k, :, 1, :], in_=src[:, k, :, 0, :]),
                         reads=[W], writes=[R])
        pending = [None, None]

        def flush_pending(stage):
            for st in range(stage + 1):
                if pending[st] is not None:
                    f = pending[st]
                    pending[st] = None
                    f()
        prep(0)
        for h in range(cfg.H):
            W, R = Wb[h % 2], Rb[h % 2]
            Q, K, V = QT.next(), KT.next(), VV.next()
            for blk in cfg.blocks:
                t0, n, is_ctx = blk
                for s2, dest in ((0, None), (1, K)):
                    p1 = pp.next()
                    for k in range(8):
                        S.op("pe", lambda e, k=k, p1=p1, s2=s2: e.matmul(p1[:, 0:n], W[:, k, s2, :], UT[:, k, t0:t0 + n],
                                                                         start=(k == 0), stop=(k == 7)),
                             reads=[W, UT], writes=[p1], sig=(k == 7))
                    if is_ctx:
                        if dest is None:
                            S.op("act", lambda e, p1=p1: e.copy(out=Q[0][0:64, t0:t0 + n], in_=p1[0:64, 0:n]),
                                 reads=[p1], writes=[Q[0]])
                            S.op("dve", lambda e, p1=p1: e.tensor_copy(out=Q[1][64:128, t0:t0 + n], in_=p1[64:128, 0:n]),
                                 reads=[p1], writes=[Q[1]])
                        else:
                            S.op("act", lambda e, p1=p1, dest=dest: e.copy(out=dest[:, t0:t0 + n], in_=p1[:, 0:n]),
                                 reads=[p1], writes=[dest])
                    else:
                        p2 = pp.next()
                        for k in range(8):
                            S.op("pe", lambda e, k=k, p2=p2, s2=s2: e.matmul(p2[:, 0:n], R[:, k, s2, :], UT[:, k, t0:t0 + n],
                                                                             start=(k == 0), stop=(k == 7)),
                                 reads=[R, UT], writes=[p2], sig=(k == 7))
                        a1, a2 = tmpa.next(), tmpa.next()
                        S.op("dve", lambda e, p1=p1, a1=a1: e.tensor_tensor(out=a1[:, 0:n], in0=p1[:, 0:n], in1=COS[:, t0:t0 + n], op=ALU.mult),
                             reads=[p1, COS], writes=[a1])
                        S.op("dve", lambda e, p2=p2, a2=a2: e.tensor_tensor(out=a2[:, 0:n], in0=p2[:, 0:n], in1=SIN[:, t0:t0 + n], op=ALU.mult),
                             reads=[p2, SIN], writes=[a2])
                        if dest is None:
                            for qi, (lo_, hi_) in enumerate(((0, 64), (64, 128))):
                                S.op("pool", lambda e, a1=a1, a2=a2, qi=qi, lo_=lo_, hi_=hi_: e.tensor_tensor(
                                    out=Q[qi][lo_:hi_, t0:t0 + n], in0=a1[lo_:hi_, 0:n], in1=a2[lo_:hi_, 0:n], op=ALU.add),
                                    reads=[a1, a2], writes=[Q[qi]])
                        else:
                            S.op("pool", lambda e, a1=a1, a2=a2, dest=dest: e.tensor_tensor(out=dest[:, t0:t0 + n], in0=a1[:, 0:n], in1=a2[:, 0:n], op=ALU.add),
                                 reads=[a1, a2], writes=[dest])
                for s in range(n // 128):
                    p1 = pp.next()
                    tt = t0 + s * 128
                    for k in range(8):
                        S.op("pe", lambda e, k=k, p1=p1, tt=tt: e.matmul(p1[:, 0:128], UT[:, k, tt:tt + 128], W[:, k, 2, :],
                                                                         start=(k == 0), stop=(k == 7)),
                             reads=[W, UT], writes=[p1], sig=(k == 7))
                    S.op("act", lambda e, p1=p1, tt=tt: e.copy(out=V[:, tt // 128, :], in_=p1[:, 0:128]), reads=[p1], writes=[V])
            if h + 1 < cfg.H:
                prep(h + 1)
            for qb in qblocks:
                q0, nq, q_ctx = qb
                kchunks = list(range(SEQ // 128, T // 128)) if q_ctx else list(range(T // 128))
                nk = len(kchunks)
                its = [(m, ki, kc) for m in range(2) for ki, kc in enumerate(kchunks)]
                LA = 2
                pss = {}
                r1, a1, r2, a2 = tmpa.next(), tmpa.next(), tmpa.next(), tmpa.next()

                def emit_scores(idx):
                    m, ki, kc = its[idx]
                    ps = pp.next()
                    S.op("pe", lambda e, ps=ps, kc=kc, m=m: e.matmul(
                        ps[:, 0:nq], K[:, kc * 128:(kc + 1) * 128], Q[m][:, q0:q0 + nq], start=True, stop=True),
                        reads=[K, Q[m]], writes=[ps])
                    pss[idx] = ps
                for idx in range(min(LA, len(its))):
                    emit_scores(idx)
                for idx in range(len(its)):
                    if idx + LA < len(its):
                        emit_scores(idx + LA)
                    m, ki, kc = its[idx]
                    ps = pss.pop(idx)
                    et = ET.next()
                    S.op("act", lambda e, ps=ps, et=et: e.activation(out=et[:, 0:nq], in_=ps[:, 0:nq], func=AF.Exp, scale=0.125),
                         reads=[ps], writes=[et])
                    fst, lst = ki == 0, ki == nk - 1
                    S.op("pe", lambda e, et=et, kc=kc, m=m, fst=fst, lst=lst: e.matmul(
                        pO[m][:, 0:nq], V[:, kc, :], et[:, 0:nq], start=fst, stop=lst),
                        reads=[V, et], writes=[pO[m]], sig=False)
                    S.op("pe", lambda e, et=et, m=m, fst=fst, lst=lst: e.matmul(
                        pL[m][:, 0:nq], self.onesbf[:], et[:, 0:nq], start=fst, stop=lst),
                        reads=[self.onesbf, et], writes=[pL[m]], sig=True)
                    if idx == min(20, nk - 1):
                        flush_pending(0)
                    if idx == min(23, nk - 1):
                        flush_pending(1)
                    if idx == nk - 1:
                        S.op("dve", lambda e, r1=r1: e.reciprocal(out=r1[:, 0:nq], in_=pL[0][:, 0:nq]), reads=[pL[0]], writes=[r1])
                        S.op("dve", lambda e, r1=r1, a1=a1: e.tensor_tensor(out=a1[:, 0:nq], in0=pO[0][:, 0:nq], in1=r1[:, 0:nq], op=ALU.mult),
                             reads=[pO[0], r1], writes=[a1])
                S.op("dve", lambda e, r2=r2: e.reciprocal(out=r2[:, 0:nq], in_=pL[1][:, 0:nq]), reads=[pL[1]], writes=[r2])
                S.op("dve", lambda e, r2=r2, a2=a2: e.tensor_tensor(out=a2[:, 0:nq], in0=pO[1][:, 0:nq], in1=r2[:, 0:nq], op=ALU.mult),
                     reads=[pO[1], r2], writes=[a2])
                S.op("dve", lambda e, a1=a1, a2=a2: e.scalar_tensor_tensor(out=a1[:, 0:nq], in0=a2[:, 0:nq], scalar=lsc[:, 4:5], in1=a1[:, 0:nq],
                                                                          op0=ALU.mult, op1=ALU.add), reads=[a1, a2, lsc], writes=[a1])
                S.op("pool", lambda e, a1=a1, r1=r1: e.tensor_tensor(out=r1[:, 0:nq], in0=a1[:, 0:nq], in1=a1[:, 0:nq], op=ALU.mult),
                     reads=[a1], writes=[r1])

                def tail1(r1=r1, nq=nq):
                    S.op("pe", lambda e: e.matmul(pR[:, 0:nq], self.ones32[:], r1[:, 0:nq], start=True, stop=True),
                         reads=[self.ones32, r1], writes=[pR])

                def tail2(a1=a1, r2=r2, q0=q0, nq=nq, h=h):
                    S.op("act", lambda e: e.activation(out=r2[:, 0:nq], in_=pR[:, 0:nq], func=AF.Ln, bias=self.cst[:, 1:2]),
                         reads=[pR, self.cst], writes=[r2])
                    S.op("act", lambda e: e.activation(out=r2[:, 0:nq], in_=r2[:, 0:nq], func=AF.Exp, scale=-0.5),
                         reads=[r2], writes=[r2])
                    o = ob.next()
                    S.op("dve", lambda e: e.scalar_tensor_tensor(out=o[:, 0:nq], in0=a1[:, 0:nq], scalar=sgt[:, 1:2], in1=r2[:, 0:nq],
                                                                 op0=ALU.mult, op1=ALU.mult), reads=[a1, sgt, r2], writes=[o])
                    S.dma("sp", self.t_MIX, mixv[h * 128:(h + 1) * 128, q0:q0 + nq], o, o[:, 0:nq])
                pending[0], pending[1] = tail1, tail2
            flush_pending(1)
        sc.close()

    def mixer_fnet(self, i, j, last):
        S, cfg = self.S, self.cfg
        parts = [(0, cfg.SEQ, cfg.lat_blocks, self.io["dftL"], 0)]
        if not last:
            parts.append((cfg.SEQ, cfg.CTX, cfg.ctx_blocks, self.io["dftC"], 1))
        for (off, N, blks, dft, col) in parts:
            sc = Scope(S)
            NCH = N // 128
            ACS = sc.sb("ACS", [P, NCH, 8, 256], BF16)
            cs128 = sc.sb("cs128", [P, 256], BF16)
            S.dma("sp", cs128, cs128[:], self.t_in, self.io["dft128"][:, :])
            sc1 = Scope(S)
            xb = Rot([sc1.sb("xb%d" % k, [P, 8, 512], F32) for k in range(2)])
            ub = Rot([sc1.sb("ub%d" % k, [P, 8, 512], BF16) for k in range(2)])
            pp = Rot([sc1.ps("pp%d" % k) for k in range(4)])
            for blk in blks:
                t0, n, is_ctx = blk
                x = xb.next()
                u = ub.next()
                st, sap = self.xsrc(i, blk)
                S.dma("sp", x, x[:, :, 0:n], st, sap)
                self.modulate([u], x, lambda c, u=u, n=n: u[:, c, 0:n], i, n, col, 0, 1)
                for g in range(8):
                    for s in range(n // 128):
                        p1 = pp.next()
                        nch = (t0 - off) // 128 + s
                        S.op("pe", lambda e, p1=p1, u=u, g=g, s=s: e.matmul(p1[:, 0:256], u[:, g, s * 128:(s + 1) * 128], cs128[:],
                                                                            start=True, stop=True), reads=[u, cs128], writes=[p1])
                        eng = "act" if (g + s) % 2 == 0 else "dve"
                        if eng == "act":
                            S.op("act", lambda e, p1=p1, nch=nch, g=g: e.copy(out=ACS[:, nch, g, :], in_=p1[:, 0:256]), reads=[p1], writes=[ACS])
                        else:
                            S.op("dve", lambda e, p1=p1, nch=nch, g=g: e.tensor_copy(out=ACS[:, nch, g, :], in_=p1[:, 0:256]), reads=[p1], writes=[ACS])
            sc1.close()
            sc2 = Scope(S)
            acc = [sc2.ps("acc%d" % g) for g in range(8)]
            CP = Rot([sc2.sb("CP%d" % k, [P, 2, 512], BF16) for k in range(4)])
            ob = Rot([sc2.sb("ob%d" % k, [P, 512], BF16) for k in range(4)])
            scale = float(1.0 / np.sqrt(N * 128.0))
            kbw = min(512, N)
            for kb in range(N // kbw):
                for nchk in range(NCH):
                    cp = CP.next()
                    for s2 in range(2):
                        S.dma("sp", cp, cp[:, s2, 0:kbw], self.t_in, dft[s2, nchk * 128:(nchk + 1) * 128, kb * kbw:(kb + 1) * kbw])
                    for g in range(8):
                        for s2 in range(2):
                            S.op("pe", lambda e, g=g, s2=s2, cp=cp, nchk=nchk: e.matmul(
                                acc[g][:, 0:kbw], ACS[:, nchk, g, s2 * 128:(s2 + 1) * 128], cp[:, s2, 0:kbw],
                                start=(nchk == 0 and s2 == 0), stop=(nchk == NCH - 1 and s2 == 1)),
                                reads=[ACS, cp], writes=[acc[g]], sig=(s2 == 1 and (g == 7 or nchk == NCH - 1)))
                for g in range(8):
                    o = ob.next()
                    if g % 2 == 0:
                        S.op("act", lambda e, o=o, g=g: e.activation(out=o[:, 0:kbw], in_=acc[g][:, 0:kbw], func=AF.Copy, scale=scale),
                             reads=[acc[g]], writes=[o])
                    else:
                        S.op("dve", lambda e, o=o, g=g: e.tensor_scalar(out=o[:, 0:kbw], in0=acc[g][:, 0:kbw], scalar1=scale, scalar2=None, op0=ALU.mult),
                             reads=[acc[g]], writes=[o])
                    S.dma("sp", self.t_MIX, self.MIX[g * 128:(g + 1) * 128, off + kb * kbw: off + (kb + 1) * kbw], o, o[:, 0:kbw])
            sc2.close()
            sc.close()

    def mixer_conv(self, i, j, last):
        S, cfg = self.S, self.cfg
        SEQ, CTX = cfg.SEQ, cfg.CTX
        sc = Scope(S)
        HW = 15 + SEQ + 30 + CTX + 15
        HG = sc.sb("HG", [P, 8, HW], BF16)
        lat_off, ctx_off = 15, 15 + SEQ + 30
        for (a, b) in ((0, 15), (15 + SEQ, 15 + SEQ + 30), (HW - 15, HW)):
            S.op("pool", lambda e, a=a, b=b: e.memset(HG[:, :, a:b], 0.0), writes=[HG])
        W1 = sc.sb("W1", [P, 8, 2048], BF16)
        w1v = self.wview(self.io["conv_w_pw1"][j])
        for q in range(4):
            S.dma("pool", W1, W1[:, :, q * 512:(q + 1) * 512], self.t_in, w1v[:, :, q * 512:(q + 1) * 512])
        b1 = sc.sb("b1", [P, 16], F32)
        S.dma("sp", b1, b1[:], self.t_in, self.io["conv_b_pw1T"][:, j * 16:(j + 1) * 16])
        wdw = sc.sb("wdw", [P, 8, 31], F32)
        S.dma("sp", wdw, wdw[:].rearrange("p a b -> p (a b)"), self.t_in, self.io["conv_w_dwT"][:, j * 248:(j + 1) * 248])
        cp = sc.sb("cp", [P, 4, 8], F32)
        S.dma("sp", cp, cp[:].rearrange("p a b -> p (a b)"), self.t_in, self.io["conv_pT"][:, j * 32:(j + 1) * 32])
        blks = cfg.blocks if not last else cfg.lat_blocks

        def hoff(blk):
            t0, n, is_ctx = blk
            return (ctx_off + t0 - SEQ) if is_ctx else (lat_off + t0)
        sc1 = Scope(S)
        xb = Rot([sc1.sb("xb%d" % k, [P, 8, 512], F32) for k in range(2)])
        ub = Rot([sc1.sb("ub%d" % k, [P, 8, 512], BF16) for k in range(2)])
        pp = Rot([sc1.ps("pp%d" % k) for k in range(6)])
        ta = Rot([sc1.sb("ta%d" % k, [P, 512], F32) for k in range(3)])
        tg = Rot([sc1.sb("tg%d" % k, [P, 512], F32) for k in range(3)])
        for blk in blks:
            t0, n, is_ctx = blk
            x, u = xb.next(), ub.next()
            st, sap = self.xsrc(i, blk)
            S.dma("sp", x, x[:, :, 0:n], st, sap)
            self.modulate([u], x, lambda c, u=u, n=n: u[:, c, 0:n], i, n, 1 if is_ctx else 0, 0, 1)
            ho = hoff(blk)
            for jj in range(8):
                pa, pg = pp.next(), pp.next()
                for (pt, cb) in ((pa, jj * 128), (pg, 1024 + jj * 128)):
                    for k in range(8):
                        S.op("pe", lambda e, pt=pt, cb=cb, k=k, u=u: e.matmul(pt[:, 0:n], W1[:, k, cb:cb + 128], u[:, k, 0:n],
                                                                             start=(k == 0), stop=(k == 7)),
                             reads=[W1, u], writes=[pt], sig=(k == 7))
                a, g = ta.next(), tg.next()
                S.op("dve", lambda e, pa=pa, a=a, jj=jj: e.tensor_scalar(out=a[:, 0:n], in0=pa[:, 0:n], scalar1=b1[:, jj:jj + 1], scalar2=None, op0=ALU.add),
                     reads=[pa, b1], writes=[a])
                S.op("act", lambda e, pg=pg, g=g, jj=jj: e.activation(out=g[:, 0:n], in_=pg[:, 0:n], func=AF.Sigmoid, bias=b1[:, 8 + jj:9 + jj]),
                     reads=[pg, b1], writes=[g])
                S.op("pool", lambda e, a=a, g=g, jj=jj, ho=ho: e.tensor_tensor(out=HG[:, jj, ho:ho + n], in0=a[:, 0:n], in1=g[:, 0:n], op=ALU.mult),
                     reads=[a, g], writes=[HG])
        sc1.close()
        sc2 = Scope(S)
        zb = Rot([sc2.sb("zb%d" % k, [P, 8, 512], F32) for k in range(2)])
        accB = Rot([sc2.sb("accB%d" % k, [P, 512], F32) for k in range(2)])
        tmp = dict(sq=sc2.sb("sq", [P, 8, 512], F32), pss=sc2.ps("pss"), psq=sc2.ps("psq"),
                   mean=sc2.sb("mean", [P, 512], F32), rstd=sc2.sb("rstd", [P, 512], F32), nb=sc2.sb("nb", [P, 512], F32),
                   t=Rot([sc2.sb("lt%d" % k, [P, 512], F32) for k in range(3)]))
        mb = Rot([sc2.sb("mb%d" % k, [P, 8, 512], BF16) for k in range(2)])
        for blk in blks:
            t0, n, is_ctx = blk
            ho = hoff(blk)
            z = zb.next()
            for c in range(8):
                ab = accB.next()
                for tap in range(31):
                    src_off = ho + tap - 15
                    if tap == 0:
                        S.op("dve", lambda e, c=c, src_off=src_off: e.tensor_scalar(
                            out=z[:, c, 0:n], in0=HG[:, c, src_off:src_off + n], scalar1=wdw[:, c, 0:1], scalar2=cp[:, 0, c:c + 1],
                            op0=ALU.mult, op1=ALU.add), reads=[HG, wdw, cp], writes=[z])
                    else:
                        S.op("dve", lambda e, c=c, src_off=src_off, tap=tap: e.scalar_tensor_tensor(
                            out=z[:, c, 0:n], in0=HG[:, c, src_off:src_off + n], scalar=wdw[:, c, tap:tap + 1], in1=z[:, c, 0:n],
                            op0=ALU.mult, op1=ALU.add), reads=[HG, wdw, z], writes=[z])
            m = mb.next()
            self.layer_norm(sc2, z, n, lambda c: cp[:, 1, c:c + 1], lambda c: cp[:, 2, c:c + 1],
                            [(m, lambda c, m=m, n=n: m[:, c, 0:n], AF.Silu)], tmp, greads=[cp])
            S.dma("sp", self.t_MIX, self.MIX.rearrange("(k p) t -> p k t", p=P)[:, :, t0:t0 + n], m, m[:, :, 0:n])
        sc2.close()
        sc.close()

    def post_moe(self, i, kind, j, last):
        S, cfg = self.S, self.cfg
        E = cfg.E
        blks = cfg.blocks if not last else cfg.lat_blocks
        sbs, cur, tot = [], [], 0
        for b in blks:
            if b[2] and cur and tot + b[1] <= cfg.SB_MAX + 256:
                cur.append(b)
                tot += b[1]
                continue
            if tot + b[1] > cfg.SB_MAX:
                sbs.append(cur)
                cur, tot = [], 0
            cur.append(b)
            tot += b[1]
        if cur:
            sbs.append(cur)
        if kind == 0:
            wo_ap, bo_ap = self.io["attn_w_o"][j], None
        elif kind == 1:
            wo_ap, bo_ap = self.io["fnet_w"][j], self.io["fnet_bT"][:, j * 8:(j + 1) * 8]
        else:
            wo_ap, bo_ap = self.io["conv_w_pw2"][j], self.io["conv_pT"][:, j * 32 + 24:j * 32 + 32]
        L = Scope(S)
        WO = L.sb("WO", [P, 8, 1024], BF16)
        wov = self.wview(wo_ap)
        for q in range(2):
            S.dma("pool", WO, WO[:, :, q * 512:(q + 1) * 512], self.t_in, wov[:, :, q * 512:(q + 1) * 512])
        bo = L.sb("bo", [P, 8], F32)
        if bo_ap is None:
            S.op("dve", lambda e: e.memset(bo[:], 0.0), writes=[bo])
        else:
            S.dma("sp", bo, bo[:], self.t_in, bo_ap)
        for col in range(2):
            S.op("dve", lambda e, col=col: e.tensor_tensor(out=self.GB1[:, i, :, col], in0=self.MOD[:, i, 16:24, col], in1=bo[:], op=ALU.mult),
                 reads=[self.MOD, bo], writes=[self.GB1])
        WR = L.sb("WR", [P, 8, E], F32)
        S.dma("sp", WR, WR[:], self.t_in, self.wview(self.io["moe_w_router"][i]))
        BR = L.sb("BR", [P, E], F32)
        S.dma("sp", BR, BR[:], self.t_in, self.io["moe_b_router"][i].partition_broadcast(P))
        B1 = L.sb("B1", [P, E, 16], F32)
        S.dma("sp", B1, B1[:].rearrange("p a b -> p (a b)"), self.t_in, self.io["moe_b1T"][:, i * E * 16:(i + 1) * E * 16])
        S.op("dve", lambda e: e.tensor_scalar(out=B1[:, :, 8:16], in0=B1[:, :, 8:16], scalar1=1.0, scalar2=None, op0=ALU.add),
             reads=[B1], writes=[B1])
        B2 = L.sb("B2", [E, 1024], F32)
        S.dma("sp", B2, B2[:], self.t_in, self.io["moe_b2"][i])
        xxv = self.XX.rearrange("(k p) t -> p k t", p=P)
        x1v = self.X1.rearrange("(k p) t -> p k t", p=P)
        mixv = self.MIX.rearrange("(k p) t -> p k t", p=P)
        outv = self.yT.rearrange("(k p) t -> p k t", p=P)
        for sb in sbs:
            Tb = sum(b[1] for b in sb)
            offs = []
            o = 0
            for b in sb:
                offs.append(o)
                o += b[1]
            sbt0 = sb[0][0]
            M = Scope(S)
            VT = M.sb("VT", [P, 8, Tb], BF16)
            A = Scope(S)
            xb = Rot([A.sb("xb%d" % k, [P, 8, 512], F32) for k in range(2)])
            mb = Rot([A.sb("mb%d" % k, [P, 8, 512], BF16) for k in range(1)])
            zbr = Rot([A.sb("zb%d" % k, [P, 8, 512], F32) for k in range(2)])
            x1b = Rot([A.sb("x1b%d" % k, [P, 8, 512], F32) for k in range(1)])
            v32 = A.sb("v32", [P, 8, 512], F32)
            pssA, psqA = A.ps("pss"), A.ps("psq")
            ltA = Rot([A.sb("lt%d" % k, [P, 512], F32) for k in range(3)])
            tmps = Rot([dict(sq=A.sb("sq%d" % k, [P, 8, 512], F32), pss=pssA, psq=psqA,
                             mean=A.sb("mean%d" % k, [P, 512], F32), rstd=A.sb("rstd%d" % k, [P, 512], F32),
                             nb=A.sb("nb%d" % k, [P, 512], F32), t=ltA) for k in range(2)])
            pp = Rot([A.ps("pp%d" % k) for k in range(3)])
            pl = Rot([A.ps("pl%d" % k) for k in range(2)])
            pt = A.ps("ptr")
            lg = Rot([A.sb("lg%d" % k, [P, E], F32) for k in range(2)])
            mx = Rot([A.sb("mx%d" % k, [P, 8], F32) for k in range(2)])
            gm = Rot([A.sb("gm%d" % k, [P, E], F32) for k in range(2)])
            ge = Rot([A.sb("ge%d" % k, [P, E], F32) for k in range(2)])
            gs = Rot([A.sb("gs%d" % k, [P, 2], F32) for k in range(2)])
            gt = Rot([A.sb("gt%d" % k, [E, 128], F32) for k in range(2)])
            for bi, blk in enumerate(sb):
                t0, n, is_ctx = blk
                col = 1 if is_ctx else 0
                x, m = xb.next(), mb.next()
                zb, tmp = zbr.next(), tmps.next()
                st, sap = self.xsrc(i, blk)
                S.dma("sp", x, x[:, :, 0:n], st, sap)
                S.dma("sp", m, m[:, :, 0:n], self.t_MIX, mixv[:, :, t0:t0 + n])
                for c in range(8):
                    S.op("act", lambda e, c=c, x=x: e.activation(out=x[:, c, 0:n], in_=x[:, c, 0:n], func=AF.Identity,
                                                                 scale=cfg.alpha, bias=self.GB1[:, i, c, col:col + 1]),
                         reads=[x, self.GB1], writes=[x])
                for dch in range(8):
                    py = pp.next()
                    for k in range(8):
                        S.op("pe", lambda e, py=py, k=k, dch=dch, m=m: e.matmul(py[:, 0:n], WO[:, k, dch * 128:(dch + 1) * 128], m[:, k, 0:n],
                                                                               start=(k == 0), stop=(k == 7)),
                             reads=[WO, m], writes=[py], sig=(k == 7))
                    S.op("dve", lambda e, py=py, dch=dch, x=x: e.scalar_tensor_tensor(
                        out=zb[:, dch, 0:n], in0=py[:, 0:n], scalar=self.MOD[:, i, 16 + dch, col:col + 1], in1=x[:, dch, 0:n],
                        op0=ALU.mult, op1=ALU.add), reads=[py, self.MOD, x], writes=[zb])
                x1 = x1b.next()
                self.layer_norm(A, zb, n, lambda c: self.LNP[:, i, 0, c:c + 1], lambda c: self.LNP[:, i, 1, c:c + 1],
                                [(x1, lambda c, x1=x1, n=n: x1[:, c, 0:n], AF.Identity)], tmp)
                S.dma("sp", self.t_X1, x1v[:, :, t0:t0 + n], x1, x1[:, :, 0:n])
                for c in range(8):
                    S.op("act", lambda e, c=c, x1=x1: e.activation(out=v32[:, c, 0:n], in_=x1[:, c, 0:n], func=AF.Identity,
                                                                   scale=self.MOD[:, i, 32 + c, col:col + 1], bias=self.MOD[:, i, 24 + c, col:col + 1]),
                         reads=[x1, self.MOD], writes=[v32])
                    eng = "dve" if c % 2 == 0 else "pool"
                    S.op(eng, lambda e, c=c, o=offs[bi]: e.tensor_copy(out=VT[:, c, o:o + n], in_=v32[:, c, 0:n]), reads=[v32], writes=[VT])
                for s in range(n // 128):
                    p1 = pl.next()
                    for k in range(8):
                        S.op("pe", lambda e, p1=p1, k=k, s=s: e.matmul(p1[:, 0:E], v32[:, k, s * 128:(s + 1) * 128], WR[:, k, :],
                                                                       start=(k == 0), stop=(k == 7)),
                             reads=[v32, WR], writes=[p1], sig=(k == 7))
                    l, mxx, gmm, gee, gss, gtt = lg.next(), mx.next(), gm.next(), ge.next(), gs.next(), gt.next()
                    S.op("dve", lambda e, l=l, p1=p1: e.tensor_tensor(out=l[:], in0=p1[:, 0:E], in1=BR[:], op=ALU.add), reads=[p1, BR], writes=[l])
                    S.op("dve", lambda e, l=l, mxx=mxx: e.max(out=mxx[:], in_=l[:]), reads=[l], writes=[mxx])
                    S.op("dve", lambda e, l=l, mxx=mxx, gmm=gmm: e.tensor_scalar(out=gmm[:], in0=l[:], scalar1=mxx[:, 3:4], scalar2=None, op0=ALU.is_ge),
                         reads=[l, mxx], writes=[gmm])
                    S.op("dve", lambda e, mxx=mxx, gss=gss: e.tensor_scalar(out=gss[:, 0:1], in0=mxx[:, 0:1], scalar1=-1.0, scalar2=None, op0=ALU.mult),
                         reads=[mxx], writes=[gss])
                    S.op("act", lambda e, l=l, gee=gee, gss=gss: e.activation(out=gee[:], in_=l[:], func=AF.Exp, bias=gss[:, 0:1]),
                         reads=[l, gss], writes=[gee])
                    S.op("dve", lambda e, gee=gee, gmm=gmm: e.tensor_tensor(out=gee[:], in0=gee[:], in1=gmm[:], op=ALU.mult), reads=[gee, gmm], writes=[gee])
                    S.op("dve", lambda e, gee=gee, gss=gss: e.reduce_sum(out=gss[:, 1:2], in_=gee[:], axis=AX.X), reads=[gee], writes=[gss])
                    S.op("dve", lambda e, gss=gss: e.reciprocal(out=gss[:, 1:2], in_=gss[:, 1:2]), reads=[gss], writes=[gss])
                    S.op("dve", lambda e, gee=gee, gss=gss: e.tensor_scalar(out=gee[:], in0=gee[:], scalar1=gss[:, 1:2], scalar2=None, op0=ALU.mult),
                         reads=[gee, gss], writes=[gee])
                    S.op("pe", lambda e, gee=gee: e.transpose(pt[0:E, 0:128], gee[:], self.ident[:]), reads=[gee, self.ident], writes=[pt])
                    S.op("act", lambda e, gtt=gtt: e.copy(out=gtt[:], in_=pt[0:E, 0:128]), reads=[pt], writes=[gtt])
                    tt = t0 + s * 128
                    S.dma("sp", self.t_GD, self.GD[:, tt:tt + 128], gtt, gtt[:])
            A.close()
            Mf = Scope(S)
            Fa = Mf.sb("Fa", [P, 8, Tb], F32)
            B = Scope(S)
            HH = [[B.sb("Hh%d_%d" % (k, bi), [P, 8, blk[1]], BF16) for bi, blk in enumerate(sb)] for k in range(2)]
            GBt = Rot([B.sb("GBt%d" % k, [P, Tb], F32) for k in range(2)])
            GTs = B.sb("GTs", [E, Tb], F32)
            S.dma("sp", GTs, GTs[:], self.t_GD, self.GD[:, sbt0:sbt0 + Tb])
            NW1 = 4
            w1bufs = [B.sb("w1r%d" % k, [P, 8, 2, 256], BF16) for k in range(NW1)]
            w2bufs = [B.sb("w2r%d" % k, [P, 8, 256], BF16) for k in range(4)]
            pg = Rot([B.ps("pg%d" % k) for k in range(3)])
            plin = Rot([B.ps("plin%d" % k) for k in range(3)])
            py2 = Rot([B.ps("py%d" % k) for k in range(2)])
            ta = Rot([B.sb("ta%d" % k, [P, 512], F32) for k in range(3)])
            ts_ = Rot([B.sb("ts%d" % k, [P, 512], F32) for k in range(3)])
            tl = Rot([B.sb("tl%d" % k, [P, 512], F32) for k in range(3)])
            nld = [0]

            def load_w1(q):
                while nld[0] <= q and nld[0] < E * 4:
                    qq = nld[0]
                    ex_, j4_ = qq // 4, qq % 4
                    w1 = w1bufs[qq % NW1]
                    w1v_ = self.wview(self.io["moe_w1"][i, ex_])
                    for s2 in range(2):
                        S.dma("pool", w1, w1[:, :, s2, :], self.t_in, w1v_[:, :, s2 * 1024 + j4_ * 256: s2 * 1024 + (j4_ + 1) * 256])
                    nld[0] += 1

            def load_w2(ex, d4):
                w2 = w2bufs[d4]
                w2v_ = self.wview(self.io["moe_w2"][i, ex])
                S.dma("pool", w2, w2[:], self.t_in, w2v_[:, :, d4 * 256:(d4 + 1) * 256])
            late = [None]

            def flush_late():
                if late[0] is not None:
                    f = late[0]
                    late[0] = None
                    f()

            def w1_unit(ex, w1, gb, jj, jc, n, o, bi):
                Hh = HH[ex % 2][bi]
                pG, pLn = pg.next(), plin.next()
                for (ptile, s2) in ((pG, 0), (pLn, 1)):
                    for k in range(8):
                        S.op("pe", lambda e, ptile=ptile, s2=s2, k=k: e.matmul(
                            ptile[:, 0:n], w1[:, k, s2, jj * 128:(jj + 1) * 128], VT[:, k, o:o + n], start=(k == 0), stop=(k == 7)),
                            reads=[w1, VT], writes=[ptile], sig=(k == 7))
                a, sg, l = ta.next(), ts_.next(), tl.next()
                S.op("dve", lambda e: e.tensor_scalar(
                    out=a[:, 0:n], in0=pG[:, 0:n], scalar1=B1[:, ex, jc:jc + 1], scalar2=7.0, op0=ALU.add, op1=ALU.min),
                    reads=[pG, B1], writes=[a])
                S.op("act", lambda e: e.activation(out=sg[:, 0:n], in_=a[:, 0:n], func=AF.Sigmoid, scale=1.702),
                     reads=[a], writes=[sg])
                S.op("act", lambda e: e.activation(out=l[:, 0:n], in_=pLn[:, 0:n], func=AF.Identity, bias=B1[:, ex, 8 + jc:9 + jc]),
                     reads=[pLn, B1], writes=[l])
                flush_late()

                def second():
                    S.op("dve", lambda e: e.tensor_scalar(out=l[:, 0:n], in0=l[:, 0:n], scalar1=-6.0, scalar2=8.0,
                                                          op0=ALU.max, op1=ALU.min), reads=[l], writes=[l])
                    S.op("dve", lambda e: e.tensor_tensor(out=sg[:, 0:n], in0=a[:, 0:n], in1=sg[:, 0:n], op=ALU.mult),
                         reads=[a, sg], writes=[sg])
                    S.op("pool", lambda e: e.tensor_tensor(out=sg[:, 0:n], in0=sg[:, 0:n], in1=l[:, 0:n], op=ALU.mult),
                         reads=[sg, l], writes=[sg])
                    S.op("pool", lambda e: e.tensor_tensor(out=Hh[:, jc, 0:n], in0=sg[:, 0:n], in1=gb[:, o:o + n], op=ALU.mult),
                         reads=[sg, gb], writes=[Hh])
                late[0] = second

            def w2_group(ex, w2, dd, dch, n, o, bi):
                Hh = HH[ex % 2][bi]
                py = py2.next()
                if ex == 0:
                    S.op("pe", lambda e: e.matmul(py[:, 0:n], B2[:, dch * 128:(dch + 1) * 128], GTs[:, o:o + n], start=True, stop=False),
                         reads=[B2, GTs], writes=[py], sig=False)
                for f in range(8):
                    S.op("pe", lambda e, f=f: e.matmul(
                        py[:, 0:n], w2[:, f, dd * 128:(dd + 1) * 128], Hh[:, f, 0:n], start=(f == 0 and ex != 0), stop=(f == 7)),
                        reads=[w2, Hh], writes=[py], sig=(f == 7))
                if ex == 0:
                    S.op("act", lambda e: e.copy(out=Fa[:, dch, o:o + n], in_=py[:, 0:n]), reads=[py], writes=[Fa])
                else:
                    S.op("dve", lambda e: e.tensor_tensor(out=Fa[:, dch, o:o + n], in0=Fa[:, dch, o:o + n], in1=py[:, 0:n], op=ALU.add),
                         reads=[py, Fa], writes=[Fa])

            load_w1(1)
            w2q = []
            LAG = min(2, len(sb))
            for st in range(E + 1):
                gb = None
                if st < E:
                    gb = GBt.next()
                    S.dma("sp", gb, gb[:], self.t_GD, self.GD[st, sbt0:sbt0 + Tb].partition_broadcast(P))
                for j4 in range(4):
                    if st < E:
                        load_w1(st * 4 + j4 + 2)
                    if j4 < 3:
                        if st >= 1:
                            load_w2(st - 1, j4 + 1)
                    elif st < E:
                        load_w2(st, 0)
                    w1 = w1bufs[(st * 4 + j4) % NW1]
                    w2 = w2bufs[j4]
                    for jj in range(2):
                        jc = j4 * 2 + jj
                        for bi, blk in enumerate(sb):
                            n, o = blk[1], offs[bi]
                            if st < E:
                                w1_unit(st, w1, gb, jj, jc, n, o, bi)
                            if st >= 1:
                                if st == E:
                                    flush_late()
                                w2q.append(lambda st=st, w2=w2, jj=jj, jc=jc, n=n, o=o, bi=bi: w2_group(st - 1, w2, jj, jc, n, o, bi))
                                while len(w2q) > LAG:
                                    w2q.pop(0)()
            flush_late()
            while w2q:
                w2q.pop(0)()
            B.close()
            C = Scope(S)
            xb = Rot([C.sb("xb%d" % k, [P, 8, 512], F32) for k in range(2)])
            zb = C.sb("zb", [P, 8, 512], F32)
            ob = Rot([C.sb("ob%d" % k, [P, 8, 512], F32) for k in range(2)])
            tmp = dict(sq=C.sb("sq", [P, 8, 512], F32), pss=C.ps("pss"), psq=C.ps("psq"),
                       mean=C.sb("mean", [P, 512], F32), rstd=C.sb("rstd", [P, 512], F32), nb=C.sb("nb", [P, 512], F32),
                       t=Rot([C.sb("lt%d" % k, [P, 512], F32) for k in range(3)]))
            for bi, blk in enumerate(sb):
                t0, n, is_ctx = blk
                col = 1 if is_ctx else 0
                o = offs[bi]
                x = xb.next()
                S.dma("sp", x, x[:, :, 0:n], self.t_X1, x1v[:, :, t0:t0 + n])
                for c in range(8):
                    S.op("act", lambda e, c=c, x=x: e.activation(out=x[:, c, 0:n], in_=x[:, c, 0:n], func=AF.Copy, scale=cfg.alpha),
                         reads=[x], writes=[x])
                    S.op("dve", lambda e, c=c, x=x, o=o: e.scalar_tensor_tensor(
                        out=zb[:, c, 0:n], in0=Fa[:, c, o:o + n], scalar=self.MOD[:, i, 40 + c, col:col + 1], in1=x[:, c, 0:n],
                        op0=ALU.mult, op1=ALU.add), reads=[Fa, self.MOD, x], writes=[zb])
                ot = ob.next()
                self.layer_norm(C, zb, n, lambda c: self.LNP[:, i, 2, c:c + 1], lambda c: self.LNP[:, i, 3, c:c + 1],
                                [(ot, lambda c, ot=ot, n=n: ot[:, c, 0:n], AF.Identity)], tmp)
                if last:
                    S.dma("sp", self.t_out, outv[:, :, t0:t0 + n], ot, ot[:, :, 0:n])
                else:
                    S.dma("sp", self.t_XX, xxv[:, :, t0:t0 + n], ot, ot[:, :, 0:n])
            C.close()
            Mf.close()
            M.close()
        L.close()


def pmajor(v, nch):
    v = np.asarray(v, np.float32)
    lead = v.shape[:-1]
    a = v.reshape(lead + (nch, P))
    a = np.moveaxis(a, -1, 0)
    return np.ascontiguousarray(a)


def host_constants(cfg):
    SEQ, CTX = cfg.SEQ, cfg.CTX
    GRID_W = 64
    t = np.arange(SEQ)
    rows = (t // GRID_W).astype(np.float64)
    cols = (t % GRID_W).astype(np.float64)
    inv_freq = (10000.0 ** (-np.arange(16, dtype=np.float64) / 16)).astype(np.float32).astype(np.float64)
    dd = np.arange(64)
    f = dd % 16
    pos = np.where(dd[:, None] < 32, rows[None, :], cols[None, :])
    ang = (pos.astype(np.float32) * inv_freq[f][:, None].astype(np.float32)).astype(np.float64)
    C = np.cos(ang).astype(np.float32)
    Sn = np.sin(ang).astype(np.float32)
    ropeC = np.concatenate([C, C], 0)
    ropeS = np.concatenate([Sn, Sn], 0)

    def dft(N):
        n = np.arange(N, dtype=np.int64)
        m = (n[:, None] * n[None, :]) % N
        a = 2.0 * np.pi * m.astype(np.float64) / N
        return np.stack([np.cos(a), -np.sin(a)]).astype(np.float32).astype(ml_dtypes.bfloat16)
    a128 = 2.0 * np.pi * ((np.arange(128)[:, None] * np.arange(128)[None, :]) % 128) / 128.0
    dft128 = np.concatenate([np.cos(a128), np.sin(a128)], 1).astype(np.float32).astype(ml_dtypes.bfloat16)
    return dict(ropeC=ropeC, ropeS=ropeS, dftL=dft(SEQ), dftC=dft(CTX), dft128=dft128, ident=np.eye(P, dtype=np.float32))


def host_inputs(cfg, inp, consts, b):
    f = lambda a: np.ascontiguousarray(np.asarray(a, np.float32))
    DEPTH, E = cfg.DEPTH, cfg.E
    m = {}
    m["xT"] = np.ascontiguousarray(np.asarray(inp["x"][b], np.float32).T)
    m["ctxT"] = np.ascontiguousarray(np.asarray(inp["ctx"][b], np.float32).T)
    m["cT"] = pmajor(inp["c"][b], 8)
    m["cctxT"] = pmajor(inp["c_ctx"], 8)
    m["w_mod"] = f(inp["w_mod"])
    m["b_modT"] = pmajor(inp["b_mod"], 48).reshape(P, DEPTH * 48)
    lnp = np.stack([np.asarray(inp[k], np.float32) for k in ("ln1_g", "ln1_b", "ln2_g", "ln2_b")], 1)
    m["lnp"] = pmajor(lnp, 8).reshape(P, DEPTH * 32)
    m["attn_w_qkv"] = f(inp["attn_w_qkv"])
    m["attn_w_o"] = f(inp["attn_w_o"])
    m["lam"] = np.ascontiguousarray(np.stack([np.asarray(inp[k], np.float32) for k in
                                             ("attn_lam_q1", "attn_lam_k1", "attn_lam_q2", "attn_lam_k2")], 1))
    m["sublnT"] = np.ascontiguousarray(np.asarray(inp["attn_subln_g"], np.float32).T)
    m["fnet_w"] = f(inp["fnet_w"])
    m["fnet_bT"] = pmajor(inp["fnet_b"], 8).reshape(P, -1)
    m["conv_w_pw1"] = f(inp["conv_w_pw1"])
    m["conv_b_pw1T"] = pmajor(inp["conv_b_pw1"], 16).reshape(P, -1)
    wdw = np.asarray(inp["conv_w_dw"], np.float32)
    wdw = np.moveaxis(wdw, 1, 2)
    nC = wdw.shape[0]
    wdw = wdw.reshape(nC, 8, P, 31)
    m["conv_w_dwT"] = np.ascontiguousarray(np.moveaxis(wdw, 2, 0)).reshape(P, nC * 8 * 31)
    cpar = np.stack([np.asarray(inp[k], np.float32) for k in ("conv_b_dw", "conv_ln_g", "conv_ln_b", "conv_b_pw2")], 1)
    m["conv_pT"] = pmajor(cpar, 8).reshape(P, nC * 32)
    m["conv_w_pw2"] = f(inp["conv_w_pw2"])
    m["moe_w_router"] = f(inp["moe_w_router"])
    m["moe_b_router"] = f(inp["moe_b_router"])
    m["moe_w1"] = f(inp["moe_w1"])
    m["moe_b1T"] = pmajor(inp["moe_b1"], 16).reshape(P, DEPTH * E * 16)
    m["moe_w2"] = f(inp["moe_w2"])
    m["moe_b2"] = f(inp["moe_b2"])
    m.update(consts)
    return m


_CACHE = {}


def run(cfg, inputs, n_cores):
    key = (cfg.SEQ, cfg.CTX, cfg.E, cfg.DEPTH)
    if key not in _CACHE:
        _CACHE[key] = Prog(cfg).build()
    nc = _CACHE[key]
    consts = host_constants(cfg)
    in_maps = [host_inputs(cfg, inputs, consts, b) for b in range(n_cores)]
    res = run_bass_kernel_spmd(nc, in_maps, core_ids=list(range(n_cores)))
    out = np.stack([np.ascontiguousarray(res.results[b]["yT"].T) for b in range(n_cores)], 0)
    return out.astype(np.float32)


def kernel(**inputs):
    cfg = Cfg()
    return run(cfg, inputs, 8)
```

```python
import contextlib
import numpy as np
import ml_dtypes
import concourse.bass as bass
import concourse.mybir as mybir
from concourse.bass_utils import run_bass_kernel_spmd

F32 = mybir.dt.float32
BF16 = mybir.dt.bfloat16
AF = mybir.ActivationFunctionType
ALU = mybir.AluOpType
AX = mybir.AxisListType
P = 128
LN_EPS = 1e-5


class Cfg:
    def __init__(self, SEQ=4096, CTX=256, E=32, DEPTH=4):
        self.SEQ, self.CTX, self.E, self.DEPTH = SEQ, CTX, E, DEPTH
        self.D = 1024
        self.KC = 8
        self.H = 8
        self.T = SEQ + CTX
        self.N_A = (DEPTH + 2) // 3
        self.N_B = (DEPTH + 1) // 3
        self.N_C = DEPTH // 3
        self.alpha = float((2 * DEPTH) ** 0.25)
        self.lat_blocks = [(t, 512, False) for t in range(0, SEQ, 512)]
        self.ctx_blocks = [(SEQ + t, min(512, CTX - t), True) for t in range(0, CTX, 512)]
        self.blocks = self.lat_blocks + self.ctx_blocks
        self.SB_MAX = 1024


class TT:
    def __init__(self, h, name):
        self.h = h
        self.name = name
        self.w = None
        self.r = {}
        self.dsem = None
        self.dcnt = 0
        self.w_is_dma = False

    def __getitem__(self, k):
        return self.h[k]


class Sched:
    def __init__(self, nc, es):
        self.nc = nc
        self.eng = {"pe": nc.tensor, "act": nc.scalar, "dve": nc.vector, "pool": nc.gpsimd, "sp": nc.sync}
        self.sem = {k: es.enter_context(nc.semaphore("e_" + k)) for k in self.eng}
        self.cnt = {k: 0 for k in self.eng}
        self.seen = {k: {} for k in self.eng}
        self.semkey = {}
        for k in self.eng:
            self.semkey[id(self.sem[k])] = k
        self.free_dsems = []
        self.all_dsems = []
        self.es = es
        self.ndsem = 0
        self.live = []
        self.ninst = 0

    def _get_dsem(self):
        if self.free_dsems:
            return self.free_dsems.pop()
        s = self.es.enter_context(self.nc.semaphore("d%d" % self.ndsem))
        self.ndsem += 1
        rec = [s, 0]
        self.all_dsems.append(rec)
        return rec

    def track(self, h, name):
        t = TT(h, name)
        self.live.append(t)
        return t

    def _wait(self, e, evs):
        own = self.sem[e]
        for (sem, val) in evs:
            if sem is own and e == "pe":
                continue
            k = id(sem)
            if self.seen[e].get(k, 0) >= val:
                continue
            self.eng[e].wait_ge(sem, val)
            self.seen[e][k] = val

    def op(self, e, fn, reads=(), writes=(), sig=True):
        deps = []
        for t in reads:
            if t.w is not None:
                deps.append(t.w)
        for t in writes:
            if t.w is not None:
                deps.append(t.w)
            for k, v in t.r.items():
                deps.append((k, v))
        deps2 = []
        for d in deps:
            deps2.append(d)
        self._wait(e, deps2)
        ins = fn(self.eng[e])
        self.ninst += 1
        if sig:
            self.cnt[e] += 1
            ins.then_inc(self.sem[e], 1)
            ev = (self.sem[e], self.cnt[e])
        else:
            ev = (self.sem[e], self.cnt[e] + 1)
        for t in writes:
            t.w = ev
            t.w_is_dma = False
            t.r = {}
        for t in reads:
            if t.r.get(ev[0], 0) < ev[1]:
                t.r[ev[0]] = ev[1]
        return ins

    def dma(self, q, out_t, out_ap, in_t, in_ap):
        deps = []
        if in_t.w is not None:
            deps.append(in_t.w)
        if out_t.w is not None and not out_t.w_is_dma:
            deps.append(out_t.w)
        for k, v in out_t.r.items():
            deps.append((k, v))
        self._wait(q, deps)
        if out_t.dsem is None:
            out_t.dsem = self._get_dsem()
        rec = out_t.dsem
        rec[1] += 16
        self.eng[q].dma_start(out=out_ap, in_=in_ap).then_inc(rec[0], 16)
        self.ninst += 1
        ev = (rec[0], rec[1])
        out_t.w = ev
        out_t.w_is_dma = True
        out_t.r = {}
        if in_t.r.get(ev[0], 0) < ev[1]:
            in_t.r[ev[0]] = ev[1]

    def drain(self, release=()):
        evs = [(self.sem[k], self.cnt[k]) for k in self.eng if self.cnt[k] > 0]
        evs += [(r[0], r[1]) for r in self.all_dsems if r[1] > 0]
        for e in self.eng:
            own = self.sem[e]
            for (sem, val) in evs:
                if sem is own:
                    continue
                k = id(sem)
                if self.seen[e].get(k, 0) >= val:
                    continue
                self.eng[e].wait_ge(sem, val)
                self.seen[e][k] = val
        for t in self.live:
            t.w = None
            t.r = {}
        for t in release:
            if t.dsem is not None:
                self.free_dsems.append(t.dsem)
                t.dsem = None
            if t in self.live:
                self.live.remove(t)


_UID = [0]


def _uid():
    _UID[0] += 1
    return _UID[0]


class Scope:
    def __init__(self, S):
        self.S = S
        self.es = contextlib.ExitStack()
        self.tiles = []
        self.n = 0

    def sb(self, name, shape, dt):
        h = self.es.enter_context(self.S.nc.sbuf_tensor("%s_%d" % (name, _uid()), list(shape), dt))
        t = self.S.track(h, name)
        self.tiles.append(t)
        return t

    def ps(self, name, shape=(P, 512), dt=F32):
        h = self.es.enter_context(self.S.nc.psum_tensor("%s_%d" % (name, _uid()), list(shape), dt))
        t = self.S.track(h, name)
        self.tiles.append(t)
        return t

    def close(self):
        self.S.drain(release=self.tiles)
        self.es.close()


class Rot:
    def __init__(self, tiles):
        self.tiles = tiles
        self.i = 0

    def next(self):
        t = self.tiles[self.i % len(self.tiles)]
        self.i += 1
        return t


class Prog:
    def __init__(self, cfg):
        self.cfg = cfg
        self.nc = bass.Bass("TRN2", target_bir_lowering=False)
        self.io = {}

    def din(self, name, shape, dt=F32):
        ap = self.nc.dram_tensor(name, list(shape), dt, kind="ExternalInput").ap()
        self.io[name] = ap
        return ap

    def build(self):
        cfg = self.cfg
        nc = self.nc
        D, T, E, SEQ, CTX, DEPTH = cfg.D, cfg.T, cfg.E, cfg.SEQ, cfg.CTX, cfg.DEPTH
        d = self.din
        d("xT", [D, SEQ]); d("ctxT", [D, CTX]); d("cT", [P, 8]); d("cctxT", [P, 8])
        d("w_mod", [DEPTH, D, 6 * D]); d("b_modT", [P, DEPTH * 48])
        d("lnp", [P, DEPTH * 32])
        d("attn_w_qkv", [cfg.N_A, D, 3 * D]); d("attn_w_o", [cfg.N_A, D, D])
        d("lam", [cfg.N_A, 4, 64]); d("sublnT", [P, cfg.N_A])
        d("fnet_w", [max(cfg.N_B, 1), D, D]); d("fnet_bT", [P, max(cfg.N_B, 1) * 8])
        nC = max(cfg.N_C, 1)
        d("conv_w_pw1", [nC, D, 2 * D]); d("conv_b_pw1T", [P, nC * 16])
        d("conv_w_dwT", [P, nC * 8 * 31]); d("conv_pT", [P, nC * 4 * 8])
        d("conv_w_pw2", [nC, D, D])
        d("moe_w_router", [DEPTH, D, E]); d("moe_b_router", [DEPTH, E])
        d("moe_w1", [DEPTH, E, D, 2 * D]); d("moe_b1T", [P, DEPTH * E * 16])
        d("moe_w2", [DEPTH, E, D, D]); d("moe_b2", [DEPTH, E, D])
        d("ropeC", [P, SEQ]); d("ropeS", [P, SEQ])
        d("dftL", [2, SEQ, SEQ], BF16); d("dftC", [2, CTX, CTX], BF16); d("dft128", [P, 256], BF16)
        d("ident", [P, P])
        self.yT = nc.dram_tensor("yT", [D, SEQ], F32, kind="ExternalOutput").ap()
        self.XX = nc.dram_tensor("XX", [D, T], F32).ap()
        self.X1 = nc.dram_tensor("X1", [D, T], F32).ap()
        self.MIX = nc.dram_tensor("MIX", [D, T], BF16).ap()
        self.GD = nc.dram_tensor("GD", [E, T], F32).ap()

        with contextlib.ExitStack() as es:
            S = Sched(nc, es)
            self.S = S
            es.enter_context(nc.Block())
            self.t_in = S.track(None, "inputs")
            self.t_XX = S.track(None, "XX")
            self.t_X1 = S.track(None, "X1")
            self.t_MIX = S.track(None, "MIX")
            self.t_GD = S.track(None, "GD")
            self.t_out = S.track(None, "yT")
            G = Scope(S)
            self.G = G
            self.ones32 = G.sb("ones32", [P, P], F32)
            self.onesbf = G.sb("onesbf", [P, P], BF16)
            self.ident = G.sb("ident", [P, P], F32)
            self.MOD = G.sb("MOD", [P, DEPTH, 48, 2], F32)
            self.LNP = G.sb("LNP", [P, DEPTH, 4, 8], F32)
            self.GB1 = G.sb("GB1", [P, DEPTH, 8, 2], F32)
            self.cst = G.sb("cst", [P, 4], F32)
            S.op("dve", lambda e: e.memset(self.cst[:, 0:1], LN_EPS), writes=[self.cst])
            S.op("dve", lambda e: e.memset(self.cst[:, 1:2], 128.0 * LN_EPS), writes=[self.cst])
            S.op("dve", lambda e: e.memset(self.ones32[:], 1.0), writes=[self.ones32])
            S.op("dve", lambda e: e.memset(self.onesbf[:], 1.0), writes=[self.onesbf])
            S.dma("sp", self.ident, self.ident[:], self.t_in, self.io["ident"][:, :])
            S.dma("sp", self.LNP, self.LNP[:].rearrange("p a b c -> p (a b c)"), self.t_in, self.io["lnp"][:, :])
            self.stage_mod()
            for i in range(DEPTH):
                kind = i % 3
                j = i // 3
                last = i == DEPTH - 1
                if kind == 0:
                    self.mixer_attn(i, j, last)
                elif kind == 1:
                    self.mixer_fnet(i, j, last)
                else:
                    self.mixer_conv(i, j, last)
                self.post_moe(i, kind, j, last)
            S.drain()
            G.close()
        return nc

    def xsrc(self, i, blk):
        t0, n, is_ctx = blk
        cfg = self.cfg
        if i == 0:
            if is_ctx:
                ap = self.io["ctxT"].rearrange("(k p) t -> p k t", p=P)[:, :, t0 - cfg.SEQ:t0 - cfg.SEQ + n]
            else:
                ap = self.io["xT"].rearrange("(k p) t -> p k t", p=P)[:, :, t0:t0 + n]
            return self.t_in, ap
        return self.t_XX, self.XX.rearrange("(k p) t -> p k t", p=P)[:, :, t0:t0 + n]

    def wview(self, ap2d):
        return ap2d.rearrange("(k p) n -> p k n", p=P)

    def modulate(self, eng_rot, x, ut_ap_fn, i, n, col, s_sh, s_sc):
        S = self.S
        for c in range(8):
            S.op("act", lambda e, c=c: e.activation(
                out=ut_ap_fn(c), in_=x[:, c, 0:n], func=AF.Identity,
                bias=self.MOD[:, i, s_sh * 8 + c, col:col + 1], scale=self.MOD[:, i, s_sc * 8 + c, col:col + 1]),
                reads=[x, self.MOD], writes=eng_rot)

    def layer_norm(self, sc, z, n, gcol, bcol, outs, tmp, greads=()):
        S = self.S
        sq, pss, psq, mean, rstd, nb = tmp["sq"], tmp["pss"], tmp["psq"], tmp["mean"], tmp["rstd"], tmp["nb"]
        for c in range(8):
            S.op("act", lambda e, c=c: e.activation(out=sq[:, c, 0:n], in_=z[:, c, 0:n], func=AF.Square),
                 reads=[z], writes=[sq])
        for c in range(8):
            S.op("pe", lambda e, c=c: e.matmul(pss[:, 0:n], self.ones32[:], z[:, c, 0:n], start=(c == 0), stop=(c == 7)),
                 reads=[self.ones32, z], writes=[pss], sig=(c == 7))
        for c in range(8):
            S.op("pe", lambda e, c=c: e.matmul(psq[:, 0:n], self.ones32[:], sq[:, c, 0:n], start=(c == 0), stop=(c == 7)),
                 reads=[self.ones32, sq], writes=[psq], sig=(c == 7))
        invd = 1.0 / 1024.0
        S.op("dve", lambda e: e.tensor_scalar(out=mean[:, 0:n], in0=pss[:, 0:n], scalar1=invd, scalar2=None, op0=ALU.mult),
             reads=[pss], writes=[mean])
        S.op("dve", lambda e: e.tensor_tensor(out=nb[:, 0:n], in0=mean[:, 0:n], in1=mean[:, 0:n], op=ALU.mult),
             reads=[mean], writes=[nb])
        S.op("dve", lambda e: e.scalar_tensor_tensor(out=rstd[:, 0:n], in0=psq[:, 0:n], scalar=invd, in1=nb[:, 0:n],
                                                     op0=ALU.mult, op1=ALU.subtract),
             reads=[psq, nb], writes=[rstd])
        S.op("act", lambda e: e.activation(out=rstd[:, 0:n], in_=rstd[:, 0:n], func=AF.Sqrt, bias=self.cst[:, 0:1]),
             reads=[rstd, self.cst], writes=[rstd])
        S.op("dve", lambda e: e.reciprocal(out=rstd[:, 0:n], in_=rstd[:, 0:n]), reads=[rstd], writes=[rstd])
        S.op("dve", lambda e: e.scalar_tensor_tensor(out=nb[:, 0:n], in0=mean[:, 0:n], scalar=-1.0, in1=rstd[:, 0:n],
                                                     op0=ALU.mult, op1=ALU.mult),
             reads=[mean, rstd], writes=[nb])
        for c in range(8):
            t = tmp["t"].next()
            eng = "dve" if c % 2 == 0 else "pool"
            S.op(eng, lambda e, c=c, t=t: e.tensor_tensor(out=t[:, 0:n], in0=z[:, c, 0:n], in1=rstd[:, 0:n], op=ALU.mult),
                 reads=[z, rstd], writes=[t])
            S.op(eng, lambda e, t=t: e.tensor_tensor(out=t[:, 0:n], in0=t[:, 0:n], in1=nb[:, 0:n], op=ALU.add),
                 reads=[t, nb], writes=[t])
            for (ot, apf, func) in outs:
                S.op("act", lambda e, c=c, t=t, apf=apf, func=func: e.activation(
                    out=apf(c), in_=t[:, 0:n], func=func, bias=bcol(c), scale=gcol(c)),
                    reads=[t, self.LNP, self.MOD] + list(greads), writes=[ot])

    def stage_mod(self):
        S, cfg = self.S, self.cfg
        sc = Scope(S)
        craw = sc.sb("craw", [P, 16], F32)
        cond = sc.sb("cond", [P, 8, 2], F32)
        bmod = sc.sb("bmod", [P, cfg.DEPTH, 48], F32)
        wm = Rot([sc.sb("wm%d" % k, [P, 8, 768], F32) for k in range(2)])
        pst = Rot([sc.ps("pmod%d" % k) for k in range(2)])
        S.dma("sp", craw, craw[:, 0:8], self.t_in, self.io["cT"][:, :])
        S.dma("sp", craw, craw[:, 8:16], self.t_in, self.io["cctxT"][:, :])
        S.dma("sp", bmod, bmod[:].rearrange("p a b -> p (a b)"), self.t_in, self.io["b_modT"][:, :])
        S.op("act", lambda e: e.activation(out=cond[:, :, 0], in_=craw[:, 0:8], func=AF.Silu), reads=[craw], writes=[cond])
        S.op("act", lambda e: e.activation(out=cond[:, :, 1], in_=craw[:, 8:16], func=AF.Silu), reads=[craw], writes=[cond])
        for i in range(cfg.DEPTH):
            wv = self.wview(self.io["w_mod"][i])
            for nb in range(8):
                w = wm.next()
                S.dma("sp", w, w[:], self.t_in, wv[:, :, nb * 768:(nb + 1) * 768])
                for c6 in range(6):
                    ch = nb * 6 + c6
                    ps = pst.next()
                    for k in range(8):
                        S.op("pe", lambda e, k=k, c6=c6, w=w, ps=ps: e.matmul(
                            ps[:, 0:2], w[:, k, c6 * 128:(c6 + 1) * 128], cond[:, k, :], start=(k == 0), stop=(k == 7)),
                            reads=[w, cond], writes=[ps], sig=(k == 7))
                    S.op("dve", lambda e, ch=ch, ps=ps, i=i: e.tensor_scalar(
                        out=self.MOD[:, i, ch, :], in0=ps[:, 0:2], scalar1=bmod[:, i, ch:ch + 1], scalar2=None, op0=ALU.add),
                        reads=[ps, bmod], writes=[self.MOD])
            for s in (1, 4):
                S.op("dve", lambda e, s=s, i=i: e.tensor_scalar(
                    out=self.MOD[:, i, s * 8:(s + 1) * 8, :], in0=self.MOD[:, i, s * 8:(s + 1) * 8, :], scalar1=1.0,
                    scalar2=None, op0=ALU.add), reads=[self.MOD], writes=[self.MOD])
        sc.close()

    def mixer_attn(self, i, j, last):
        S, cfg = self.S, self.cfg
        SEQ, T = cfg.SEQ, cfg.T
        lam_init = 0.8 - 0.6 * float(np.exp(-0.3 * i))
        sc = Scope(S)
        UT = sc.sb("UT", [P, 8, T], BF16)
        sc0 = Scope(S)
        xb = Rot([sc0.sb("xb%d" % k, [P, 8, 512], F32) for k in range(2)])
        for blk in cfg.blocks:
            t0, n, is_ctx = blk
            x = xb.next()
            st, sap = self.xsrc(i, blk)
            S.dma("sp", x, x[:, :, 0:n], st, sap)
            self.modulate([UT], x, lambda c, t0=t0, n=n: UT[:, c, t0:t0 + n], i, n, 1 if is_ctx else 0, 0, 1)
        sc0.close()
        COS = sc.sb("COS", [P, SEQ], F32)
        SIN = sc.sb("SIN", [P, SEQ], F32)
        S.dma("sp", COS, COS[:], self.t_in, self.io["ropeC"][:, :])
        S.dma("sp", SIN, SIN[:], self.t_in, self.io["ropeS"][:, :])
        lamt = sc.sb("lamt", [P, 4, 64], F32)
        S.dma("sp", lamt, lamt[:].rearrange("p a b -> p (a b)"), self.t_in,
              self.io["lam"][j].rearrange("a b -> (a b)").partition_broadcast(P))
        lsc = sc.sb("lsc", [P, 8], F32)
        S.op("dve", lambda e: e.tensor_tensor(out=lamt[:, 0, :], in0=lamt[:, 0, :], in1=lamt[:, 1, :], op=ALU.mult),
             reads=[lamt], writes=[lamt])
        S.op("dve", lambda e: e.tensor_tensor(out=lamt[:, 2, :], in0=lamt[:, 2, :], in1=lamt[:, 3, :], op=ALU.mult),
             reads=[lamt], writes=[lamt])
        S.op("dve", lambda e: e.reduce_sum(out=lsc[:, 0:1], in_=lamt[:, 0, :], axis=AX.X), reads=[lamt], writes=[lsc])
        S.op("dve", lambda e: e.reduce_sum(out=lsc[:, 1:2], in_=lamt[:, 2, :], axis=AX.X), reads=[lamt], writes=[lsc])
        S.op("act", lambda e: e.activation(out=lsc[:, 2:4], in_=lsc[:, 0:2], func=AF.Exp), reads=[lsc], writes=[lsc])
        S.op("dve", lambda e: e.scalar_tensor_tensor(out=lsc[:, 4:5], in0=lsc[:, 3:4], scalar=-lam_init, in1=lsc[:, 2:3],
                                                     op0=ALU.add, op1=ALU.subtract), reads=[lsc], writes=[lsc])
        NA = cfg.N_A
        sgt0 = sc.sb("sgt0", [P, NA], F32)
        sgt = sc.sb("sgt", [P, 2], F32)
        S.dma("sp", sgt0, sgt0[:], self.t_in, self.io["sublnT"][:, :])
        S.op("dve", lambda e: e.tensor_scalar(out=sgt[:, 1:2], in0=sgt0[:, j:j + 1], scalar1=float((1.0 - lam_init) * np.sqrt(128.0)),
                                              scalar2=None, op0=ALU.mult), reads=[sgt0], writes=[sgt])
        WQ = Rot([sc.sb("WQ%d" % k, [P, 8, 3, 128], BF16) for k in range(2)])
        WR = Rot([sc.sb("WR%d" % k, [P, 8, 2, 128], BF16) for k in range(2)])
        QT = Rot([[sc.sb("QA%d" % k, [P, T], BF16), sc.sb("QB%d" % k, [P, T], BF16)] for k in range(2)])
        for qpair in QT.tiles:
            for qt_ in qpair:
                S.op("pool", lambda e, qt_=qt_: e.memset(qt_[:], 0.0), writes=[qt_])
        KT = Rot([sc.sb("KT%d" % k, [P, T], BF16) for k in range(2)])
        VV = Rot([sc.sb("VV%d" % k, [P, T // 128, 128], BF16) for k in range(2)])
        pp = Rot([sc.ps("pp%d" % k) for k in range(3)])
        pO = [sc.ps("pO%d" % k) for k in range(2)]
        pL = [sc.ps("pL%d" % k) for k in range(2)]
        pR = sc.ps("pR")
        tmpa = Rot([sc.sb("tmpa%d" % k, [P, 512], F32) for k in range(4)])
        ET = Rot([sc.sb("ET%d" % k, [P, 512], BF16) for k in range(3)])
        ob = Rot([sc.sb("ob%d" % k, [P, 512], BF16) for k in range(2)])
        wq = self.wview(self.io["attn_w_qkv"][j])
        mixv = self.MIX
        qblocks = cfg.blocks if not last else cfg.lat_blocks
        Wb, Rb = WQ.tiles, WR.tiles

        def prep(h):
            W, R = Wb[h % 2], Rb[h % 2]
            for s3 in range(3):
                S.dma("pool", W, W[:, :, s3, :], self.t_in, wq[:, :, s3 * 1024 + h * 128: s3 * 1024 + (h + 1) * 128])
            for s2 in range(2):
                src = W[:, :, s2, :].rearrange("p k (b h j) -> p k b h j", b=4, h=2)
                dst = R[:, :, s2, :].rearrange("p k (b h j) -> p k b h j", b=4, h=2)
                for k in range(8):
                    S.op("dve", lambda e, src=src, dst=dst, k=k: e.tensor_scalar(
                        out=dst[:, k, :, 0, :], in0=src[:, k, :, 1, :], scalar1=-1.0, scalar2=None, op0=ALU.mult),
                        reads=[W], writes=[R])
                    S.op("dve", lambda e, src=src, dst=dst, k=k: e.tensor_copy(out=dst[:, k, :, 1, :], in_=src[:, k, :, 0, :]),
                         reads=[W], writes=[R])
        pending = [None, None]

        def flush_pending(stage):
            for st in range(stage + 1):
                if pending[st] is not None:
                    f = pending[st]
                    pending[st] = None
                    f()
        prep(0)
        for h in range(cfg.H):
            W, R = Wb[h % 2], Rb[h % 2]
            Q, K, V = QT.next(), KT.next(), VV.next()
            for blk in cfg.blocks:
                t0, n, is_ctx = blk
                for s2, dest in ((0, None), (1, K)):
                    p1 = pp.next()
                    for k in range(8):
                        S.op("pe", lambda e, k=k, p1=p1, s2=s2: e.matmul(p1[:, 0:n], W[:, k, s2, :], UT[:, k, t0:t0 + n],
                                                                         start=(k == 0), stop=(k == 7)),
                             reads=[W, UT], writes=[p1], sig=(k == 7))
                    if is_ctx:
                        if dest is None:
                            S.op("act", lambda e, p1=p1: e.copy(out=Q[0][0:64, t0:t0 + n], in_=p1[0:64, 0:n]),
                                 reads=[p1], writes=[Q[0]])
                            S.op("dve", lambda e, p1=p1: e.tensor_copy(out=Q[1][64:128, t0:t0 + n], in_=p1[64:128, 0:n]),
                                 reads=[p1], writes=[Q[1]])
                        else:
                            S.op("act", lambda e, p1=p1, dest=dest: e.copy(out=dest[:, t0:t0 + n], in_=p1[:, 0:n]),
                                 reads=[p1], writes=[dest])
                    else:
                        p2 = pp.next()
                        for k in range(8):
                            S.op("pe", lambda e, k=k, p2=p2, s2=s2: e.matmul(p2[:, 0:n], R[:, k, s2, :], UT[:, k, t0:t0 + n],
                                                                             start=(k == 0), stop=(k == 7)),
                                 reads=[R, UT], writes=[p2], sig=(k == 7))
                        a1, a2 = tmpa.next(), tmpa.next()
                        S.op("dve", lambda e, p1=p1, a1=a1: e.tensor_tensor(out=a1[:, 0:n], in0=p1[:, 0:n], in1=COS[:, t0:t0 + n], op=ALU.mult),
                             reads=[p1, COS], writes=[a1])
                        S.op("dve", lambda e, p2=p2, a2=a2: e.tensor_tensor(out=a2[:, 0:n], in0=p2[:, 0:n], in1=SIN[:, t0:t0 + n], op=ALU.mult),
                             reads=[p2, SIN], writes=[a2])
                        if dest is None:
                            for qi, (lo_, hi_) in enumerate(((0, 64), (64, 128))):
                                S.op("pool", lambda e, a1=a1, a2=a2, qi=qi, lo_=lo_, hi_=hi_: e.tensor_tensor(
                                    out=Q[qi][lo_:hi_, t0:t0 + n], in0=a1[lo_:hi_, 0:n], in1=a2[lo_:hi_, 0:n], op=ALU.add),
                                    reads=[a1, a2], writes=[Q[qi]])
                        else:
                            S.op("pool", lambda e, a1=a1, a2=a2, dest=dest: e.tensor_tensor(out=dest[:, t0:t0 + n], in0=a1[:, 0:n], in1=a2[:, 0:n], op=ALU.add),
                                 reads=[a1, a2], writes=[dest])
                for s in range(n // 128):
                    p1 = pp.next()
                    tt = t0 + s * 128
                    for k in range(8):
                        S.op("pe", lambda e, k=k, p1=p1, tt=tt: e.matmul(p1[:, 0:128], UT[:, k, tt:tt + 128], W[:, k, 2, :],
                                                                         start=(k == 0), stop=(k == 7)),
                             reads=[W, UT], writes=[p1], sig=(k == 7))
                    S.op("act", lambda e, p1=p1, tt=tt: e.copy(out=V[:, tt // 128, :], in_=p1[:, 0:128]), reads=[p1], writes=[V])
            if h + 1 < cfg.H:
                prep(h + 1)
            for qb in qblocks:
                q0, nq, q_ctx = qb
                kchunks = list(range(SEQ // 128, T // 128)) if q_ctx else list(range(T // 128))
                nk = len(kchunks)
                its = [(m, ki, kc) for m in range(2) for ki, kc in enumerate(kchunks)]
                LA = 2
                pss = {}
                r1, a1, r2, a2 = tmpa.next(), tmpa.next(), tmpa.next(), tmpa.next()

                def emit_scores(idx):
                    m, ki, kc = its[idx]
                    ps = pp.next()
                    S.op("pe", lambda e, ps=ps, kc=kc, m=m: e.matmul(
                        ps[:, 0:nq], K[:, kc * 128:(kc + 1) * 128], Q[m][:, q0:q0 + nq], start=True, stop=True),
                        reads=[K, Q[m]], writes=[ps])
                    pss[idx] = ps
                for idx in range(min(LA, len(its))):
                    emit_scores(idx)
                for idx in range(len(its)):
                    if idx + LA < len(its):
                        emit_scores(idx + LA)
                    m, ki, kc = its[idx]
                    ps = pss.pop(idx)
                    et = ET.next()
                    S.op("act", lambda e, ps=ps, et=et: e.activation(out=et[:, 0:nq], in_=ps[:, 0:nq], func=AF.Exp, scale=0.125),
                         reads=[ps], writes=[et])
                    fst, lst = ki == 0, ki == nk - 1
                    S.op("pe", lambda e, et=et, kc=kc, m=m, fst=fst, lst=lst: e.matmul(
                        pO[m][:, 0:nq], V[:, kc, :], et[:, 0:nq], start=fst, stop=lst),
                        reads=[V, et], writes=[pO[m]], sig=False)
                    S.op("pe", lambda e, et=et, m=m, fst=fst, lst=lst: e.matmul(
                        pL[m][:, 0:nq], self.onesbf[:], et[:, 0:nq], start=fst, stop=lst),
                        reads=[self.onesbf, et], writes=[pL[m]], sig=True)
                    if idx == min(20, nk - 1):
                        flush_pending(0)
                    if idx == min(23, nk - 1):
                        flush_pending(1)
                    if idx == nk - 1:
                        S.op("dve", lambda e, r1=r1: e.reciprocal(out=r1[:, 0:nq], in_=pL[0][:, 0:nq]), reads=[pL[0]], writes=[r1])
                        S.op("dve", lambda e, r1=r1, a1=a1: e.tensor_tensor(out=a1[:, 0:nq], in0=pO[0][:, 0:nq], in1=r1[:, 0:nq], op=ALU.mult),
                             reads=[pO[0], r1], writes=[a1])
                S.op("dve", lambda e, r2=r2: e.reciprocal(out=r2[:, 0:nq], in_=pL[1][:, 0:nq]), reads=[pL[1]], writes=[r2])
                S.op("dve", lambda e, r2=r2, a2=a2: e.tensor_tensor(out=a2[:, 0:nq], in0=pO[1][:, 0:nq], in1=r2[:, 0:nq], op=ALU.mult),
                     reads=[pO[1], r2], writes=[a2])
                S.op("dve", lambda e, a1=a1, a2=a2: e.scalar_tensor_tensor(out=a1[:, 0:nq], in0=a2[:, 0:nq], scalar=lsc[:, 4:5], in1=a1[:, 0:nq],
                                                                          op0=ALU.mult, op1=ALU.add), reads=[a1, a2, lsc], writes=[a1])
                S.op("pool", lambda e, a1=a1, r1=r1: e.tensor_tensor(out=r1[:, 0:nq], in0=a1[:, 0:nq], in1=a1[:, 0:nq], op=ALU.mult),
                     reads=[a1], writes=[r1])

                def tail1(r1=r1, nq=nq):
                    S.op("pe", lambda e: e.matmul(pR[:, 0:nq], self.ones32[:], r1[:, 0:nq], start=True, stop=True),
                         reads=[self.ones32, r1], writes=[pR])

                def tail2(a1=a1, r2=r2, q0=q0, nq=nq, h=h):
                    S.op("act", lambda e: e.activation(out=r2[:, 0:nq], in_=pR[:, 0:nq], func=AF.Ln, bias=self.cst[:, 1:2]),
                         reads=[pR, self.cst], writes=[r2])
                    S.op("act", lambda e: e.activation(out=r2[:, 0:nq], in_=r2[:, 0:nq], func=AF.Exp, scale=-0.5),
                         reads=[r2], writes=[r2])
                    o = ob.next()
                    S.op("dve", lambda e: e.scalar_tensor_tensor(out=o[:, 0:nq], in0=a1[:, 0:nq], scalar=sgt[:, 1:2], in1=r2[:, 0:nq],
                                                                 op0=ALU.mult, op1=ALU.mult), reads=[a1, sgt, r2], writes=[o])
                    S.dma("sp", self.t_MIX, mixv[h * 128:(h + 1) * 128, q0:q0 + nq], o, o[:, 0:nq])
                pending[0], pending[1] = tail1, tail2
            flush_pending(1)
        sc.close()

    def mixer_fnet(self, i, j, last):
        S, cfg = self.S, self.cfg
        parts = [(0, cfg.SEQ, cfg.lat_blocks, self.io["dftL"], 0)]
        if not last:
            parts.append((cfg.SEQ, cfg.CTX, cfg.ctx_blocks, self.io["dftC"], 1))
        for (off, N, blks, dft, col) in parts:
            sc = Scope(S)
            NCH = N // 128
            ACS = sc.sb("ACS", [P, NCH, 8, 256], BF16)
            cs128 = sc.sb("cs128", [P, 256], BF16)
            S.dma("sp", cs128, cs128[:], self.t_in, self.io["dft128"][:, :])
            sc1 = Scope(S)
            xb = Rot([sc1.sb("xb%d" % k, [P, 8, 512], F32) for k in range(2)])
            ub = Rot([sc1.sb("ub%d" % k, [P, 8, 512], BF16) for k in range(2)])
            pp = Rot([sc1.ps("pp%d" % k) for k in range(4)])
            for blk in blks:
                t0, n, is_ctx = blk
                x = xb.next()
                u = ub.next()
                st, sap = self.xsrc(i, blk)
                S.dma("sp", x, x[:, :, 0:n], st, sap)
                self.modulate([u], x, lambda c, u=u, n=n: u[:, c, 0:n], i, n, col, 0, 1)
                for g in range(8):
                    for s in range(n // 128):
                        p1 = pp.next()
                        nch = (t0 - off) // 128 + s
                        S.op("pe", lambda e, p1=p1, u=u, g=g, s=s: e.matmul(p1[:, 0:256], u[:, g, s * 128:(s + 1) * 128], cs128[:],
                                                                            start=True, stop=True), reads=[u, cs128], writes=[p1])
                        eng = "act" if (g + s) % 2 == 0 else "dve"
                        if eng == "act":
                            S.op("act", lambda e, p1=p1, nch=nch, g=g: e.copy(out=ACS[:, nch, g, :], in_=p1[:, 0:256]), reads=[p1], writes=[ACS])
                        else:
                            S.op("dve", lambda e, p1=p1, nch=nch, g=g: e.tensor_copy(out=ACS[:, nch, g, :], in_=p1[:, 0:256]), reads=[p1], writes=[ACS])
            sc1.close()
            sc2 = Scope(S)
            acc = [sc2.ps("acc%d" % g) for g in range(8)]
            CP = Rot([sc2.sb("CP%d" % k, [P, 2, 512], BF16) for k in range(4)])
            ob = Rot([sc2.sb("ob%d" % k, [P, 512], BF16) for k in range(4)])
            scale = float(1.0 / np.sqrt(N * 128.0))
            kbw = min(512, N)
            for kb in range(N // kbw):
                for nchk in range(NCH):
                    cp = CP.next()
                    for s2 in range(2):
                        S.dma("sp", cp, cp[:, s2, 0:kbw], self.t_in, dft[s2, nchk * 128:(nchk + 1) * 128, kb * kbw:(kb + 1) * kbw])
                    for g in range(8):
                        for s2 in range(2):
                            S.op("pe", lambda e, g=g, s2=s2, cp=cp, nchk=nchk: e.matmul(
                                acc[g][:, 0:kbw], ACS[:, nchk, g, s2 * 128:(s2 + 1) * 128], cp[:, s2, 0:kbw],
                                start=(nchk == 0 and s2 == 0), stop=(nchk == NCH - 1 and s2 == 1)),
                                reads=[ACS, cp], writes=[acc[g]], sig=(s2 == 1 and (g == 7 or nchk == NCH - 1)))
                for g in range(8):
                    o = ob.next()
                    if g % 2 == 0:
                        S.op("act", lambda e, o=o, g=g: e.activation(out=o[:, 0:kbw], in_=acc[g][:, 0:kbw], func=AF.Copy, scale=scale),
                             reads=[acc[g]], writes=[o])
                    else:
                        S.op("dve", lambda e, o=o, g=g: e.tensor_scalar(out=o[:, 0:kbw], in0=acc[g][:, 0:kbw], scalar1=scale, scalar2=None, op0=ALU.mult),
                             reads=[acc[g]], writes=[o])
                    S.dma("sp", self.t_MIX, self.MIX[g * 128:(g + 1) * 128, off + kb * kbw: off + (kb + 1) * kbw], o, o[:, 0:kbw])
            sc2.close()
            sc.close()

    def mixer_conv(self, i, j, last):
        S, cfg = self.S, self.cfg
        SEQ, CTX = cfg.SEQ, cfg.CTX
        sc = Scope(S)
        HW = 15 + SEQ + 30 + CTX + 15
        HG = sc.sb("HG", [P, 8, HW], BF16)
        lat_off, ctx_off = 15, 15 + SEQ + 30
        for (a, b) in ((0, 15), (15 + SEQ, 15 + SEQ + 30), (HW - 15, HW)):
            S.op("pool", lambda e, a=a, b=b: e.memset(HG[:, :, a:b], 0.0), writes=[HG])
        W1 = sc.sb("W1", [P, 8, 2048], BF16)
        w1v = self.wview(self.io["conv_w_pw1"][j])
        for q in range(4):
            S.dma("pool", W1, W1[:, :, q * 512:(q + 1) * 512], self.t_in, w1v[:, :, q * 512:(q + 1) * 512])
        b1 = sc.sb("b1", [P, 16], F32)
        S.dma("sp", b1, b1[:], self.t_in, self.io["conv_b_pw1T"][:, j * 16:(j + 1) * 16])
        wdw = sc.sb("wdw", [P, 8, 31], F32)
        S.dma("sp", wdw, wdw[:].rearrange("p a b -> p (a b)"), self.t_in, self.io["conv_w_dwT"][:, j * 248:(j + 1) * 248])
        cp = sc.sb("cp", [P, 4, 8], F32)
        S.dma("sp", cp, cp[:].rearrange("p a b -> p (a b)"), self.t_in, self.io["conv_pT"][:, j * 32:(j + 1) * 32])
        blks = cfg.blocks if not last else cfg.lat_blocks

        def hoff(blk):
            t0, n, is_ctx = blk
            return (ctx_off + t0 - SEQ) if is_ctx else (lat_off + t0)
        sc1 = Scope(S)
        xb = Rot([sc1.sb("xb%d" % k, [P, 8, 512], F32) for k in range(2)])
        ub = Rot([sc1.sb("ub%d" % k, [P, 8, 512], BF16) for k in range(2)])
        pp = Rot([sc1.ps("pp%d" % k) for k in range(6)])
        ta = Rot([sc1.sb("ta%d" % k, [P, 512], F32) for k in range(3)])
        tg = Rot([sc1.sb("tg%d" % k, [P, 512], F32) for k in range(3)])
        for blk in blks:
            t0, n, is_ctx = blk
            x, u = xb.next(), ub.next()
            st, sap = self.xsrc(i, blk)
            S.dma("sp", x, x[:, :, 0:n], st, sap)
            self.modulate([u], x, lambda c, u=u, n=n: u[:, c, 0:n], i, n, 1 if is_ctx else 0, 0, 1)
            ho = hoff(blk)
            for jj in range(8):
                pa, pg = pp.next(), pp.next()
                for (pt, cb) in ((pa, jj * 128), (pg, 1024 + jj * 128)):
                    for k in range(8):
                        S.op("pe", lambda e, pt=pt, cb=cb, k=k, u=u: e.matmul(pt[:, 0:n], W1[:, k, cb:cb + 128], u[:, k, 0:n],
                                                                             start=(k == 0), stop=(k == 7)),
                             reads=[W1, u], writes=[pt], sig=(k == 7))
                a, g = ta.next(), tg.next()
                S.op("dve", lambda e, pa=pa, a=a, jj=jj: e.tensor_scalar(out=a[:, 0:n], in0=pa[:, 0:n], scalar1=b1[:, jj:jj + 1], scalar2=None, op0=ALU.add),
                     reads=[pa, b1], writes=[a])
                S.op("act", lambda e, pg=pg, g=g, jj=jj: e.activation(out=g[:, 0:n], in_=pg[:, 0:n], func=AF.Sigmoid, bias=b1[:, 8 + jj:9 + jj]),
                     reads=[pg, b1], writes=[g])
                S.op("pool", lambda e, a=a, g=g, jj=jj, ho=ho: e.tensor_tensor(out=HG[:, jj, ho:ho + n], in0=a[:, 0:n], in1=g[:, 0:n], op=ALU.mult),
                     reads=[a, g], writes=[HG])
        sc1.close()
        sc2 = Scope(S)
        zb = Rot([sc2.sb("zb%d" % k, [P, 8, 512], F32) for k in range(2)])
        accB = Rot([sc2.sb("accB%d" % k, [P, 512], F32) for k in range(2)])
        tmp = dict(sq=sc2.sb("sq", [P, 8, 512], F32), pss=sc2.ps("pss"), psq=sc2.ps("psq"),
                   mean=sc2.sb("mean", [P, 512], F32), rstd=sc2.sb("rstd", [P, 512], F32), nb=sc2.sb("nb", [P, 512], F32),
                   t=Rot([sc2.sb("lt%d" % k, [P, 512], F32) for k in range(3)]))
        mb = Rot([sc2.sb("mb%d" % k, [P, 8, 512], BF16) for k in range(2)])
        for blk in blks:
            t0, n, is_ctx = blk
            ho = hoff(blk)
            z = zb.next()
            for c in range(8):
                ab = accB.next()
                for tap in range(31):
                    src_off = ho + tap - 15
                    if tap == 0:
                        S.op("dve", lambda e, c=c, src_off=src_off: e.tensor_scalar(
                            out=z[:, c, 0:n], in0=HG[:, c, src_off:src_off + n], scalar1=wdw[:, c, 0:1], scalar2=cp[:, 0, c:c + 1],
                            op0=ALU.mult, op1=ALU.add), reads=[HG, wdw, cp], writes=[z])
                    else:
                        S.op("dve", lambda e, c=c, src_off=src_off, tap=tap: e.scalar_tensor_tensor(
                            out=z[:, c, 0:n], in0=HG[:, c, src_off:src_off + n], scalar=wdw[:, c, tap:tap + 1], in1=z[:, c, 0:n],
                            op0=ALU.mult, op1=ALU.add), reads=[HG, wdw, z], writes=[z])
            m = mb.next()
            self.layer_norm(sc2, z, n, lambda c: cp[:, 1, c:c + 1], lambda c: cp[:, 2, c:c + 1],
                            [(m, lambda c, m=m, n=n: m[:, c, 0:n], AF.Silu)], tmp, greads=[cp])
            S.dma("sp", self.t_MIX, self.MIX.rearrange("(k p) t -> p k t", p=P)[:, :, t0:t0 + n], m, m[:, :, 0:n])
        sc2.close()
        sc.close()

    def post_moe(self, i, kind, j, last):
        S, cfg = self.S, self.cfg
        E = cfg.E
        blks = cfg.blocks if not last else cfg.lat_blocks
        sbs, cur, tot = [], [], 0
        for b in blks:
            if b[2] and cur and tot + b[1] <= cfg.SB_MAX + 256:
                cur.append(b)
                tot += b[1]
                continue
            if tot + b[1] > cfg.SB_MAX:
                sbs.append(cur)
                cur, tot = [], 0
            cur.append(b)
            tot += b[1]
        if cur:
            sbs.append(cur)
        if kind == 0:
            wo_ap, bo_ap = self.io["attn_w_o"][j], None
        elif kind == 1:
            wo_ap, bo_ap = self.io["fnet_w"][j], self.io["fnet_bT"][:, j * 8:(j + 1) * 8]
        else:
            wo_ap, bo_ap = self.io["conv_w_pw2"][j], self.io["conv_pT"][:, j * 32 + 24:j * 32 + 32]
        L = Scope(S)
        WO = L.sb("WO", [P, 8, 1024], BF16)
        wov = self.wview(wo_ap)
        for q in range(2):
            S.dma("pool", WO, WO[:, :, q * 512:(q + 1) * 512], self.t_in, wov[:, :, q * 512:(q + 1) * 512])
        bo = L.sb("bo", [P, 8], F32)
        if bo_ap is None:
            S.op("dve", lambda e: e.memset(bo[:], 0.0), writes=[bo])
        else:
            S.dma("sp", bo, bo[:], self.t_in, bo_ap)
        for col in range(2):
            S.op("dve", lambda e, col=col: e.tensor_tensor(out=self.GB1[:, i, :, col], in0=self.MOD[:, i, 16:24, col], in1=bo[:], op=ALU.mult),
                 reads=[self.MOD, bo], writes=[self.GB1])
        WR = L.sb("WR", [P, 8, E], F32)
        S.dma("sp", WR, WR[:], self.t_in, self.wview(self.io["moe_w_router"][i]))
        BR = L.sb("BR", [P, E], F32)
        S.dma("sp", BR, BR[:], self.t_in, self.io["moe_b_router"][i].partition_broadcast(P))
        B1 = L.sb("B1", [P, E, 16], F32)
        S.dma("sp", B1, B1[:].rearrange("p a b -> p (a b)"), self.t_in, self.io["moe_b1T"][:, i * E * 16:(i + 1) * E * 16])
        S.op("dve", lambda e: e.tensor_scalar(out=B1[:, :, 8:16], in0=B1[:, :, 8:16], scalar1=1.0, scalar2=None, op0=ALU.add),
             reads=[B1], writes=[B1])
        B2 = L.sb("B2", [E, 1024], F32)
        S.dma("sp", B2, B2[:], self.t_in, self.io["moe_b2"][i])
        xxv = self.XX.rearrange("(k p) t -> p k t", p=P)
        x1v = self.X1.rearrange("(k p) t -> p k t", p=P)
        mixv = self.MIX.rearrange("(k p) t -> p k t", p=P)
        outv = self.yT.rearrange("(k p) t -> p k t", p=P)
        for sb in sbs:
            Tb = sum(b[1] for b in sb)
            offs = []
            o = 0
            for b in sb:
                offs.append(o)
                o += b[1]
            sbt0 = sb[0][0]
            M = Scope(S)
            VT = M.sb("VT", [P, 8, Tb], BF16)
            A = Scope(S)
            xb = Rot([A.sb("xb%d" % k, [P, 8, 512], F32) for k in range(2)])
            mb = Rot([A.sb("mb%d" % k, [P, 8, 512], BF16) for k in range(1)])
            zbr = Rot([A.sb("zb%d" % k, [P, 8, 512], F32) for k in range(2)])
            x1b = Rot([A.sb("x1b%d" % k, [P, 8, 512], F32) for k in range(1)])
            v32 = A.sb("v32", [P, 8, 512], F32)
            pssA, psqA = A.ps("pss"), A.ps("psq")
            ltA = Rot([A.sb("lt%d" % k, [P, 512], F32) for k in range(3)])
            tmps = Rot([dict(sq=A.sb("sq%d" % k, [P, 8, 512], F32), pss=pssA, psq=psqA,
                             mean=A.sb("mean%d" % k, [P, 512], F32), rstd=A.sb("rstd%d" % k, [P, 512], F32),
                             nb=A.sb("nb%d" % k, [P, 512], F32), t=ltA) for k in range(2)])
            pp = Rot([A.ps("pp%d" % k) for k in range(3)])
            pl = Rot([A.ps("pl%d" % k) for k in range(2)])
            pt = A.ps("ptr")
            lg = Rot([A.sb("lg%d" % k, [P, E], F32) for k in range(2)])
            mx = Rot([A.sb("mx%d" % k, [P, 8], F32) for k in range(2)])
            gm = Rot([A.sb("gm%d" % k, [P, E], F32) for k in range(2)])
            ge = Rot([A.sb("ge%d" % k, [P, E], F32) for k in range(2)])
            gs = Rot([A.sb("gs%d" % k, [P, 2], F32) for k in range(2)])
            gt = Rot([A.sb("gt%d" % k, [E, 128], F32) for k in range(2)])
            for bi, blk in enumerate(sb):
                t0, n, is_ctx = blk
                col = 1 if is_ctx else 0
                x, m = xb.next(), mb.next()
                zb, tmp = zbr.next(), tmps.next()
                st, sap = self.xsrc(i, blk)
                S.dma("sp", x, x[:, :, 0:n], st, sap)
                S.dma("sp", m, m[:, :, 0:n], self.t_MIX, mixv[:, :, t0:t0 + n])
                for c in range(8):
                    S.op("act", lambda e, c=c, x=x: e.activation(out=x[:, c, 0:n], in_=x[:, c, 0:n], func=AF.Identity,
                                                                 scale=cfg.alpha, bias=self.GB1[:, i, c, col:col + 1]),
                         reads=[x, self.GB1], writes=[x])
                for dch in range(8):
                    py = pp.next()
                    for k in range(8):
                        S.op("pe", lambda e, py=py, k=k, dch=dch, m=m: e.matmul(py[:, 0:n], WO[:, k, dch * 128:(dch + 1) * 128], m[:, k, 0:n],
                                                                               start=(k == 0), stop=(k == 7)),
                             reads=[WO, m], writes=[py], sig=(k == 7))
                    S.op("dve", lambda e, py=py, dch=dch, x=x: e.scalar_tensor_tensor(
                        out=zb[:, dch, 0:n], in0=py[:, 0:n], scalar=self.MOD[:, i, 16 + dch, col:col + 1], in1=x[:, dch, 0:n],
                        op0=ALU.mult, op1=ALU.add), reads=[py, self.MOD, x], writes=[zb])
                x1 = x1b.next()
                self.layer_norm(A, zb, n, lambda c: self.LNP[:, i, 0, c:c + 1], lambda c: self.LNP[:, i, 1, c:c + 1],
                                [(x1, lambda c, x1=x1, n=n: x1[:, c, 0:n], AF.Identity)], tmp)
                S.dma("sp", self.t_X1, x1v[:, :, t0:t0 + n], x1, x1[:, :, 0:n])
                for c in range(8):
                    S.op("act", lambda e, c=c, x1=x1: e.activation(out=v32[:, c, 0:n], in_=x1[:, c, 0:n], func=AF.Identity,
                                                                   scale=self.MOD[:, i, 32 + c, col:col + 1], bias=self.MOD[:, i, 24 + c, col:col + 1]),
                         reads=[x1, self.MOD], writes=[v32])
                    eng = "dve" if c % 2 == 0 else "pool"
                    S.op(eng, lambda e, c=c, o=offs[bi]: e.tensor_copy(out=VT[:, c, o:o + n], in_=v32[:, c, 0:n]), reads=[v32], writes=[VT])
                for s in range(n // 128):
                    p1 = pl.next()
                    for k in range(8):
                        S.op("pe", lambda e, p1=p1, k=k, s=s: e.matmul(p1[:, 0:E], v32[:, k, s * 128:(s + 1) * 128], WR[:, k, :],
                                                                       start=(k == 0), stop=(k == 7)),
                             reads=[v32, WR], writes=[p1], sig=(k == 7))
                    l, mxx, gmm, gee, gss, gtt = lg.next(), mx.next(), gm.next(), ge.next(), gs.next(), gt.next()
                    S.op("dve", lambda e, l=l, p1=p1: e.tensor_tensor(out=l[:], in0=p1[:, 0:E], in1=BR[:], op=ALU.add), reads=[p1, BR], writes=[l])
                    S.op("dve", lambda e, l=l, mxx=mxx: e.max(out=mxx[:], in_=l[:]), reads=[l], writes=[mxx])
                    S.op("dve", lambda e, l=l, mxx=mxx, gmm=gmm: e.tensor_scalar(out=gmm[:], in0=l[:], scalar1=mxx[:, 3:4], scalar2=None, op0=ALU.is_ge),
                         reads=[l, mxx], writes=[gmm])
                    S.op("dve", lambda e, mxx=mxx, gss=gss: e.tensor_scalar(out=gss[:, 0:1], in0=mxx[:, 0:1], scalar1=-1.0, scalar2=None, op0=ALU.mult),
                         reads=[mxx], writes=[gss])
                    S.op("act", lambda e, l=l, gee=gee, gss=gss: e.activation(out=gee[:], in_=l[:], func=AF.Exp, bias=gss[:, 0:1]),
                         reads=[l, gss], writes=[gee])
                    S.op("dve", lambda e, gee=gee, gmm=gmm: e.tensor_tensor(out=gee[:], in0=gee[:], in1=gmm[:], op=ALU.mult), reads=[gee, gmm], writes=[gee])
                    S.op("dve", lambda e, gee=gee, gss=gss: e.reduce_sum(out=gss[:, 1:2], in_=gee[:], axis=AX.X), reads=[gee], writes=[gss])
                    S.op("dve", lambda e, gss=gss: e.reciprocal(out=gss[:, 1:2], in_=gss[:, 1:2]), reads=[gss], writes=[gss])
                    S.op("dve", lambda e, gee=gee, gss=gss: e.tensor_scalar(out=gee[:], in0=gee[:], scalar1=gss[:, 1:2], scalar2=None, op0=ALU.mult),
                         reads=[gee, gss], writes=[gee])
                    S.op("pe", lambda e, gee=gee: e.transpose(pt[0:E, 0:128], gee[:], self.ident[:]), reads=[gee, self.ident], writes=[pt])
                    S.op("act", lambda e, gtt=gtt: e.copy(out=gtt[:], in_=pt[0:E, 0:128]), reads=[pt], writes=[gtt])
                    tt = t0 + s * 128
                    S.dma("sp", self.t_GD, self.GD[:, tt:tt + 128], gtt, gtt[:])
            A.close()
            Mf = Scope(S)
            Fa = Mf.sb("Fa", [P, 8, Tb], F32)
            B = Scope(S)
            HH = [[B.sb("Hh%d_%d" % (k, bi), [P, 8, blk[1]], BF16) for bi, blk in enumerate(sb)] for k in range(2)]
            GBt = Rot([B.sb("GBt%d" % k, [P, Tb], F32) for k in range(2)])
            GTs = B.sb("GTs", [E, Tb], F32)
            S.dma("sp", GTs, GTs[:], self.t_GD, self.GD[:, sbt0:sbt0 + Tb])
            NW1 = 4
            w1bufs = [B.sb("w1r%d" % k, [P, 8, 2, 256], BF16) for k in range(NW1)]
            w2bufs = [B.sb("w2r%d" % k, [P, 8, 256], BF16) for k in range(4)]
            pg = Rot([B.ps("pg%d" % k) for k in range(3)])
            plin = Rot([B.ps("plin%d" % k) for k in range(3)])
            py2 = Rot([B.ps("py%d" % k) for k in range(2)])
            ta = Rot([B.sb("ta%d" % k, [P, 512], F32) for k in range(3)])
            ts_ = Rot([B.sb("ts%d" % k, [P, 512], F32) for k in range(3)])
            tl = Rot([B.sb("tl%d" % k, [P, 512], F32) for k in range(3)])
            nld = [0]

            def load_w1(q):
                while nld[0] <= q and nld[0] < E * 4:
                    qq = nld[0]
                    ex_, j4_ = qq // 4, qq % 4
                    w1 = w1bufs[qq % NW1]
                    w1v_ = self.wview(self.io["moe_w1"][i, ex_])
                    for s2 in range(2):
                        S.dma("pool", w1, w1[:, :, s2, :], self.t_in, w1v_[:, :, s2 * 1024 + j4_ * 256: s2 * 1024 + (j4_ + 1) * 256])
                    nld[0] += 1

            def load_w2(ex, d4):
                w2 = w2bufs[d4]
                w2v_ = self.wview(self.io["moe_w2"][i, ex])
                S.dma("pool", w2, w2[:], self.t_in, w2v_[:, :, d4 * 256:(d4 + 1) * 256])
            late = [None]

            def flush_late():
                if late[0] is not None:
                    f = late[0]
                    late[0] = None
                    f()

            def w1_unit(ex, w1, gb, jj, jc, n, o, bi):
                Hh = HH[ex % 2][bi]
                pG, pLn = pg.next(), plin.next()
                for (ptile, s2) in ((pG, 0), (pLn, 1)):
                    for k in range(8):
                        S.op("pe", lambda e, ptile=ptile, s2=s2, k=k: e.matmul(
                            ptile[:, 0:n], w1[:, k, s2, jj * 128:(jj + 1) * 128], VT[:, k, o:o + n], start=(k == 0), stop=(k == 7)),
                            reads=[w1, VT], writes=[ptile], sig=(k == 7))
                a, sg, l = ta.next(), ts_.next(), tl.next()
                S.op("dve", lambda e: e.tensor_scalar(
                    out=a[:, 0:n], in0=pG[:, 0:n], scalar1=B1[:, ex, jc:jc + 1], scalar2=7.0, op0=ALU.add, op1=ALU.min),
                    reads=[pG, B1], writes=[a])
                S.op("act", lambda e: e.activation(out=sg[:, 0:n], in_=a[:, 0:n], func=AF.Sigmoid, scale=1.702),
                     reads=[a], writes=[sg])
                S.op("act", lambda e: e.activation(out=l[:, 0:n], in_=pLn[:, 0:n], func=AF.Identity, bias=B1[:, ex, 8 + jc:9 + jc]),
                     reads=[pLn, B1], writes=[l])
                flush_late()

                def second():
                    S.op("dve", lambda e: e.tensor_scalar(out=l[:, 0:n], in0=l[:, 0:n], scalar1=-6.0, scalar2=8.0,
                                                          op0=ALU.max, op1=ALU.min), reads=[l], writes=[l])
                    S.op("dve", lambda e: e.tensor_tensor(out=sg[:, 0:n], in0=a[:, 0:n], in1=sg[:, 0:n], op=ALU.mult),
                         reads=[a, sg], writes=[sg])
                    S.op("pool", lambda e: e.tensor_tensor(out=sg[:, 0:n], in0=sg[:, 0:n], in1=l[:, 0:n], op=ALU.mult),
                         reads=[sg, l], writes=[sg])
                    S.op("pool", lambda e: e.tensor_tensor(out=Hh[:, jc, 0:n], in0=sg[:, 0:n], in1=gb[:, o:o + n], op=ALU.mult),
                         reads=[sg, gb], writes=[Hh])
                late[0] = second

            def w2_group(ex, w2, dd, dch, n, o, bi):
                Hh = HH[ex % 2][bi]
                py = py2.next()
                if ex == 0:
                    S.op("pe", lambda e: e.matmul(py[:, 0:n], B2[:, dch * 128:(dch + 1) * 128], GTs[:, o:o + n], start=True, stop=False),
                         reads=[B2, GTs], writes=[py], sig=False)
                for f in range(8):
                    S.op("pe", lambda e, f=f: e.matmul(
                        py[:, 0:n], w2[:, f, dd * 128:(dd + 1) * 128], Hh[:, f, 0:n], start=(f == 0 and ex != 0), stop=(f == 7)),
                        reads=[w2, Hh], writes=[py], sig=(f == 7))
                if ex == 0:
                    S.op("act", lambda e: e.copy(out=Fa[:, dch, o:o + n], in_=py[:, 0:n]), reads=[py], writes=[Fa])
                else:
                    S.op("dve", lambda e: e.tensor_tensor(out=Fa[:, dch, o:o + n], in0=Fa[:, dch, o:o + n], in1=py[:, 0:n], op=ALU.add),
                         reads=[py, Fa], writes=[Fa])

            load_w1(1)
            w2q = []
            LAG = min(2, len(sb))
            for st in range(E + 1):
                gb = None
                if st < E:
                    gb = GBt.next()
                    S.dma("sp", gb, gb[:], self.t_GD, self.GD[st, sbt0:sbt0 + Tb].partition_broadcast(P))
                for j4 in range(4):
                    if st < E:
                        load_w1(st * 4 + j4 + 2)
                    if j4 < 3:
                        if st >= 1:
                            load_w2(st - 1, j4 + 1)
                    elif st < E:
                        load_w2(st, 0)
                    w1 = w1bufs[(st * 4 + j4) % NW1]
                    w2 = w2bufs[j4]
                    for jj in range(2):
                        jc = j4 * 2 + jj
                        for bi, blk in enumerate(sb):
                            n, o = blk[1], offs[bi]
                            if st < E:
                                w1_unit(st, w1, gb, jj, jc, n, o, bi)
                            if st >= 1:
                                if st == E:
                                    flush_late()
                                w2q.append(lambda st=st, w2=w2, jj=jj, jc=jc, n=n, o=o, bi=bi: w2_group(st - 1, w2, jj, jc, n, o, bi))
                                while len(w2q) > LAG:
                                    w2q.pop(0)()
            flush_late()
            while w2q:
                w2q.pop(0)()
            B.close()
            C = Scope(S)
            xb = Rot([C.sb("xb%d" % k, [P, 8, 512], F32) for k in range(2)])
            zbC = Rot([C.sb("zb%d" % k, [P, 8, 512], F32) for k in range(2)])
            ob = Rot([C.sb("ob%d" % k, [P, 8, 512], F32) for k in range(1)])
            sqC, pssC, psqC = C.sb("sq", [P, 8, 512], F32), C.ps("pss"), C.ps("psq")
            ltC = Rot([C.sb("lt%d" % k, [P, 512], F32) for k in range(3)])
            tmpsC = Rot([dict(sq=sqC, pss=pssC, psq=psqC, mean=C.sb("mean%d" % k, [P, 512], F32),
                              rstd=C.sb("rstd%d" % k, [P, 512], F32), nb=C.sb("nb%d" % k, [P, 512], F32), t=ltC) for k in range(2)])
            for bi, blk in enumerate(sb):
                t0, n, is_ctx = blk
                col = 1 if is_ctx else 0
                o = offs[bi]
                x = xb.next()
                zb, tmp = zbC.next(), tmpsC.next()
                S.dma("sp", x, x[:, :, 0:n], self.t_X1, x1v[:, :, t0:t0 + n])
                for c in range(8):
                    S.op("act", lambda e, c=c, x=x: e.activation(out=x[:, c, 0:n], in_=x[:, c, 0:n], func=AF.Copy, scale=cfg.alpha),
                         reads=[x], writes=[x])
                    S.op("dve", lambda e, c=c, x=x, o=o: e.scalar_tensor_tensor(
                        out=zb[:, c, 0:n], in0=Fa[:, c, o:o + n], scalar=self.MOD[:, i, 40 + c, col:col + 1], in1=x[:, c, 0:n],
                        op0=ALU.mult, op1=ALU.add), reads=[Fa, self.MOD, x], writes=[zb])
                ot = ob.next()
                self.layer_norm(C, zb, n, lambda c: self.LNP[:, i, 2, c:c + 1], lambda c: self.LNP[:, i, 3, c:c + 1],
                                [(ot, lambda c, ot=ot, n=n: ot[:, c, 0:n], AF.Identity)], tmp)
                if last:
                    S.dma("sp", self.t_out, outv[:, :, t0:t0 + n], ot, ot[:, :, 0:n])
                else:
                    S.dma("sp", self.t_XX, xxv[:, :, t0:t0 + n], ot, ot[:, :, 0:n])
            C.close()
            Mf.close()
            M.close()
        L.close()


def pmajor(v, nch):
    v = np.asarray(v, np.float32)
    lead = v.shape[:-1]
    a = v.reshape(lead + (nch, P))
    a = np.moveaxis(a, -1, 0)
    return np.ascontiguousarray(a)


def host_constants(cfg):
    SEQ, CTX = cfg.SEQ, cfg.CTX
    GRID_W = 64
    t = np.arange(SEQ)
    rows = (t // GRID_W).astype(np.float64)
    cols = (t % GRID_W).astype(np.float64)
    inv_freq = (10000.0 ** (-np.arange(16, dtype=np.float64) / 16)).astype(np.float32).astype(np.float64)
    dd = np.arange(64)
    f = dd % 16
    pos = np.where(dd[:, None] < 32, rows[None, :], cols[None, :])
    ang = (pos.astype(np.float32) * inv_freq[f][:, None].astype(np.float32)).astype(np.float64)
    C = np.cos(ang).astype(np.float32)
    Sn = np.sin(ang).astype(np.float32)
    ropeC = np.concatenate([C, C], 0)
    ropeS = np.concatenate([Sn, Sn], 0)

    def dft(N):
        n = np.arange(N, dtype=np.int64)
        m = (n[:, None] * n[None, :]) % N
        a = 2.0 * np.pi * m.astype(np.float64) / N
        return np.stack([np.cos(a), -np.sin(a)]).astype(np.float32).astype(ml_dtypes.bfloat16)
    a128 = 2.0 * np.pi * ((np.arange(128)[:, None] * np.arange(128)[None, :]) % 128) / 128.0
    dft128 = np.concatenate([np.cos(a128), np.sin(a128)], 1).astype(np.float32).astype(ml_dtypes.bfloat16)
    return dict(ropeC=ropeC, ropeS=ropeS, dftL=dft(SEQ), dftC=dft(CTX), dft128=dft128, ident=np.eye(P, dtype=np.float32))


def host_inputs(cfg, inp, consts, b):
    f = lambda a: np.ascontiguousarray(np.asarray(a, np.float32))
    DEPTH, E = cfg.DEPTH, cfg.E
    m = {}
    m["xT"] = np.ascontiguousarray(np.asarray(inp["x"][b], np.float32).T)
    m["ctxT"] = np.ascontiguousarray(np.asarray(inp["ctx"][b], np.float32).T)
    m["cT"] = pmajor(inp["c"][b], 8)
    m["cctxT"] = pmajor(inp["c_ctx"], 8)
    m["w_mod"] = f(inp["w_mod"])
    m["b_modT"] = pmajor(inp["b_mod"], 48).reshape(P, DEPTH * 48)
    lnp = np.stack([np.asarray(inp[k], np.float32) for k in ("ln1_g", "ln1_b", "ln2_g", "ln2_b")], 1)
    m["lnp"] = pmajor(lnp, 8).reshape(P, DEPTH * 32)
    m["attn_w_qkv"] = f(inp["attn_w_qkv"])
    m["attn_w_o"] = f(inp["attn_w_o"])
    m["lam"] = np.ascontiguousarray(np.stack([np.asarray(inp[k], np.float32) for k in
                                             ("attn_lam_q1", "attn_lam_k1", "attn_lam_q2", "attn_lam_k2")], 1))
    m["sublnT"] = np.ascontiguousarray(np.asarray(inp["attn_subln_g"], np.float32).T)
    m["fnet_w"] = f(inp["fnet_w"])
    m["fnet_bT"] = pmajor(inp["fnet_b"], 8).reshape(P, -1)
    m["conv_w_pw1"] = f(inp["conv_w_pw1"])
    m["conv_b_pw1T"] = pmajor(inp["conv_b_pw1"], 16).reshape(P, -1)
    wdw = np.asarray(inp["conv_w_dw"], np.float32)
    wdw = np.moveaxis(wdw, 1, 2)
    nC = wdw.shape[0]
    wdw = wdw.reshape(nC, 8, P, 31)
    m["conv_w_dwT"] = np.ascontiguousarray(np.moveaxis(wdw, 2, 0)).reshape(P, nC * 8 * 31)
    cpar = np.stack([np.asarray(inp[k], np.float32) for k in ("conv_b_dw", "conv_ln_g", "conv_ln_b", "conv_b_pw2")], 1)
    m["conv_pT"] = pmajor(cpar, 8).reshape(P, nC * 32)
    m["conv_w_pw2"] = f(inp["conv_w_pw2"])
    m["moe_w_router"] = f(inp["moe_w_router"])
    m["moe_b_router"] = f(inp["moe_b_router"])
    m["moe_w1"] = f(inp["moe_w1"])
    m["moe_b1T"] = pmajor(inp["moe_b1"], 16).reshape(P, DEPTH * E * 16)
    m["moe_w2"] = f(inp["moe_w2"])
    m["moe_b2"] = f(inp["moe_b2"])
    m.update(consts)
    return m


_CACHE = {}


def run(cfg, inputs, n_cores):
    key = (cfg.SEQ, cfg.CTX, cfg.E, cfg.DEPTH)
    if key not in _CACHE:
        _CACHE[key] = Prog(cfg).build()
    nc = _CACHE[key]
    consts = host_constants(cfg)
    in_maps = [host_inputs(cfg, inputs, consts, b) for b in range(n_cores)]
    res = run_bass_kernel_spmd(nc, in_maps, core_ids=list(range(n_cores)))
    out = np.stack([np.ascontiguousarray(res.results[b]["yT"].T) for b in range(n_cores)], 0)
    return out.astype(np.float32)


def kernel(**inputs):
    cfg = Cfg()
    return run(cfg, inputs, 8)
```

```python
import contextlib
import numpy as np
import ml_dtypes
import concourse.bass as bass
import concourse.mybir as mybir
from concourse.bass_utils import run_bass_kernel_spmd

F32 = mybir.dt.float32
BF16 = mybir.dt.bfloat16
AF = mybir.ActivationFunctionType
ALU = mybir.AluOpType
AX = mybir.AxisListType
P = 128
LN_EPS = 1e-5


class Cfg:
    def __init__(self, SEQ=4096, CTX=256, E=32, DEPTH=4):
        self.SEQ, self.CTX, self.E, self.DEPTH = SEQ, CTX, E, DEPTH
        self.D = 1024
        self.KC = 8
        self.H = 8
        self.T = SEQ + CTX
        self.N_A = (DEPTH + 2) // 3
        self.N_B = (DEPTH + 1) // 3
        self.N_C = DEPTH // 3
        self.alpha = float((2 * DEPTH) ** 0.25)
        self.lat_blocks = [(t, 512, False) for t in range(0, SEQ, 512)]
        self.ctx_blocks = [(SEQ + t, min(512, CTX - t), True) for t in range(0, CTX, 512)]
        self.blocks = self.lat_blocks + self.ctx_blocks
        self.SB_MAX = 1024


class TT:
    def __init__(self, h, name):
        self.h = h
        self.name = name
        self.w = None
        self.r = {}
        self.dsem = None
        self.dcnt = 0
        self.w_is_dma = False

    def __getitem__(self, k):
        return self.h[k]


class Sched:
    def __init__(self, nc, es):
        self.nc = nc
        self.eng = {"pe": nc.tensor, "act": nc.scalar, "dve": nc.vector, "pool": nc.gpsimd, "sp": nc.sync}
        self.sem = {k: es.enter_context(nc.semaphore("e_" + k)) for k in self.eng}
        self.cnt = {k: 0 for k in self.eng}
        self.seen = {k: {} for k in self.eng}
        self.semkey = {}
        for k in self.eng:
            self.semkey[id(self.sem[k])] = k
        self.free_dsems = []
        self.all_dsems = []
        self.es = es
        self.ndsem = 0
        self.live = []
        self.ninst = 0

    def _get_dsem(self):
        if self.free_dsems:
            return self.free_dsems.pop()
        s = self.es.enter_context(self.nc.semaphore("d%d" % self.ndsem))
        self.ndsem += 1
        rec = [s, 0]
        self.all_dsems.append(rec)
        return rec

    def track(self, h, name):
        t = TT(h, name)
        self.live.append(t)
        return t

    def _wait(self, e, evs):
        own = self.sem[e]
        for (sem, val) in evs:
            if sem is own and e == "pe":
                continue
            k = id(sem)
            if self.seen[e].get(k, 0) >= val:
                continue
            self.eng[e].wait_ge(sem, val)
            self.seen[e][k] = val

    def op(self, e, fn, reads=(), writes=(), sig=True):
        deps = []
        for t in reads:
            if t.w is not None:
                deps.append(t.w)
        for t in writes:
            if t.w is not None:
                deps.append(t.w)
            for k, v in t.r.items():
                deps.append((k, v))
        deps2 = []
        for d in deps:
            deps2.append(d)
        self._wait(e, deps2)
        ins = fn(self.eng[e])
        self.ninst += 1
        if sig:
            self.cnt[e] += 1
            ins.then_inc(self.sem[e], 1)
            ev = (self.sem[e], self.cnt[e])
        else:
            ev = (self.sem[e], self.cnt[e] + 1)
        for t in writes:
            t.w = ev
            t.w_is_dma = False
            t.r = {}
        for t in reads:
            if t.r.get(ev[0], 0) < ev[1]:
                t.r[ev[0]] = ev[1]
        return ins

    def dma(self, q, out_t, out_ap, in_t, in_ap):
        deps = []
        if in_t.w is not None:
            deps.append(in_t.w)
        if out_t.w is not None and not out_t.w_is_dma:
            deps.append(out_t.w)
        for k, v in out_t.r.items():
            deps.append((k, v))
        self._wait(q, deps)
        if out_t.dsem is None:
            out_t.dsem = self._get_dsem()
        rec = out_t.dsem
        rec[1] += 16
        self.eng[q].dma_start(out=out_ap, in_=in_ap).then_inc(rec[0], 16)
        self.ninst += 1
        ev = (rec[0], rec[1])
        out_t.w = ev
        out_t.w_is_dma = True
        out_t.r = {}
        if in_t.r.get(ev[0], 0) < ev[1]:
            in_t.r[ev[0]] = ev[1]

    def drain(self, release=()):
        evs = [(self.sem[k], self.cnt[k]) for k in self.eng if self.cnt[k] > 0]
        evs += [(r[0], r[1]) for r in self.all_dsems if r[1] > 0]
        for e in self.eng:
            own = self.sem[e]
            for (sem, val) in evs:
                if sem is own:
                    continue
                k = id(sem)
                if self.seen[e].get(k, 0) >= val:
                    continue
                self.eng[e].wait_ge(sem, val)
                self.seen[e][k] = val
        for t in self.live:
            t.w = None
            t.r = {}
        for t in release:
            if t.dsem is not None:
                self.free_dsems.append(t.dsem)
                t.dsem = None
            if t in self.live:
                self.live.remove(t)


_UID = [0]


def _uid():
    _UID[0] += 1
    return _UID[0]


class Scope:
    def __init__(self, S):
        self.S = S
        self.es = contextlib.ExitStack()
        self.tiles = []
        self.n = 0

    def sb(self, name, shape, dt):
        h = self.es.enter_context(self.S.nc.sbuf_tensor("%s_%d" % (name, _uid()), list(shape), dt))
        t = self.S.track(h, name)
        self.tiles.append(t)
        return t

    def ps(self, name, shape=(P, 512), dt=F32):
        h = self.es.enter_context(self.S.nc.psum_tensor("%s_%d" % (name, _uid()), list(shape), dt))
        t = self.S.track(h, name)
        self.tiles.append(t)
        return t

    def close(self):
        self.S.drain(release=self.tiles)
        self.es.close()


class Rot:
    def __init__(self, tiles):
        self.tiles = tiles
        self.i = 0

    def next(self):
        t = self.tiles[self.i % len(self.tiles)]
        self.i += 1
        return t


class Prog:
    def __init__(self, cfg):
        self.cfg = cfg
        self.nc = bass.Bass("TRN2", target_bir_lowering=False)
        self.io = {}

    def din(self, name, shape, dt=F32):
        ap = self.nc.dram_tensor(name, list(shape), dt, kind="ExternalInput").ap()
        self.io[name] = ap
        return ap

    def build(self):
        cfg = self.cfg
        nc = self.nc
        D, T, E, SEQ, CTX, DEPTH = cfg.D, cfg.T, cfg.E, cfg.SEQ, cfg.CTX, cfg.DEPTH
        d = self.din
        d("xT", [D, SEQ]); d("ctxT", [D, CTX]); d("cT", [P, 8]); d("cctxT", [P, 8])
        d("w_mod", [DEPTH, D, 6 * D]); d("b_modT", [P, DEPTH * 48])
        d("lnp", [P, DEPTH * 32])
        d("attn_w_qkv", [cfg.N_A, D, 3 * D]); d("attn_w_o", [cfg.N_A, D, D])
        d("lam", [cfg.N_A, 4, 64]); d("sublnT", [P, cfg.N_A])
        d("fnet_w", [max(cfg.N_B, 1), D, D]); d("fnet_bT", [P, max(cfg.N_B, 1) * 8])
        nC = max(cfg.N_C, 1)
        d("conv_w_pw1", [nC, D, 2 * D]); d("conv_b_pw1T", [P, nC * 16])
        d("conv_w_dwT", [P, nC * 8 * 31]); d("conv_pT", [P, nC * 4 * 8])
        d("conv_w_pw2", [nC, D, D])
        d("moe_w_router", [DEPTH, D, E]); d("moe_b_router", [DEPTH, E])
        d("moe_w1", [DEPTH, E, D, 2 * D]); d("moe_b1T", [P, DEPTH * E * 16])
        d("moe_w2", [DEPTH, E, D, D]); d("moe_b2", [DEPTH, E, D])
        d("ropeC", [P, SEQ]); d("ropeS", [P, SEQ])
        d("dftL", [2, SEQ, SEQ], BF16); d("dftC", [2, CTX, CTX], BF16); d("dft128", [P, 256], BF16)
        d("ident", [P, P])
        self.yT = nc.dram_tensor("yT", [D, SEQ], F32, kind="ExternalOutput").ap()
        self.XX = nc.dram_tensor("XX", [D, T], F32).ap()
        self.X1 = nc.dram_tensor("X1", [D, T], F32).ap()
        self.MIX = nc.dram_tensor("MIX", [D, T], BF16).ap()
        self.GD = nc.dram_tensor("GD", [E, T], F32).ap()

        with contextlib.ExitStack() as es:
            S = Sched(nc, es)
            self.S = S
            es.enter_context(nc.Block())
            self.t_in = S.track(None, "inputs")
            self.t_XX = S.track(None, "XX")
            self.t_X1 = S.track(None, "X1")
            self.t_MIX = S.track(None, "MIX")
            self.t_GD = S.track(None, "GD")
            self.t_out = S.track(None, "yT")
            G = Scope(S)
            self.G = G
            self.ones32 = G.sb("ones32", [P, P], F32)
            self.onesbf = G.sb("onesbf", [P, P], BF16)
            self.ident = G.sb("ident", [P, P], F32)
            self.MOD = G.sb("MOD", [P, DEPTH, 48, 2], F32)
            self.LNP = G.sb("LNP", [P, DEPTH, 4, 8], F32)
            self.GB1 = G.sb("GB1", [P, DEPTH, 8, 2], F32)
            self.cst = G.sb("cst", [P, 4], F32)
            S.op("dve", lambda e: e.memset(self.cst[:, 0:1], LN_EPS), writes=[self.cst])
            S.op("dve", lambda e: e.memset(self.cst[:, 1:2], 128.0 * LN_EPS), writes=[self.cst])
            S.op("dve", lambda e: e.memset(self.ones32[:], 1.0), writes=[self.ones32])
            S.op("dve", lambda e: e.memset(self.onesbf[:], 1.0), writes=[self.onesbf])
            S.dma("sp", self.ident, self.ident[:], self.t_in, self.io["ident"][:, :])
            S.dma("sp", self.LNP, self.LNP[:].rearrange("p a b c -> p (a b c)"), self.t_in, self.io["lnp"][:, :])
            self.stage_mod()
            for i in range(DEPTH):
                kind = i % 3
                j = i // 3
                last = i == DEPTH - 1
                if kind == 0:
                    self.mixer_attn(i, j, last)
                elif kind == 1:
                    self.mixer_fnet(i, j, last)
                else:
                    self.mixer_conv(i, j, last)
                self.post_moe(i, kind, j, last)
            S.drain()
            G.close()
        return nc

    def xsrc(self, i, blk):
        t0, n, is_ctx = blk
        cfg = self.cfg
        if i == 0:
            if is_ctx:
                ap = self.io["ctxT"].rearrange("(k p) t -> p k t", p=P)[:, :, t0 - cfg.SEQ:t0 - cfg.SEQ + n]
            else:
                ap = self.io["xT"].rearrange("(k p) t -> p k t", p=P)[:, :, t0:t0 + n]
            return self.t_in, ap
        return self.t_XX, self.XX.rearrange("(k p) t -> p k t", p=P)[:, :, t0:t0 + n]

    def wview(self, ap2d):
        return ap2d.rearrange("(k p) n -> p k n", p=P)

    def modulate(self, eng_rot, x, ut_ap_fn, i, n, col, s_sh, s_sc):
        S = self.S
        for c in range(8):
            S.op("act", lambda e, c=c: e.activation(
                out=ut_ap_fn(c), in_=x[:, c, 0:n], func=AF.Identity,
                bias=self.MOD[:, i, s_sh * 8 + c, col:col + 1], scale=self.MOD[:, i, s_sc * 8 + c, col:col + 1]),
                reads=[x, self.MOD], writes=eng_rot)

    def layer_norm(self, sc, z, n, gcol, bcol, outs, tmp, greads=()):
        S = self.S
        sq, pss, psq, mean, rstd, nb = tmp["sq"], tmp["pss"], tmp["psq"], tmp["mean"], tmp["rstd"], tmp["nb"]
        for c in range(8):
            S.op("act", lambda e, c=c: e.activation(out=sq[:, c, 0:n], in_=z[:, c, 0:n], func=AF.Square),
                 reads=[z], writes=[sq])
        for c in range(8):
            S.op("pe", lambda e, c=c: e.matmul(pss[:, 0:n], self.ones32[:], z[:, c, 0:n], start=(c == 0), stop=(c == 7)),
                 reads=[self.ones32, z], writes=[pss], sig=(c == 7))
        for c in range(8):
            S.op("pe", lambda e, c=c: e.matmul(psq[:, 0:n], self.ones32[:], sq[:, c, 0:n], start=(c == 0), stop=(c == 7)),
                 reads=[self.ones32, sq], writes=[psq], sig=(c == 7))
        invd = 1.0 / 1024.0
        S.op("dve", lambda e: e.tensor_scalar(out=mean[:, 0:n], in0=pss[:, 0:n], scalar1=invd, scalar2=None, op0=ALU.mult),
             reads=[pss], writes=[mean])
        S.op("dve", lambda e: e.tensor_tensor(out=nb[:, 0:n], in0=mean[:, 0:n], in1=mean[:, 0:n], op=ALU.mult),
             reads=[mean], writes=[nb])
        S.op("dve", lambda e: e.scalar_tensor_tensor(out=rstd[:, 0:n], in0=psq[:, 0:n], scalar=invd, in1=nb[:, 0:n],
                                                     op0=ALU.mult, op1=ALU.subtract),
             reads=[psq, nb], writes=[rstd])
        S.op("act", lambda e: e.activation(out=rstd[:, 0:n], in_=rstd[:, 0:n], func=AF.Ln, bias=self.cst[:, 0:1]),
             reads=[rstd, self.cst], writes=[rstd])
        S.op("act", lambda e: e.activation(out=rstd[:, 0:n], in_=rstd[:, 0:n], func=AF.Exp, scale=-0.5),
             reads=[rstd], writes=[rstd])
        S.op("dve", lambda e: e.scalar_tensor_tensor(out=nb[:, 0:n], in0=mean[:, 0:n], scalar=-1.0, in1=rstd[:, 0:n],
                                                     op0=ALU.mult, op1=ALU.mult),
             reads=[mean, rstd], writes=[nb])
        for c in range(8):
            t = tmp["t"].next()
            eng = "dve" if c % 2 == 0 else "pool"
            S.op(eng, lambda e, c=c, t=t: e.tensor_tensor(out=t[:, 0:n], in0=z[:, c, 0:n], in1=rstd[:, 0:n], op=ALU.mult),
                 reads=[z, rstd], writes=[t])
            S.op(eng, lambda e, t=t: e.tensor_tensor(out=t[:, 0:n], in0=t[:, 0:n], in1=nb[:, 0:n], op=ALU.add),
                 reads=[t, nb], writes=[t])
            for (ot, apf, func) in outs:
                S.op("act", lambda e, c=c, t=t, apf=apf, func=func: e.activation(
                    out=apf(c), in_=t[:, 0:n], func=func, bias=bcol(c), scale=gcol(c)),
                    reads=[t, self.LNP, self.MOD] + list(greads), writes=[ot])

    def stage_mod(self):
        S, cfg = self.S, self.cfg
        sc = Scope(S)
        craw = sc.sb("craw", [P, 16], F32)
        cond = sc.sb("cond", [P, 8, 2], F32)
        bmod = sc.sb("bmod", [P, cfg.DEPTH, 48], F32)
        wm = Rot([sc.sb("wm%d" % k, [P, 8, 768], F32) for k in range(2)])
        pst = Rot([sc.ps("pmod%d" % k) for k in range(2)])
        S.dma("sp", craw, craw[:, 0:8], self.t_in, self.io["cT"][:, :])
        S.dma("sp", craw, craw[:, 8:16], self.t_in, self.io["cctxT"][:, :])
        S.dma("sp", bmod, bmod[:].rearrange("p a b -> p (a b)"), self.t_in, self.io["b_modT"][:, :])
        S.op("act", lambda e: e.activation(out=cond[:, :, 0], in_=craw[:, 0:8], func=AF.Silu), reads=[craw], writes=[cond])
        S.op("act", lambda e: e.activation(out=cond[:, :, 1], in_=craw[:, 8:16], func=AF.Silu), reads=[craw], writes=[cond])
        for i in range(cfg.DEPTH):
            wv = self.wview(self.io["w_mod"][i])
            for nb in range(8):
                w = wm.next()
                S.dma("sp", w, w[:], self.t_in, wv[:, :, nb * 768:(nb + 1) * 768])
                for c6 in range(6):
                    ch = nb * 6 + c6
                    ps = pst.next()
                    for k in range(8):
                        S.op("pe", lambda e, k=k, c6=c6, w=w, ps=ps: e.matmul(
                            ps[:, 0:2], w[:, k, c6 * 128:(c6 + 1) * 128], cond[:, k, :], start=(k == 0), stop=(k == 7)),
                            reads=[w, cond], writes=[ps], sig=(k == 7))
                    S.op("dve", lambda e, ch=ch, ps=ps, i=i: e.tensor_scalar(
                        out=self.MOD[:, i, ch, :], in0=ps[:, 0:2], scalar1=bmod[:, i, ch:ch + 1], scalar2=None, op0=ALU.add),
                        reads=[ps, bmod], writes=[self.MOD])
            for s in (1, 4):
                S.op("dve", lambda e, s=s, i=i: e.tensor_scalar(
                    out=self.MOD[:, i, s * 8:(s + 1) * 8, :], in0=self.MOD[:, i, s * 8:(s + 1) * 8, :], scalar1=1.0,
                    scalar2=None, op0=ALU.add), reads=[self.MOD], writes=[self.MOD])
        sc.close()

    def mixer_attn(self, i, j, last):
        S, cfg = self.S, self.cfg
        SEQ, T = cfg.SEQ, cfg.T
        lam_init = 0.8 - 0.6 * float(np.exp(-0.3 * i))
        sc = Scope(S)
        UT = sc.sb("UT", [P, 8, T], BF16)
        sc0 = Scope(S)
        xb = Rot([sc0.sb("xb%d" % k, [P, 8, 512], F32) for k in range(2)])
        for blk in cfg.blocks:
            t0, n, is_ctx = blk
            x = xb.next()
            st, sap = self.xsrc(i, blk)
            S.dma("sp", x, x[:, :, 0:n], st, sap)
            self.modulate([UT], x, lambda c, t0=t0, n=n: UT[:, c, t0:t0 + n], i, n, 1 if is_ctx else 0, 0, 1)
        sc0.close()
        COS = sc.sb("COS", [P, SEQ], F32)
        SIN = sc.sb("SIN", [P, SEQ], F32)
        S.dma("sp", COS, COS[:], self.t_in, self.io["ropeC"][:, :])
        S.dma("sp", SIN, SIN[:], self.t_in, self.io["ropeS"][:, :])
        lamt = sc.sb("lamt", [P, 4, 64], F32)
        S.dma("sp", lamt, lamt[:].rearrange("p a b -> p (a b)"), self.t_in,
              self.io["lam"][j].rearrange("a b -> (a b)").partition_broadcast(P))
        lsc = sc.sb("lsc", [P, 8], F32)
        S.op("dve", lambda e: e.tensor_tensor(out=lamt[:, 0, :], in0=lamt[:, 0, :], in1=lamt[:, 1, :], op=ALU.mult),
             reads=[lamt], writes=[lamt])
        S.op("dve", lambda e: e.tensor_tensor(out=lamt[:, 2, :], in0=lamt[:, 2, :], in1=lamt[:, 3, :], op=ALU.mult),
             reads=[lamt], writes=[lamt])
        S.op("dve", lambda e: e.reduce_sum(out=lsc[:, 0:1], in_=lamt[:, 0, :], axis=AX.X), reads=[lamt], writes=[lsc])
        S.op("dve", lambda e: e.reduce_sum(out=lsc[:, 1:2], in_=lamt[:, 2, :], axis=AX.X), reads=[lamt], writes=[lsc])
        S.op("act", lambda e: e.activation(out=lsc[:, 2:4], in_=lsc[:, 0:2], func=AF.Exp), reads=[lsc], writes=[lsc])
        S.op("dve", lambda e: e.scalar_tensor_tensor(out=lsc[:, 4:5], in0=lsc[:, 3:4], scalar=-lam_init, in1=lsc[:, 2:3],
                                                     op0=ALU.add, op1=ALU.subtract), reads=[lsc], writes=[lsc])
        NA = cfg.N_A
        sgt0 = sc.sb("sgt0", [P, NA], F32)
        sgt = sc.sb("sgt", [P, 2], F32)
        S.dma("sp", sgt0, sgt0[:], self.t_in, self.io["sublnT"][:, :])
        S.op("dve", lambda e: e.tensor_scalar(out=sgt[:, 1:2], in0=sgt0[:, j:j + 1], scalar1=float((1.0 - lam_init) * np.sqrt(128.0)),
                                              scalar2=None, op0=ALU.mult), reads=[sgt0], writes=[sgt])
        WQ = Rot([sc.sb("WQ%d" % k, [P, 8, 3, 128], BF16) for k in range(2)])
        WR = Rot([sc.sb("WR%d" % k, [P, 8, 2, 128], BF16) for k in range(2)])
        QT = Rot([[sc.sb("QA%d" % k, [P, T], BF16), sc.sb("QB%d" % k, [P, T], BF16)] for k in range(2)])
        for qpair in QT.tiles:
            for qt_ in qpair:
                S.op("pool", lambda e, qt_=qt_: e.memset(qt_[:], 0.0), writes=[qt_])
        KT = Rot([sc.sb("KT%d" % k, [P, T], BF16) for k in range(2)])
        VV = Rot([sc.sb("VV%d" % k, [P, T // 128, 128], BF16) for k in range(2)])
        pp = Rot([sc.ps("pp%d" % k) for k in range(3)])
        pO = [sc.ps("pO%d" % k) for k in range(2)]
        pL = [sc.ps("pL%d" % k) for k in range(2)]
        pR = sc.ps("pR")
        tmpa = Rot([sc.sb("tmpa%d" % k, [P, 512], F32) for k in range(4)])
        ET = Rot([sc.sb("ET%d" % k, [P, 512], BF16) for k in range(3)])
        ob = Rot([sc.sb("ob%d" % k, [P, 512], BF16) for k in range(2)])
        wq = self.wview(self.io["attn_w_qkv"][j])
        mixv = self.MIX
        qblocks = cfg.blocks if not last else cfg.lat_blocks
        Wb, Rb = WQ.tiles, WR.tiles

        def prep(h):
            W, R = Wb[h % 2], Rb[h % 2]
            for s3 in range(3):
                S.dma("pool", W, W[:, :, s3, :], self.t_in, wq[:, :, s3 * 1024 + h * 128: s3 * 1024 + (h + 1) * 128])
            for s2 in range(2):
                src = W[:, :, s2, :].rearrange("p k (b h j) -> p k b h j", b=4, h=2)
                dst = R[:, :, s2, :].rearrange("p k (b h j) -> p k b h j", b=4, h=2)
                for k in range(8):
                    S.op("dve", lambda e, src=src, dst=dst, k=k: e.tensor_scalar(
                        out=dst[:, k, :, 0, :], in0=src[:, k, :, 1, :], scalar1=-1.0, scalar2=None, op0=ALU.mult),
                        reads=[W], writes=[R])
                    S.op("dve", lambda e, src=src, dst=dst, k=k: e.tensor_copy(out=dst[:, k, :, 1, :], in_=src[:, k, :, 0, :]),
                         reads=[W], writes=[R])
        pending = [None, None]

        def flush_pending(stage):
            for st in range(stage + 1):
                if pending[st] is not None:
                    f = pending[st]
                    pending[st] = None
                    f()
        prep(0)
        for h in range(cfg.H):
            W, R = Wb[h % 2], Rb[h % 2]
            Q, K, V = QT.next(), KT.next(), VV.next()
            for blk in cfg.blocks:
                t0, n, is_ctx = blk
                for s2, dest in ((0, None), (1, K)):
                    p1 = pp.next()
                    for k in range(8):
                        S.op("pe", lambda e, k=k, p1=p1, s2=s2: e.matmul(p1[:, 0:n], W[:, k, s2, :], UT[:, k, t0:t0 + n],
                                                                         start=(k == 0), stop=(k == 7)),
                             reads=[W, UT], writes=[p1], sig=(k == 7))
                    if is_ctx:
                        if dest is None:
                            S.op("act", lambda e, p1=p1: e.copy(out=Q[0][0:64, t0:t0 + n], in_=p1[0:64, 0:n]),
                                 reads=[p1], writes=[Q[0]])
                            S.op("dve", lambda e, p1=p1: e.tensor_copy(out=Q[1][64:128, t0:t0 + n], in_=p1[64:128, 0:n]),
                                 reads=[p1], writes=[Q[1]])
                        else:
                            S.op("act", lambda e, p1=p1, dest=dest: e.copy(out=dest[:, t0:t0 + n], in_=p1[:, 0:n]),
                                 reads=[p1], writes=[dest])
                    else:
                        p2 = pp.next()
                        for k in range(8):
                            S.op("pe", lambda e, k=k, p2=p2, s2=s2: e.matmul(p2[:, 0:n], R[:, k, s2, :], UT[:, k, t0:t0 + n],
                                                                             start=(k == 0), stop=(k == 7)),
                                 reads=[R, UT], writes=[p2], sig=(k == 7))
                        a1, a2 = tmpa.next(), tmpa.next()
                        S.op("dve", lambda e, p1=p1, a1=a1: e.tensor_tensor(out=a1[:, 0:n], in0=p1[:, 0:n], in1=COS[:, t0:t0 + n], op=ALU.mult),
                             reads=[p1, COS], writes=[a1])
                        S.op("dve", lambda e, p2=p2, a2=a2: e.tensor_tensor(out=a2[:, 0:n], in0=p2[:, 0:n], in1=SIN[:, t0:t0 + n], op=ALU.mult),
                             reads=[p2, SIN], writes=[a2])
                        if dest is None:
                            for qi, (lo_, hi_) in enumerate(((0, 64), (64, 128))):
                                S.op("pool", lambda e, a1=a1, a2=a2, qi=qi, lo_=lo_, hi_=hi_: e.tensor_tensor(
                                    out=Q[qi][lo_:hi_, t0:t0 + n], in0=a1[lo_:hi_, 0:n], in1=a2[lo_:hi_, 0:n], op=ALU.add),
                                    reads=[a1, a2], writes=[Q[qi]])
                        else:
                            S.op("pool", lambda e, a1=a1, a2=a2, dest=dest: e.tensor_tensor(out=dest[:, t0:t0 + n], in0=a1[:, 0:n], in1=a2[:, 0:n], op=ALU.add),
                                 reads=[a1, a2], writes=[dest])
                for s in range(n // 128):
                    p1 = pp.next()
                    tt = t0 + s * 128
                    for k in range(8):
                        S.op("pe", lambda e, k=k, p1=p1, tt=tt: e.matmul(p1[:, 0:128], UT[:, k, tt:tt + 128], W[:, k, 2, :],
                                                                         start=(k == 0), stop=(k == 7)),
                             reads=[W, UT], writes=[p1], sig=(k == 7))
                    S.op("act", lambda e, p1=p1, tt=tt: e.copy(out=V[:, tt // 128, :], in_=p1[:, 0:128]), reads=[p1], writes=[V])
            if h + 1 < cfg.H:
                prep(h + 1)
            for qb in qblocks:
                q0, nq, q_ctx = qb
                kchunks = list(range(SEQ // 128, T // 128)) if q_ctx else list(range(T // 128))
                nk = len(kchunks)
                its = [(m, ki, kc) for m in range(2) for ki, kc in enumerate(kchunks)]
                LA = 2
                pss = {}
                r1, a1, r2, a2 = tmpa.next(), tmpa.next(), tmpa.next(), tmpa.next()

                def emit_scores(idx):
                    m, ki, kc = its[idx]
                    ps = pp.next()
                    S.op("pe", lambda e, ps=ps, kc=kc, m=m: e.matmul(
                        ps[:, 0:nq], K[:, kc * 128:(kc + 1) * 128], Q[m][:, q0:q0 + nq], start=True, stop=True),
                        reads=[K, Q[m]], writes=[ps])
                    pss[idx] = ps
                for idx in range(min(LA, len(its))):
                    emit_scores(idx)
                for idx in range(len(its)):
                    if idx + LA < len(its):
                        emit_scores(idx + LA)
                    m, ki, kc = its[idx]
                    ps = pss.pop(idx)
                    et = ET.next()
                    S.op("act", lambda e, ps=ps, et=et: e.activation(out=et[:, 0:nq], in_=ps[:, 0:nq], func=AF.Exp, scale=0.125),
                         reads=[ps], writes=[et])
                    fst, lst = ki == 0, ki == nk - 1
                    S.op("pe", lambda e, et=et, kc=kc, m=m, fst=fst, lst=lst: e.matmul(
                        pO[m][:, 0:nq], V[:, kc, :], et[:, 0:nq], start=fst, stop=lst),
                        reads=[V, et], writes=[pO[m]], sig=False)
                    S.op("pe", lambda e, et=et, m=m, fst=fst, lst=lst: e.matmul(
                        pL[m][:, 0:nq], self.onesbf[:], et[:, 0:nq], start=fst, stop=lst),
                        reads=[self.onesbf, et], writes=[pL[m]], sig=True)
                    if idx == min(20, nk - 1):
                        flush_pending(0)
                    if idx == min(23, nk - 1):
                        flush_pending(1)
                    if idx == nk - 1:
                        S.op("dve", lambda e, r1=r1: e.reciprocal(out=r1[:, 0:nq], in_=pL[0][:, 0:nq]), reads=[pL[0]], writes=[r1])
                        S.op("dve", lambda e, r1=r1, a1=a1: e.tensor_tensor(out=a1[:, 0:nq], in0=pO[0][:, 0:nq], in1=r1[:, 0:nq], op=ALU.mult),
                             reads=[pO[0], r1], writes=[a1])
                S.op("dve", lambda e, r2=r2: e.reciprocal(out=r2[:, 0:nq], in_=pL[1][:, 0:nq]), reads=[pL[1]], writes=[r2])
                S.op("dve", lambda e, r2=r2, a2=a2: e.tensor_tensor(out=a2[:, 0:nq], in0=pO[1][:, 0:nq], in1=r2[:, 0:nq], op=ALU.mult),
                     reads=[pO[1], r2], writes=[a2])
                S.op("dve", lambda e, a1=a1, a2=a2: e.scalar_tensor_tensor(out=a1[:, 0:nq], in0=a2[:, 0:nq], scalar=lsc[:, 4:5], in1=a1[:, 0:nq],
                                                                          op0=ALU.mult, op1=ALU.add), reads=[a1, a2, lsc], writes=[a1])
                S.op("pool", lambda e, a1=a1, r1=r1: e.tensor_tensor(out=r1[:, 0:nq], in0=a1[:, 0:nq], in1=a1[:, 0:nq], op=ALU.mult),
                     reads=[a1], writes=[r1])

                def tail1(r1=r1, nq=nq):
                    S.op("pe", lambda e: e.matmul(pR[:, 0:nq], self.ones32[:], r1[:, 0:nq], start=True, stop=True),
                         reads=[self.ones32, r1], writes=[pR])

                def tail2(a1=a1, r2=r2, q0=q0, nq=nq, h=h):
                    S.op("act", lambda e: e.activation(out=r2[:, 0:nq], in_=pR[:, 0:nq], func=AF.Ln, bias=self.cst[:, 1:2]),
                         reads=[pR, self.cst], writes=[r2])
                    S.op("act", lambda e: e.activation(out=r2[:, 0:nq], in_=r2[:, 0:nq], func=AF.Exp, scale=-0.5),
                         reads=[r2], writes=[r2])
                    o = ob.next()
                    S.op("dve", lambda e: e.scalar_tensor_tensor(out=o[:, 0:nq], in0=a1[:, 0:nq], scalar=sgt[:, 1:2], in1=r2[:, 0:nq],
                                                                 op0=ALU.mult, op1=ALU.mult), reads=[a1, sgt, r2], writes=[o])
                    S.dma("sp", self.t_MIX, mixv[h * 128:(h + 1) * 128, q0:q0 + nq], o, o[:, 0:nq])
                pending[0], pending[1] = tail1, tail2
            flush_pending(1)
        sc.close()

    def mixer_fnet(self, i, j, last):
        S, cfg = self.S, self.cfg
        parts = [(0, cfg.SEQ, cfg.lat_blocks, self.io["dftL"], 0)]
        if not last:
            parts.append((cfg.SEQ, cfg.CTX, cfg.ctx_blocks, self.io["dftC"], 1))
        for (off, N, blks, dft, col) in parts:
            sc = Scope(S)
            NCH = N // 128
            ACS = sc.sb("ACS", [P, NCH, 8, 256], BF16)
            cs128 = sc.sb("cs128", [P, 256], BF16)
            S.dma("sp", cs128, cs128[:], self.t_in, self.io["dft128"][:, :])
            sc1 = Scope(S)
            xb = Rot([sc1.sb("xb%d" % k, [P, 8, 512], F32) for k in range(2)])
            ub = Rot([sc1.sb("ub%d" % k, [P, 8, 512], BF16) for k in range(2)])
            pp = Rot([sc1.ps("pp%d" % k) for k in range(4)])
            for blk in blks:
                t0, n, is_ctx = blk
                x = xb.next()
                u = ub.next()
                st, sap = self.xsrc(i, blk)
                S.dma("sp", x, x[:, :, 0:n], st, sap)
                self.modulate([u], x, lambda c, u=u, n=n: u[:, c, 0:n], i, n, col, 0, 1)
                for g in range(8):
                    for s in range(n // 128):
                        p1 = pp.next()
                        nch = (t0 - off) // 128 + s
                        S.op("pe", lambda e, p1=p1, u=u, g=g, s=s: e.matmul(p1[:, 0:256], u[:, g, s * 128:(s + 1) * 128], cs128[:],
                                                                            start=True, stop=True), reads=[u, cs128], writes=[p1])
                        eng = "act" if (g + s) % 2 == 0 else "dve"
                        if eng == "act":
                            S.op("act", lambda e, p1=p1, nch=nch, g=g: e.copy(out=ACS[:, nch, g, :], in_=p1[:, 0:256]), reads=[p1], writes=[ACS])
                        else:
                            S.op("dve", lambda e, p1=p1, nch=nch, g=g: e.tensor_copy(out=ACS[:, nch, g, :], in_=p1[:, 0:256]), reads=[p1], writes=[ACS])
            sc1.close()
            sc2 = Scope(S)
            acc = [sc2.ps("acc%d" % g) for g in range(8)]
            CP = Rot([sc2.sb("CP%d" % k, [P, 2, 512], BF16) for k in range(4)])
            ob = Rot([sc2.sb("ob%d" % k, [P, 512], BF16) for k in range(4)])
            scale = float(1.0 / np.sqrt(N * 128.0))
            kbw = min(512, N)
            for kb in range(N // kbw):
                for nchk in range(NCH):
                    cp = CP.next()
                    for s2 in range(2):
                        S.dma("sp", cp, cp[:, s2, 0:kbw], self.t_in, dft[s2, nchk * 128:(nchk + 1) * 128, kb * kbw:(kb + 1) * kbw])
                    for g in range(8):
                        for s2 in range(2):
                            S.op("pe", lambda e, g=g, s2=s2, cp=cp, nchk=nchk: e.matmul(
                                acc[g][:, 0:kbw], ACS[:, nchk, g, s2 * 128:(s2 + 1) * 128], cp[:, s2, 0:kbw],
                                start=(nchk == 0 and s2 == 0), stop=(nchk == NCH - 1 and s2 == 1)),
                                reads=[ACS, cp], writes=[acc[g]], sig=(s2 == 1 and (g == 7 or nchk == NCH - 1)))
                for g in range(8):
                    o = ob.next()
                    if g % 2 == 0:
                        S.op("act", lambda e, o=o, g=g: e.activation(out=o[:, 0:kbw], in_=acc[g][:, 0:kbw], func=AF.Copy, scale=scale),
                             reads=[acc[g]], writes=[o])
                    else:
                        S.op("dve", lambda e, o=o, g=g: e.tensor_scalar(out=o[:, 0:kbw], in0=acc[g][:, 0:kbw], scalar1=scale, scalar2=None, op0=ALU.mult),
                             reads=[acc[g]], writes=[o])
                    S.dma("sp", self.t_MIX, self.MIX[g * 128:(g + 1) * 128, off + kb * kbw: off + (kb + 1) * kbw], o, o[:, 0:kbw])
            sc2.close()
            sc.close()

    def mixer_conv(self, i, j, last):
        S, cfg = self.S, self.cfg
        SEQ, CTX = cfg.SEQ, cfg.CTX
        sc = Scope(S)
        HW = 15 + SEQ + 30 + CTX + 15
        HG = sc.sb("HG", [P, 8, HW], BF16)
        lat_off, ctx_off = 15, 15 + SEQ + 30
        for (a, b) in ((0, 15), (15 + SEQ, 15 + SEQ + 30), (HW - 15, HW)):
            S.op("pool", lambda e, a=a, b=b: e.memset(HG[:, :, a:b], 0.0), writes=[HG])
        W1 = sc.sb("W1", [P, 8, 2048], BF16)
        w1v = self.wview(self.io["conv_w_pw1"][j])
        for q in range(4):
            S.dma("pool", W1, W1[:, :, q * 512:(q + 1) * 512], self.t_in, w1v[:, :, q * 512:(q + 1) * 512])
        b1 = sc.sb("b1", [P, 16], F32)
        S.dma("sp", b1, b1[:], self.t_in, self.io["conv_b_pw1T"][:, j * 16:(j + 1) * 16])
        wdw = sc.sb("wdw", [P, 8, 31], F32)
        S.dma("sp", wdw, wdw[:].rearrange("p a b -> p (a b)"), self.t_in, self.io["conv_w_dwT"][:, j * 248:(j + 1) * 248])
        cp = sc.sb("cp", [P, 4, 8], F32)
        S.dma("sp", cp, cp[:].rearrange("p a b -> p (a b)"), self.t_in, self.io["conv_pT"][:, j * 32:(j + 1) * 32])
        blks = cfg.blocks if not last else cfg.lat_blocks

        def hoff(blk):
            t0, n, is_ctx = blk
            return (ctx_off + t0 - SEQ) if is_ctx else (lat_off + t0)
        sc1 = Scope(S)
        xb = Rot([sc1.sb("xb%d" % k, [P, 8, 512], F32) for k in range(2)])
        ub = Rot([sc1.sb("ub%d" % k, [P, 8, 512], BF16) for k in range(2)])
        pp = Rot([sc1.ps("pp%d" % k) for k in range(6)])
        ta = Rot([sc1.sb("ta%d" % k, [P, 512], F32) for k in range(3)])
        tg = Rot([sc1.sb("tg%d" % k, [P, 512], F32) for k in range(3)])
        for blk in blks:
            t0, n, is_ctx = blk
            x, u = xb.next(), ub.next()
            st, sap = self.xsrc(i, blk)
            S.dma("sp", x, x[:, :, 0:n], st, sap)
            self.modulate([u], x, lambda c, u=u, n=n: u[:, c, 0:n], i, n, 1 if is_ctx else 0, 0, 1)
            ho = hoff(blk)
            for jj in range(8):
                pa, pg = pp.next(), pp.next()
                for (pt, cb) in ((pa, jj * 128), (pg, 1024 + jj * 128)):
                    for k in range(8):
                        S.op("pe", lambda e, pt=pt, cb=cb, k=k, u=u: e.matmul(pt[:, 0:n], W1[:, k, cb:cb + 128], u[:, k, 0:n],
                                                                             start=(k == 0), stop=(k == 7)),
                             reads=[W1, u], writes=[pt], sig=(k == 7))
                a, g = ta.next(), tg.next()
                S.op("dve", lambda e, pa=pa, a=a, jj=jj: e.tensor_scalar(out=a[:, 0:n], in0=pa[:, 0:n], scalar1=b1[:, jj:jj + 1], scalar2=None, op0=ALU.add),
                     reads=[pa, b1], writes=[a])
                S.op("act", lambda e, pg=pg, g=g, jj=jj: e.activation(out=g[:, 0:n], in_=pg[:, 0:n], func=AF.Sigmoid, bias=b1[:, 8 + jj:9 + jj]),
                     reads=[pg, b1], writes=[g])
                S.op("pool", lambda e, a=a, g=g, jj=jj, ho=ho: e.tensor_tensor(out=HG[:, jj, ho:ho + n], in0=a[:, 0:n], in1=g[:, 0:n], op=ALU.mult),
                     reads=[a, g], writes=[HG])
        sc1.close()
        sc2 = Scope(S)
        zb = Rot([sc2.sb("zb%d" % k, [P, 8, 512], F32) for k in range(2)])
        accB = Rot([sc2.sb("accB%d" % k, [P, 512], F32) for k in range(2)])
        tmp = dict(sq=sc2.sb("sq", [P, 8, 512], F32), pss=sc2.ps("pss"), psq=sc2.ps("psq"),
                   mean=sc2.sb("mean", [P, 512], F32), rstd=sc2.sb("rstd", [P, 512], F32), nb=sc2.sb("nb", [P, 512], F32),
                   t=Rot([sc2.sb("lt%d" % k, [P, 512], F32) for k in range(3)]))
        mb = Rot([sc2.sb("mb%d" % k, [P, 8, 512], BF16) for k in range(2)])
        for blk in blks:
            t0, n, is_ctx = blk
            ho = hoff(blk)
            z = zb.next()
            for c in range(8):
                ab = accB.next()
                for tap in range(31):
                    src_off = ho + tap - 15
                    if tap == 0:
                        S.op("dve", lambda e, c=c, src_off=src_off: e.tensor_scalar(
                            out=z[:, c, 0:n], in0=HG[:, c, src_off:src_off + n], scalar1=wdw[:, c, 0:1], scalar2=cp[:, 0, c:c + 1],
                            op0=ALU.mult, op1=ALU.add), reads=[HG, wdw, cp], writes=[z])
                    else:
                        S.op("dve", lambda e, c=c, src_off=src_off, tap=tap: e.scalar_tensor_tensor(
                            out=z[:, c, 0:n], in0=HG[:, c, src_off:src_off + n], scalar=wdw[:, c, tap:tap + 1], in1=z[:, c, 0:n],
                            op0=ALU.mult, op1=ALU.add), reads=[HG, wdw, z], writes=[z])
            m = mb.next()
            self.layer_norm(sc2, z, n, lambda c: cp[:, 1, c:c + 1], lambda c: cp[:, 2, c:c + 1],
                            [(m, lambda c, m=m, n=n: m[:, c, 0:n], AF.Silu)], tmp, greads=[cp])
            S.dma("sp", self.t_MIX, self.MIX.rearrange("(k p) t -> p k t", p=P)[:, :, t0:t0 + n], m, m[:, :, 0:n])
        sc2.close()
        sc.close()

    def post_moe(self, i, kind, j, last):
        S, cfg = self.S, self.cfg
        E = cfg.E
        blks = cfg.blocks if not last else cfg.lat_blocks
        sbs, cur, tot = [], [], 0
        for b in blks:
            if b[2] and cur and tot + b[1] <= cfg.SB_MAX + 256:
                cur.append(b)
                tot += b[1]
                continue
            if tot + b[1] > cfg.SB_MAX:
                sbs.append(cur)
                cur, tot = [], 0
            cur.append(b)
            tot += b[1]
        if cur:
            sbs.append(cur)
        if kind == 0:
            wo_ap, bo_ap = self.io["attn_w_o"][j], None
        elif kind == 1:
            wo_ap, bo_ap = self.io["fnet_w"][j], self.io["fnet_bT"][:, j * 8:(j + 1) * 8]
        else:
            wo_ap, bo_ap = self.io["conv_w_pw2"][j], self.io["conv_pT"][:, j * 32 + 24:j * 32 + 32]
        L = Scope(S)
        WO = L.sb("WO", [P, 8, 1024], BF16)
        wov = self.wview(wo_ap)
        for q in range(2):
            S.dma("pool", WO, WO[:, :, q * 512:(q + 1) * 512], self.t_in, wov[:, :, q * 512:(q + 1) * 512])
        bo = L.sb("bo", [P, 8], F32)
        if bo_ap is None:
            S.op("dve", lambda e: e.memset(bo[:], 0.0), writes=[bo])
        else:
            S.dma("sp", bo, bo[:], self.t_in, bo_ap)
        for col in range(2):
            S.op("dve", lambda e, col=col: e.tensor_tensor(out=self.GB1[:, i, :, col], in0=self.MOD[:, i, 16:24, col], in1=bo[:], op=ALU.mult),
                 reads=[self.MOD, bo], writes=[self.GB1])
        WR = L.sb("WR", [P, 8, E], F32)
        S.dma("sp", WR, WR[:], self.t_in, self.wview(self.io["moe_w_router"][i]))
        BR = L.sb("BR", [P, E], F32)
        S.dma("sp", BR, BR[:], self.t_in, self.io["moe_b_router"][i].partition_broadcast(P))
        B1 = L.sb("B1", [P, E, 16], F32)
        S.dma("sp", B1, B1[:].rearrange("p a b -> p (a b)"), self.t_in, self.io["moe_b1T"][:, i * E * 16:(i + 1) * E * 16])
        S.op("dve", lambda e: e.tensor_scalar(out=B1[:, :, 8:16], in0=B1[:, :, 8:16], scalar1=1.0, scalar2=None, op0=ALU.add),
             reads=[B1], writes=[B1])
        B2 = L.sb("B2", [E, 1024], F32)
        S.dma("sp", B2, B2[:], self.t_in, self.io["moe_b2"][i])
        xxv = self.XX.rearrange("(k p) t -> p k t", p=P)
        x1v = self.X1.rearrange("(k p) t -> p k t", p=P)
        mixv = self.MIX.rearrange("(k p) t -> p k t", p=P)
        outv = self.yT.rearrange("(k p) t -> p k t", p=P)
        for sb in sbs:
            Tb = sum(b[1] for b in sb)
            offs = []
            o = 0
            for b in sb:
                offs.append(o)
                o += b[1]
            sbt0 = sb[0][0]
            M = Scope(S)
            VT = M.sb("VT", [P, 8, Tb], BF16)
            A = Scope(S)
            xb = Rot([A.sb("xb%d" % k, [P, 8, 512], F32) for k in range(2)])
            mb = Rot([A.sb("mb%d" % k, [P, 8, 512], BF16) for k in range(1)])
            zbr = Rot([A.sb("zb%d" % k, [P, 8, 512], F32) for k in range(2)])
            x1b = Rot([A.sb("x1b%d" % k, [P, 8, 512], F32) for k in range(1)])
            v32 = A.sb("v32", [P, 8, 512], F32)
            pssA, psqA = A.ps("pss"), A.ps("psq")
            ltA = Rot([A.sb("lt%d" % k, [P, 512], F32) for k in range(3)])
            tmps = Rot([dict(sq=A.sb("sq%d" % k, [P, 8, 512], F32), pss=pssA, psq=psqA,
                             mean=A.sb("mean%d" % k, [P, 512], F32), rstd=A.sb("rstd%d" % k, [P, 512], F32),
                             nb=A.sb("nb%d" % k, [P, 512], F32), t=ltA) for k in range(2)])
            pp = Rot([A.ps("pp%d" % k) for k in range(3)])
            pl = Rot([A.ps("pl%d" % k) for k in range(2)])
            pt = A.ps("ptr")
            lg = Rot([A.sb("lg%d" % k, [P, E], F32) for k in range(2)])
            mx = Rot([A.sb("mx%d" % k, [P, 8], F32) for k in range(2)])
            gm = Rot([A.sb("gm%d" % k, [P, E], F32) for k in range(2)])
            ge = Rot([A.sb("ge%d" % k, [P, E], F32) for k in range(2)])
            gs = Rot([A.sb("gs%d" % k, [P, 2], F32) for k in range(2)])
            gt = Rot([A.sb("gt%d" % k, [E, 128], F32) for k in range(2)])
            for bi, blk in enumerate(sb):
                t0, n, is_ctx = blk
                col = 1 if is_ctx else 0
                x, m = xb.next(), mb.next()
                zb, tmp = zbr.next(), tmps.next()
                st, sap = self.xsrc(i, blk)
                S.dma("sp", x, x[:, :, 0:n], st, sap)
                S.dma("sp", m, m[:, :, 0:n], self.t_MIX, mixv[:, :, t0:t0 + n])
                for c in range(8):
                    S.op("act", lambda e, c=c, x=x: e.activation(out=x[:, c, 0:n], in_=x[:, c, 0:n], func=AF.Identity,
                                                                 scale=cfg.alpha, bias=self.GB1[:, i, c, col:col + 1]),
                         reads=[x, self.GB1], writes=[x])
                for dch in range(8):
                    py = pp.next()
                    for k in range(8):
                        S.op("pe", lambda e, py=py, k=k, dch=dch, m=m: e.matmul(py[:, 0:n], WO[:, k, dch * 128:(dch + 1) * 128], m[:, k, 0:n],
                                                                               start=(k == 0), stop=(k == 7)),
                             reads=[WO, m], writes=[py], sig=(k == 7))
                    S.op("dve", lambda e, py=py, dch=dch, x=x: e.scalar_tensor_tensor(
                        out=zb[:, dch, 0:n], in0=py[:, 0:n], scalar=self.MOD[:, i, 16 + dch, col:col + 1], in1=x[:, dch, 0:n],
                        op0=ALU.mult, op1=ALU.add), reads=[py, self.MOD, x], writes=[zb])
                x1 = x1b.next()
                self.layer_norm(A, zb, n, lambda c: self.LNP[:, i, 0, c:c + 1], lambda c: self.LNP[:, i, 1, c:c + 1],
                                [(x1, lambda c, x1=x1, n=n: x1[:, c, 0:n], AF.Identity)], tmp)
                S.dma("sp", self.t_X1, x1v[:, :, t0:t0 + n], x1, x1[:, :, 0:n])
                for c in range(8):
                    S.op("act", lambda e, c=c, x1=x1: e.activation(out=v32[:, c, 0:n], in_=x1[:, c, 0:n], func=AF.Identity,
                                                                   scale=self.MOD[:, i, 32 + c, col:col + 1], bias=self.MOD[:, i, 24 + c, col:col + 1]),
                         reads=[x1, self.MOD], writes=[v32])
                    eng = "dve" if c % 2 == 0 else "pool"
                    S.op(eng, lambda e, c=c, o=offs[bi]: e.tensor_copy(out=VT[:, c, o:o + n], in_=v32[:, c, 0:n]), reads=[v32], writes=[VT])
                for s in range(n // 128):
                    p1 = pl.next()
                    for k in range(8):
                        S.op("pe", lambda e, p1=p1, k=k, s=s: e.matmul(p1[:, 0:E], v32[:, k, s * 128:(s + 1) * 128], WR[:, k, :],
                                                                       start=(k == 0), stop=(k == 7)),
                             reads=[v32, WR], writes=[p1], sig=(k == 7))
                    l, mxx, gmm, gee, gss, gtt = lg.next(), mx.next(), gm.next(), ge.next(), gs.next(), gt.next()
                    S.op("dve", lambda e, l=l, p1=p1: e.tensor_tensor(out=l[:], in0=p1[:, 0:E], in1=BR[:], op=ALU.add), reads=[p1, BR], writes=[l])
                    S.op("dve", lambda e, l=l, mxx=mxx: e.max(out=mxx[:], in_=l[:]), reads=[l], writes=[mxx])
                    S.op("dve", lambda e, l=l, mxx=mxx, gmm=gmm: e.tensor_scalar(out=gmm[:], in0=l[:], scalar1=mxx[:, 3:4], scalar2=None, op0=ALU.is_ge),
                         reads=[l, mxx], writes=[gmm])
                    S.op("dve", lambda e, mxx=mxx, gss=gss: e.tensor_scalar(out=gss[:, 0:1], in0=mxx[:, 0:1], scalar1=-1.0, scalar2=None, op0=ALU.mult),
                         reads=[mxx], writes=[gss])
                    S.op("act", lambda e, l=l, gee=gee, gss=gss: e.activation(out=gee[:], in_=l[:], func=AF.Exp, bias=gss[:, 0:1]),
                         reads=[l, gss], writes=[gee])
                    S.op("dve", lambda e, gee=gee, gmm=gmm: e.tensor_tensor(out=gee[:], in0=gee[:], in1=gmm[:], op=ALU.mult), reads=[gee, gmm], writes=[gee])
                    S.op("dve", lambda e, gee=gee, gss=gss: e.reduce_sum(out=gss[:, 1:2], in_=gee[:], axis=AX.X), reads=[gee], writes=[gss])
                    S.op("dve", lambda e, gss=gss: e.reciprocal(out=gss[:, 1:2], in_=gss[:, 1:2]), reads=[gss], writes=[gss])
                    S.op("dve", lambda e, gee=gee, gss=gss: e.tensor_scalar(out=gee[:], in0=gee[:], scalar1=gss[:, 1:2], scalar2=None, op0=ALU.mult),
                         reads=[gee, gss], writes=[gee])
                    S.op("pe", lambda e, gee=gee: e.transpose(pt[0:E, 0:128], gee[:], self.ident[:]), reads=[gee, self.ident], writes=[pt])
                    S.op("act", lambda e, gtt=gtt: e.copy(out=gtt[:], in_=pt[0:E, 0:128]), reads=[pt], writes=[gtt])
                    tt = t0 + s * 128
                    S.dma("sp", self.t_GD, self.GD[:, tt:tt + 128], gtt, gtt[:])
            A.close()
            Mf = Scope(S)
            Fa = Mf.sb("Fa", [P, 8, Tb], F32)
            B = Scope(S)
            HH = [[B.sb("Hh%d_%d" % (k, bi), [P, 8, blk[1]], BF16) for bi, blk in enumerate(sb)] for k in range(2)]
            GBt = Rot([B.sb("GBt%d" % k, [P, Tb], F32) for k in range(2)])
            GTs = B.sb("GTs", [E, Tb], F32)
            S.dma("sp", GTs, GTs[:], self.t_GD, self.GD[:, sbt0:sbt0 + Tb])
            NW1 = 4
            w1bufs = [B.sb("w1r%d" % k, [P, 8, 2, 256], BF16) for k in range(NW1)]
            w2bufs = [B.sb("w2r%d" % k, [P, 8, 256], BF16) for k in range(4)]
            pg = Rot([B.ps("pg%d" % k) for k in range(3)])
            plin = Rot([B.ps("plin%d" % k) for k in range(3)])
            py2 = Rot([B.ps("py%d" % k) for k in range(2)])
            ta = Rot([B.sb("ta%d" % k, [P, 512], F32) for k in range(3)])
            ts_ = Rot([B.sb("ts%d" % k, [P, 512], F32) for k in range(3)])
            tl = Rot([B.sb("tl%d" % k, [P, 512], F32) for k in range(3)])
            nld = [0]

            def load_w1(q):
                while nld[0] <= q and nld[0] < E * 4:
                    qq = nld[0]
                    ex_, j4_ = qq // 4, qq % 4
                    w1 = w1bufs[qq % NW1]
                    w1v_ = self.wview(self.io["moe_w1"][i, ex_])
                    for s2 in range(2):
                        S.dma("pool", w1, w1[:, :, s2, :], self.t_in, w1v_[:, :, s2 * 1024 + j4_ * 256: s2 * 1024 + (j4_ + 1) * 256])
                    nld[0] += 1

            def load_w2(ex, d4):
                w2 = w2bufs[d4]
                w2v_ = self.wview(self.io["moe_w2"][i, ex])
                S.dma("pool", w2, w2[:], self.t_in, w2v_[:, :, d4 * 256:(d4 + 1) * 256])
            late = [None]

            def flush_late():
                if late[0] is not None:
                    f = late[0]
                    late[0] = None
                    f()

            def w1_unit(ex, w1, gb, jj, jc, n, o, bi):
                Hh = HH[ex % 2][bi]
                pG, pLn = pg.next(), plin.next()
                for (ptile, s2) in ((pG, 0), (pLn, 1)):
                    for k in range(8):
                        S.op("pe", lambda e, ptile=ptile, s2=s2, k=k: e.matmul(
                            ptile[:, 0:n], w1[:, k, s2, jj * 128:(jj + 1) * 128], VT[:, k, o:o + n], start=(k == 0), stop=(k == 7)),
                            reads=[w1, VT], writes=[ptile], sig=(k == 7))
                a, sg, l = ta.next(), ts_.next(), tl.next()
                S.op("dve", lambda e: e.tensor_scalar(
                    out=a[:, 0:n], in0=pG[:, 0:n], scalar1=B1[:, ex, jc:jc + 1], scalar2=7.0, op0=ALU.add, op1=ALU.min),
                    reads=[pG, B1], writes=[a])
                S.op("act", lambda e: e.activation(out=sg[:, 0:n], in_=a[:, 0:n], func=AF.Sigmoid, scale=1.702),
                     reads=[a], writes=[sg])
                S.op("act", lambda e: e.activation(out=l[:, 0:n], in_=pLn[:, 0:n], func=AF.Identity, bias=B1[:, ex, 8 + jc:9 + jc]),
                     reads=[pLn, B1], writes=[l])
                flush_late()

                def second():
                    S.op("dve", lambda e: e.tensor_scalar(out=l[:, 0:n], in0=l[:, 0:n], scalar1=-6.0, scalar2=8.0,
                                                          op0=ALU.max, op1=ALU.min), reads=[l], writes=[l])
                    S.op("dve", lambda e: e.tensor_tensor(out=sg[:, 0:n], in0=a[:, 0:n], in1=sg[:, 0:n], op=ALU.mult),
                         reads=[a, sg], writes=[sg])
                    S.op("pool", lambda e: e.tensor_tensor(out=sg[:, 0:n], in0=sg[:, 0:n], in1=l[:, 0:n], op=ALU.mult),
                         reads=[sg, l], writes=[sg])
                    S.op("pool", lambda e: e.tensor_tensor(out=Hh[:, jc, 0:n], in0=sg[:, 0:n], in1=gb[:, o:o + n], op=ALU.mult),
                         reads=[sg, gb], writes=[Hh])
                late[0] = second

            def w2_group(ex, w2, dd, dch, n, o, bi):
                Hh = HH[ex % 2][bi]
                py = py2.next()
                if ex == 0:
                    S.op("pe", lambda e: e.matmul(py[:, 0:n], B2[:, dch * 128:(dch + 1) * 128], GTs[:, o:o + n], start=True, stop=False),
                         reads=[B2, GTs], writes=[py], sig=False)
                for f in range(8):
                    S.op("pe", lambda e, f=f: e.matmul(
                        py[:, 0:n], w2[:, f, dd * 128:(dd + 1) * 128], Hh[:, f, 0:n], start=(f == 0 and ex != 0), stop=(f == 7)),
                        reads=[w2, Hh], writes=[py], sig=(f == 7))
                if ex == 0:
                    S.op("act", lambda e: e.copy(out=Fa[:, dch, o:o + n], in_=py[:, 0:n]), reads=[py], writes=[Fa])
                else:
                    S.op("dve", lambda e: e.tensor_tensor(out=Fa[:, dch, o:o + n], in0=Fa[:, dch, o:o + n], in1=py[:, 0:n], op=ALU.add),
                         reads=[py, Fa], writes=[Fa])

            load_w1(1)
            w2q = []
            LAG = min(2, len(sb))
            for st in range(E + 1):
                gb = None
                if st < E:
                    gb = GBt.next()
                    S.dma("sp", gb, gb[:], self.t_GD, self.GD[st, sbt0:sbt0 + Tb].partition_broadcast(P))
                for j4 in range(4):
                    if st < E:
                        load_w1(st * 4 + j4 + 2)
                    if j4 < 3:
                        if st >= 1:
                            load_w2(st - 1, j4 + 1)
                    elif st < E:
                        load_w2(st, 0)
                    w1 = w1bufs[(st * 4 + j4) % NW1]
                    w2 = w2bufs[j4]
                    for jj in range(2):
                        jc = j4 * 2 + jj
                        for bi, blk in enumerate(sb):
                            n, o = blk[1], offs[bi]
                            if st < E:
                                w1_unit(st, w1, gb, jj, jc, n, o, bi)
                            if st >= 1:
                                if st == E:
                                    flush_late()
                                w2q.append(lambda st=st, w2=w2, jj=jj, jc=jc, n=n, o=o, bi=bi: w2_group(st - 1, w2, jj, jc, n, o, bi))
                                while len(w2q) > LAG:
                                    w2q.pop(0)()
            flush_late()
            while w2q:
                w2q.pop(0)()
            B.close()
            C = Scope(S)
            xb = Rot([C.sb("xb%d" % k, [P, 8, 512], F32) for k in range(2)])
            zb = C.sb("zb", [P, 8, 512], F32)
            ob = Rot([C.sb("ob%d" % k, [P, 8, 512], F32) for k in range(2)])
            tmp = dict(sq=C.sb("sq", [P, 8, 512], F32), pss=C.ps("pss"), psq=C.ps("psq"),
                       mean=C.sb("mean", [P, 512], F32), rstd=C.sb("rstd", [P, 512], F32), nb=C.sb("nb", [P, 512], F32),
                       t=Rot([C.sb("lt%d" % k, [P, 512], F32) for k in range(3)]))
            for bi, blk in enumerate(sb):
                t0, n, is_ctx = blk
                col = 1 if is_ctx else 0
                o = offs[bi]
                x = xb.next()
                S.dma("sp", x, x[:, :, 0:n], self.t_X1, x1v[:, :, t0:t0 + n])
                for c in range(8):
                    S.op("act", lambda e, c=c, x=x: e.activation(out=x[:, c, 0:n], in_=x[:, c, 0:n], func=AF.Copy, scale=cfg.alpha),
                         reads=[x], writes=[x])
                    S.op("dve", lambda e, c=c, x=x, o=o: e.scalar_tensor_tensor(
                        out=zb[:, c, 0:n], in0=Fa[:, c, o:o + n], scalar=self.MOD[:, i, 40 + c, col:col + 1], in1=x[:, c, 0:n],
                        op0=ALU.mult, op1=ALU.add), reads=[Fa, self.MOD, x], writes=[zb])
                ot = ob.next()
                self.layer_norm(C, zb, n, lambda c: self.LNP[:, i, 2, c:c + 1], lambda c: self.LNP[:, i, 3, c:c + 1],
                                [(ot, lambda c, ot=ot, n=n: ot[:, c, 0:n], AF.Identity)], tmp)
                if last:
                    S.dma("sp", self.t_out, outv[:, :, t0:t0 + n], ot, ot[:, :, 0:n])
                else:
                    S.dma("sp", self.t_XX, xxv[:, :, t0:t0 + n], ot, ot[:, :, 0:n])
            C.close()
            Mf.close()
            M.close()
        L.close()


def pmajor(v, nch):
    v = np.asarray(v, np.float32)
    lead = v.shape[:-1]
    a = v.reshape(lead + (nch, P))
    a = np.moveaxis(a, -1, 0)
    return np.ascontiguousarray(a)


def host_constants(cfg):
    SEQ, CTX = cfg.SEQ, cfg.CTX
    GRID_W = 64
    t = np.arange(SEQ)
    rows = (t // GRID_W).astype(np.float64)
    cols = (t % GRID_W).astype(np.float64)
    inv_freq = (10000.0 ** (-np.arange(16, dtype=np.float64) / 16)).astype(np.float32).astype(np.float64)
    dd = np.arange(64)
    f = dd % 16
    pos = np.where(dd[:, None] < 32, rows[None, :], cols[None, :])
    ang = (pos.astype(np.float32) * inv_freq[f][:, None].astype(np.float32)).astype(np.float64)
    C = np.cos(ang).astype(np.float32)
    Sn = np.sin(ang).astype(np.float32)
    ropeC = np.concatenate([C, C], 0)
    ropeS = np.concatenate([Sn, Sn], 0)

    def dft(N):
        n = np.arange(N, dtype=np.int64)
        m = (n[:, None] * n[None, :]) % N
        a = 2.0 * np.pi * m.astype(np.float64) / N
        return np.stack([np.cos(a), -np.sin(a)]).astype(np.float32).astype(ml_dtypes.bfloat16)
    a128 = 2.0 * np.pi * ((np.arange(128)[:, None] * np.arange(128)[None, :]) % 128) / 128.0
    dft128 = np.concatenate([np.cos(a128), np.sin(a128)], 1).astype(np.float32).astype(ml_dtypes.bfloat16)
    return dict(ropeC=ropeC, ropeS=ropeS, dftL=dft(SEQ), dftC=dft(CTX), dft128=dft128, ident=np.eye(P, dtype=np.float32))


def host_inputs(cfg, inp, consts, b):
    f = lambda a: np.ascontiguousarray(np.asarray(a, np.float32))
    DEPTH, E = cfg.DEPTH, cfg.E
    m = {}
    m["xT"] = np.ascontiguousarray(np.asarray(inp["x"][b], np.float32).T)
    m["ctxT"] = np.ascontiguousarray(np.asarray(inp["ctx"][b], np.float32).T)
    m["cT"] = pmajor(inp["c"][b], 8)
    m["cctxT"] = pmajor(inp["c_ctx"], 8)
    m["w_mod"] = f(inp["w_mod"])
    m["b_modT"] = pmajor(inp["b_mod"], 48).reshape(P, DEPTH * 48)
    lnp = np.stack([np.asarray(inp[k], np.float32) for k in ("ln1_g", "ln1_b", "ln2_g", "ln2_b")], 1)
    m["lnp"] = pmajor(lnp, 8).reshape(P, DEPTH * 32)
    m["attn_w_qkv"] = f(inp["attn_w_qkv"])
    m["attn_w_o"] = f(inp["attn_w_o"])
    m["lam"] = np.ascontiguousarray(np.stack([np.asarray(inp[k], np.float32) for k in
                                             ("attn_lam_q1", "attn_lam_k1", "attn_lam_q2", "attn_lam_k2")], 1))
    m["sublnT"] = np.ascontiguousarray(np.asarray(inp["attn_subln_g"], np.float32).T)
    m["fnet_w"] = f(inp["fnet_w"])
    m["fnet_bT"] = pmajor(inp["fnet_b"], 8).reshape(P, -1)
    m["conv_w_pw1"] = f(inp["conv_w_pw1"])
    m["conv_b_pw1T"] = pmajor(inp["conv_b_pw1"], 16).reshape(P, -1)
    wdw = np.asarray(inp["conv_w_dw"], np.float32)
    wdw = np.moveaxis(wdw, 1, 2)
    nC = wdw.shape[0]
    wdw = wdw.reshape(nC, 8, P, 31)
    m["conv_w_dwT"] = np.ascontiguousarray(np.moveaxis(wdw, 2, 0)).reshape(P, nC * 8 * 31)
    cpar = np.stack([np.asarray(inp[k], np.float32) for k in ("conv_b_dw", "conv_ln_g", "conv_ln_b", "conv_b_pw2")], 1)
    m["conv_pT"] = pmajor(cpar, 8).reshape(P, nC * 32)
    m["conv_w_pw2"] = f(inp["conv_w_pw2"])
    m["moe_w_router"] = f(inp["moe_w_router"])
    m["moe_b_router"] = f(inp["moe_b_router"])
    m["moe_w1"] = f(inp["moe_w1"])
    m["moe_b1T"] = pmajor(inp["moe_b1"], 16).reshape(P, DEPTH * E * 16)
    m["moe_w2"] = f(inp["moe_w2"])
    m["moe_b2"] = f(inp["moe_b2"])
    m.update(consts)
    return m


_CACHE = {}


def run(cfg, inputs, n_cores):
    key = (cfg.SEQ, cfg.CTX, cfg.E, cfg.DEPTH)
    if key not in _CACHE:
        _CACHE[key] = Prog(cfg).build()
    nc = _CACHE[key]
    consts = host_constants(cfg)
    in_maps = [host_inputs(cfg, inputs, consts, b) for b in range(n_cores)]
    res = run_bass_kernel_spmd(nc, in_maps, core_ids=list(range(n_cores)))
    out = np.stack([np.ascontiguousarray(res.results[b]["yT"].T) for b in range(n_cores)], 0)
    return out.astype(np.float32)


def kernel(**inputs):
    cfg = Cfg()
    return run(cfg, inputs, 8)
```
